# Optimizing a Trainium2 kernel written in Bass

```python
import jax, jax.numpy as jnp
from jax import lax
import numpy as np

D_MODEL = 1024
BATCH = 16
SEQ = 2048
DEPTH = 1

CTX_LEN = 256
GRID_W = 64
POS_THETA = 10000.0
RMS_EPS = 1e-6
N_MOD = 6
RG_WIDTH = 512
RG_BLOCKS = 8
RG_C = 8.0
CONV_W = 4
GLA_HEADS = 4
GLA_DK = 64
GLA_DV = 128
GLA_WIDTH = GLA_HEADS * GLA_DV
GLA_RANK = 16
GLA_GATE_NORM = 16.0
GLA_CHUNK = 64
MIX_WIDTH = RG_WIDTH + GLA_WIDTH
IN_SPLITS = (RG_WIDTH, RG_WIDTH, GLA_HEADS * GLA_DK, GLA_HEADS * GLA_DK, GLA_WIDTH, GLA_WIDTH, 2 * GLA_RANK)
IN_COLS = 2 * RG_WIDTH + 2 * GLA_HEADS * GLA_DK + 2 * GLA_WIDTH + 2 * GLA_RANK
N_KEYS = 128
N_EXPERTS = N_KEYS * N_KEYS
PEER_HEADS = 8
PEER_DQ = 256
PEER_TOPK = 16
PEER_TOKEN_BLOCK = 128

kernel_name = "hymba_rglru_gla_peer_dit_block"


def _rmsnorm(x, g):
    xf = x.astype(jnp.float32)
    y = xf * lax.rsqrt(jnp.mean(xf * xf, axis=-1, keepdims=True) + RMS_EPS)
    return (y * g.astype(jnp.float32)).astype(x.dtype)


def _modulate(x, shift, scale):
    return x * (1 + scale) + shift


def _sincos_2d(n_tokens, dim, dtype):
    rows = n_tokens // GRID_W
    r, col = jnp.meshgrid(jnp.arange(rows, dtype=jnp.float32), jnp.arange(GRID_W, dtype=jnp.float32), indexing="ij")
    quarter = dim // 4
    omega = POS_THETA ** (-jnp.arange(quarter, dtype=jnp.float32) / quarter)

    def axis_embed(p):
        ang = p.reshape(-1)[:, None] * omega[None, :]
        return jnp.concatenate([jnp.sin(ang), jnp.cos(ang)], axis=-1)

    return jnp.concatenate([axis_embed(r), axis_embed(col)], axis=-1).astype(dtype)


def _split_cols(h):
    bounds = np.cumsum(np.array(IN_SPLITS))[:-1].tolist()
    return jnp.split(h, bounds, axis=-1)


def _centred_dwconv(x, w, b):
    t = x.shape[1]
    left = CONV_W // 2
    right = CONV_W - 1 - left
    xp = jnp.pad(x, ((0, 0), (left, right), (0, 0)))
    out = xp[:, 0:t] * w[0]
    for j in range(1, CONV_W):
        out = out + xp[:, j:j + t] * w[j]
    return out + b


def _affine_combine(left, right):
    a_l, b_l = left
    a_r, b_r = right
    return a_l * a_r, a_r * b_l + b_r


def _linear_scan(a, b, h0, reverse):
    if reverse:
        a = jnp.flip(a, axis=1)
        b = jnp.flip(b, axis=1)
    b = b.at[:, 0].add(a[:, 0] * h0)
    _, h = lax.associative_scan(_affine_combine, (a, b), axis=1)
    if reverse:
        h = jnp.flip(h, axis=1)
    return h


def _rglru_direction(xc, w_a, b_a, w_x, b_x, lam, h0, reverse):
    bsz, t, width = xc.shape
    xf = xc.astype(jnp.float32)
    xb = xf.reshape(bsz, t, RG_BLOCKS, width // RG_BLOCKS)
    gate_r = jax.nn.sigmoid(jnp.einsum("btgi,gij->btgj", xb, w_a.astype(jnp.float32)).reshape(bsz, t, width) + b_a.astype(jnp.float32))
    gate_i = jax.nn.sigmoid(jnp.einsum("btgi,gij->btgj", xb, w_x.astype(jnp.float32)).reshape(bsz, t, width) + b_x.astype(jnp.float32))
    log_a = -RG_C * gate_r * jax.nn.softplus(-lam.astype(jnp.float32))
    a = jnp.exp(log_a)
    b = jnp.sqrt(-jnp.expm1(2.0 * log_a)) * gate_i * xf
    return _linear_scan(a, b, h0, reverse)


def _rglru_mixer(x_ctx, x_lat, conv_w, conv_b, w_a, b_a, w_x, b_x, lam):
    xc_ctx = _centred_dwconv(x_ctx, conv_w, conv_b)
    xc_lat = _centred_dwconv(x_lat, conv_w, conv_b)
    bsz = x_lat.shape[0]
    h0 = jnp.zeros((bsz, RG_WIDTH), jnp.float32)
    h_ctx_f = _rglru_direction(xc_ctx, w_a[0], b_a[0], w_x[0], b_x[0], lam[0], h0, False)
    h_lat_f = _rglru_direction(xc_lat, w_a[0], b_a[0], w_x[0], b_x[0], lam[0], h_ctx_f[:, -1], False)
    h_ctx_b = _rglru_direction(xc_ctx, w_a[1], b_a[1], w_x[1], b_x[1], lam[1], h0, True)
    h_lat_b = _rglru_direction(xc_lat, w_a[1], b_a[1], w_x[1], b_x[1], lam[1], h_ctx_b[:, 0], True)
    return h_ctx_f + h_ctx_b, h_lat_f + h_lat_b


def _to_heads(t, dh):
    bsz, n, _ = t.shape
    return t.reshape(bsz, n, GLA_HEADS, dh).transpose(0, 2, 1, 3).astype(jnp.float32)


def _gla_chunked(q, k, v, log_alpha, s0, reverse, with_output):
    if reverse:
        q, k, v, log_alpha = (jnp.flip(z, axis=2) for z in (q, k, v, log_alpha))
    bsz, heads, t, _ = q.shape
    dv = v.shape[-1]
    n_chunks = t // GLA_CHUNK

    def chunk(z):
        return z.reshape(bsz, heads, n_chunks, GLA_CHUNK, z.shape[-1])

    q, k, v, log_alpha = chunk(q), chunk(k), chunk(v), chunk(log_alpha)
    b = jnp.cumsum(log_alpha, axis=3)
    b_last = b[:, :, :, -1:, :]
    ds = jnp.einsum("bhncd,bhncv->bhndv", k * jnp.exp(b_last - b), v)
    gamma = jnp.broadcast_to(jnp.exp(b_last[:, :, :, 0, :])[..., None], ds.shape)
    ds = ds.at[:, :, 0].add(gamma[:, :, 0] * s0)
    _, s_out = lax.associative_scan(_affine_combine, (gamma, ds), axis=2)
    s_end = s_out[:, :, -1]
    if not with_output:
        return None, s_end
    s_in = jnp.concatenate([s0[:, :, None], s_out[:, :, :-1]], axis=2)
    q_dec = q * jnp.exp(b)
    k_inv = k * jnp.exp(-b)
    lower = jnp.tril(jnp.ones((GLA_CHUNK, GLA_CHUNK), dtype=bool))
    scores = jnp.where(lower, jnp.einsum("bhnid,bhnjd->bhnij", q_dec, k_inv), 0.0)
    o = jnp.einsum("bhnij,bhnjv->bhniv", scores, v) + jnp.einsum("bhnid,bhndv->bhniv", q_dec, s_in)
    o = o.reshape(bsz, heads, t, dv)
    if reverse:
        o = jnp.flip(o, axis=2)
    return o, s_end


def _log_gate(lr, w_g, b_g, direction):
    z = lr[..., direction * GLA_RANK:(direction + 1) * GLA_RANK].astype(jnp.float32) @ w_g.astype(jnp.float32) + b_g.astype(jnp.float32)
    return _to_heads(jax.nn.log_sigmoid(z) / GLA_GATE_NORM, GLA_DK)


def _gla_mixer(q_c, k_c, v_c, lr_c, q_l, k_l, v_l, lr_l, w_g, b_g, with_ctx_out):
    scale = GLA_DK ** -0.5
    qc, kc, vc = _to_heads(q_c, GLA_DK) * scale, _to_heads(k_c, GLA_DK), _to_heads(v_c, GLA_DV)
    ql, kl, vl = _to_heads(q_l, GLA_DK) * scale, _to_heads(k_l, GLA_DK), _to_heads(v_l, GLA_DV)
    bsz = ql.shape[0]
    s0 = jnp.zeros((bsz, GLA_HEADS, GLA_DK, GLA_DV), jnp.float32)
    outs_c, outs_l = [], []
    for d, reverse in enumerate((False, True)):
        o_c, s_ctx = _gla_chunked(qc, kc, vc, _log_gate(lr_c, w_g[d], b_g[d], d), s0, reverse, with_ctx_out)
        o_l, _ = _gla_chunked(ql, kl, vl, _log_gate(lr_l, w_g[d], b_g[d], d), s_ctx, reverse, True)
        outs_c.append(o_c)
        outs_l.append(o_l)
    o_ctx = outs_c[0] + outs_c[1] if with_ctx_out else None
    return o_ctx, outs_l[0] + outs_l[1]


def _merge_groups(y_rg, gate_rg, o_gla, r_gla, norm_g, w_out):
    dtype = gate_rg.dtype
    bsz, t, _ = gate_rg.shape
    rg = y_rg.astype(dtype) * jax.nn.gelu(gate_rg, approximate=False)
    gla = _rmsnorm(o_gla, norm_g).transpose(0, 2, 1, 3).reshape(bsz, t, GLA_WIDTH).astype(dtype)
    gla = gla * jax.nn.silu(r_gla)
    return jnp.concatenate([rg, gla], axis=-1) @ w_out


def _peer(u, w_q, keys, u_tab, v_tab):
    bsz, t, dim = u.shape
    q = (u @ w_q).reshape(bsz, t, PEER_HEADS, 2, PEER_DQ // 2).astype(jnp.float32)
    s = jnp.einsum("bthpd,hpkd->bthpk", q, keys.astype(jnp.float32))
    s1, i1 = lax.top_k(s[..., 0, :], PEER_TOPK)
    s2, i2 = lax.top_k(s[..., 1, :], PEER_TOPK)
    n_cand = PEER_TOPK * PEER_TOPK
    cand_s = (s1[..., :, None] + s2[..., None, :]).reshape(bsz, t, PEER_HEADS, n_cand)
    cand_e = (i1[..., :, None] * N_KEYS + i2[..., None, :]).reshape(bsz, t, PEER_HEADS, n_cand)
    top_s, pos = lax.top_k(cand_s, PEER_TOPK)
    expert = jnp.take_along_axis(cand_e, pos, axis=-1)
    weight = jax.nn.softmax(top_s, axis=-1).astype(u.dtype)
    n_blocks = (bsz * t) // PEER_TOKEN_BLOCK
    n_sel = PEER_HEADS * PEER_TOPK
    u_blk = u.reshape(n_blocks, PEER_TOKEN_BLOCK, dim)
    e_blk = expert.reshape(n_blocks, PEER_TOKEN_BLOCK, n_sel)
    w_blk = weight.reshape(n_blocks, PEER_TOKEN_BLOCK, n_sel)

    def block(args):
        xb, eb, wb = args
        act = jax.nn.gelu(jnp.einsum("td,tkd->tk", xb, u_tab[eb]), approximate=False) * wb
        return jnp.einsum("tk,tkd->td", act, v_tab[eb])

    y = lax.map(block, (u_blk, e_blk, w_blk))
    return y.reshape(bsz, t, dim)


def setup_inputs(seed: int = 0) -> dict:
    key = jax.random.key(seed)
    ks = jax.random.split(key, 25)
    f32 = jnp.float32
    d = D_MODEL

    def nrm(k, shape, s):
        return s * jax.random.normal(k, shape, f32)

    a0 = jax.random.uniform(ks[14], (DEPTH, 2, RG_WIDTH), f32, 0.9, 0.999)
    sig = a0 ** (1.0 / RG_C)
    rg_lambda = jnp.log(sig) - jnp.log1p(-sig)
    blk = RG_WIDTH // RG_BLOCKS
    return {
        "x": nrm(ks[0], (BATCH, SEQ, d), 1.0),
        "c": nrm(ks[1], (BATCH, d), 1.0),
        "ctx": nrm(ks[2], (BATCH, CTX_LEN, d), 1.0),
        "c_ctx": nrm(ks[3], (d,), 1.0),
        "ada_w": nrm(ks[4], (DEPTH, d, N_MOD * d), 0.3 * d ** -0.5),
        "ada_b": nrm(ks[5], (DEPTH, N_MOD * d), 0.02),
        "norm1_g": 1.0 + nrm(ks[6], (DEPTH, d), 0.05),
        "w_in": nrm(ks[7], (DEPTH, d, IN_COLS), d ** -0.5),
        "conv_w": nrm(ks[8], (DEPTH, CONV_W, RG_WIDTH), 0.5),
        "conv_b": nrm(ks[9], (DEPTH, RG_WIDTH), 0.02),
        "rg_w_a": nrm(ks[10], (DEPTH, 2, RG_BLOCKS, blk, blk), blk ** -0.5),
        "rg_b_a": nrm(ks[11], (DEPTH, 2, RG_WIDTH), 0.1),
        "rg_w_x": nrm(ks[12], (DEPTH, 2, RG_BLOCKS, blk, blk), blk ** -0.5),
        "rg_b_x": nrm(ks[13], (DEPTH, 2, RG_WIDTH), 0.1),
        "rg_lambda": rg_lambda,
        "gla_w_g": nrm(ks[15], (DEPTH, 2, GLA_RANK, GLA_HEADS * GLA_DK), GLA_RANK ** -0.5),
        "gla_b_g": 2.0 + nrm(ks[16], (DEPTH, 2, GLA_HEADS * GLA_DK), 0.5),
        "gla_norm_g": 1.0 + nrm(ks[17], (DEPTH, GLA_DV), 0.05),
        "w_out": nrm(ks[18], (DEPTH, MIX_WIDTH, d), MIX_WIDTH ** -0.5),
        "norm2_g": 1.0 + nrm(ks[19], (DEPTH, d), 0.05),
        "peer_w_q": nrm(ks[20], (DEPTH, d, PEER_HEADS * PEER_DQ), d ** -0.5),
        "peer_keys": nrm(ks[21], (DEPTH, PEER_HEADS, 2, N_KEYS, PEER_DQ // 2), (PEER_DQ // 2) ** -0.5),
        "peer_u": nrm(ks[22], (DEPTH, N_EXPERTS, d), d ** -0.5),
        "peer_v": nrm(ks[23], (DEPTH, N_EXPERTS, d), 1.0),
        "final_norm_g": 1.0 + nrm(ks[24], (d,), 0.05),
    }


def reference(x, c, ctx, c_ctx, ada_w, ada_b, norm1_g, w_in, conv_w, conv_b, rg_w_a, rg_b_a, rg_w_x, rg_b_x, rg_lambda, gla_w_g, gla_b_g, gla_norm_g, w_out, norm2_g, peer_w_q, peer_keys, peer_u, peer_v, final_norm_g):
    bsz, n_lat, dim = x.shape
    x = x + _sincos_2d(n_lat, dim, x.dtype)[None]
    for l in range(DEPTH):
        update_ctx = l < DEPTH - 1
        mod_l = (jax.nn.silu(c) @ ada_w[l] + ada_b[l]).reshape(bsz, N_MOD, 1, dim)
        mod_c = (jax.nn.silu(c_ctx) @ ada_w[l] + ada_b[l]).reshape(N_MOD, 1, 1, dim)
        h_l = _modulate(_rmsnorm(x, norm1_g[l]), mod_l[:, 0], mod_l[:, 1]) @ w_in[l]
        h_c = _modulate(_rmsnorm(ctx, norm1_g[l]), mod_c[0], mod_c[1]) @ w_in[l]
        rx_l, rgate_l, q_l, k_l, v_l, r_l, lr_l = _split_cols(h_l)
        rx_c, rgate_c, q_c, k_c, v_c, r_c, lr_c = _split_cols(h_c)
        y_c, y_l = _rglru_mixer(rx_c, rx_l, conv_w[l], conv_b[l], rg_w_a[l], rg_b_a[l], rg_w_x[l], rg_b_x[l], rg_lambda[l])
        o_c, o_l = _gla_mixer(q_c, k_c, v_c, lr_c, q_l, k_l, v_l, lr_l, gla_w_g[l], gla_b_g[l], update_ctx)
        x = x + mod_l[:, 2] * _merge_groups(y_l, rgate_l, o_l, r_l, gla_norm_g[l], w_out[l])
        u_l = _modulate(_rmsnorm(x, norm2_g[l]), mod_l[:, 3], mod_l[:, 4])
        x = x + mod_l[:, 5] * _peer(u_l, peer_w_q[l], peer_keys[l], peer_u[l], peer_v[l])
        if update_ctx:
            ctx = ctx + mod_c[2] * _merge_groups(y_c, rgate_c, o_c, r_c, gla_norm_g[l], w_out[l])
            u_c = _modulate(_rmsnorm(ctx, norm2_g[l]), mod_c[3], mod_c[4])
            ctx = ctx + mod_c[5] * _peer(u_c, peer_w_q[l], peer_keys[l], peer_u[l], peer_v[l])
    return _rmsnorm(x, final_norm_g)
```

```python
import contextlib
import numpy as np
import concourse.bass as bass
import concourse.mybir as mybir
from concourse.bass_utils import run_bass_kernel_spmd

F32 = mybir.dt.float32
BF16 = mybir.dt.bfloat16
U32 = mybir.dt.uint32
ALU = mybir.AluOpType
AF = mybir.ActivationFunctionType
AX = mybir.AxisListType

NCORES = 8
D = 1024
SEQ = 2048
CTX = 256
S = CTX + SEQ
NT = S // 128
INC = 2592
NEXP = 16384
TB = 256
EPS = 1e-6
NEG = -1.0e30


class KB:
    def __init__(self):
        self.nc = bass.Bass("TRN2", target_bir_lowering=False)
        nc = self.nc
        self.eng = {"pe": nc.tensor, "act": nc.scalar, "dve": nc.vector, "pool": nc.gpsimd, "sp": nc.sync}
        self.stack = contextlib.ExitStack()
        self.sem = {}
        self.cnt = {}
        for n in self.eng:
            self.sem[("e", n)] = self.stack.enter_context(nc.semaphore("s_" + n))
            self.cnt[n] = 0
        self.nd = 24
        self.dcnt = [0] * self.nd
        for i in range(self.nd):
            self.sem[("d", i)] = self.stack.enter_context(nc.semaphore("d%d" % i))
        self.dnext = 0
        self.waited = {}
        self.lastw = {}
        self.readers = {}

    def _wait(self, en, deps):
        for sk, v in deps.items():
            if en == "pe" and sk == ("e", "pe"):
                continue
            if self.waited.get((en, sk), 0) >= v:
                continue
            self.eng[en].wait_ge(self.sem[sk], v)
            self.waited[(en, sk)] = v

    def _deps(self, r, w):
        d = {}
        for k in r:
            t = self.lastw.get(k)
            if t:
                d[t[0]] = max(d.get(t[0], 0), t[1])
        for k in w:
            t = self.lastw.get(k)
            if t:
                d[t[0]] = max(d.get(t[0], 0), t[1])
            for sk, v in self.readers.get(k, {}).items():
                d[sk] = max(d.get(sk, 0), v)
        return d

    def _commit(self, tok, r, w):
        for k in w:
            self.lastw[k] = tok
            self.readers[k] = {}
        for k in r:
            rd = self.readers.setdefault(k, {})
            rd[tok[0]] = max(rd.get(tok[0], 0), tok[1])

    def I(self, en, fn, r=(), w=()):
        self._wait(en, self._deps(r, w))
        inst = fn(self.eng[en])
        self.cnt[en] += 1
        inst.then_inc(self.sem[("e", en)], 1)
        self._commit((("e", en), self.cnt[en]), r, w)

    def D(self, out, in_, r=(), w=(), q="sp"):
        i = self.dnext
        self.dnext = (i + 1) % self.nd
        deps = self._deps(r, w)
        if self.dcnt[i] > 0:
            deps[("d", i)] = max(deps.get(("d", i), 0), self.dcnt[i])
        self._wait(q, deps)
        inst = self.eng[q].dma_start(out=out, in_=in_)
        self.dcnt[i] += 16
        inst.then_inc(self.sem[("d", i)], 16)
        self._commit((("d", i), self.dcnt[i]), r, w)

    def barrier(self, engines=("pe", "act", "dve", "pool", "sp")):
        deps = {}
        for n in self.eng:
            if self.cnt[n] > 0:
                deps[("e", n)] = self.cnt[n]
        for i in range(self.nd):
            if self.dcnt[i] > 0:
                deps[("d", i)] = self.dcnt[i]
        for en in engines:
            d = dict(deps)
            d.pop(("e", en), None)
            self._wait(en, d)
        self.lastw = {}
        self.readers = {}


def build(upto=None, dbg=False):
    kb = KB()
    nc = kb.nc
    I, Dm = kb.I, kb.D

    def dram(name, shape, dt=F32, kind="ExternalInput"):
        return nc.dram_tensor(name, list(shape), dt, kind=kind).ap()

    x_d = dram("x", [2, SEQ, D])
    ctx_d = dram("ctx", [2, CTX, D])
    cT_d = dram("cT", [128, 8, 3])
    adaw_d = dram("ada_w", [D, 6 * D])
    adab_d = dram("ada_b", [1, 6 * D])
    adabT_d = dram("ada_bT", [128, 48])
    g1_d = dram("norm1_g", [1, D])
    g2_d = dram("norm2_g", [1, D])
    gf_d = dram("final_g", [1, D])
    win_d = dram("w_in", [D, INC])
    convw_d = dram("convwT", [128, 4, 4])
    convb_d = dram("convbT", [128, 4])
    rgwa_d = dram("rg_w_a", [2, 8, 64, 64])
    rgwx_d = dram("rg_w_x", [2, 8, 64, 64])
    rgba_d = dram("rgbaT", [128, 2, 4])
    rgbx_d = dram("rgbxT", [128, 2, 4])
    rglam_d = dram("rglamT", [128, 2, 4])
    glawg_d = dram("gla_w_g", [2, 16, 256])
    glabg_d = dram("glabgT", [128, 2, 2])
    glang_d = dram("glangT", [128, 1])
    wout_d = dram("w_out", [D, D])
    wq_d = dram("peer_w_q", [D, 2048])
    keysT_d = dram("keysT", [128, 16, 128])
    pu_d = dram("peer_u", [NEXP, D])
    pv_d = dram("peer_v", [NEXP, D])
    pos_d = dram("pos", [SEQ, D])
    ident_d = dram("ident", [128, 128])
    iota_d = dram("iota", [128, 128])
    maskf_d = dram("maskf", [128, 256])
    maskb_d = dram("maskb", [128, 256])
    out_d = dram("out", [2, SEQ, D], kind="ExternalOutput")
    x1_d = dram("x1s", [2 * SEQ, D], kind="ExternalOutput" if dbg else "Internal")
    grow_d = dram("grow_scr", [4, D], kind="Internal")
    ut_d = dram("ut_scr", [NEXP // 256, 128, 8, 256], BF16, kind="Internal")
    vs_d = dram("v_scr", [NEXP, D], BF16, kind="Internal")

    es = contextlib.ExitStack()

    uid = [0]

    def sb(stack, name, shape, dt=F32):
        uid[0] += 1
        return stack.enter_context(nc.sbuf_tensor("sb%d_%s" % (uid[0], name), list(shape), dt))

    def ps(stack, name, shape, dt=F32):
        uid[0] += 1
        return stack.enter_context(nc.psum_tensor("ps%d_%s" % (uid[0], name), list(shape), dt))

    ident = sb(es, "ident", [128, 128])
    identb = sb(es, "identb", [128, 128], BF16)
    iota = sb(es, "iota", [128, 128])
    ones = sb(es, "ones", [128, 128])
    modp = sb(es, "modp", [128, 6, 8, 3])
    epsb = sb(es, "epsb", [128, 1])

    Dm(ident[:], ident_d[:, :], w=["ident"])
    Dm(iota[:], iota_d[:, :], w=["iota"])
    I("dve", lambda e: e.tensor_copy(out=identb[:], in_=ident[:]), r=["ident"], w=["identb"])
    I("dve", lambda e: e.memset(ones[:], 1.0), w=["ones"])
    I("dve", lambda e: e.memset(epsb[:], EPS), w=["epsb"])

    with contextlib.ExitStack() as st:
        cT = sb(st, "cT", [128, 8, 3])
        scT = sb(st, "scT", [128, 8, 3])
        rep = sb(st, "rep", [128, 2, 8, 128])
        abT = sb(st, "abT", [128, 48])
        abrow = sb(st, "abrow", [128, D])
        growt = [sb(st, "growt%d" % i, [128, D]) for i in range(2)]
        aw = [sb(st, "aw%d" % i, [128, 8, D]) for i in range(2)]
        pm = [ps(st, "pm%d" % i, [128, 512]) for i in range(4)]
        Dm(cT[:], cT_d[:, :, :], w=["cT"])
        Dm(abT[:], adabT_d[:, :], w=["abT"])
        I("act", lambda e: e.activation(out=scT[:], in_=cT[:], func=AF.Silu), r=["cT"], w=["scT"])
        for b in range(2):
            I("dve", lambda e, b=b: e.tensor_copy(out=rep[:, b], in_=scT[:, :, b:b + 1].to_broadcast([128, 8, 128])),
              r=["scT"], w=["rep"])
        pi = 0
        for m in range(6):
            a = aw[m % 2]
            ak = "aw%d" % (m % 2)
            Dm(a[:], adaw_d[:, m * D:(m + 1) * D].rearrange("(kc p) n -> p kc n", p=128), w=[ak])
            if m in (0, 1, 3, 4):
                p = pm[pi % 4]
                pk = "pm%d" % (pi % 4)
                pi += 1
                for fc in range(8):
                    for kc in range(8):
                        I("pe", lambda e, fc=fc, kc=kc, p=p, a=a: e.matmul(
                            p[:, fc * 3:fc * 3 + 3], lhsT=a[:, kc, fc * 128:(fc + 1) * 128], rhs=scT[:, kc, :],
                            start=(kc == 0), stop=(kc == 7)), r=[ak, "scT"], w=[pk])
                I("dve", lambda e, m=m, p=p: e.tensor_tensor(
                    out=modp[:, m], in0=p[:, 0:24].rearrange("p (f c) -> p f c", c=3),
                    in1=abT[:, m * 8:(m + 1) * 8].unsqueeze(2).to_broadcast([128, 8, 3]), op=ALU.add),
                  r=[pk, "abT"], w=["modp"])
                if m in (1, 4):
                    I("dve", lambda e, m=m: e.tensor_scalar_add(out=modp[:, m], in0=modp[:, m], scalar1=1.0),
                      r=["modp"], w=["modp"])
            else:
                mi = 0 if m == 2 else 1
                Dm(abrow[:], adab_d[0:1, m * D:(m + 1) * D].partition_broadcast(128), w=["abrow"])
                for b in range(2):
                    for hf in range(2):
                        p = pm[pi % 4]
                        pk = "pm%d" % (pi % 4)
                        pi += 1
                        for kc in range(8):
                            I("pe", lambda e, kc=kc, p=p, a=a, b=b, hf=hf: e.matmul(
                                p[:, :], lhsT=rep[:, b, kc, :], rhs=a[:, kc, hf * 512:(hf + 1) * 512],
                                start=(kc == 0), stop=(kc == 7)), r=[ak, "rep"], w=[pk])
                        I("dve", lambda e, p=p, b=b, hf=hf, mi=mi: e.tensor_tensor(
                            out=growt[b][:, hf * 512:(hf + 1) * 512], in0=p[:, :],
                            in1=abrow[:, hf * 512:(hf + 1) * 512], op=ALU.add),
                          r=[pk, "abrow"], w=["growt%d" % b])
                    Dm(grow_d[mi * 2 + b:mi * 2 + b + 1, :], growt[b][0:1, :], r=["growt%d" % b], w=["grow_scr"])
        kb.barrier()
    if upto == "M0":
        return nc

    with contextlib.ExitStack() as sm:
        g1row = sb(sm, "g1row", [128, D])
        g1gate = sb(sm, "g1gate", [128, 2, D])
        for b in range(2):
            Dm(g1gate[:, b, :], grow_d[b:b + 1, :].partition_broadcast(128), r=["grow_scr"], w=["g1gate"])
        maskf = sb(sm, "maskf", [128, 256], BF16)
        maskb = sb(sm, "maskb", [128, 256], BF16)
        mstage = sb(sm, "mstage", [128, 256])
        cw = sb(sm, "cw", [128, 4, 4])
        cb = sb(sm, "cb", [128, 4])
        rba = sb(sm, "rba", [128, 2, 4])
        rbx = sb(sm, "rbx", [128, 2, 4])
        rlam = sb(sm, "rlam", [128, 2, 4])
        coef = sb(sm, "coef", [128, 2, 4])
        gbg = sb(sm, "gbg", [128, 2, 2])
        ngbg = sb(sm, "ngbg", [128, 2, 2])
        gng = sb(sm, "gng", [128, 1])
        wg = sb(sm, "wg", [32, 2, 256])
        wbd = sb(sm, "wbd", [128, 2, 2, 4, 128])
        hm = sb(sm, "hm", [128, 2])
        bdm = sb(sm, "bdm", [128, 256])
        I("dve", lambda e: e.memset(hm[:], 0.0), w=["hm"])
        I("dve", lambda e: e.memset(hm[0:64, 0:1], 1.0), w=["hm"])
        I("dve", lambda e: e.memset(hm[64:128, 1:2], 1.0), w=["hm"])
        I("dve", lambda e: e.memset(bdm[:], 0.0), w=["bdm"])
        I("dve", lambda e: e.memset(bdm[0:64, 0:128], 1.0), w=["bdm"])
        I("dve", lambda e: e.memset(bdm[64:128, 128:256], 1.0), w=["bdm"])
        Dm(g1row[:], g1_d[0:1, :].partition_broadcast(128), w=["g1row"])
        Dm(mstage[:], maskf_d[:, :], w=["mstage"])
        I("dve", lambda e: e.tensor_copy(out=maskf[:], in_=mstage[:]), r=["mstage"], w=["maskf"])
        Dm(mstage[:], maskb_d[:, :], w=["mstage"])
        I("dve", lambda e: e.tensor_copy(out=maskb[:], in_=mstage[:]), r=["mstage"], w=["maskb"])
        Dm(cw[:], convw_d[:, :, :], w=["cw"])
        Dm(cb[:], convb_d[:, :], w=["cb"])
        Dm(rba[:], rgba_d[:, :, :], w=["rba"])
        Dm(rbx[:], rgbx_d[:, :, :], w=["rbx"])
        Dm(rlam[:], rglam_d[:, :, :], w=["rlam"])
        Dm(gbg[:], glabg_d[:, :, :], w=["gbg"])
        Dm(gng[:], glang_d[:, :], w=["gng"])
        I("act", lambda e: e.activation(out=coef[:], in_=rlam[:], func=AF.Exp, scale=-1.0), r=["rlam"], w=["coef"])
        I("act", lambda e: e.activation(out=coef[:], in_=coef[:], func=AF.Ln, bias=1.0), r=["coef"], w=["coef"])
        I("dve", lambda e: e.tensor_scalar_mul(out=coef[:], in0=coef[:], scalar1=-8.0), r=["coef"], w=["coef"])
        I("dve", lambda e: e.tensor_scalar_mul(out=ngbg[:], in0=gbg[:], scalar1=-1.0), r=["gbg"], w=["ngbg"])
        I("dve", lambda e: e.memset(wg[:], 0.0), w=["wg"])
        for d in range(2):
            Dm(wg[16 * d:16 * d + 16, d, :], glawg_d[d, :, :], w=["wg"])
        I("dve", lambda e: e.memset(wbd[:], 0.0), w=["wbd"])
        for gi, src in enumerate((rgwa_d, rgwx_d)):
            for d in range(2):
                for cc in range(4):
                    for h in range(2):
                        Dm(wbd[64 * h:64 * h + 64, gi, d, cc, 64 * h:64 * h + 64], src[d, 2 * cc + h, :, :], w=["wbd"])

        for b in range(2):
            with contextlib.ExitStack() as sbt:
                mgT = sb(sbt, "mgT", [128, 8, SEQ], BF16)
                qT = sb(sbt, "qT", [128, 2, SEQ], BF16)
                kT = sb(sbt, "kT", [128, 2, S], BF16)
                vtok = sb(sbt, "vtok", [128, NT, 512], BF16)
                lrT = sb(sbt, "lrT", [32, S])
                sR = contextlib.ExitStack()
                rxT = sb(sR, "rxT", [128, 4, S], BF16)
                with contextlib.ExitStack() as s1:
                    winb = sb(s1, "winb", [128, 8, INC], BF16)
                    wst0 = sb(s1, "wst0", [128, INC // 2])
                    wst = [wst0, wst0]
                    uT0 = sb(s1, "uT0", [128, 8, 512], BF16)
                    uT = [uT0, uT0]
                    xt = [sb(s1, "xt%d" % i, [128, D]) for i in range(2)]
                    pt0 = sb(s1, "pt0", [128, D])
                    pt = [pt0, pt0]
                    xn = [sb(s1, "xn%d" % i, [128, D], BF16) for i in range(2)]
                    sq = sb(s1, "sqj", [128, D], BF16)
                    ss = [sb(s1, "ss%d" % i, [128, 1]) for i in range(2)]
                    tp = [ps(s1, "tp%d" % i, [128, 8, 128], BF16) for i in range(2)]
                    pj = [ps(s1, "pj%d" % i, [128, 512]) for i in range(4)]
                    hw = INC // 2
                    for kc in range(8):
                        for hh in range(2):
                            w_ = wst[hh]
                            Dm(w_[:], win_d[kc * 128:(kc + 1) * 128, hh * hw:(hh + 1) * hw], w=["wst0"])
                            I("pool" if hh else "dve", lambda e, w_=w_, kc=kc, hh=hh: e.tensor_copy(
                                out=winb[:, kc, hh * hw:(hh + 1) * hw], in_=w_[:]), r=["wst0"], w=["winb"])
                    ngroups = 5
                    pjc = 0
                    for g in range(ngroups):
                        t0 = g * 4
                        nt = min(4, NT - t0)
                        ntok = nt * 128
                        u = uT[g % 2]
                        uk = "uT0"
                        for ti in range(nt):
                            t = t0 + ti
                            pb = t % 2
                            xk, xnk, ssk, tpk, ptk = "xt%d" % pb, "xn%d" % pb, "ss%d" % pb, "tp%d" % pb, "pt0"
                            if t < 2:
                                Dm(xt[pb][:], ctx_d[b, t * 128:(t + 1) * 128, :], w=[xk])
                                col = 2
                            else:
                                l0 = (t - 2) * 128
                                Dm(xt[pb][:], x_d[b, l0:l0 + 128, :], w=[xk])
                                Dm(pt[pb][:], pos_d[l0:l0 + 128, :], w=[ptk])
                                I("pool", lambda e, pb=pb: e.tensor_tensor(out=xt[pb][:], in0=xt[pb][:], in1=pt[pb][:], op=ALU.add),
                                  r=[xk, ptk], w=[xk])
                                col = b
                            I("act", lambda e, pb=pb: e.activation(out=sq[:], in_=xt[pb][:], func=AF.Square, accum_out=ss[pb][:]),
                              r=[xk], w=["sqj", ssk])
                            I("act", lambda e, pb=pb: e.activation(out=ss[pb][:], in_=ss[pb][:], func=AF.Sqrt, scale=1.0 / D, bias=epsb[:]),
                              r=[ssk, "epsb"], w=[ssk])
                            I("dve", lambda e, pb=pb: e.reciprocal(out=ss[pb][:], in_=ss[pb][:]), r=[ssk], w=[ssk])
                            I("dve", lambda e, pb=pb: e.scalar_tensor_tensor(
                                out=xn[pb][:], in0=xt[pb][:], scalar=ss[pb][:], in1=g1row[:], op0=ALU.mult, op1=ALU.mult),
                              r=[xk, ssk, "g1row"], w=[xnk])
                            for fc in range(8):
                                I("pe", lambda e, pb=pb, fc=fc: e.transpose(tp[pb][:, fc, :], xn[pb][:, fc * 128:(fc + 1) * 128], identb[:]),
                                  r=[xnk, "identb"], w=[tpk])
                            for fc in range(8):
                                I("dve" if fc % 2 else "pool" if False else "dve", lambda e, pb=pb, fc=fc, ti=ti, u=u, col=col: e.tensor_scalar(
                                    out=u[:, fc, ti * 128:(ti + 1) * 128], in0=tp[pb][:, fc, :],
                                    scalar1=modp[:, 1, fc, col:col + 1], scalar2=modp[:, 0, fc, col:col + 1],
                                    op0=ALU.mult, op1=ALU.add), r=[tpk, "modp"], w=[uk])
                        c0 = t0 * 128
                        lat0 = max(c0, CTX)
                        for cch in range(21):
                            cs_ = cch * 128
                            ncol = 128 if cch < 20 else 32
                            p = pj[pjc % 4]
                            pk = "pj%d" % (pjc % 4)
                            pjc += 1
                            if 12 <= cch < 16:
                                continue
                            lat_only = (4 <= cch < 10) or (16 <= cch < 20)
                            if lat_only and lat0 >= c0 + ntok:
                                continue
                            for kc in range(8):
                                I("pe", lambda e, kc=kc, p=p, u=u, cs_=cs_, ncol=ncol, ntok=ntok: e.matmul(
                                    p[0:ncol, 0:ntok], lhsT=winb[:, kc, cs_:cs_ + ncol], rhs=u[:, kc, 0:ntok],
                                    start=(kc == 0), stop=(kc == 7)), r=["winb", uk], w=[pk])
                            o0 = lat0 - c0
                            if cch < 4:
                                I("act", lambda e, p=p, cch=cch, c0=c0, ntok=ntok: e.copy(out=rxT[:, cch, c0:c0 + ntok], in_=p[:, 0:ntok]),
                                  r=[pk], w=["rxT"])
                            elif cch < 8:
                                I("act", lambda e, p=p, cch=cch, o0=o0, lat0=lat0, ntok=ntok: e.activation(
                                    out=mgT[:, cch - 4, lat0 - CTX:lat0 - CTX + ntok - o0], in_=p[:, o0:ntok], func=AF.Gelu),
                                  r=[pk], w=["mgT"])
                            elif cch < 10:
                                I("dve", lambda e, p=p, cch=cch, o0=o0, lat0=lat0, ntok=ntok: e.tensor_scalar_mul(
                                    out=qT[:, cch - 8, lat0 - CTX:lat0 - CTX + ntok - o0], in0=p[:, o0:ntok], scalar1=0.125),
                                  r=[pk], w=["qT"])
                            elif cch < 12:
                                I("dve", lambda e, p=p, cch=cch, c0=c0, ntok=ntok: e.tensor_copy(out=kT[:, cch - 10, c0:c0 + ntok], in_=p[:, 0:ntok]),
                                  r=[pk], w=["kT"])
                            elif cch < 20:
                                I("act", lambda e, p=p, cch=cch, o0=o0, lat0=lat0, ntok=ntok: e.activation(
                                    out=mgT[:, 4 + cch - 16, lat0 - CTX:lat0 - CTX + ntok - o0], in_=p[:, o0:ntok], func=AF.Silu),
                                  r=[pk], w=["mgT"])
                            else:
                                I("dve", lambda e, p=p, c0=c0, ntok=ntok: e.tensor_copy(out=lrT[:, c0:c0 + ntok], in_=p[0:32, 0:ntok]),
                                  r=[pk], w=["lrT"])
                        for ti in range(nt):
                            p = pj[pjc % 4]
                            pk = "pj%d" % (pjc % 4)
                            pjc += 1
                            for kc in range(8):
                                I("pe", lambda e, kc=kc, p=p, u=u, ti=ti: e.matmul(
                                    p[:, :], lhsT=u[:, kc, ti * 128:(ti + 1) * 128], rhs=winb[:, kc, 1536:2048],
                                    start=(kc == 0), stop=(kc == 7)), r=["winb", uk], w=[pk])
                            I("act", lambda e, p=p, t=t0 + ti: e.copy(out=vtok[:, t, :], in_=p[:, :]), r=[pk], w=["vtok"])
                    kb.barrier()

                if upto == "M1a":
                    sR.close()
                    return nc
                with contextlib.ExitStack() as s2:
                    xc = sb(s2, "xc", [128, S])
                    gr = sb(s2, "gr", [128, S])
                    gi_ = sb(s2, "gi", [128, S])
                    aa = sb(s2, "aa", [128, S])
                    bb = sb(s2, "bb", [128, S])
                    hf_ = sb(s2, "hf", [128, S])
                    hb_ = sb(s2, "hb", [128, S])
                    pg = [ps(s2, "pg%d" % i, [128, 512]) for i in range(4)]
                    pgc = 0
                    segs = ((0, CTX), (CTX, S))
                    for cc in range(4):
                        I("dve", lambda e, cc=cc: e.tensor_scalar(
                            out=xc[:], in0=rxT[:, cc, :], scalar1=cw[:, cc, 2:3], scalar2=cb[:, cc:cc + 1],
                            op0=ALU.mult, op1=ALU.add), r=["rxT", "cw", "cb"], w=["xc"])
                        for (a0, a1) in segs:
                            for j, sh in ((0, -2), (1, -1), (3, 1)):
                                lo = max(a0, a0 - sh)
                                hi = min(a1, a1 - sh)
                                I("dve", lambda e, cc=cc, j=j, sh=sh, lo=lo, hi=hi: e.scalar_tensor_tensor(
                                    out=xc[:, lo:hi], in0=rxT[:, cc, lo + sh:hi + sh], scalar=cw[:, cc, j:j + 1],
                                    in1=xc[:, lo:hi], op0=ALU.mult, op1=ALU.add), r=["rxT", "cw", "xc"], w=["xc"])
                        for d in range(2):
                            for gi, (gt, gk, bias_t) in enumerate(((gr, "gr", rba), (gi_, "gi", rbx))):
                                for g in range(5):
                                    c0 = g * 512
                                    n = min(512, S - c0)
                                    p = pg[pgc % 4]
                                    pk = "pg%d" % (pgc % 4)
                                    pgc += 1
                                    I("pe", lambda e, p=p, gi=gi, d=d, cc=cc, c0=c0, n=n: e.matmul(
                                        p[:, 0:n], lhsT=wbd[:, gi, d, cc, :], rhs=xc[:, c0:c0 + n], start=True, stop=True),
                                      r=["wbd", "xc"], w=[pk])
                                    I("act", lambda e, p=p, gt=gt, bias_t=bias_t, d=d, cc=cc, c0=c0, n=n: e.activation(
                                        out=gt[:, c0:c0 + n], in_=p[:, 0:n], func=AF.Sigmoid, bias=bias_t[:, d, cc:cc + 1]),
                                      r=[pk], w=[gk])
                            I("act", lambda e, d=d, cc=cc: e.activation(out=aa[:], in_=gr[:], func=AF.Exp, scale=coef[:, d, cc:cc + 1]),
                              r=["gr", "coef"], w=["aa"])
                            I("pool", lambda e: e.tensor_tensor(out=bb[:], in0=aa[:], in1=aa[:], op=ALU.mult), r=["aa"], w=["bb"])
                            I("act", lambda e: e.activation(out=bb[:], in_=bb[:], func=AF.Sqrt, scale=-1.0, bias=1.0), r=["bb"], w=["bb"])
                            I("pool", lambda e: e.tensor_tensor(out=bb[:], in0=bb[:], in1=gi_[:], op=ALU.mult), r=["bb", "gi"], w=["bb"])
                            I("pool", lambda e: e.tensor_tensor(out=bb[:], in0=bb[:], in1=xc[:], op=ALU.mult), r=["bb", "xc"], w=["bb"])
                            if d == 0:
                                I("dve", lambda e: e.tensor_tensor_scan(out=hf_[:], data0=aa[:], data1=bb[:], initial=0.0,
                                                                         op0=ALU.mult, op1=ALU.add), r=["aa", "bb"], w=["hf"])
                            else:
                                I("dve", lambda e: e.tensor_tensor_scan(out=hb_[:, 0:CTX][:, ::-1], data0=aa[:, 0:CTX][:, ::-1],
                                                                         data1=bb[:, 0:CTX][:, ::-1], initial=0.0,
                                                                         op0=ALU.mult, op1=ALU.add), r=["aa", "bb"], w=["hb"])
                                I("dve", lambda e: e.tensor_tensor_scan(out=hb_[:, CTX:S][:, ::-1], data0=aa[:, CTX:S][:, ::-1],
                                                                         data1=bb[:, CTX:S][:, ::-1], initial=hb_[:, 0:1],
                                                                         op0=ALU.mult, op1=ALU.add), r=["aa", "bb", "hb"], w=["hb"])
                        I("dve", lambda e: e.tensor_tensor(out=hf_[:, CTX:S], in0=hf_[:, CTX:S], in1=hb_[:, CTX:S], op=ALU.add),
                          r=["hf", "hb"], w=["hf"])
                        I("dve", lambda e, cc=cc: e.tensor_tensor(out=mgT[:, cc, :], in0=mgT[:, cc, :], in1=hf_[:, CTX:S], op=ALU.mult),
                          r=["hf", "mgT"], w=["mgT"])
                    kb.barrier()

                if upto == "M1b":
                    sR.close()
                    return nc
                sR.close()
                with contextlib.ExitStack() as s3:
                    otot = sb(s3, "otot", [128, 4, SEQ])
                    la = sb(s3, "la", [128, S])
                    bc = sb(s3, "bc", [128, S])
                    eb = sb(s3, "eb", [128, S])
                    qd = sb(s3, "qd", [128, SEQ], BF16)
                    ki = sb(s3, "ki", [128, S], BF16)
                    kih = [sb(s3, "kih%d" % i, [128, S], BF16) for i in range(2)]
                    kit = [sb(s3, "kit%d" % i, [128, 128], BF16) for i in range(2)]
                    Sst = sb(s3, "Sst", [128, 256])
                    Sbf = [sb(s3, "Sbf%d" % i, [128, 256], BF16) for i in range(2)]
                    dsg = [sb(s3, "dsg%d" % i, [128, 256]) for i in range(2)]
                    scb = [sb(s3, "scb%d" % i, [128, 256], BF16) for i in range(2)]
                    pz = [ps(s3, "pz%d" % i, [128, 512]) for i in range(2)]
                    ptr = [ps(s3, "ptr%d" % i, [128, 128], BF16) for i in range(2)]
                    pds = [ps(s3, "pds%d" % i, [128, 256]) for i in range(2)]
                    psc = [ps(s3, "psc%d" % i, [128, 256]) for i in range(1)]
                    po = [ps(s3, "po%d" % i, [128, 256]) for i in range(1)]
                    zc = 0
                    cn = 0
                    first_o = {0: True, 1: True}
                    for d in range(2):
                        order = list(range(NT)) if d == 0 else [1, 0] + list(range(NT - 1, 1, -1))
                        mk, mkk = (maskf, "maskf") if d == 0 else (maskb, "maskb")
                        for pr in range(2):
                            for g in range(5):
                                c0 = g * 512
                                n = min(512, S - c0)
                                p = pz[zc % 2]
                                pk = "pz%d" % (zc % 2)
                                zc += 1
                                I("pe", lambda e, p=p, d=d, pr=pr, c0=c0, n=n: e.matmul(
                                    p[:, 0:n], lhsT=wg[:, d, pr * 128:(pr + 1) * 128], rhs=lrT[:, c0:c0 + n], start=True, stop=True),
                                  r=["wg", "lrT"], w=[pk])
                                I("act", lambda e, p=p, d=d, pr=pr, c0=c0, n=n: e.activation(
                                    out=la[:, c0:c0 + n], in_=p[:, 0:n], func=AF.Exp, scale=-1.0, bias=ngbg[:, d, pr:pr + 1]),
                                  r=[pk, "ngbg"], w=["la"])
                            I("act", lambda e: e.activation(out=la[:], in_=la[:], func=AF.Ln, bias=1.0), r=["la"], w=["la"])
                            I("pool", lambda e: e.tensor_scalar_mul(out=la[:], in0=la[:], scalar1=-1.0 / 16.0), r=["la"], w=["la"])
                            for n_ in range(NT):
                                c0 = n_ * 128
                                if d == 0:
                                    I("dve", lambda e, c0=c0: e.tensor_tensor_scan(
                                        out=bc[:, c0:c0 + 128], data0=ones[:, :], data1=la[:, c0:c0 + 128], initial=0.0,
                                        op0=ALU.mult, op1=ALU.add), r=["la", "ones"], w=["bc"])
                                else:
                                    I("dve", lambda e, c0=c0: e.tensor_tensor_scan(
                                        out=bc[:, c0:c0 + 128][:, ::-1], data0=ones[:, :], data1=la[:, c0:c0 + 128][:, ::-1], initial=0.0,
                                        op0=ALU.mult, op1=ALU.add), r=["la", "ones"], w=["bc"])
                            I("act", lambda e: e.activation(out=eb[:], in_=bc[:], func=AF.Exp), r=["bc"], w=["eb"])
                            I("act", lambda e: e.activation(out=la[:], in_=bc[:], func=AF.Exp, scale=-1.0), r=["bc"], w=["la"])
                            I("pool", lambda e, pr=pr: e.tensor_tensor(out=qd[:], in0=qT[:, pr, :], in1=eb[:, CTX:S], op=ALU.mult),
                              r=["qT", "eb"], w=["qd"])
                            I("dve", lambda e, pr=pr: e.tensor_tensor(out=ki[:], in0=kT[:, pr, :], in1=la[:], op=ALU.mult),
                              r=["kT", "la"], w=["ki"])
                            for h in range(2):
                                I("pool", lambda e, h=h: e.tensor_scalar_mul(out=kih[h][:], in0=ki[:], scalar1=hm[:, h:h + 1]),
                                  r=["ki", "hm"], w=["kih%d" % h])
                            I("dve", lambda e: e.memset(Sst[:], 0.0), w=["Sst"])
                            if upto == "G1":
                                kb.barrier()
                                return nc
                            for n_ in order:
                                c0 = n_ * 128
                                gcol = c0 + 127 if d == 0 else c0
                                j = cn % 2
                                cn += 1
                                I("pe", lambda e, j=j, c0=c0: e.transpose(ptr[j][:, :], ki[:, c0:c0 + 128], identb[:]),
                                  r=["ki", "identb"], w=["ptr%d" % j])
                                I("act", lambda e, j=j: e.copy(out=kit[j][:], in_=ptr[j][:, :]), r=["ptr%d" % j], w=["kit%d" % j])
                                I("pe", lambda e, j=j, n_=n_, pr=pr: e.matmul(
                                    pds[j][:, :], lhsT=kit[j][:, :], rhs=vtok[:, n_, pr * 256:(pr + 1) * 256], start=True, stop=True),
                                  r=["kit%d" % j, "vtok"], w=["pds%d" % j])
                                I("pool", lambda e, j=j: e.tensor_tensor(out=Sbf[j][:], in0=Sst[:], in1=bdm[:], op=ALU.mult),
                                  r=["Sst", "bdm"], w=["Sbf%d" % j])
                                if n_ >= 2:
                                    l0 = c0 - CTX
                                    for h in range(2):
                                        I("pe", lambda e, h=h, c0=c0, l0=l0: e.matmul(
                                            psc[0][:, h * 128:(h + 1) * 128], lhsT=kih[h][:, c0:c0 + 128],
                                            rhs=qd[:, l0:l0 + 128], start=True, stop=True),
                                          r=["kih%d" % h, "qd"], w=["psc0"])
                                    I("dve", lambda e, j=j, mk=mk: e.tensor_tensor(out=scb[j][:], in0=psc[0][:, :], in1=mk[:], op=ALU.mult),
                                      r=["psc0", mkk], w=["scb%d" % j])
                                    for h in range(2):
                                        hd = pr * 2 + h
                                        I("pe", lambda e, h=h, hd=hd, j=j, n_=n_: e.matmul(
                                            po[0][:, h * 128:(h + 1) * 128], lhsT=vtok[:, n_, hd * 128:(hd + 1) * 128],
                                            rhs=scb[j][:, h * 128:(h + 1) * 128], start=True, stop=False),
                                          r=["vtok", "scb%d" % j], w=["po0"])
                                        I("pe", lambda e, h=h, j=j, l0=l0: e.matmul(
                                            po[0][:, h * 128:(h + 1) * 128], lhsT=Sbf[j][:, h * 128:(h + 1) * 128],
                                            rhs=qd[:, l0:l0 + 128], start=False, stop=True),
                                          r=["Sbf%d" % j, "qd"], w=["po0"])
                                    ov = otot[:, pr * 2:pr * 2 + 2, l0:l0 + 128]
                                    pv_ = po[0][:, :].rearrange("p (h t) -> p h t", h=2)
                                    if d == 0:
                                        I("act", lambda e, ov=ov, pv_=pv_: e.copy(out=ov, in_=pv_), r=["po0"], w=["otot"])
                                    else:
                                        I("dve", lambda e, ov=ov, pv_=pv_: e.tensor_tensor(out=ov, in0=ov, in1=pv_, op=ALU.add),
                                          r=["po0", "otot"], w=["otot"])
                                I("dve", lambda e, j=j: e.tensor_tensor(out=dsg[j][:], in0=pds[j][:, :], in1=Sst[:], op=ALU.add),
                                  r=["pds%d" % j, "Sst"], w=["dsg%d" % j])
                                I("pool", lambda e, j=j, gcol=gcol: e.tensor_scalar_mul(out=Sst[:], in0=dsg[j][:], scalar1=eb[:, gcol:gcol + 1]),
                                  r=["dsg%d" % j, "eb", "Sbf%d" % j], w=["Sst"])
                                if (upto == "G2" and n_ == 0) or (upto == "G3" and n_ == 2):
                                    kb.barrier()
                                    return nc
                    sqo = sb(s3, "sqo", [128, 512])
                    rs = sb(s3, "rs", [128, 512])
                    tmpo = sb(s3, "tmpo", [128, 512])
                    for hd in range(4):
                        for g in range(4):
                            c0 = g * 512
                            p = pz[zc % 2]
                            pk = "pz%d" % (zc % 2)
                            zc += 1
                            I("act", lambda e, hd=hd, c0=c0: e.activation(out=sqo[:], in_=otot[:, hd, c0:c0 + 512], func=AF.Square),
                              r=["otot"], w=["sqo"])
                            I("pe", lambda e, p=p: e.matmul(p[:, :], lhsT=ones[:, :], rhs=sqo[:], start=True, stop=True),
                              r=["ones", "sqo"], w=[pk])
                            I("act", lambda e, p=p: e.activation(out=rs[:], in_=p[:, :], func=AF.Sqrt, scale=1.0 / 128.0, bias=epsb[:]),
                              r=[pk, "epsb"], w=["rs"])
                            I("dve", lambda e: e.reciprocal(out=rs[:], in_=rs[:]), r=["rs"], w=["rs"])
                            I("dve", lambda e, hd=hd, c0=c0: e.scalar_tensor_tensor(
                                out=tmpo[:], in0=otot[:, hd, c0:c0 + 512], scalar=gng[:, 0:1], in1=rs[:], op0=ALU.mult, op1=ALU.mult),
                              r=["otot", "gng", "rs"], w=["tmpo"])
                            I("pool", lambda e, hd=hd, c0=c0: e.tensor_tensor(
                                out=mgT[:, 4 + hd, c0:c0 + 512], in0=mgT[:, 4 + hd, c0:c0 + 512], in1=tmpo[:], op=ALU.mult),
                              r=["tmpo", "mgT"], w=["mgT"])
                    kb.barrier()

                if upto == "M1c":
                    return nc
                with contextlib.ExitStack() as s4:
                    woutb = sb(s4, "woutb", [128, 8, D], BF16)
                    wst2 = [sb(s4, "wso%d" % i, [128, D]) for i in range(2)]
                    xt = [sb(s4, "xo%d" % i, [128, D]) for i in range(2)]
                    pt = [sb(s4, "po_%d" % i, [128, D]) for i in range(2)]
                    tm = [sb(s4, "tm%d" % i, [128, D]) for i in range(2)]
                    pw = [ps(s4, "pw%d" % i, [128, 2, 512]) for i in range(2)]
                    for kc in range(8):
                        w_ = wst2[kc % 2]
                        Dm(w_[:], wout_d[kc * 128:(kc + 1) * 128, :], w=["wso%d" % (kc % 2)])
                        I("dve", lambda e, w_=w_, kc=kc: e.tensor_copy(out=woutb[:, kc, :], in_=w_[:]), r=["wso%d" % (kc % 2)], w=["woutb"])
                    for t in range(16):
                        pb = t % 2
                        l0 = t * 128
                        Dm(xt[pb][:], x_d[b, l0:l0 + 128, :], w=["xo%d" % pb])
                        Dm(pt[pb][:], pos_d[l0:l0 + 128, :], w=["po_%d" % pb])
                        I("pool", lambda e, pb=pb: e.tensor_tensor(out=xt[pb][:], in0=xt[pb][:], in1=pt[pb][:], op=ALU.add),
                          r=["xo%d" % pb, "po_%d" % pb], w=["xo%d" % pb])
                        for hf in range(2):
                            for mc in range(8):
                                I("pe", lambda e, pb=pb, hf=hf, mc=mc, l0=l0: e.matmul(
                                    pw[pb][:, hf, :], lhsT=mgT[:, mc, l0:l0 + 128], rhs=woutb[:, mc, hf * 512:(hf + 1) * 512],
                                    start=(mc == 0), stop=(mc == 7)), r=["mgT", "woutb"], w=["pw%d" % pb])
                        I("dve", lambda e, pb=pb: e.tensor_tensor(
                            out=tm[pb][:], in0=pw[pb][:, :, :].rearrange("p a n -> p (a n)"), in1=g1gate[:, b, :], op=ALU.mult),
                          r=["pw%d" % pb, "g1gate"], w=["tm%d" % pb])
                        I("pool", lambda e, pb=pb: e.tensor_tensor(out=tm[pb][:], in0=tm[pb][:], in1=xt[pb][:], op=ALU.add),
                          r=["tm%d" % pb, "xo%d" % pb], w=["tm%d" % pb])
                        Dm(x1_d[b * SEQ + l0:b * SEQ + l0 + 128, :], tm[pb][:], r=["tm%d" % pb], w=["x1s"])
                    kb.barrier()
                if upto == "M1d":
                    return nc

    if upto == "M1":
        return nc
    with contextlib.ExitStack() as sc:
        uin = [sb(sc, "uin%d" % i, [128, 4, D]) for i in range(2)]
        ubf = [sb(sc, "ubf%d" % i, [128, 4, D], BF16) for i in range(2)]
        uts = [sb(sc, "uts%d" % i, [128, 8, 512], BF16) for i in range(2)]
        vin = [sb(sc, "vin%d" % i, [128, 4, D]) for i in range(2)]
        vbf = [sb(sc, "vbf%d" % i, [128, 4, D], BF16) for i in range(2)]
        ptc = [ps(sc, "ptc%d" % i, [128, 8, 128], BF16) for i in range(4)]
        tcn = 0
        for g in range(32):
            j = g % 2
            e0 = g * 512
            Dm(uin[j][:], pu_d[e0:e0 + 512, :].rearrange("(c p) n -> p c n", p=128), w=["uin%d" % j])
            Dm(vin[j][:], pv_d[e0:e0 + 512, :].rearrange("(c p) n -> p c n", p=128), w=["vin%d" % j])
            I("dve", lambda e, j=j: e.tensor_copy(out=ubf[j][:], in_=uin[j][:]), r=["uin%d" % j], w=["ubf%d" % j])
            I("pool", lambda e, j=j: e.tensor_copy(out=vbf[j][:], in_=vin[j][:]), r=["vin%d" % j], w=["vbf%d" % j])
            Dm(vs_d[e0:e0 + 512, :].rearrange("(c p) n -> p c n", p=128), vbf[j][:], r=["vbf%d" % j], w=["v_scr"])
            for c in range(4):
                p = ptc[tcn % 4]
                pk = "ptc%d" % (tcn % 4)
                tcn += 1
                for fc in range(8):
                    I("pe", lambda e, p=p, j=j, c=c, fc=fc: e.transpose(p[:, fc, :], ubf[j][:, c, fc * 128:(fc + 1) * 128], identb[:]),
                      r=["ubf%d" % j, "identb"], w=[pk])
                I("act", lambda e, p=p, j=j, c=c: e.copy(out=uts[j][:, :, c * 128:(c + 1) * 128], in_=p[:, :, :]),
                  r=[pk], w=["uts%d" % j])
            for hh in range(2):
                Dm(ut_d[2 * g + hh, :, :, :], uts[j][:, :, hh * 256:(hh + 1) * 256], r=["uts%d" % j], w=["ut_scr"])
        kb.barrier()

    if upto == "C":
        return nc
    with contextlib.ExitStack() as sp_:
        g2row = sb(sp_, "g2row", [128, D])
        gfrow = sb(sp_, "gfrow", [128, D])
        g2gate = sb(sp_, "g2gate", [128, D])
        wqb = sb(sp_, "wqb", [128, 8, 2048], BF16)
        keysb = sb(sp_, "keysb", [128, 16, 128], BF16)
        WT = sb(sp_, "WT", [128, 128, TB], BF16)
        NBUF = 4
        utb = [sb(sp_, "utb%d" % i, [128, 8, 256], BF16) for i in range(NBUF)]
        vtb = [sb(sp_, "vtb%d" % i, [128, 2, D], BF16) for i in range(NBUF)]
        x1t = [sb(sp_, "x1t%d" % i, [128, D]) for i in range(2)]
        u2T = sb(sp_, "u2T", [128, 8, TB], BF16)
        q2T = sb(sp_, "q2T", [128, 16, TB], BF16)
        sc_ = sb(sp_, "sc", [128, 16, 128])
        eq = sc_[:, :, :].rearrange("p (h a) (b c) -> p h (a b) c", a=2, c=16)
        cs = sb(sp_, "cs", [128, 8, 256])
        A16 = [sc_[:, 8 * i:8 * i + 8, :].rearrange("p a n -> p (a n)").bitcast(BF16).rearrange("p (t i) -> p t i", i=128) for i in range(2)]
        B16 = [cs[:, 4 * i:4 * i + 4, :].rearrange("p a n -> p (a n)").bitcast(BF16).rearrange("p (t i) -> p t i", i=128) for i in range(2)]
        wk = sb(sp_, "wk", [128, 128])
        tv = sb(sp_, "tv", [128, 16, 16])
        tix = sb(sp_, "tix", [128, 16, 16], U32)
        tif = sb(sp_, "tif", [128, 16, 16])
        sv = sb(sp_, "sv", [128, 8, 16])
        spx = sb(sp_, "spx", [128, 8, 16], U32)
        spi = sb(sp_, "spi", [128, 8, 16], U32)
        pf = sb(sp_, "pf", [128, 8, 16])
        qf = sb(sp_, "qf", [128, 8, 16])
        If_ = sb(sp_, "If", [128, 128])
        Jf_ = sb(sp_, "Jf", [128, 128])
        Wf_ = sb(sp_, "Wf", [128, 128])
        zs = sb(sp_, "zs", [128, 8])
        IT = sb(sp_, "IT", [128, 128])
        JT = sb(sp_, "JT", [128, 128])
        WTr = sb(sp_, "WTr", [128, 128])
        NSL = 4
        gl = [sb(sp_, "gl%d" % i, [128, TB]) for i in range(NSL)]
        awt = [sb(sp_, "awt%d" % i, [128, TB], BF16) for i in range(NSL)]
        sq2 = sb(sp_, "sq2", [128, D])
        ss2 = sb(sp_, "ss2", [128, 1])
        tmy = sb(sp_, "tmy", [128, D])
        py = [ps(sp_, "py%d" % i, [128, 2, 512]) for i in range(2)]
        pa = [ps(sp_, "pa%d" % i, [128, 512]) for i in range(4)]

        Dm(g2row[:], g2_d[0:1, :].partition_broadcast(128), w=["g2row"])
        Dm(gfrow[:], gf_d[0:1, :].partition_broadcast(128), w=["gfrow"])
        for kc in range(8):
            for hh in range(2):
                Dm(tmy[:], wq_d[kc * 128:(kc + 1) * 128, hh * 1024:(hh + 1) * 1024], w=["tmy"])
                I("dve", lambda e, kc=kc, hh=hh: e.tensor_copy(out=wqb[:, kc, hh * 1024:(hh + 1) * 1024], in_=tmy[:]), r=["tmy"], w=["wqb"])
        for hh in range(2):
            Dm(tmy[:], keysT_d[:, hh * 8:(hh + 1) * 8, :].rearrange("p a n -> p (a n)"), w=["tmy"])
            I("dve", lambda e, hh=hh: e.tensor_copy(out=keysb[:, hh * 8:(hh + 1) * 8, :].rearrange("p a n -> p (a n)"), in_=tmy[:]), r=["tmy"], w=["keysb"])

        NG = 64
        nblk = 2 * SEQ // TB
        if upto is not None and upto.startswith('P'):
            nblk = int(upto[1:])

        def load_group(gi):
            j = gi % NBUF
            e0 = (gi % NG) * 256
            Dm(utb[j][:], ut_d[gi % NG, :, :, :], r=["ut_scr"], w=["utb%d" % j])
            Dm(vtb[j][:], vs_d[e0:e0 + 256, :].rearrange("(c p) n -> p c n", p=128), r=["v_scr"], w=["vtb%d" % j])

        total_groups = nblk * NG
        for g_ in range(min(NBUF, total_groups)):
            load_group(g_)
        pac = [0]

        def nextpa():
            k = pac[0] % 4
            pac[0] += 1
            return pa[k], "pa%d" % k

        LOOK = 3
        for tb_ in range(nblk):
            b = tb_ // (SEQ // TB)
            r0 = tb_ * TB
            if tb_ % (SEQ // TB) == 0:
                Dm(g2gate[:], grow_d[2 + b:3 + b, :].partition_broadcast(128), r=["grow_scr"], w=["g2gate"])
            for ti in range(2):
                xk = "x1t%d" % ti
                Dm(x1t[ti][:], x1_d[r0 + ti * 128:r0 + (ti + 1) * 128, :], r=["x1s"], w=[xk])
                I("act", lambda e, ti=ti: e.activation(out=tmy[:], in_=x1t[ti][:], func=AF.Square, accum_out=ss2[:]),
                  r=[xk], w=["tmy", "ss2"])
                I("act", lambda e: e.activation(out=ss2[:], in_=ss2[:], func=AF.Sqrt, scale=1.0 / D, bias=epsb[:]),
                  r=["ss2", "epsb"], w=["ss2"])
                I("dve", lambda e: e.reciprocal(out=ss2[:], in_=ss2[:]), r=["ss2"], w=["ss2"])
                I("dve", lambda e, ti=ti: e.scalar_tensor_tensor(
                    out=sq2[:], in0=x1t[ti][:], scalar=ss2[:], in1=g2row[:], op0=ALU.mult, op1=ALU.mult),
                  r=[xk, "ss2", "g2row"], w=["sq2"])
                for hf in range(2):
                    p, pk = nextpa()
                    for f4 in range(4):
                        fc = hf * 4 + f4
                        I("pe", lambda e, p=p, fc=fc, f4=f4: e.transpose(p[:, f4 * 128:(f4 + 1) * 128], sq2[:, fc * 128:(fc + 1) * 128], ident[:]),
                          r=["sq2", "ident"], w=[pk])
                    for f4 in range(4):
                        fc = hf * 4 + f4
                        I("dve", lambda e, p=p, fc=fc, f4=f4, ti=ti, b=b: e.tensor_scalar(
                            out=u2T[:, fc, ti * 128:(ti + 1) * 128], in0=p[:, f4 * 128:(f4 + 1) * 128],
                            scalar1=modp[:, 4, fc, b:b + 1], scalar2=modp[:, 3, fc, b:b + 1], op0=ALU.mult, op1=ALU.add),
                          r=[pk, "modp"], w=["u2T"])
            for hp in range(16):
                p, pk = nextpa()
                for kc in range(8):
                    I("pe", lambda e, p=p, hp=hp, kc=kc: e.matmul(
                        p[:, 0:TB], lhsT=wqb[:, kc, hp * 128:(hp + 1) * 128], rhs=u2T[:, kc, :], start=(kc == 0), stop=(kc == 7)),
                      r=["wqb", "u2T"], w=[pk])
                I("act", lambda e, p=p, hp=hp: e.copy(out=q2T[:, hp, :], in_=p[:, 0:TB]), r=[pk], w=["q2T"])
            for ti in range(2):
                for qd_ in range(4):
                    p, pk = nextpa()
                    for hh in range(4):
                        hp = qd_ * 4 + hh
                        I("pe", lambda e, p=p, hp=hp, hh=hh, ti=ti: e.matmul(
                            p[:, hh * 128:(hh + 1) * 128], lhsT=q2T[:, hp, ti * 128:(ti + 1) * 128], rhs=keysb[:, hp, :],
                            start=True, stop=True), r=["q2T", "keysb"], w=[pk])
                    I("act", lambda e, p=p, qd_=qd_: e.copy(
                        out=sc_[:, qd_ * 4:(qd_ + 1) * 4, :], in_=p[:, :].rearrange("p (a n) -> p a n", n=128)),
                      r=[pk], w=["sc0", "sc1"])
                for hp in range(16):
                    I("dve", lambda e, hp=hp: e.max(out=tv[:, hp, 0:8], in_=sc_[:, hp, :]), r=["sc0", "sc1"], w=["tv"])
                    I("dve", lambda e, hp=hp: e.max_index(out=tix[:, hp, 0:8], in_max=tv[:, hp, 0:8], in_values=sc_[:, hp, :]),
                      r=["sc0", "sc1", "tv"], w=["tix"])
                    I("dve", lambda e, hp=hp: e.match_replace(out=wk[:], in_to_replace=tv[:, hp, 0:8], in_values=sc_[:, hp, :], imm_value=NEG),
                      r=["sc0", "sc1", "tv"], w=["wk"])
                    I("dve", lambda e, hp=hp: e.max(out=tv[:, hp, 8:16], in_=wk[:]), r=["wk"], w=["tv"])
                    I("dve", lambda e, hp=hp: e.max_index(out=tix[:, hp, 8:16], in_max=tv[:, hp, 8:16], in_values=wk[:]),
                      r=["wk", "tv"], w=["tix"])
                I("dve", lambda e: e.tensor_copy(out=tif[:], in_=tix[:]), r=["tix"], w=["tif"])
                tv4 = tv[:, :, :].rearrange("p (h s) k -> p h s k", s=2)
                tif4 = tif[:, :, :].rearrange("p (h s) k -> p h s k", s=2)
                I("dve", lambda e, tv4=tv4: e.tensor_tensor(
                    out=cs[:, :, :].rearrange("p h (a c) -> p h a c", c=16),
                    in0=tv4[:, :, 0, :].unsqueeze(3).to_broadcast([128, 8, 16, 16]),
                    in1=tv4[:, :, 1, :].unsqueeze(2).to_broadcast([128, 8, 16, 16]), op=ALU.add), r=["tv"], w=["cs0", "cs1"])
                for h in range(8):
                    I("dve", lambda e, h=h: e.max(out=sv[:, h, 0:8], in_=cs[:, h, :]), r=["cs0", "cs1"], w=["sv"])
                    I("dve", lambda e, h=h: e.max_index(out=spx[:, h, 0:8], in_max=sv[:, h, 0:8], in_values=cs[:, h, :]),
                      r=["cs0", "cs1", "sv"], w=["spx"])
                    I("dve", lambda e, h=h: e.match_replace(out=cs[:, h, :], in_to_replace=sv[:, h, 0:8], in_values=cs[:, h, :], imm_value=NEG),
                      r=["cs0", "cs1", "sv"], w=["cs0", "cs1"])
                    I("dve", lambda e, h=h: e.max(out=sv[:, h, 8:16], in_=cs[:, h, :]), r=["cs0", "cs1"], w=["sv"])
                    I("dve", lambda e, h=h: e.max_index(out=spx[:, h, 8:16], in_max=sv[:, h, 8:16], in_values=cs[:, h, :]),
                      r=["cs0", "cs1", "sv"], w=["spx"])
                I("dve", lambda e: e.tensor_single_scalar(out=spi[:], in_=spx[:], scalar=4, op=ALU.logical_shift_right), r=["spx"], w=["spi"])
                I("dve", lambda e: e.tensor_copy(out=pf[:], in_=spi[:]), r=["spi"], w=["pf"])
                I("dve", lambda e: e.tensor_single_scalar(out=spi[:], in_=spx[:], scalar=15, op=ALU.bitwise_and), r=["spx", "pf"], w=["spi"])
                I("dve", lambda e: e.tensor_copy(out=qf[:], in_=spi[:]), r=["spi"], w=["qf"])
                io16 = iota[:, 0:16].unsqueeze(1).unsqueeze(1).to_broadcast([128, 8, 16, 16])
                for (rf, side, dst, dk) in ((pf, 0, If_, "If"), (qf, 1, Jf_, "Jf")):
                    I("dve", lambda e, rf=rf: e.tensor_tensor(
                        out=eq, in0=rf[:, :, :].unsqueeze(3).to_broadcast([128, 8, 16, 16]), in1=io16, op=ALU.is_equal),
                      r=["pf", "qf", "iota"], w=["sc0", "sc1"])
                    I("dve", lambda e, side=side, tif4=tif4: e.tensor_tensor(
                        out=eq, in0=eq, in1=tif4[:, :, side, :].unsqueeze(2).to_broadcast([128, 8, 16, 16]), op=ALU.mult),
                      r=["sc0", "sc1", "tif"], w=["sc0", "sc1"])
                    I("dve", lambda e, dst=dst: e.tensor_reduce(
                        out=dst[:, :].rearrange("p (h k) -> p h k", k=16), in_=eq, axis=AX.X, op=ALU.add), r=["sc0", "sc1"], w=[dk])
                I("dve", lambda e: e.tensor_tensor(out=sv[:], in0=sv[:], in1=sv[:, :, 0:1].to_broadcast([128, 8, 16]), op=ALU.subtract),
                  r=["sv"], w=["sv"])
                I("act", lambda e: e.activation(out=sv[:], in_=sv[:], func=AF.Exp), r=["sv"], w=["sv"])
                I("dve", lambda e: e.tensor_reduce(out=zs[:], in_=sv[:], axis=AX.X, op=ALU.add), r=["sv"], w=["zs"])
                I("dve", lambda e: e.reciprocal(out=zs[:], in_=zs[:]), r=["zs"], w=["zs"])
                I("dve", lambda e: e.tensor_tensor(
                    out=Wf_[:, :].rearrange("p (h k) -> p h k", k=16), in0=sv[:], in1=zs[:, :].unsqueeze(2).to_broadcast([128, 8, 16]),
                    op=ALU.mult), r=["sv", "zs"], w=["Wf"])
                for (src, sk, dst, dk) in ((If_, "If", IT, "IT"), (Jf_, "Jf", JT, "JT"), (Wf_, "Wf", WTr, "WTr")):
                    p, pk = nextpa()
                    I("pe", lambda e, p=p, src=src: e.transpose(p[:, 0:128], src[:], ident[:]), r=[sk, "ident"], w=[pk])
                    I("act", lambda e, p=p, dst=dst: e.copy(out=dst[:], in_=p[:, 0:128]), r=[pk], w=[dk])
                if upto == "S0" and ti == 1:
                    kb.barrier()
                    return nc
                iob = iota[:, :].unsqueeze(1).to_broadcast([128, 16, 128])
                for bi in range(8):
                    j2 = bi % 2
                    t0 = bi * 16

                    I("dve", lambda e, j2=j2, t0=t0: e.tensor_tensor(
                        out=A16[j2], in0=iob, in1=IT[:, t0:t0 + 16].unsqueeze(2).to_broadcast([128, 16, 128]), op=ALU.is_equal),
                      r=["iota", "IT"], w=["sc%d" % j2])
                    I("pool", lambda e, j2=j2, t0=t0: e.tensor_tensor(
                        out=A16[j2], in0=A16[j2], in1=WTr[:, t0:t0 + 16].unsqueeze(2).to_broadcast([128, 16, 128]), op=ALU.mult),
                      r=["sc%d" % j2, "WTr"], w=["sc%d" % j2])
                    I("dve", lambda e, j2=j2, t0=t0: e.tensor_tensor(
                        out=B16[j2], in0=iob, in1=JT[:, t0:t0 + 16].unsqueeze(2).to_broadcast([128, 16, 128]), op=ALU.is_equal),
                      r=["iota", "JT"], w=["cs%d" % j2])
                    for t4 in range(4):
                        p, pk = nextpa()
                        for tt in range(4):
                            tl = t4 * 4 + tt
                            I("pe", lambda e, p=p, tt=tt, tl=tl, j2=j2: e.matmul(
                                p[:, tt * 128:(tt + 1) * 128], lhsT=B16[j2][:, tl, :], rhs=A16[j2][:, tl, :], start=True, stop=True),
                              r=["sc%d" % j2, "cs%d" % j2], w=[pk])
                        tg0 = ti * 128 + t0 + t4 * 4
                        I("act", lambda e, p=p, tg0=tg0: e.copy(
                            out=WT[:, :, tg0:tg0 + 4].rearrange("p i t -> p t i"), in_=p[:, :].rearrange("p (t i) -> p t i", i=128)),
                          r=[pk], w=["WT"])
            if upto == "S1":
                kb.barrier()
                return nc
            slots = {}

            def emitU(ic):
                gi = tb_ * NG + ic // 2
                j = gi % NBUF
                c = ic % 2
                p, pk = nextpa()
                sl = ic % NSL
                slots[ic] = sl
                for kc in range(8):
                    I("pe", lambda e, p=p, j=j, c=c, kc=kc: e.matmul(
                        p[:, 0:TB], lhsT=utb[j][:, kc, c * 128:(c + 1) * 128], rhs=u2T[:, kc, :], start=(kc == 0), stop=(kc == 7)),
                      r=["utb%d" % j, "u2T"], w=[pk])
                I("act", lambda e, p=p, sl=sl: e.activation(out=gl[sl][:], in_=p[:, 0:TB], func=AF.Gelu), r=[pk], w=["gl%d" % sl])
                I("dve", lambda e, sl=sl, ic=ic: e.tensor_tensor(out=awt[sl][:], in0=gl[sl][:], in1=WT[:, ic, :], op=ALU.mult),
                  r=["gl%d" % sl, "WT"], w=["awt%d" % sl])

            def emitV(ic):
                gi = tb_ * NG + ic // 2
                j = gi % NBUF
                c = ic % 2
                sl = slots.pop(ic)
                for ti in range(2):
                    for hf in range(2):
                        I("pe", lambda e, sl=sl, ti=ti, hf=hf, j=j, c=c, ic=ic: e.matmul(
                            py[ti][:, hf, :], lhsT=awt[sl][:, ti * 128:(ti + 1) * 128], rhs=vtb[j][:, c, hf * 512:(hf + 1) * 512],
                            start=(ic == 0), stop=(ic == 127)), r=["awt%d" % sl, "vtb%d" % j], w=["py%d" % ti])
                if c == 1 and gi + NBUF < total_groups:
                    load_group(gi + NBUF)

            for ic in range(-LOOK, 128):
                if ic + LOOK < 128:
                    emitU(ic + LOOK)
                if ic >= 0:
                    emitV(ic)
            for ti in range(2):
                xk = "x1t%d" % ti
                I("dve", lambda e, ti=ti: e.tensor_tensor(
                    out=tmy[:], in0=py[ti][:, :, :].rearrange("p a n -> p (a n)"), in1=g2gate[:], op=ALU.mult),
                  r=["py%d" % ti, "g2gate"], w=["tmy"])
                I("pool", lambda e, ti=ti: e.tensor_tensor(out=tmy[:], in0=tmy[:], in1=x1t[ti][:], op=ALU.add), r=["tmy", xk], w=["tmy"])
                I("act", lambda e: e.activation(out=sq2[:], in_=tmy[:], func=AF.Square, accum_out=ss2[:]), r=["tmy"], w=["sq2", "ss2"])
                I("act", lambda e: e.activation(out=ss2[:], in_=ss2[:], func=AF.Sqrt, scale=1.0 / D, bias=epsb[:]),
                  r=["ss2", "epsb"], w=["ss2"])
                I("dve", lambda e: e.reciprocal(out=ss2[:], in_=ss2[:]), r=["ss2"], w=["ss2"])
                I("dve", lambda e: e.scalar_tensor_tensor(
                    out=sq2[:], in0=tmy[:], scalar=ss2[:], in1=gfrow[:], op0=ALU.mult, op1=ALU.mult),
                  r=["tmy", "ss2", "gfrow"], w=["sq2"])
                l0 = (tb_ % (SEQ // TB)) * TB + ti * 128
                Dm(out_d[b, l0:l0 + 128, :], sq2[:], r=["sq2"], w=["out"])
        kb.barrier(engines=("sp",))
    return nc


def _pos_table():
    rows = SEQ // 64
    r, col = np.meshgrid(np.arange(rows, dtype=np.float32), np.arange(64, dtype=np.float32), indexing="ij")
    quarter = D // 4
    omega = (np.float32(10000.0) ** (-np.arange(quarter, dtype=np.float32) / np.float32(quarter))).astype(np.float32)

    def emb(p):
        ang = p.reshape(-1)[:, None].astype(np.float32) * omega[None, :]
        return np.concatenate([np.sin(ang), np.cos(ang)], axis=-1)

    return np.concatenate([emb(r), emb(col)], axis=-1).astype(np.float32)


_NC_CACHE = {}


def _fm(v, nchunk):
    return np.ascontiguousarray(np.asarray(v, np.float32).reshape(nchunk, 128).T)


def kernel(x, c, ctx, c_ctx, ada_w, ada_b, norm1_g, w_in, conv_w, conv_b, rg_w_a, rg_b_a, rg_w_x, rg_b_x, rg_lambda,
           gla_w_g, gla_b_g, gla_norm_g, w_out, norm2_g, peer_w_q, peer_keys, peer_u, peer_v, final_norm_g):
    if "nc" not in _NC_CACHE:
        _NC_CACHE["nc"] = build()
    nc = _NC_CACHE["nc"]
    in_maps = make_in_maps(x, c, ctx, c_ctx, ada_w, ada_b, norm1_g, w_in, conv_w, conv_b, rg_w_a, rg_b_a, rg_w_x, rg_b_x, rg_lambda,
                           gla_w_g, gla_b_g, gla_norm_g, w_out, norm2_g, peer_w_q, peer_keys, peer_u, peer_v, final_norm_g)
    res = run_bass_kernel_spmd(nc, in_maps, core_ids=list(range(NCORES)))
    out = np.concatenate([np.asarray(r["out"], dtype=np.float32) for r in res.results], axis=0)
    return out


def make_in_maps(x, c, ctx, c_ctx, ada_w, ada_b, norm1_g, w_in, conv_w, conv_b, rg_w_a, rg_b_a, rg_w_x, rg_b_x, rg_lambda,
                 gla_w_g, gla_b_g, gla_norm_g, w_out, norm2_g, peer_w_q, peer_keys, peer_u, peer_v, final_norm_g):
    f = lambda a: np.ascontiguousarray(np.asarray(a, dtype=np.float32))
    x, c, ctx, c_ctx = f(x), f(c), f(ctx), f(c_ctx)
    jj, ii = np.meshgrid(np.arange(128), np.arange(128), indexing="ij")
    mf = (jj <= ii).astype(np.float32)
    mb = (jj >= ii).astype(np.float32)
    shared = {
        "ada_w": f(ada_w[0]),
        "ada_b": f(ada_b[0]).reshape(1, -1),
        "ada_bT": _fm(ada_b[0], 48),
        "norm1_g": f(norm1_g[0]).reshape(1, -1),
        "norm2_g": f(norm2_g[0]).reshape(1, -1),
        "final_g": f(final_norm_g).reshape(1, -1),
        "w_in": f(w_in[0]),
        "convwT": np.ascontiguousarray(f(conv_w[0]).reshape(4, 4, 128).transpose(2, 1, 0)),
        "convbT": _fm(conv_b[0], 4),
        "rg_w_a": f(rg_w_a[0]),
        "rg_w_x": f(rg_w_x[0]),
        "rgbaT": np.ascontiguousarray(f(rg_b_a[0]).reshape(2, 4, 128).transpose(2, 0, 1)),
        "rgbxT": np.ascontiguousarray(f(rg_b_x[0]).reshape(2, 4, 128).transpose(2, 0, 1)),
        "rglamT": np.ascontiguousarray(f(rg_lambda[0]).reshape(2, 4, 128).transpose(2, 0, 1)),
        "gla_w_g": f(gla_w_g[0]),
        "glabgT": np.ascontiguousarray(f(gla_b_g[0]).reshape(2, 2, 128).transpose(2, 0, 1)),
        "glangT": f(gla_norm_g[0]).reshape(128, 1),
        "w_out": f(w_out[0]),
        "peer_w_q": f(peer_w_q[0]),
        "keysT": np.ascontiguousarray(f(peer_keys[0]).reshape(16, 128, 128).transpose(2, 0, 1)),
        "peer_u": f(peer_u[0]),
        "peer_v": f(peer_v[0]),
        "pos": _pos_table(),
        "ident": np.eye(128, dtype=np.float32),
        "iota": np.tile(np.arange(128, dtype=np.float32)[None, :], (128, 1)),
        "maskf": np.ascontiguousarray(np.concatenate([mf, mf], axis=1)),
        "maskb": np.ascontiguousarray(np.concatenate([mb, mb], axis=1)),
    }
    in_maps = []
    for i in range(NCORES):
        b0 = 2 * i
        cm = np.stack([c[b0], c[b0 + 1], c_ctx], axis=0)
        cT = np.ascontiguousarray(cm.reshape(3, 8, 128).transpose(2, 1, 0))
        m = dict(shared)
        m["x"] = np.ascontiguousarray(x[b0:b0 + 2])
        m["ctx"] = np.ascontiguousarray(ctx[b0:b0 + 2])
        m["cT"] = cT
        in_maps.append(m)
    return in_maps
```

```python
import contextlib
import numpy as np
import concourse.bass as bass
import concourse.mybir as mybir
from concourse.bass_utils import run_bass_kernel_spmd

F32 = mybir.dt.float32
BF16 = mybir.dt.bfloat16
U32 = mybir.dt.uint32
ALU = mybir.AluOpType
AF = mybir.ActivationFunctionType
AX = mybir.AxisListType

NCORES = 8
D = 1024
SEQ = 2048
CTX = 256
S = CTX + SEQ
NT = S // 128
INC = 2592
NEXP = 16384
TB = 256
EPS = 1e-6
NEG = -1.0e30


class KB:
    def __init__(self):
        self.nc = bass.Bass("TRN2", target_bir_lowering=False)
        nc = self.nc
        self.eng = {"pe": nc.tensor, "act": nc.scalar, "dve": nc.vector, "pool": nc.gpsimd, "sp": nc.sync}
        self.stack = contextlib.ExitStack()
        self.sem = {}
        self.cnt = {}
        for n in self.eng:
            self.sem[("e", n)] = self.stack.enter_context(nc.semaphore("s_" + n))
            self.cnt[n] = 0
        self.nd = 24
        self.dcnt = [0] * self.nd
        for i in range(self.nd):
            self.sem[("d", i)] = self.stack.enter_context(nc.semaphore("d%d" % i))
        self.dnext = 0
        self.waited = {}
        self.lastw = {}
        self.readers = {}

    def _wait(self, en, deps):
        for sk, v in deps.items():
            if en == "pe" and sk == ("e", "pe"):
                continue
            if self.waited.get((en, sk), 0) >= v:
                continue
            self.eng[en].wait_ge(self.sem[sk], v)
            self.waited[(en, sk)] = v

    def _deps(self, r, w):
        d = {}
        for k in r:
            t = self.lastw.get(k)
            if t:
                d[t[0]] = max(d.get(t[0], 0), t[1])
        for k in w:
            t = self.lastw.get(k)
            if t:
                d[t[0]] = max(d.get(t[0], 0), t[1])
            for sk, v in self.readers.get(k, {}).items():
                d[sk] = max(d.get(sk, 0), v)
        return d

    def _commit(self, tok, r, w):
        for k in w:
            self.lastw[k] = tok
            self.readers[k] = {}
        for k in r:
            rd = self.readers.setdefault(k, {})
            rd[tok[0]] = max(rd.get(tok[0], 0), tok[1])

    def I(self, en, fn, r=(), w=()):
        self._wait(en, self._deps(r, w))
        inst = fn(self.eng[en])
        self.cnt[en] += 1
        inst.then_inc(self.sem[("e", en)], 1)
        self._commit((("e", en), self.cnt[en]), r, w)

    def D(self, out, in_, r=(), w=(), q="sp"):
        i = self.dnext
        self.dnext = (i + 1) % self.nd
        deps = self._deps(r, w)
        if self.dcnt[i] > 0:
            deps[("d", i)] = max(deps.get(("d", i), 0), self.dcnt[i])
        self._wait(q, deps)
        inst = self.eng[q].dma_start(out=out, in_=in_)
        self.dcnt[i] += 16
        inst.then_inc(self.sem[("d", i)], 16)
        self._commit((("d", i), self.dcnt[i]), r, w)

    def barrier(self, engines=("pe", "act", "dve", "pool", "sp")):
        deps = {}
        for n in self.eng:
            if self.cnt[n] > 0:
                deps[("e", n)] = self.cnt[n]
        for i in range(self.nd):
            if self.dcnt[i] > 0:
                deps[("d", i)] = self.dcnt[i]
        for en in engines:
            d = dict(deps)
            d.pop(("e", en), None)
            self._wait(en, d)
        self.lastw = {}
        self.readers = {}


def build(upto=None, dbg=False):
    kb = KB()
    nc = kb.nc
    I, Dm = kb.I, kb.D

    def dram(name, shape, dt=F32, kind="ExternalInput"):
        return nc.dram_tensor(name, list(shape), dt, kind=kind).ap()

    x_d = dram("x", [2, SEQ, D])
    ctx_d = dram("ctx", [2, CTX, D])
    cT_d = dram("cT", [128, 8, 3])
    adaw_d = dram("ada_w", [D, 6 * D])
    adab_d = dram("ada_b", [1, 6 * D])
    adabT_d = dram("ada_bT", [128, 48])
    g1_d = dram("norm1_g", [1, D])
    g2_d = dram("norm2_g", [1, D])
    gf_d = dram("final_g", [1, D])
    win_d = dram("w_in", [D, INC])
    convw_d = dram("convwT", [128, 4, 4])
    convb_d = dram("convbT", [128, 4])
    rgwa_d = dram("rg_w_a", [2, 8, 64, 64])
    rgwx_d = dram("rg_w_x", [2, 8, 64, 64])
    rgba_d = dram("rgbaT", [128, 2, 4])
    rgbx_d = dram("rgbxT", [128, 2, 4])
    rglam_d = dram("rglamT", [128, 2, 4])
    glawg_d = dram("gla_w_g", [2, 16, 256])
    glabg_d = dram("glabgT", [128, 2, 2])
    glang_d = dram("glangT", [128, 1])
    wout_d = dram("w_out", [D, D])
    wq_d = dram("peer_w_q", [D, 2048])
    keysT_d = dram("keysT", [128, 16, 128])
    pu_d = dram("peer_u", [NEXP, D])
    pv_d = dram("peer_v", [NEXP, D])
    pos_d = dram("pos", [SEQ, D])
    ident_d = dram("ident", [128, 128])
    iota_d = dram("iota", [128, 128])
    maskf_d = dram("maskf", [128, 256])
    maskb_d = dram("maskb", [128, 256])
    out_d = dram("out", [2, SEQ, D], kind="ExternalOutput")
    x1_d = dram("x1s", [2 * SEQ, D], kind="ExternalOutput" if dbg else "Internal")
    grow_d = dram("grow_scr", [4, D], kind="Internal")
    ut_d = dram("ut_scr", [NEXP // 256, 128, 8, 256], BF16, kind="Internal")
    vs_d = dram("v_scr", [NEXP, D], BF16, kind="Internal")

    es = contextlib.ExitStack()

    uid = [0]

    def sb(stack, name, shape, dt=F32):
        uid[0] += 1
        return stack.enter_context(nc.sbuf_tensor("sb%d_%s" % (uid[0], name), list(shape), dt))

    def ps(stack, name, shape, dt=F32):
        uid[0] += 1
        return stack.enter_context(nc.psum_tensor("ps%d_%s" % (uid[0], name), list(shape), dt))

    ident = sb(es, "ident", [128, 128])
    identb = sb(es, "identb", [128, 128], BF16)
    iota = sb(es, "iota", [128, 128])
    ones = sb(es, "ones", [128, 128])
    modp = sb(es, "modp", [128, 6, 8, 3])
    epsb = sb(es, "epsb", [128, 1])

    Dm(ident[:], ident_d[:, :], w=["ident"])
    Dm(iota[:], iota_d[:, :], w=["iota"])
    I("dve", lambda e: e.tensor_copy(out=identb[:], in_=ident[:]), r=["ident"], w=["identb"])
    I("dve", lambda e: e.memset(ones[:], 1.0), w=["ones"])
    I("dve", lambda e: e.memset(epsb[:], EPS), w=["epsb"])

    with contextlib.ExitStack() as st:
        cT = sb(st, "cT", [128, 8, 3])
        scT = sb(st, "scT", [128, 8, 3])
        rep = sb(st, "rep", [128, 2, 8, 128])
        abT = sb(st, "abT", [128, 48])
        abrow = sb(st, "abrow", [128, D])
        growt = [sb(st, "growt%d" % i, [128, D]) for i in range(2)]
        aw = [sb(st, "aw%d" % i, [128, 8, D]) for i in range(2)]
        pm = [ps(st, "pm%d" % i, [128, 512]) for i in range(4)]
        Dm(cT[:], cT_d[:, :, :], w=["cT"])
        Dm(abT[:], adabT_d[:, :], w=["abT"])
        I("act", lambda e: e.activation(out=scT[:], in_=cT[:], func=AF.Silu), r=["cT"], w=["scT"])
        for b in range(2):
            I("dve", lambda e, b=b: e.tensor_copy(out=rep[:, b], in_=scT[:, :, b:b + 1].to_broadcast([128, 8, 128])),
              r=["scT"], w=["rep"])
        pi = 0
        for m in range(6):
            a = aw[m % 2]
            ak = "aw%d" % (m % 2)
            Dm(a[:], adaw_d[:, m * D:(m + 1) * D].rearrange("(kc p) n -> p kc n", p=128), w=[ak])
            if m in (0, 1, 3, 4):
                p = pm[pi % 4]
                pk = "pm%d" % (pi % 4)
                pi += 1
                for fc in range(8):
                    for kc in range(8):
                        I("pe", lambda e, fc=fc, kc=kc, p=p, a=a: e.matmul(
                            p[:, fc * 3:fc * 3 + 3], lhsT=a[:, kc, fc * 128:(fc + 1) * 128], rhs=scT[:, kc, :],
                            start=(kc == 0), stop=(kc == 7)), r=[ak, "scT"], w=[pk])
                I("dve", lambda e, m=m, p=p: e.tensor_tensor(
                    out=modp[:, m], in0=p[:, 0:24].rearrange("p (f c) -> p f c", c=3),
                    in1=abT[:, m * 8:(m + 1) * 8].unsqueeze(2).to_broadcast([128, 8, 3]), op=ALU.add),
                  r=[pk, "abT"], w=["modp"])
                if m in (1, 4):
                    I("dve", lambda e, m=m: e.tensor_scalar_add(out=modp[:, m], in0=modp[:, m], scalar1=1.0),
                      r=["modp"], w=["modp"])
            else:
                mi = 0 if m == 2 else 1
                Dm(abrow[:], adab_d[0:1, m * D:(m + 1) * D].partition_broadcast(128), w=["abrow"])
                for b in range(2):
                    for hf in range(2):
                        p = pm[pi % 4]
                        pk = "pm%d" % (pi % 4)
                        pi += 1
                        for kc in range(8):
                            I("pe", lambda e, kc=kc, p=p, a=a, b=b, hf=hf: e.matmul(
                                p[:, :], lhsT=rep[:, b, kc, :], rhs=a[:, kc, hf * 512:(hf + 1) * 512],
                                start=(kc == 0), stop=(kc == 7)), r=[ak, "rep"], w=[pk])
                        I("dve", lambda e, p=p, b=b, hf=hf, mi=mi: e.tensor_tensor(
                            out=growt[b][:, hf * 512:(hf + 1) * 512], in0=p[:, :],
                            in1=abrow[:, hf * 512:(hf + 1) * 512], op=ALU.add),
                          r=[pk, "abrow"], w=["growt%d" % b])
                    Dm(grow_d[mi * 2 + b:mi * 2 + b + 1, :], growt[b][0:1, :], r=["growt%d" % b], w=["grow_scr"])
        kb.barrier()
    if upto == "M0":
        return nc

    with contextlib.ExitStack() as sm:
        g1row = sb(sm, "g1row", [128, D])
        g1gate = sb(sm, "g1gate", [128, 2, D])
        for b in range(2):
            Dm(g1gate[:, b, :], grow_d[b:b + 1, :].partition_broadcast(128), r=["grow_scr"], w=["g1gate"])
        maskf = sb(sm, "maskf", [128, 256], BF16)
        maskb = sb(sm, "maskb", [128, 256], BF16)
        mstage = sb(sm, "mstage", [128, 256])
        cw = sb(sm, "cw", [128, 4, 4])
        cb = sb(sm, "cb", [128, 4])
        rba = sb(sm, "rba", [128, 2, 4])
        rbx = sb(sm, "rbx", [128, 2, 4])
        rlam = sb(sm, "rlam", [128, 2, 4])
        coef = sb(sm, "coef", [128, 2, 4])
        gbg = sb(sm, "gbg", [128, 2, 2])
        ngbg = sb(sm, "ngbg", [128, 2, 2])
        gng = sb(sm, "gng", [128, 1])
        wg = sb(sm, "wg", [32, 2, 256])
        wbd = sb(sm, "wbd", [128, 2, 2, 4, 128])
        hm = sb(sm, "hm", [128, 2])
        bdm = sb(sm, "bdm", [128, 256])
        I("dve", lambda e: e.memset(hm[:], 0.0), w=["hm"])
        I("dve", lambda e: e.memset(hm[0:64, 0:1], 1.0), w=["hm"])
        I("dve", lambda e: e.memset(hm[64:128, 1:2], 1.0), w=["hm"])
        I("dve", lambda e: e.memset(bdm[:], 0.0), w=["bdm"])
        I("dve", lambda e: e.memset(bdm[0:64, 0:128], 1.0), w=["bdm"])
        I("dve", lambda e: e.memset(bdm[64:128, 128:256], 1.0), w=["bdm"])
        Dm(g1row[:], g1_d[0:1, :].partition_broadcast(128), w=["g1row"])
        Dm(mstage[:], maskf_d[:, :], w=["mstage"])
        I("dve", lambda e: e.tensor_copy(out=maskf[:], in_=mstage[:]), r=["mstage"], w=["maskf"])
        Dm(mstage[:], maskb_d[:, :], w=["mstage"])
        I("dve", lambda e: e.tensor_copy(out=maskb[:], in_=mstage[:]), r=["mstage"], w=["maskb"])
        Dm(cw[:], convw_d[:, :, :], w=["cw"])
        Dm(cb[:], convb_d[:, :], w=["cb"])
        Dm(rba[:], rgba_d[:, :, :], w=["rba"])
        Dm(rbx[:], rgbx_d[:, :, :], w=["rbx"])
        Dm(rlam[:], rglam_d[:, :, :], w=["rlam"])
        Dm(gbg[:], glabg_d[:, :, :], w=["gbg"])
        Dm(gng[:], glang_d[:, :], w=["gng"])
        I("act", lambda e: e.activation(out=coef[:], in_=rlam[:], func=AF.Exp, scale=-1.0), r=["rlam"], w=["coef"])
        I("act", lambda e: e.activation(out=coef[:], in_=coef[:], func=AF.Ln, bias=1.0), r=["coef"], w=["coef"])
        I("dve", lambda e: e.tensor_scalar_mul(out=coef[:], in0=coef[:], scalar1=-8.0), r=["coef"], w=["coef"])
        I("dve", lambda e: e.tensor_scalar_mul(out=ngbg[:], in0=gbg[:], scalar1=-1.0), r=["gbg"], w=["ngbg"])
        I("dve", lambda e: e.memset(wg[:], 0.0), w=["wg"])
        for d in range(2):
            Dm(wg[16 * d:16 * d + 16, d, :], glawg_d[d, :, :], w=["wg"])
        I("dve", lambda e: e.memset(wbd[:], 0.0), w=["wbd"])
        for gi, src in enumerate((rgwa_d, rgwx_d)):
            for d in range(2):
                for cc in range(4):
                    for h in range(2):
                        Dm(wbd[64 * h:64 * h + 64, gi, d, cc, 64 * h:64 * h + 64], src[d, 2 * cc + h, :, :], w=["wbd"])

        for b in range(2):
            with contextlib.ExitStack() as sbt:
                mgT = sb(sbt, "mgT", [128, 8, SEQ], BF16)
                qT = sb(sbt, "qT", [128, 2, SEQ], BF16)
                kT = sb(sbt, "kT", [128, 2, S], BF16)
                vtok = sb(sbt, "vtok", [128, NT, 512], BF16)
                lrT = sb(sbt, "lrT", [32, S])
                sR = contextlib.ExitStack()
                rxT = sb(sR, "rxT", [128, 4, S], BF16)
                with contextlib.ExitStack() as s1:
                    winb = sb(s1, "winb", [128, 8, INC], BF16)
                    wst0 = sb(s1, "wst0", [128, INC // 2])
                    wst = [wst0, wst0]
                    uT0 = sb(s1, "uT0", [128, 8, 512], BF16)
                    uT = [uT0, uT0]
                    xt = [sb(s1, "xt%d" % i, [128, D]) for i in range(2)]
                    pt0 = sb(s1, "pt0", [128, D])
                    pt = [pt0, pt0]
                    xn = [sb(s1, "xn%d" % i, [128, D], BF16) for i in range(2)]
                    sq = sb(s1, "sqj", [128, D], BF16)
                    ss = [sb(s1, "ss%d" % i, [128, 1]) for i in range(2)]
                    tp = [ps(s1, "tp%d" % i, [128, 8, 128], BF16) for i in range(2)]
                    pj = [ps(s1, "pj%d" % i, [128, 512]) for i in range(4)]
                    hw = INC // 2
                    for kc in range(8):
                        for hh in range(2):
                            w_ = wst[hh]
                            Dm(w_[:], win_d[kc * 128:(kc + 1) * 128, hh * hw:(hh + 1) * hw], w=["wst0"])
                            I("pool" if hh else "dve", lambda e, w_=w_, kc=kc, hh=hh: e.tensor_copy(
                                out=winb[:, kc, hh * hw:(hh + 1) * hw], in_=w_[:]), r=["wst0"], w=["winb"])
                    ngroups = 5
                    pjc = 0
                    for g in range(ngroups):
                        t0 = g * 4
                        nt = min(4, NT - t0)
                        ntok = nt * 128
                        u = uT[g % 2]
                        uk = "uT0"
                        for ti in range(nt):
                            t = t0 + ti
                            pb = t % 2
                            xk, xnk, ssk, tpk, ptk = "xt%d" % pb, "xn%d" % pb, "ss%d" % pb, "tp%d" % pb, "pt0"
                            if t < 2:
                                Dm(xt[pb][:], ctx_d[b, t * 128:(t + 1) * 128, :], w=[xk])
                                col = 2
                            else:
                                l0 = (t - 2) * 128
                                Dm(xt[pb][:], x_d[b, l0:l0 + 128, :], w=[xk])
                                Dm(pt[pb][:], pos_d[l0:l0 + 128, :], w=[ptk])
                                I("pool", lambda e, pb=pb: e.tensor_tensor(out=xt[pb][:], in0=xt[pb][:], in1=pt[pb][:], op=ALU.add),
                                  r=[xk, ptk], w=[xk])
                                col = b
                            I("act", lambda e, pb=pb: e.activation(out=sq[:], in_=xt[pb][:], func=AF.Square, accum_out=ss[pb][:]),
                              r=[xk], w=["sqj", ssk])
                            I("act", lambda e, pb=pb: e.activation(out=ss[pb][:], in_=ss[pb][:], func=AF.Sqrt, scale=1.0 / D, bias=epsb[:]),
                              r=[ssk, "epsb"], w=[ssk])
                            I("dve", lambda e, pb=pb: e.reciprocal(out=ss[pb][:], in_=ss[pb][:]), r=[ssk], w=[ssk])
                            I("dve", lambda e, pb=pb: e.scalar_tensor_tensor(
                                out=xn[pb][:], in0=xt[pb][:], scalar=ss[pb][:], in1=g1row[:], op0=ALU.mult, op1=ALU.mult),
                              r=[xk, ssk, "g1row"], w=[xnk])
                            for fc in range(8):
                                I("pe", lambda e, pb=pb, fc=fc: e.transpose(tp[pb][:, fc, :], xn[pb][:, fc * 128:(fc + 1) * 128], identb[:]),
                                  r=[xnk, "identb"], w=[tpk])
                            for fc in range(8):
                                I("dve" if fc % 2 else "pool" if False else "dve", lambda e, pb=pb, fc=fc, ti=ti, u=u, col=col: e.tensor_scalar(
                                    out=u[:, fc, ti * 128:(ti + 1) * 128], in0=tp[pb][:, fc, :],
                                    scalar1=modp[:, 1, fc, col:col + 1], scalar2=modp[:, 0, fc, col:col + 1],
                                    op0=ALU.mult, op1=ALU.add), r=[tpk, "modp"], w=[uk])
                        c0 = t0 * 128
                        lat0 = max(c0, CTX)
                        for cch in range(21):
                            cs_ = cch * 128
                            ncol = 128 if cch < 20 else 32
                            p = pj[pjc % 4]
                            pk = "pj%d" % (pjc % 4)
                            pjc += 1
                            if 12 <= cch < 16:
                                continue
                            lat_only = (4 <= cch < 10) or (16 <= cch < 20)
                            if lat_only and lat0 >= c0 + ntok:
                                continue
                            for kc in range(8):
                                I("pe", lambda e, kc=kc, p=p, u=u, cs_=cs_, ncol=ncol, ntok=ntok: e.matmul(
                                    p[0:ncol, 0:ntok], lhsT=winb[:, kc, cs_:cs_ + ncol], rhs=u[:, kc, 0:ntok],
                                    start=(kc == 0), stop=(kc == 7)), r=["winb", uk], w=[pk])
                            o0 = lat0 - c0
                            if cch < 4:
                                I("act", lambda e, p=p, cch=cch, c0=c0, ntok=ntok: e.copy(out=rxT[:, cch, c0:c0 + ntok], in_=p[:, 0:ntok]),
                                  r=[pk], w=["rxT"])
                            elif cch < 8:
                                I("act", lambda e, p=p, cch=cch, o0=o0, lat0=lat0, ntok=ntok: e.activation(
                                    out=mgT[:, cch - 4, lat0 - CTX:lat0 - CTX + ntok - o0], in_=p[:, o0:ntok], func=AF.Gelu),
                                  r=[pk], w=["mgT"])
                            elif cch < 10:
                                I("dve", lambda e, p=p, cch=cch, o0=o0, lat0=lat0, ntok=ntok: e.tensor_scalar_mul(
                                    out=qT[:, cch - 8, lat0 - CTX:lat0 - CTX + ntok - o0], in0=p[:, o0:ntok], scalar1=0.125),
                                  r=[pk], w=["qT"])
                            elif cch < 12:
                                I("dve", lambda e, p=p, cch=cch, c0=c0, ntok=ntok: e.tensor_copy(out=kT[:, cch - 10, c0:c0 + ntok], in_=p[:, 0:ntok]),
                                  r=[pk], w=["kT"])
                            elif cch < 20:
                                I("act", lambda e, p=p, cch=cch, o0=o0, lat0=lat0, ntok=ntok: e.activation(
                                    out=mgT[:, 4 + cch - 16, lat0 - CTX:lat0 - CTX + ntok - o0], in_=p[:, o0:ntok], func=AF.Silu),
                                  r=[pk], w=["mgT"])
                            else:
                                I("dve", lambda e, p=p, c0=c0, ntok=ntok: e.tensor_copy(out=lrT[:, c0:c0 + ntok], in_=p[0:32, 0:ntok]),
                                  r=[pk], w=["lrT"])
                        for ti in range(nt):
                            p = pj[pjc % 4]
                            pk = "pj%d" % (pjc % 4)
                            pjc += 1
                            for kc in range(8):
                                I("pe", lambda e, kc=kc, p=p, u=u, ti=ti: e.matmul(
                                    p[:, :], lhsT=u[:, kc, ti * 128:(ti + 1) * 128], rhs=winb[:, kc, 1536:2048],
                                    start=(kc == 0), stop=(kc == 7)), r=["winb", uk], w=[pk])
                            I("act", lambda e, p=p, t=t0 + ti: e.copy(out=vtok[:, t, :], in_=p[:, :]), r=[pk], w=["vtok"])
                    kb.barrier()

                if upto == "M1a":
                    sR.close()
                    return nc
                with contextlib.ExitStack() as s2:
                    xc = sb(s2, "xc", [128, S])
                    gr = sb(s2, "gr", [128, S])
                    gi_ = sb(s2, "gi", [128, S])
                    aa = sb(s2, "aa", [128, S])
                    bb = sb(s2, "bb", [128, S])
                    hf_ = sb(s2, "hf", [128, S])
                    hb_ = sb(s2, "hb", [128, S])
                    pg = [ps(s2, "pg%d" % i, [128, 512]) for i in range(4)]
                    pgc = 0
                    segs = ((0, CTX), (CTX, S))
                    for cc in range(4):
                        I("dve", lambda e, cc=cc: e.tensor_scalar(
                            out=xc[:], in0=rxT[:, cc, :], scalar1=cw[:, cc, 2:3], scalar2=cb[:, cc:cc + 1],
                            op0=ALU.mult, op1=ALU.add), r=["rxT", "cw", "cb"], w=["xc"])
                        for (a0, a1) in segs:
                            for j, sh in ((0, -2), (1, -1), (3, 1)):
                                lo = max(a0, a0 - sh)
                                hi = min(a1, a1 - sh)
                                I("dve", lambda e, cc=cc, j=j, sh=sh, lo=lo, hi=hi: e.scalar_tensor_tensor(
                                    out=xc[:, lo:hi], in0=rxT[:, cc, lo + sh:hi + sh], scalar=cw[:, cc, j:j + 1],
                                    in1=xc[:, lo:hi], op0=ALU.mult, op1=ALU.add), r=["rxT", "cw", "xc"], w=["xc"])
                        for d in range(2):
                            for gi, (gt, gk, bias_t) in enumerate(((gr, "gr", rba), (gi_, "gi", rbx))):
                                for g in range(5):
                                    c0 = g * 512
                                    n = min(512, S - c0)
                                    p = pg[pgc % 4]
                                    pk = "pg%d" % (pgc % 4)
                                    pgc += 1
                                    I("pe", lambda e, p=p, gi=gi, d=d, cc=cc, c0=c0, n=n: e.matmul(
                                        p[:, 0:n], lhsT=wbd[:, gi, d, cc, :], rhs=xc[:, c0:c0 + n], start=True, stop=True),
                                      r=["wbd", "xc"], w=[pk])
                                    I("act", lambda e, p=p, gt=gt, bias_t=bias_t, d=d, cc=cc, c0=c0, n=n: e.activation(
                                        out=gt[:, c0:c0 + n], in_=p[:, 0:n], func=AF.Sigmoid, bias=bias_t[:, d, cc:cc + 1]),
                                      r=[pk], w=[gk])
                            I("act", lambda e, d=d, cc=cc: e.activation(out=aa[:], in_=gr[:], func=AF.Exp, scale=coef[:, d, cc:cc + 1]),
                              r=["gr", "coef"], w=["aa"])
                            I("pool", lambda e: e.tensor_tensor(out=bb[:], in0=aa[:], in1=aa[:], op=ALU.mult), r=["aa"], w=["bb"])
                            I("act", lambda e: e.activation(out=bb[:], in_=bb[:], func=AF.Sqrt, scale=-1.0, bias=1.0), r=["bb"], w=["bb"])
                            I("pool", lambda e: e.tensor_tensor(out=bb[:], in0=bb[:], in1=gi_[:], op=ALU.mult), r=["bb", "gi"], w=["bb"])
                            I("pool", lambda e: e.tensor_tensor(out=bb[:], in0=bb[:], in1=xc[:], op=ALU.mult), r=["bb", "xc"], w=["bb"])
                            if d == 0:
                                I("dve", lambda e: e.tensor_tensor_scan(out=hf_[:], data0=aa[:], data1=bb[:], initial=0.0,
                                                                         op0=ALU.mult, op1=ALU.add), r=["aa", "bb"], w=["hf"])
                            else:
                                I("dve", lambda e: e.tensor_tensor_scan(out=hb_[:, 0:CTX][:, ::-1], data0=aa[:, 0:CTX][:, ::-1],
                                                                         data1=bb[:, 0:CTX][:, ::-1], initial=0.0,
                                                                         op0=ALU.mult, op1=ALU.add), r=["aa", "bb"], w=["hb"])
                                I("dve", lambda e: e.tensor_tensor_scan(out=hb_[:, CTX:S][:, ::-1], data0=aa[:, CTX:S][:, ::-1],
                                                                         data1=bb[:, CTX:S][:, ::-1], initial=hb_[:, 0:1],
                                                                         op0=ALU.mult, op1=ALU.add), r=["aa", "bb", "hb"], w=["hb"])
                        I("dve", lambda e: e.tensor_tensor(out=hf_[:, CTX:S], in0=hf_[:, CTX:S], in1=hb_[:, CTX:S], op=ALU.add),
                          r=["hf", "hb"], w=["hf"])
                        I("dve", lambda e, cc=cc: e.tensor_tensor(out=mgT[:, cc, :], in0=mgT[:, cc, :], in1=hf_[:, CTX:S], op=ALU.mult),
                          r=["hf", "mgT"], w=["mgT"])
                    kb.barrier()

                if upto == "M1b":
                    sR.close()
                    return nc
                sR.close()
                with contextlib.ExitStack() as s3:
                    otot = sb(s3, "otot", [128, 4, SEQ])
                    la = sb(s3, "la", [128, S])
                    bc = sb(s3, "bc", [128, S])
                    eb = sb(s3, "eb", [128, S])
                    qd = sb(s3, "qd", [128, SEQ], BF16)
                    ki = sb(s3, "ki", [128, S], BF16)
                    kih = [sb(s3, "kih%d" % i, [128, S], BF16) for i in range(2)]
                    kit = [sb(s3, "kit%d" % i, [128, 128], BF16) for i in range(2)]
                    Sst = sb(s3, "Sst", [128, 256])
                    Sbf = [sb(s3, "Sbf%d" % i, [128, 256], BF16) for i in range(2)]
                    dsg = [sb(s3, "dsg%d" % i, [128, 256]) for i in range(2)]
                    scb = [sb(s3, "scb%d" % i, [128, 256], BF16) for i in range(2)]
                    pz = [ps(s3, "pz%d" % i, [128, 512]) for i in range(2)]
                    ptr = [ps(s3, "ptr%d" % i, [128, 128], BF16) for i in range(2)]
                    pds = [ps(s3, "pds%d" % i, [128, 256]) for i in range(2)]
                    psc = [ps(s3, "psc%d" % i, [128, 256]) for i in range(1)]
                    po = [ps(s3, "po%d" % i, [128, 256]) for i in range(1)]
                    zc = 0
                    cn = 0
                    first_o = {0: True, 1: True}
                    for d in range(2):
                        order = list(range(NT)) if d == 0 else [1, 0] + list(range(NT - 1, 1, -1))
                        mk, mkk = (maskf, "maskf") if d == 0 else (maskb, "maskb")
                        for pr in range(2):
                            for g in range(5):
                                c0 = g * 512
                                n = min(512, S - c0)
                                p = pz[zc % 2]
                                pk = "pz%d" % (zc % 2)
                                zc += 1
                                I("pe", lambda e, p=p, d=d, pr=pr, c0=c0, n=n: e.matmul(
                                    p[:, 0:n], lhsT=wg[:, d, pr * 128:(pr + 1) * 128], rhs=lrT[:, c0:c0 + n], start=True, stop=True),
                                  r=["wg", "lrT"], w=[pk])
                                I("act", lambda e, p=p, d=d, pr=pr, c0=c0, n=n: e.activation(
                                    out=la[:, c0:c0 + n], in_=p[:, 0:n], func=AF.Exp, scale=-1.0, bias=ngbg[:, d, pr:pr + 1]),
                                  r=[pk, "ngbg"], w=["la"])
                            I("act", lambda e: e.activation(out=la[:], in_=la[:], func=AF.Ln, bias=1.0), r=["la"], w=["la"])
                            I("pool", lambda e: e.tensor_scalar_mul(out=la[:], in0=la[:], scalar1=-1.0 / 16.0), r=["la"], w=["la"])
                            for n_ in range(NT):
                                c0 = n_ * 128
                                if d == 0:
                                    I("dve", lambda e, c0=c0: e.tensor_tensor_scan(
                                        out=bc[:, c0:c0 + 128], data0=ones[:, :], data1=la[:, c0:c0 + 128], initial=0.0,
                                        op0=ALU.mult, op1=ALU.add), r=["la", "ones"], w=["bc"])
                                else:
                                    I("dve", lambda e, c0=c0: e.tensor_tensor_scan(
                                        out=bc[:, c0:c0 + 128][:, ::-1], data0=ones[:, :], data1=la[:, c0:c0 + 128][:, ::-1], initial=0.0,
                                        op0=ALU.mult, op1=ALU.add), r=["la", "ones"], w=["bc"])
                            I("act", lambda e: e.activation(out=eb[:], in_=bc[:], func=AF.Exp), r=["bc"], w=["eb"])
                            I("act", lambda e: e.activation(out=la[:], in_=bc[:], func=AF.Exp, scale=-1.0), r=["bc"], w=["la"])
                            I("pool", lambda e, pr=pr: e.tensor_tensor(out=qd[:], in0=qT[:, pr, :], in1=eb[:, CTX:S], op=ALU.mult),
                              r=["qT", "eb"], w=["qd"])
                            I("dve", lambda e, pr=pr: e.tensor_tensor(out=ki[:], in0=kT[:, pr, :], in1=la[:], op=ALU.mult),
                              r=["kT", "la"], w=["ki"])
                            for h in range(2):
                                I("pool", lambda e, h=h: e.tensor_scalar_mul(out=kih[h][:], in0=ki[:], scalar1=hm[:, h:h + 1]),
                                  r=["ki", "hm"], w=["kih%d" % h])
                            I("dve", lambda e: e.memset(Sst[:], 0.0), w=["Sst"])
                            if upto == "G1":
                                kb.barrier()
                                return nc
                            for n_ in order:
                                c0 = n_ * 128
                                gcol = c0 + 127 if d == 0 else c0
                                j = cn % 2
                                cn += 1
                                I("pe", lambda e, j=j, c0=c0: e.transpose(ptr[j][:, :], ki[:, c0:c0 + 128], identb[:]),
                                  r=["ki", "identb"], w=["ptr%d" % j])
                                I("act", lambda e, j=j: e.copy(out=kit[j][:], in_=ptr[j][:, :]), r=["ptr%d" % j], w=["kit%d" % j])
                                I("pe", lambda e, j=j, n_=n_, pr=pr: e.matmul(
                                    pds[j][:, :], lhsT=kit[j][:, :], rhs=vtok[:, n_, pr * 256:(pr + 1) * 256], start=True, stop=True),
                                  r=["kit%d" % j, "vtok"], w=["pds%d" % j])
                                I("pool", lambda e, j=j: e.tensor_tensor(out=Sbf[j][:], in0=Sst[:], in1=bdm[:], op=ALU.mult),
                                  r=["Sst", "bdm"], w=["Sbf%d" % j])
                                if n_ >= 2:
                                    l0 = c0 - CTX
                                    for h in range(2):
                                        I("pe", lambda e, h=h, c0=c0, l0=l0: e.matmul(
                                            psc[0][:, h * 128:(h + 1) * 128], lhsT=kih[h][:, c0:c0 + 128],
                                            rhs=qd[:, l0:l0 + 128], start=True, stop=True),
                                          r=["kih%d" % h, "qd"], w=["psc0"])
                                    I("dve", lambda e, j=j, mk=mk: e.tensor_tensor(out=scb[j][:], in0=psc[0][:, :], in1=mk[:], op=ALU.mult),
                                      r=["psc0", mkk], w=["scb%d" % j])
                                    for h in range(2):
                                        hd = pr * 2 + h
                                        I("pe", lambda e, h=h, hd=hd, j=j, n_=n_: e.matmul(
                                            po[0][:, h * 128:(h + 1) * 128], lhsT=vtok[:, n_, hd * 128:(hd + 1) * 128],
                                            rhs=scb[j][:, h * 128:(h + 1) * 128], start=True, stop=False),
                                          r=["vtok", "scb%d" % j], w=["po0"])
                                        I("pe", lambda e, h=h, j=j, l0=l0: e.matmul(
                                            po[0][:, h * 128:(h + 1) * 128], lhsT=Sbf[j][:, h * 128:(h + 1) * 128],
                                            rhs=qd[:, l0:l0 + 128], start=False, stop=True),
                                          r=["Sbf%d" % j, "qd"], w=["po0"])
                                    ov = otot[:, pr * 2:pr * 2 + 2, l0:l0 + 128]
                                    pv_ = po[0][:, :].rearrange("p (h t) -> p h t", h=2)
                                    if d == 0:
                                        I("act", lambda e, ov=ov, pv_=pv_: e.copy(out=ov, in_=pv_), r=["po0"], w=["otot"])
                                    else:
                                        I("dve", lambda e, ov=ov, pv_=pv_: e.tensor_tensor(out=ov, in0=ov, in1=pv_, op=ALU.add),
                                          r=["po0", "otot"], w=["otot"])
                                I("dve", lambda e, j=j: e.tensor_tensor(out=dsg[j][:], in0=pds[j][:, :], in1=Sst[:], op=ALU.add),
                                  r=["pds%d" % j, "Sst"], w=["dsg%d" % j])
                                I("pool", lambda e, j=j, gcol=gcol: e.tensor_scalar_mul(out=Sst[:], in0=dsg[j][:], scalar1=eb[:, gcol:gcol + 1]),
                                  r=["dsg%d" % j, "eb", "Sbf%d" % j], w=["Sst"])
                                if (upto == "G2" and n_ == 0) or (upto == "G3" and n_ == 2):
                                    kb.barrier()
                                    return nc
                    sqo = sb(s3, "sqo", [128, 512])
                    rs = sb(s3, "rs", [128, 512])
                    tmpo = sb(s3, "tmpo", [128, 512])
                    for hd in range(4):
                        for g in range(4):
                            c0 = g * 512
                            p = pz[zc % 2]
                            pk = "pz%d" % (zc % 2)
                            zc += 1
                            I("act", lambda e, hd=hd, c0=c0: e.activation(out=sqo[:], in_=otot[:, hd, c0:c0 + 512], func=AF.Square),
                              r=["otot"], w=["sqo"])
                            I("pe", lambda e, p=p: e.matmul(p[:, :], lhsT=ones[:, :], rhs=sqo[:], start=True, stop=True),
                              r=["ones", "sqo"], w=[pk])
                            I("act", lambda e, p=p: e.activation(out=rs[:], in_=p[:, :], func=AF.Sqrt, scale=1.0 / 128.0, bias=epsb[:]),
                              r=[pk, "epsb"], w=["rs"])
                            I("dve", lambda e: e.reciprocal(out=rs[:], in_=rs[:]), r=["rs"], w=["rs"])
                            I("dve", lambda e, hd=hd, c0=c0: e.scalar_tensor_tensor(
                                out=tmpo[:], in0=otot[:, hd, c0:c0 + 512], scalar=gng[:, 0:1], in1=rs[:], op0=ALU.mult, op1=ALU.mult),
                              r=["otot", "gng", "rs"], w=["tmpo"])
                            I("pool", lambda e, hd=hd, c0=c0: e.tensor_tensor(
                                out=mgT[:, 4 + hd, c0:c0 + 512], in0=mgT[:, 4 + hd, c0:c0 + 512], in1=tmpo[:], op=ALU.mult),
                              r=["tmpo", "mgT"], w=["mgT"])
                    kb.barrier()

                if upto == "M1c":
                    return nc
                with contextlib.ExitStack() as s4:
                    woutb = sb(s4, "woutb", [128, 8, D], BF16)
                    wst2 = [sb(s4, "wso%d" % i, [128, D]) for i in range(2)]
                    xt = [sb(s4, "xo%d" % i, [128, D]) for i in range(2)]
                    pt = [sb(s4, "po_%d" % i, [128, D]) for i in range(2)]
                    tm = [sb(s4, "tm%d" % i, [128, D]) for i in range(2)]
                    pw = [ps(s4, "pw%d" % i, [128, 2, 512]) for i in range(2)]
                    for kc in range(8):
                        w_ = wst2[kc % 2]
                        Dm(w_[:], wout_d[kc * 128:(kc + 1) * 128, :], w=["wso%d" % (kc % 2)])
                        I("dve", lambda e, w_=w_, kc=kc: e.tensor_copy(out=woutb[:, kc, :], in_=w_[:]), r=["wso%d" % (kc % 2)], w=["woutb"])
                    for t in range(16):
                        pb = t % 2
                        l0 = t * 128
                        Dm(xt[pb][:], x_d[b, l0:l0 + 128, :], w=["xo%d" % pb])
                        Dm(pt[pb][:], pos_d[l0:l0 + 128, :], w=["po_%d" % pb])
                        I("pool", lambda e, pb=pb: e.tensor_tensor(out=xt[pb][:], in0=xt[pb][:], in1=pt[pb][:], op=ALU.add),
                          r=["xo%d" % pb, "po_%d" % pb], w=["xo%d" % pb])
                        for hf in range(2):
                            for mc in range(8):
                                I("pe", lambda e, pb=pb, hf=hf, mc=mc, l0=l0: e.matmul(
                                    pw[pb][:, hf, :], lhsT=mgT[:, mc, l0:l0 + 128], rhs=woutb[:, mc, hf * 512:(hf + 1) * 512],
                                    start=(mc == 0), stop=(mc == 7)), r=["mgT", "woutb"], w=["pw%d" % pb])
                        I("dve", lambda e, pb=pb: e.tensor_tensor(
                            out=tm[pb][:], in0=pw[pb][:, :, :].rearrange("p a n -> p (a n)"), in1=g1gate[:, b, :], op=ALU.mult),
                          r=["pw%d" % pb, "g1gate"], w=["tm%d" % pb])
                        I("pool", lambda e, pb=pb: e.tensor_tensor(out=tm[pb][:], in0=tm[pb][:], in1=xt[pb][:], op=ALU.add),
                          r=["tm%d" % pb, "xo%d" % pb], w=["tm%d" % pb])
                        Dm(x1_d[b * SEQ + l0:b * SEQ + l0 + 128, :], tm[pb][:], r=["tm%d" % pb], w=["x1s"])
                    kb.barrier()
                if upto == "M1d":
                    return nc

    if upto == "M1":
        return nc
    with contextlib.ExitStack() as sc:
        uin = [sb(sc, "uin%d" % i, [128, 4, D]) for i in range(2)]
        ubf = [sb(sc, "ubf%d" % i, [128, 4, D], BF16) for i in range(2)]
        uts = [sb(sc, "uts%d" % i, [128, 8, 512], BF16) for i in range(2)]
        vin = [sb(sc, "vin%d" % i, [128, 4, D]) for i in range(2)]
        vbf = [sb(sc, "vbf%d" % i, [128, 4, D], BF16) for i in range(2)]
        ptc = [ps(sc, "ptc%d" % i, [128, 8, 128], BF16) for i in range(4)]
        tcn = 0
        for g in range(32):
            j = g % 2
            e0 = g * 512
            Dm(uin[j][:], pu_d[e0:e0 + 512, :].rearrange("(c p) n -> p c n", p=128), w=["uin%d" % j])
            Dm(vin[j][:], pv_d[e0:e0 + 512, :].rearrange("(c p) n -> p c n", p=128), w=["vin%d" % j])
            I("dve", lambda e, j=j: e.tensor_copy(out=ubf[j][:], in_=uin[j][:]), r=["uin%d" % j], w=["ubf%d" % j])
            I("pool", lambda e, j=j: e.tensor_copy(out=vbf[j][:], in_=vin[j][:]), r=["vin%d" % j], w=["vbf%d" % j])
            Dm(vs_d[e0:e0 + 512, :].rearrange("(c p) n -> p c n", p=128), vbf[j][:], r=["vbf%d" % j], w=["v_scr"])
            for c in range(4):
                p = ptc[tcn % 4]
                pk = "ptc%d" % (tcn % 4)
                tcn += 1
                for fc in range(8):
                    I("pe", lambda e, p=p, j=j, c=c, fc=fc: e.transpose(p[:, fc, :], ubf[j][:, c, fc * 128:(fc + 1) * 128], identb[:]),
                      r=["ubf%d" % j, "identb"], w=[pk])
                I("act", lambda e, p=p, j=j, c=c: e.copy(out=uts[j][:, :, c * 128:(c + 1) * 128], in_=p[:, :, :]),
                  r=[pk], w=["uts%d" % j])
            for hh in range(2):
                Dm(ut_d[2 * g + hh, :, :, :], uts[j][:, :, hh * 256:(hh + 1) * 256], r=["uts%d" % j], w=["ut_scr"])
        kb.barrier()

    if upto == "C":
        return nc
    with contextlib.ExitStack() as sp_:
        g2row = sb(sp_, "g2row", [128, D])
        gfrow = sb(sp_, "gfrow", [128, D])
        g2gate = sb(sp_, "g2gate", [128, D])
        wqb = sb(sp_, "wqb", [128, 8, 2048], BF16)
        keysb = sb(sp_, "keysb", [128, 16, 128], BF16)
        WT = sb(sp_, "WT", [128, TB, 128], BF16)
        NBUF = 4
        utb = [sb(sp_, "utb%d" % i, [128, 8, 256], BF16) for i in range(NBUF)]
        vtb = [sb(sp_, "vtb%d" % i, [128, 2, D], BF16) for i in range(NBUF)]
        x1s_ = sb(sp_, "x1s_", [128, D])
        u2T = [sb(sp_, "u2T%d" % i, [128, 8, TB], BF16) for i in range(2)]
        q2T = sb(sp_, "q2T", [128, 16, TB], BF16)
        sc_ = sb(sp_, "sc", [128, 16, 128])
        eq = sc_[:, :, :].rearrange("p (h a) (b c) -> p h (a b) c", a=2, c=16)
        cs = sb(sp_, "cs", [128, 8, 256])
        A16 = [sc_[:, 8 * i:8 * i + 4, :].rearrange("p a n -> p (a n)").bitcast(BF16).rearrange("p (t i) -> p t i", i=64) for i in range(2)]
        B16 = [cs[:, 4 * i:4 * i + 4, :].rearrange("p a n -> p (a n)").bitcast(BF16).rearrange("p (t i) -> p t i", i=128) for i in range(2)]
        tv = sb(sp_, "tv", [128, 16, 16])
        tix = sb(sp_, "tix", [128, 16, 16], U32)
        tif = sb(sp_, "tif", [128, 16, 16])
        sv = sb(sp_, "sv", [128, 8, 16])
        spx = sb(sp_, "spx", [128, 8, 16], U32)
        spi = sb(sp_, "spi", [128, 8, 16], U32)
        pf = sb(sp_, "pf", [128, 8, 16])
        qf = sb(sp_, "qf", [128, 8, 16])
        If_ = sb(sp_, "If", [128, 128])
        Jf_ = sb(sp_, "Jf", [128, 128])
        Wf_ = sb(sp_, "Wf", [128, 128])
        zs = sb(sp_, "zs", [128, 8])
        IT = sb(sp_, "IT", [128, 2, 128])
        JT = sb(sp_, "JT", [128, 2, 128])
        WTr = sb(sp_, "WTr", [128, 2, 128])
        NSL = 3
        gl = [sb(sp_, "gl%d" % i, [128, 2 * TB]) for i in range(NSL)]
        awt = [sb(sp_, "awt%d" % i, [128, 2 * TB], BF16) for i in range(NSL)]
        sq2 = sb(sp_, "sq2", [128, D])
        ss2 = sb(sp_, "ss2", [128, 1])
        ss3 = sb(sp_, "ss3", [128, 1])
        py = [ps(sp_, "py%d" % i, [128, 2, 512]) for i in range(2)]
        pg_ = [ps(sp_, "pg%d" % i, [128, 512]) for i in range(3)]
        pb_ = [ps(sp_, "pb%d" % i, [128, 512]) for i in range(1)]
        if dbg:
            print("phase P sbuf bytes remaining:", nc.sbuf_bytes_remaining)

        Dm(g2row[:], g2_d[0:1, :].partition_broadcast(128), w=["g2row"])
        Dm(gfrow[:], gf_d[0:1, :].partition_broadcast(128), w=["gfrow"])
        for kc in range(8):
            for hh in range(2):
                Dm(sq2[:], wq_d[kc * 128:(kc + 1) * 128, hh * 1024:(hh + 1) * 1024], w=["sq2"])
                I("dve", lambda e, kc=kc, hh=hh: e.tensor_copy(out=wqb[:, kc, hh * 1024:(hh + 1) * 1024], in_=sq2[:]), r=["sq2"], w=["wqb"])
        for hh in range(2):
            Dm(sq2[:], keysT_d[:, hh * 8:(hh + 1) * 8, :].rearrange("p a n -> p (a n)"), w=["sq2"])
            I("dve", lambda e, hh=hh: e.tensor_copy(out=keysb[:, hh * 8:(hh + 1) * 8, :].rearrange("p a n -> p (a n)"), in_=sq2[:]), r=["sq2"], w=["keysb"])

        NG = 64
        nblk = 2 * SEQ // TB
        if upto is not None and upto.startswith('P'):
            nblk = int(upto[1:])
        NBB = SEQ // TB

        def load_group(gi):
            j = gi % NBUF
            e0 = (gi % NG) * 256
            Dm(utb[j][:], ut_d[gi % NG, :, :, :], r=["ut_scr"], w=["utb%d" % j])
            Dm(vtb[j][:], vs_d[e0:e0 + 256, :].rearrange("(c p) n -> p c n", p=128), r=["v_scr"], w=["vtb%d" % j])

        total_groups = nblk * NG
        for g_ in range(min(NBUF, total_groups)):
            load_group(g_)
        pbc = [0]

        def nextpb():
            k = 0
            pbc[0] += 1
            return pb_[k], "pb%d" % k

        LOOK = 2
        PULL = 4
        SC = ["sc_%d" % i for i in range(16)]
        CS = ["cs_%d" % i for i in range(8)]
        TV = ["tv_%d" % i for i in range(16)]
        TIX = ["tix_%d" % i for i in range(16)]
        SV = ["sv_%d" % i for i in range(8)]
        SPX = ["spx_%d" % i for i in range(8)]
        SCH = [SC[0:4], SC[8:12]]
        CSH = [CS[0:4], CS[4:8]]

        def stageS(tb_):
            b = tb_ // NBB
            r0 = tb_ * TB
            u2 = u2T[tb_ % 2]
            uk = "u2T%d" % (tb_ % 2)
            for ti in range(2):
                Dm(x1s_[:], x1_d[r0 + ti * 128:r0 + (ti + 1) * 128, :], r=["x1s"], w=["x1s_"])
                I("act", lambda e: e.activation(out=sq2[:], in_=x1s_[:], func=AF.Square, accum_out=ss2[:]),
                  r=["x1s_"], w=["sq2", "ss2"])
                I("act", lambda e: e.activation(out=ss2[:], in_=ss2[:], func=AF.Sqrt, scale=1.0 / D, bias=epsb[:]),
                  r=["ss2", "epsb"], w=["ss2"])
                I("dve", lambda e: e.reciprocal(out=ss2[:], in_=ss2[:]), r=["ss2"], w=["ss2"])
                I("dve", lambda e: e.scalar_tensor_tensor(
                    out=sq2[:], in0=x1s_[:], scalar=ss2[:], in1=g2row[:], op0=ALU.mult, op1=ALU.mult),
                  r=["x1s_", "ss2", "g2row"], w=["sq2"])
                yield
                for hf in range(2):
                    p, pk = nextpb()
                    for f4 in range(4):
                        fc = hf * 4 + f4
                        I("pe", lambda e, p=p, fc=fc, f4=f4: e.transpose(p[:, f4 * 128:(f4 + 1) * 128], sq2[:, fc * 128:(fc + 1) * 128], ident[:]),
                          r=["sq2", "ident"], w=[pk])
                    yield
                    yield
                    for f4 in range(4):
                        fc = hf * 4 + f4
                        I("dve", lambda e, p=p, fc=fc, f4=f4, ti=ti, b=b: e.tensor_scalar(
                            out=u2[:, fc, ti * 128:(ti + 1) * 128], in0=p[:, f4 * 128:(f4 + 1) * 128],
                            scalar1=modp[:, 4, fc, b:b + 1], scalar2=modp[:, 3, fc, b:b + 1], op0=ALU.mult, op1=ALU.add),
                          r=[pk, "modp"], w=[uk])
                    yield
            for hp in range(16):
                p, pk = nextpb()
                for kc in range(8):
                    I("pe", lambda e, p=p, hp=hp, kc=kc: e.matmul(
                        p[:, 0:TB], lhsT=wqb[:, kc, hp * 128:(hp + 1) * 128], rhs=u2[:, kc, :], start=(kc == 0), stop=(kc == 7)),
                      r=["wqb", uk], w=[pk])
                yield
                yield
                I("act", lambda e, p=p, hp=hp: e.copy(out=q2T[:, hp, :], in_=p[:, 0:TB]), r=[pk], w=["q2T"])
                yield
            for ti in range(2):
                for qd_ in range(4):
                    p, pk = nextpb()
                    for hh in range(4):
                        hp = qd_ * 4 + hh
                        I("pe", lambda e, p=p, hp=hp, hh=hh, ti=ti: e.matmul(
                            p[:, hh * 128:(hh + 1) * 128], lhsT=q2T[:, hp, ti * 128:(ti + 1) * 128], rhs=keysb[:, hp, :],
                            start=True, stop=True), r=["q2T", "keysb"], w=[pk])
                    yield
                    yield
                    I("act", lambda e, p=p, qd_=qd_: e.copy(
                        out=sc_[:, qd_ * 4:(qd_ + 1) * 4, :], in_=p[:, :].rearrange("p (a n) -> p a n", n=128)),
                      r=[pk], w=SC[qd_ * 4:(qd_ + 1) * 4])
                    yield
                for hp in range(16):
                    I("dve", lambda e, hp=hp: e.max(out=tv[:, hp, 0:8], in_=sc_[:, hp, :]), r=["sc_%d" % hp], w=["tv_%d" % hp])
                    if hp % 4 == 3:
                        yield
                for hp in range(16):
                    I("dve", lambda e, hp=hp: e.max_index(out=tix[:, hp, 0:8], in_max=tv[:, hp, 0:8], in_values=sc_[:, hp, :]),
                      r=["sc_%d" % hp, "tv_%d" % hp], w=["tix_%d" % hp])
                    if hp % 4 == 3:
                        yield
                for hp in range(16):
                    I("dve", lambda e, hp=hp: e.match_replace(out=sc_[:, hp, :], in_to_replace=tv[:, hp, 0:8], in_values=sc_[:, hp, :], imm_value=NEG),
                      r=["tv_%d" % hp], w=["sc_%d" % hp])
                    if hp % 4 == 3:
                        yield
                for hp in range(16):
                    I("dve", lambda e, hp=hp: e.max(out=tv[:, hp, 8:16], in_=sc_[:, hp, :]), r=["sc_%d" % hp], w=["tv_%d" % hp])
                    if hp % 4 == 3:
                        yield
                for hp in range(16):
                    I("dve", lambda e, hp=hp: e.max_index(out=tix[:, hp, 8:16], in_max=tv[:, hp, 8:16], in_values=sc_[:, hp, :]),
                      r=["sc_%d" % hp, "tv_%d" % hp], w=["tix_%d" % hp])
                    if hp % 4 == 3:
                        yield
                I("dve", lambda e: e.tensor_copy(out=tif[:], in_=tix[:]), r=TIX, w=["tif"])
                tv4 = tv[:, :, :].rearrange("p (h s) k -> p h s k", s=2)
                tif4 = tif[:, :, :].rearrange("p (h s) k -> p h s k", s=2)
                I("dve", lambda e, tv4=tv4: e.tensor_tensor(
                    out=cs[:, :, :].rearrange("p h (a c) -> p h a c", c=16),
                    in0=tv4[:, :, 0, :].unsqueeze(3).to_broadcast([128, 8, 16, 16]),
                    in1=tv4[:, :, 1, :].unsqueeze(2).to_broadcast([128, 8, 16, 16]), op=ALU.add), r=TV, w=CS)
                yield
                for h in range(8):
                    I("dve", lambda e, h=h: e.max(out=sv[:, h, 0:8], in_=cs[:, h, :]), r=["cs_%d" % h], w=["sv_%d" % h])
                yield
                for h in range(8):
                    I("dve", lambda e, h=h: e.max_index(out=spx[:, h, 0:8], in_max=sv[:, h, 0:8], in_values=cs[:, h, :]),
                      r=["cs_%d" % h, "sv_%d" % h], w=["spx_%d" % h])
                yield
                for h in range(8):
                    I("dve", lambda e, h=h: e.match_replace(out=cs[:, h, :], in_to_replace=sv[:, h, 0:8], in_values=cs[:, h, :], imm_value=NEG),
                      r=["sv_%d" % h], w=["cs_%d" % h])
                yield
                for h in range(8):
                    I("dve", lambda e, h=h: e.max(out=sv[:, h, 8:16], in_=cs[:, h, :]), r=["cs_%d" % h], w=["sv_%d" % h])
                yield
                for h in range(8):
                    I("dve", lambda e, h=h: e.max_index(out=spx[:, h, 8:16], in_max=sv[:, h, 8:16], in_values=cs[:, h, :]),
                      r=["cs_%d" % h, "sv_%d" % h], w=["spx_%d" % h])
                yield
                I("dve", lambda e: e.tensor_single_scalar(out=spi[:], in_=spx[:], scalar=4, op=ALU.logical_shift_right), r=SPX, w=["spi"])
                I("dve", lambda e: e.tensor_copy(out=pf[:], in_=spi[:]), r=["spi"], w=["pf"])
                I("dve", lambda e: e.tensor_single_scalar(out=spi[:], in_=spx[:], scalar=15, op=ALU.bitwise_and), r=SPX + ["pf"], w=["spi"])
                I("dve", lambda e: e.tensor_copy(out=qf[:], in_=spi[:]), r=["spi"], w=["qf"])
                yield
                io16 = iota[:, 0:16].unsqueeze(1).unsqueeze(1).to_broadcast([128, 8, 16, 16])
                for (rf, side, dst, dk) in ((pf, 0, If_, "If"), (qf, 1, Jf_, "Jf")):
                    I("dve", lambda e, rf=rf: e.tensor_tensor(
                        out=eq, in0=rf[:, :, :].unsqueeze(3).to_broadcast([128, 8, 16, 16]), in1=io16, op=ALU.is_equal),
                      r=["pf", "qf", "iota"], w=SC)
                    yield
                    I("dve", lambda e, side=side, tif4=tif4: e.tensor_tensor(
                        out=eq, in0=eq, in1=tif4[:, :, side, :].unsqueeze(2).to_broadcast([128, 8, 16, 16]), op=ALU.mult),
                      r=SC + ["tif"], w=SC)
                    yield
                    I("dve", lambda e, dst=dst: e.tensor_reduce(
                        out=dst[:, :].rearrange("p (h k) -> p h k", k=16), in_=eq, axis=AX.X, op=ALU.add), r=SC, w=[dk])
                    yield
                I("dve", lambda e: e.tensor_tensor(out=sv[:], in0=sv[:], in1=sv[:, :, 0:1].to_broadcast([128, 8, 16]), op=ALU.subtract),
                  r=SV, w=SV)
                I("act", lambda e: e.activation(out=sv[:], in_=sv[:], func=AF.Exp), r=SV, w=SV)
                I("dve", lambda e: e.tensor_reduce(out=zs[:], in_=sv[:], axis=AX.X, op=ALU.add), r=SV, w=["zs"])
                I("dve", lambda e: e.reciprocal(out=zs[:], in_=zs[:]), r=["zs"], w=["zs"])
                I("dve", lambda e: e.tensor_tensor(
                    out=Wf_[:, :].rearrange("p (h k) -> p h k", k=16), in0=sv[:], in1=zs[:, :].unsqueeze(2).to_broadcast([128, 8, 16]),
                    op=ALU.mult), r=SV + ["zs"], w=["Wf"])
                yield
                for (src, sk, dst, dk) in ((If_, "If", IT, "IT"), (Jf_, "Jf", JT, "JT"), (Wf_, "Wf", WTr, "WTr")):
                    p, pk = nextpb()
                    yield
                    I("pe", lambda e, p=p, src=src: e.transpose(p[:, 0:128], src[:], ident[:]), r=[sk, "ident"], w=[pk])
                    yield
                    yield
                    I("act", lambda e, p=p, dst=dst, ti=ti: e.copy(out=dst[:, ti, :], in_=p[:, 0:128]), r=[pk], w=[dk])
                yield

        def stageW(tb_, half):
            iob = iota[:, :].unsqueeze(1).to_broadcast([128, 16, 128])
            ioh = iota[:, half * 64:(half + 1) * 64].unsqueeze(1).to_broadcast([128, 16, 64])
            wkey = "WT%d" % half

            def gen(bi):
                ti = bi // 8
                j2 = bi % 2
                t0 = (bi % 8) * 16
                I("dve", lambda e: e.tensor_tensor(
                    out=A16[j2], in0=ioh, in1=IT[:, ti, t0:t0 + 16].unsqueeze(2).to_broadcast([128, 16, 64]), op=ALU.is_equal),
                  r=["iota", "IT"], w=SCH[j2])
                I("dve", lambda e: e.tensor_tensor(
                    out=B16[j2], in0=iob, in1=JT[:, ti, t0:t0 + 16].unsqueeze(2).to_broadcast([128, 16, 128]), op=ALU.is_equal),
                  r=["iota", "JT"], w=CSH[j2])
                I("pool", lambda e: e.tensor_tensor(
                    out=A16[j2], in0=A16[j2], in1=WTr[:, ti, t0:t0 + 16].unsqueeze(2).to_broadcast([128, 16, 64]), op=ALU.mult),
                  r=SCH[j2] + ["WTr"], w=SCH[j2])

            def mm(bi, t8):
                ti = bi // 8
                j2 = bi % 2
                t0 = (bi % 8) * 16
                p, pk = nextpb()
                for tt in range(8):
                    tl = t8 * 8 + tt
                    I("pe", lambda e, tt=tt, tl=tl: e.matmul(
                        p[:, tt * 64:(tt + 1) * 64], lhsT=B16[j2][:, tl, :], rhs=A16[j2][:, tl, :], start=True, stop=True),
                      r=SCH[j2] + CSH[j2], w=[pk])
                return p, pk, ti * 128 + t0 + t8 * 8

            def cp(p, pk, tg0):
                I("act", lambda e: e.copy(
                    out=WT[:, tg0:tg0 + 8, half * 64:(half + 1) * 64], in_=p[:, :].rearrange("p (t i) -> p t i", i=64)),
                  r=[pk], w=[wkey])

            gen(0)
            yield
            for bi in range(16):
                if bi + 1 < 16:
                    gen(bi + 1)
                yield
                a = mm(bi, 0)
                yield
                yield
                cp(*a)
                yield
                a = mm(bi, 1)
                yield
                yield
                cp(*a)
                yield

        def gemm_and_epilogue(tb_, bg):
            b = tb_ // NBB
            u2 = u2T[tb_ % 2]
            uk = "u2T%d" % (tb_ % 2)
            slots = {}

            def pull(ic, n):
                for ent in bg:
                    if ent[0] is None:
                        continue
                    if ent[1] > ic:
                        return
                    while n > 0:
                        try:
                            next(ent[0])
                            n -= 1
                        except StopIteration:
                            ent[0] = None
                            break
                    if n == 0:
                        return

            def drain(pred):
                for ent in bg:
                    if ent[0] is not None and pred(ent):
                        for _ in ent[0]:
                            pass
                        ent[0] = None

            def emitU(pr):
                gi = tb_ * NG + pr
                j = gi % NBUF
                sl = pr % NSL
                p = pg_[sl]
                pk = "pgs%d" % sl
                slots[pr] = sl
                for c in range(2):
                    for kc in range(8):
                        I("pe", lambda e, p=p, j=j, c=c, kc=kc: e.matmul(
                            p[:, c * TB:(c + 1) * TB], lhsT=utb[j][:, kc, c * 128:(c + 1) * 128], rhs=u2[:, kc, :],
                            start=(kc == 0), stop=(kc == 7)), r=["utb%d" % j, uk], w=[pk])
                I("act", lambda e, p=p, sl=sl: e.activation(out=gl[sl][:], in_=p[:, :], func=AF.Gelu), r=[pk], w=["gl%d" % sl])
                I("dve", lambda e, sl=sl, pr=pr: e.tensor_tensor(
                    out=awt[sl][:, :].rearrange("p (c t) -> p c t", c=2), in0=gl[sl][:, :].rearrange("p (c t) -> p c t", c=2),
                    in1=WT[:, :, 2 * pr:2 * pr + 2].rearrange("p t i -> p i t"), op=ALU.mult),
                  r=["gl%d" % sl, "WT%d" % (pr // 32)], w=["awt%d" % sl])

            def emitV(pr):
                gi = tb_ * NG + pr
                j = gi % NBUF
                sl = slots.pop(pr)
                for c in range(2):
                    ic = 2 * pr + c
                    for ti in range(2):
                        for hf in range(2):
                            I("pe", lambda e, sl=sl, ti=ti, hf=hf, j=j, c=c, ic=ic: e.matmul(
                                py[ti][:, hf, :], lhsT=awt[sl][:, c * TB + ti * 128:c * TB + (ti + 1) * 128],
                                rhs=vtb[j][:, c, hf * 512:(hf + 1) * 512],
                                start=(ic == 0), stop=(ic == 127)), r=["awt%d" % sl, "vtb%d" % j], w=["py%d" % ti])
                if gi + NBUF < total_groups:
                    load_group(gi + NBUF)

            for pr in range(-LOOK, 64):
                if pr + LOOK < 64:
                    nu = pr + LOOK
                    if nu == 32:
                        drain(lambda ent: ent[2] <= 64)
                    emitU(nu)
                    if pr >= 0:
                        pull(2 * pr, PULL)
                if pr >= 0:
                    emitV(pr)
                    pull(2 * pr, PULL)
            drain(lambda ent: True)
            for ti in range(2):
                r0 = tb_ * TB + ti * 128
                Dm(x1s_[:], x1_d[r0:r0 + 128, :], r=["x1s"], w=["x1s_"])
                I("dve", lambda e, ti=ti: e.tensor_tensor(
                    out=sq2[:], in0=py[ti][:, :, :].rearrange("p a n -> p (a n)"), in1=g2gate[:], op=ALU.mult),
                  r=["py%d" % ti, "g2gate"], w=["sq2"])
                I("pool", lambda e: e.tensor_tensor(out=sq2[:], in0=sq2[:], in1=x1s_[:], op=ALU.add), r=["sq2", "x1s_"], w=["sq2"])
                I("act", lambda e: e.activation(out=x1s_[:], in_=sq2[:], func=AF.Square, accum_out=ss3[:]), r=["sq2"], w=["x1s_", "ss3"])
                I("act", lambda e: e.activation(out=ss3[:], in_=ss3[:], func=AF.Sqrt, scale=1.0 / D, bias=epsb[:]),
                  r=["ss3", "epsb"], w=["ss3"])
                I("dve", lambda e: e.reciprocal(out=ss3[:], in_=ss3[:]), r=["ss3"], w=["ss3"])
                I("dve", lambda e: e.scalar_tensor_tensor(
                    out=x1s_[:], in0=sq2[:], scalar=ss3[:], in1=gfrow[:], op0=ALU.mult, op1=ALU.mult),
                  r=["sq2", "ss3", "gfrow"], w=["x1s_"])
                l0 = (tb_ % NBB) * TB + ti * 128
                Dm(out_d[b, l0:l0 + 128, :], x1s_[:], r=["x1s_"], w=["out"])

        Dm(g2gate[:], grow_d[2:3, :].partition_broadcast(128), r=["grow_scr"], w=["g2gate"])
        for _ in stageS(0):
            pass
        if upto == "S0":
            kb.barrier()
            return nc
        for _ in stageW(0, 0):
            pass
        if upto == "S1":
            kb.barrier()
            return nc
        for tb_ in range(nblk):
            bg = [[stageW(tb_, 1), 0, 64]]
            if tb_ + 1 < nblk:
                bg.append([stageS(tb_ + 1), 0, 999])
                bg.append([stageW(tb_ + 1, 0), 64, 999])
            gemm_and_epilogue(tb_, bg)
            if tb_ + 1 < nblk and (tb_ + 1) % NBB == 0:
                Dm(g2gate[:], grow_d[2 + (tb_ + 1) // NBB:3 + (tb_ + 1) // NBB, :].partition_broadcast(128), r=["grow_scr"], w=["g2gate"])
        kb.barrier(engines=("sp",))
    return nc


def _pos_table():
    rows = SEQ // 64
    r, col = np.meshgrid(np.arange(rows, dtype=np.float32), np.arange(64, dtype=np.float32), indexing="ij")
    quarter = D // 4
    omega = (np.float32(10000.0) ** (-np.arange(quarter, dtype=np.float32) / np.float32(quarter))).astype(np.float32)

    def emb(p):
        ang = p.reshape(-1)[:, None].astype(np.float32) * omega[None, :]
        return np.concatenate([np.sin(ang), np.cos(ang)], axis=-1)

    return np.concatenate([emb(r), emb(col)], axis=-1).astype(np.float32)


_NC_CACHE = {}


def _fm(v, nchunk):
    return np.ascontiguousarray(np.asarray(v, np.float32).reshape(nchunk, 128).T)


def kernel(x, c, ctx, c_ctx, ada_w, ada_b, norm1_g, w_in, conv_w, conv_b, rg_w_a, rg_b_a, rg_w_x, rg_b_x, rg_lambda,
           gla_w_g, gla_b_g, gla_norm_g, w_out, norm2_g, peer_w_q, peer_keys, peer_u, peer_v, final_norm_g):
    if "nc" not in _NC_CACHE:
        _NC_CACHE["nc"] = build()
    nc = _NC_CACHE["nc"]
    in_maps = make_in_maps(x, c, ctx, c_ctx, ada_w, ada_b, norm1_g, w_in, conv_w, conv_b, rg_w_a, rg_b_a, rg_w_x, rg_b_x, rg_lambda,
                           gla_w_g, gla_b_g, gla_norm_g, w_out, norm2_g, peer_w_q, peer_keys, peer_u, peer_v, final_norm_g)
    res = run_bass_kernel_spmd(nc, in_maps, core_ids=list(range(NCORES)))
    out = np.concatenate([np.asarray(r["out"], dtype=np.float32) for r in res.results], axis=0)
    return out


def make_in_maps(x, c, ctx, c_ctx, ada_w, ada_b, norm1_g, w_in, conv_w, conv_b, rg_w_a, rg_b_a, rg_w_x, rg_b_x, rg_lambda,
                 gla_w_g, gla_b_g, gla_norm_g, w_out, norm2_g, peer_w_q, peer_keys, peer_u, peer_v, final_norm_g):
    f = lambda a: np.ascontiguousarray(np.asarray(a, dtype=np.float32))
    x, c, ctx, c_ctx = f(x), f(c), f(ctx), f(c_ctx)
    jj, ii = np.meshgrid(np.arange(128), np.arange(128), indexing="ij")
    mf = (jj <= ii).astype(np.float32)
    mb = (jj >= ii).astype(np.float32)
    shared = {
        "ada_w": f(ada_w[0]),
        "ada_b": f(ada_b[0]).reshape(1, -1),
        "ada_bT": _fm(ada_b[0], 48),
        "norm1_g": f(norm1_g[0]).reshape(1, -1),
        "norm2_g": f(norm2_g[0]).reshape(1, -1),
        "final_g": f(final_norm_g).reshape(1, -1),
        "w_in": f(w_in[0]),
        "convwT": np.ascontiguousarray(f(conv_w[0]).reshape(4, 4, 128).transpose(2, 1, 0)),
        "convbT": _fm(conv_b[0], 4),
        "rg_w_a": f(rg_w_a[0]),
        "rg_w_x": f(rg_w_x[0]),
        "rgbaT": np.ascontiguousarray(f(rg_b_a[0]).reshape(2, 4, 128).transpose(2, 0, 1)),
        "rgbxT": np.ascontiguousarray(f(rg_b_x[0]).reshape(2, 4, 128).transpose(2, 0, 1)),
        "rglamT": np.ascontiguousarray(f(rg_lambda[0]).reshape(2, 4, 128).transpose(2, 0, 1)),
        "gla_w_g": f(gla_w_g[0]),
        "glabgT": np.ascontiguousarray(f(gla_b_g[0]).reshape(2, 2, 128).transpose(2, 0, 1)),
        "glangT": f(gla_norm_g[0]).reshape(128, 1),
        "w_out": f(w_out[0]),
        "peer_w_q": f(peer_w_q[0]),
        "keysT": np.ascontiguousarray(f(peer_keys[0]).reshape(16, 128, 128).transpose(2, 0, 1)),
        "peer_u": f(peer_u[0]),
        "peer_v": f(peer_v[0]),
        "pos": _pos_table(),
        "ident": np.eye(128, dtype=np.float32),
        "iota": np.tile(np.arange(128, dtype=np.float32)[None, :], (128, 1)),
        "maskf": np.ascontiguousarray(np.concatenate([mf, mf], axis=1)),
        "maskb": np.ascontiguousarray(np.concatenate([mb, mb], axis=1)),
    }
    in_maps = []
    for i in range(NCORES):
        b0 = 2 * i
        cm = np.stack([c[b0], c[b0 + 1], c_ctx], axis=0)
        cT = np.ascontiguousarray(cm.reshape(3, 8, 128).transpose(2, 1, 0))
        m = dict(shared)
        m["x"] = np.ascontiguousarray(x[b0:b0 + 2])
        m["ctx"] = np.ascontiguousarray(ctx[b0:b0 + 2])
        m["cT"] = cT
        in_maps.append(m)
    return in_maps
```

```python
import contextlib
import numpy as np
import concourse.bass as bass
import concourse.mybir as mybir
from concourse.bass_utils import run_bass_kernel_spmd

F32 = mybir.dt.float32
BF16 = mybir.dt.bfloat16
U32 = mybir.dt.uint32
ALU = mybir.AluOpType
AF = mybir.ActivationFunctionType
AX = mybir.AxisListType

NCORES = 8
D = 1024
SEQ = 2048
CTX = 256
S = CTX + SEQ
NT = S // 128
INC = 2592
NEXP = 16384
TB = 256
EPS = 1e-6
NEG = -1.0e30


class KB:
    def __init__(self):
        self.nc = bass.Bass("TRN2", target_bir_lowering=False)
        nc = self.nc
        self.eng = {"pe": nc.tensor, "act": nc.scalar, "dve": nc.vector, "pool": nc.gpsimd, "sp": nc.sync}
        self.stack = contextlib.ExitStack()
        self.sem = {}
        self.cnt = {}
        for n in self.eng:
            self.sem[("e", n)] = self.stack.enter_context(nc.semaphore("s_" + n))
            self.cnt[n] = 0
        self.nd = 24
        self.dcnt = [0] * self.nd
        for i in range(self.nd):
            self.sem[("d", i)] = self.stack.enter_context(nc.semaphore("d%d" % i))
        self.dnext = 0
        self.waited = {}
        self.lastw = {}
        self.readers = {}

    def _wait(self, en, deps):
        for sk, v in deps.items():
            if en == "pe" and sk == ("e", "pe"):
                continue
            if self.waited.get((en, sk), 0) >= v:
                continue
            self.eng[en].wait_ge(self.sem[sk], v)
            self.waited[(en, sk)] = v

    def _deps(self, r, w):
        d = {}
        for k in r:
            t = self.lastw.get(k)
            if t:
                d[t[0]] = max(d.get(t[0], 0), t[1])
        for k in w:
            t = self.lastw.get(k)
            if t:
                d[t[0]] = max(d.get(t[0], 0), t[1])
            for sk, v in self.readers.get(k, {}).items():
                d[sk] = max(d.get(sk, 0), v)
        return d

    def _commit(self, tok, r, w):
        for k in w:
            self.lastw[k] = tok
            self.readers[k] = {}
        for k in r:
            rd = self.readers.setdefault(k, {})
            rd[tok[0]] = max(rd.get(tok[0], 0), tok[1])

    def I(self, en, fn, r=(), w=()):
        self._wait(en, self._deps(r, w))
        inst = fn(self.eng[en])
        self.cnt[en] += 1
        inst.then_inc(self.sem[("e", en)], 1)
        self._commit((("e", en), self.cnt[en]), r, w)

    def D(self, out, in_, r=(), w=(), q="sp"):
        i = self.dnext
        self.dnext = (i + 1) % self.nd
        deps = self._deps(r, w)
        if self.dcnt[i] > 0:
            deps[("d", i)] = max(deps.get(("d", i), 0), self.dcnt[i])
        self._wait(q, deps)
        inst = self.eng[q].dma_start(out=out, in_=in_)
        self.dcnt[i] += 16
        inst.then_inc(self.sem[("d", i)], 16)
        self._commit((("d", i), self.dcnt[i]), r, w)

    def barrier(self, engines=("pe", "act", "dve", "pool", "sp")):
        deps = {}
        for n in self.eng:
            if self.cnt[n] > 0:
                deps[("e", n)] = self.cnt[n]
        for i in range(self.nd):
            if self.dcnt[i] > 0:
                deps[("d", i)] = self.dcnt[i]
        for en in engines:
            d = dict(deps)
            d.pop(("e", en), None)
            self._wait(en, d)
        self.lastw = {}
        self.readers = {}


def build(upto=None, dbg=False):
    kb = KB()
    nc = kb.nc
    I, Dm = kb.I, kb.D

    def dram(name, shape, dt=F32, kind="ExternalInput"):
        return nc.dram_tensor(name, list(shape), dt, kind=kind).ap()

    x_d = dram("x", [2, SEQ, D])
    ctx_d = dram("ctx", [2, CTX, D])
    cT_d = dram("cT", [128, 8, 3])
    adaw_d = dram("ada_w", [D, 6 * D])
    adab_d = dram("ada_b", [1, 6 * D])
    adabT_d = dram("ada_bT", [128, 48])
    g1_d = dram("norm1_g", [1, D])
    g2_d = dram("norm2_g", [1, D])
    gf_d = dram("final_g", [1, D])
    win_d = dram("w_in", [D, INC])
    convw_d = dram("convwT", [128, 4, 4])
    convb_d = dram("convbT", [128, 4])
    rgwa_d = dram("rg_w_a", [2, 8, 64, 64])
    rgwx_d = dram("rg_w_x", [2, 8, 64, 64])
    rgba_d = dram("rgbaT", [128, 2, 4])
    rgbx_d = dram("rgbxT", [128, 2, 4])
    rglam_d = dram("rglamT", [128, 2, 4])
    glawg_d = dram("gla_w_g", [2, 16, 256])
    glabg_d = dram("glabgT", [128, 2, 2])
    glang_d = dram("glangT", [128, 1])
    wout_d = dram("w_out", [D, D])
    wq_d = dram("peer_w_q", [D, 2048])
    keysT_d = dram("keysT", [128, 16, 128])
    pu_d = dram("peer_u", [NEXP, D])
    pv_d = dram("peer_v", [NEXP, D])
    pos_d = dram("pos", [SEQ, D])
    ident_d = dram("ident", [128, 128])
    iota_d = dram("iota", [128, 128])
    maskf_d = dram("maskf", [128, 256])
    maskb_d = dram("maskb", [128, 256])
    out_d = dram("out", [2, SEQ, D], kind="ExternalOutput")
    x1_d = dram("x1s", [2 * SEQ, D], kind="ExternalOutput" if dbg else "Internal")
    grow_d = dram("grow_scr", [4, D], kind="Internal")
    ut_d = dram("ut_scr", [NEXP // 256, 128, 8, 256], BF16, kind="Internal")
    vs_d = dram("v_scr", [NEXP, D], BF16, kind="Internal")

    es = contextlib.ExitStack()

    uid = [0]

    def sb(stack, name, shape, dt=F32):
        uid[0] += 1
        return stack.enter_context(nc.sbuf_tensor("sb%d_%s" % (uid[0], name), list(shape), dt))

    def ps(stack, name, shape, dt=F32):
        uid[0] += 1
        return stack.enter_context(nc.psum_tensor("ps%d_%s" % (uid[0], name), list(shape), dt))

    ident = sb(es, "ident", [128, 128])
    identb = sb(es, "identb", [128, 128], BF16)
    iota = sb(es, "iota", [128, 128])
    ones = sb(es, "ones", [128, 128])
    modp = sb(es, "modp", [128, 6, 8, 3])
    epsb = sb(es, "epsb", [128, 1])

    Dm(ident[:], ident_d[:, :], w=["ident"])
    Dm(iota[:], iota_d[:, :], w=["iota"])
    I("dve", lambda e: e.tensor_copy(out=identb[:], in_=ident[:]), r=["ident"], w=["identb"])
    I("dve", lambda e: e.memset(ones[:], 1.0), w=["ones"])
    I("dve", lambda e: e.memset(epsb[:], EPS), w=["epsb"])

    with contextlib.ExitStack() as st:
        cT = sb(st, "cT", [128, 8, 3])
        scT = sb(st, "scT", [128, 8, 3])
        rep = sb(st, "rep", [128, 2, 8, 128])
        abT = sb(st, "abT", [128, 48])
        abrow = sb(st, "abrow", [128, D])
        growt = [sb(st, "growt%d" % i, [128, D]) for i in range(2)]
        aw = [sb(st, "aw%d" % i, [128, 8, D]) for i in range(2)]
        pm = [ps(st, "pm%d" % i, [128, 512]) for i in range(4)]
        Dm(cT[:], cT_d[:, :, :], w=["cT"])
        Dm(abT[:], adabT_d[:, :], w=["abT"])
        I("act", lambda e: e.activation(out=scT[:], in_=cT[:], func=AF.Silu), r=["cT"], w=["scT"])
        for b in range(2):
            I("dve", lambda e, b=b: e.tensor_copy(out=rep[:, b], in_=scT[:, :, b:b + 1].to_broadcast([128, 8, 128])),
              r=["scT"], w=["rep"])
        pi = 0
        for m in range(6):
            a = aw[m % 2]
            ak = "aw%d" % (m % 2)
            Dm(a[:], adaw_d[:, m * D:(m + 1) * D].rearrange("(kc p) n -> p kc n", p=128), w=[ak])
            if m in (0, 1, 3, 4):
                p = pm[pi % 4]
                pk = "pm%d" % (pi % 4)
                pi += 1
                for fc in range(8):
                    for kc in range(8):
                        I("pe", lambda e, fc=fc, kc=kc, p=p, a=a: e.matmul(
                            p[:, fc * 3:fc * 3 + 3], lhsT=a[:, kc, fc * 128:(fc + 1) * 128], rhs=scT[:, kc, :],
                            start=(kc == 0), stop=(kc == 7)), r=[ak, "scT"], w=[pk])
                I("dve", lambda e, m=m, p=p: e.tensor_tensor(
                    out=modp[:, m], in0=p[:, 0:24].rearrange("p (f c) -> p f c", c=3),
                    in1=abT[:, m * 8:(m + 1) * 8].unsqueeze(2).to_broadcast([128, 8, 3]), op=ALU.add),
                  r=[pk, "abT"], w=["modp"])
                if m in (1, 4):
                    I("dve", lambda e, m=m: e.tensor_scalar_add(out=modp[:, m], in0=modp[:, m], scalar1=1.0),
                      r=["modp"], w=["modp"])
            else:
                mi = 0 if m == 2 else 1
                Dm(abrow[:], adab_d[0:1, m * D:(m + 1) * D].partition_broadcast(128), w=["abrow"])
                for b in range(2):
                    for hf in range(2):
                        p = pm[pi % 4]
                        pk = "pm%d" % (pi % 4)
                        pi += 1
                        for kc in range(8):
                            I("pe", lambda e, kc=kc, p=p, a=a, b=b, hf=hf: e.matmul(
                                p[:, :], lhsT=rep[:, b, kc, :], rhs=a[:, kc, hf * 512:(hf + 1) * 512],
                                start=(kc == 0), stop=(kc == 7)), r=[ak, "rep"], w=[pk])
                        I("dve", lambda e, p=p, b=b, hf=hf, mi=mi: e.tensor_tensor(
                            out=growt[b][:, hf * 512:(hf + 1) * 512], in0=p[:, :],
                            in1=abrow[:, hf * 512:(hf + 1) * 512], op=ALU.add),
                          r=[pk, "abrow"], w=["growt%d" % b])
                    Dm(grow_d[mi * 2 + b:mi * 2 + b + 1, :], growt[b][0:1, :], r=["growt%d" % b], w=["grow_scr"])
        kb.barrier()
    if upto == "M0":
        return nc

    with contextlib.ExitStack() as sm:
        g1row = sb(sm, "g1row", [128, D])
        g1gate = sb(sm, "g1gate", [128, 2, D])
        for b in range(2):
            Dm(g1gate[:, b, :], grow_d[b:b + 1, :].partition_broadcast(128), r=["grow_scr"], w=["g1gate"])
        maskf = sb(sm, "maskf", [128, 256], BF16)
        maskb = sb(sm, "maskb", [128, 256], BF16)
        mstage = sb(sm, "mstage", [128, 256])
        cw = sb(sm, "cw", [128, 4, 4])
        cb = sb(sm, "cb", [128, 4])
        rba = sb(sm, "rba", [128, 2, 4])
        rbx = sb(sm, "rbx", [128, 2, 4])
        rlam = sb(sm, "rlam", [128, 2, 4])
        coef = sb(sm, "coef", [128, 2, 4])
        gbg = sb(sm, "gbg", [128, 2, 2])
        ngbg = sb(sm, "ngbg", [128, 2, 2])
        gng = sb(sm, "gng", [128, 1])
        wg = sb(sm, "wg", [32, 2, 256])
        wbd = sb(sm, "wbd", [128, 2, 2, 4, 128])
        hm = sb(sm, "hm", [128, 2])
        bdm = sb(sm, "bdm", [128, 256])
        I("dve", lambda e: e.memset(hm[:], 0.0), w=["hm"])
        I("dve", lambda e: e.memset(hm[0:64, 0:1], 1.0), w=["hm"])
        I("dve", lambda e: e.memset(hm[64:128, 1:2], 1.0), w=["hm"])
        I("dve", lambda e: e.memset(bdm[:], 0.0), w=["bdm"])
        I("dve", lambda e: e.memset(bdm[0:64, 0:128], 1.0), w=["bdm"])
        I("dve", lambda e: e.memset(bdm[64:128, 128:256], 1.0), w=["bdm"])
        Dm(g1row[:], g1_d[0:1, :].partition_broadcast(128), w=["g1row"])
        Dm(mstage[:], maskf_d[:, :], w=["mstage"])
        I("dve", lambda e: e.tensor_copy(out=maskf[:], in_=mstage[:]), r=["mstage"], w=["maskf"])
        Dm(mstage[:], maskb_d[:, :], w=["mstage"])
        I("dve", lambda e: e.tensor_copy(out=maskb[:], in_=mstage[:]), r=["mstage"], w=["maskb"])
        Dm(cw[:], convw_d[:, :, :], w=["cw"])
        Dm(cb[:], convb_d[:, :], w=["cb"])
        Dm(rba[:], rgba_d[:, :, :], w=["rba"])
        Dm(rbx[:], rgbx_d[:, :, :], w=["rbx"])
        Dm(rlam[:], rglam_d[:, :, :], w=["rlam"])
        Dm(gbg[:], glabg_d[:, :, :], w=["gbg"])
        Dm(gng[:], glang_d[:, :], w=["gng"])
        I("act", lambda e: e.activation(out=coef[:], in_=rlam[:], func=AF.Exp, scale=-1.0), r=["rlam"], w=["coef"])
        I("act", lambda e: e.activation(out=coef[:], in_=coef[:], func=AF.Ln, bias=1.0), r=["coef"], w=["coef"])
        I("dve", lambda e: e.tensor_scalar_mul(out=coef[:], in0=coef[:], scalar1=-8.0), r=["coef"], w=["coef"])
        I("dve", lambda e: e.tensor_scalar_mul(out=ngbg[:], in0=gbg[:], scalar1=-1.0), r=["gbg"], w=["ngbg"])
        I("dve", lambda e: e.memset(wg[:], 0.0), w=["wg"])
        for d in range(2):
            Dm(wg[16 * d:16 * d + 16, d, :], glawg_d[d, :, :], w=["wg"])
        I("dve", lambda e: e.memset(wbd[:], 0.0), w=["wbd"])
        for gi, src in enumerate((rgwa_d, rgwx_d)):
            for d in range(2):
                for cc in range(4):
                    for h in range(2):
                        Dm(wbd[64 * h:64 * h + 64, gi, d, cc, 64 * h:64 * h + 64], src[d, 2 * cc + h, :, :], w=["wbd"])

        for b in range(2):
            with contextlib.ExitStack() as sbt:
                mgT = sb(sbt, "mgT", [128, 8, SEQ], BF16)
                qT = sb(sbt, "qT", [128, 2, SEQ], BF16)
                kT = sb(sbt, "kT", [128, 2, S], BF16)
                vtok = sb(sbt, "vtok", [128, NT, 512], BF16)
                lrT = sb(sbt, "lrT", [32, S])
                sR = contextlib.ExitStack()
                rxT = sb(sR, "rxT", [128, 4, S], BF16)
                with contextlib.ExitStack() as s1:
                    winb = sb(s1, "winb", [128, 8, INC], BF16)
                    wst0 = sb(s1, "wst0", [128, INC // 2])
                    wst = [wst0, wst0]
                    uT = [sb(s1, "uT%d" % i, [128, 8, 512], BF16) for i in range(2)]
                    xt = [sb(s1, "xt%d" % i, [128, D]) for i in range(2)]
                    pt0 = sb(s1, "pt0", [128, D])
                    pt = [pt0, pt0]
                    xn = [sb(s1, "xn%d" % i, [128, D], BF16) for i in range(2)]
                    sq = sb(s1, "sqj", [128, D], BF16)
                    ss = [sb(s1, "ss%d" % i, [128, 1]) for i in range(2)]
                    tp = [ps(s1, "tp%d" % i, [128, 8, 128], BF16) for i in range(2)]
                    pj = [ps(s1, "pj%d" % i, [128, 512]) for i in range(4)]
                    hw = INC // 2
                    for kc in range(8):
                        for hh in range(2):
                            w_ = wst[hh]
                            Dm(w_[:], win_d[kc * 128:(kc + 1) * 128, hh * hw:(hh + 1) * hw], w=["wst0"])
                            I("pool" if hh else "dve", lambda e, w_=w_, kc=kc, hh=hh: e.tensor_copy(
                                out=winb[:, kc, hh * hw:(hh + 1) * hw], in_=w_[:]), r=["wst0"], w=["winb"])
                    ngroups = 5
                    pjc = 0
                    for g in range(ngroups):
                        t0 = g * 4
                        nt = min(4, NT - t0)
                        ntok = nt * 128
                        u = uT[g % 2]
                        uk = "uT%d" % (g % 2)
                        for ti in range(nt):
                            t = t0 + ti
                            pb = t % 2
                            xk, xnk, ssk, tpk, ptk = "xt%d" % pb, "xn%d" % pb, "ss%d" % pb, "tp%d" % pb, "pt0"
                            if t < 2:
                                Dm(xt[pb][:], ctx_d[b, t * 128:(t + 1) * 128, :], w=[xk])
                                col = 2
                            else:
                                l0 = (t - 2) * 128
                                Dm(xt[pb][:], x_d[b, l0:l0 + 128, :], w=[xk])
                                Dm(pt[pb][:], pos_d[l0:l0 + 128, :], w=[ptk])
                                I("pool", lambda e, pb=pb: e.tensor_tensor(out=xt[pb][:], in0=xt[pb][:], in1=pt[pb][:], op=ALU.add),
                                  r=[xk, ptk], w=[xk])
                                col = b
                            I("act", lambda e, pb=pb: e.activation(out=sq[:], in_=xt[pb][:], func=AF.Square, accum_out=ss[pb][:]),
                              r=[xk], w=["sqj", ssk])
                            I("act", lambda e, pb=pb: e.activation(out=ss[pb][:], in_=ss[pb][:], func=AF.Sqrt, scale=1.0 / D, bias=epsb[:]),
                              r=[ssk, "epsb"], w=[ssk])
                            I("dve", lambda e, pb=pb: e.reciprocal(out=ss[pb][:], in_=ss[pb][:]), r=[ssk], w=[ssk])
                            I("dve", lambda e, pb=pb: e.scalar_tensor_tensor(
                                out=xn[pb][:], in0=xt[pb][:], scalar=ss[pb][:], in1=g1row[:], op0=ALU.mult, op1=ALU.mult),
                              r=[xk, ssk, "g1row"], w=[xnk])
                            for fc in range(8):
                                I("pe", lambda e, pb=pb, fc=fc: e.transpose(tp[pb][:, fc, :], xn[pb][:, fc * 128:(fc + 1) * 128], identb[:]),
                                  r=[xnk, "identb"], w=[tpk])
                            for fc in range(8):
                                I("dve" if fc % 2 else "pool" if False else "dve", lambda e, pb=pb, fc=fc, ti=ti, u=u, col=col: e.tensor_scalar(
                                    out=u[:, fc, ti * 128:(ti + 1) * 128], in0=tp[pb][:, fc, :],
                                    scalar1=modp[:, 1, fc, col:col + 1], scalar2=modp[:, 0, fc, col:col + 1],
                                    op0=ALU.mult, op1=ALU.add), r=[tpk, "modp"], w=[uk])
                        c0 = t0 * 128
                        lat0 = max(c0, CTX)
                        for cch in range(21):
                            cs_ = cch * 128
                            ncol = 128 if cch < 20 else 32
                            p = pj[pjc % 4]
                            pk = "pj%d" % (pjc % 4)
                            pjc += 1
                            if 12 <= cch < 16:
                                continue
                            lat_only = (4 <= cch < 10) or (16 <= cch < 20)
                            if lat_only and lat0 >= c0 + ntok:
                                continue
                            for kc in range(8):
                                I("pe", lambda e, kc=kc, p=p, u=u, cs_=cs_, ncol=ncol, ntok=ntok: e.matmul(
                                    p[0:ncol, 0:ntok], lhsT=winb[:, kc, cs_:cs_ + ncol], rhs=u[:, kc, 0:ntok],
                                    start=(kc == 0), stop=(kc == 7)), r=["winb", uk], w=[pk])
                            o0 = lat0 - c0
                            if cch < 4:
                                I("act", lambda e, p=p, cch=cch, c0=c0, ntok=ntok: e.copy(out=rxT[:, cch, c0:c0 + ntok], in_=p[:, 0:ntok]),
                                  r=[pk], w=["rxT"])
                            elif cch < 8:
                                I("act", lambda e, p=p, cch=cch, o0=o0, lat0=lat0, ntok=ntok: e.activation(
                                    out=mgT[:, cch - 4, lat0 - CTX:lat0 - CTX + ntok - o0], in_=p[:, o0:ntok], func=AF.Gelu),
                                  r=[pk], w=["mgT"])
                            elif cch < 10:
                                I("dve", lambda e, p=p, cch=cch, o0=o0, lat0=lat0, ntok=ntok: e.tensor_scalar_mul(
                                    out=qT[:, cch - 8, lat0 - CTX:lat0 - CTX + ntok - o0], in0=p[:, o0:ntok], scalar1=0.125),
                                  r=[pk], w=["qT"])
                            elif cch < 12:
                                I("dve", lambda e, p=p, cch=cch, c0=c0, ntok=ntok: e.tensor_copy(out=kT[:, cch - 10, c0:c0 + ntok], in_=p[:, 0:ntok]),
                                  r=[pk], w=["kT"])
                            elif cch < 20:
                                I("act", lambda e, p=p, cch=cch, o0=o0, lat0=lat0, ntok=ntok: e.activation(
                                    out=mgT[:, 4 + cch - 16, lat0 - CTX:lat0 - CTX + ntok - o0], in_=p[:, o0:ntok], func=AF.Silu),
                                  r=[pk], w=["mgT"])
                            else:
                                I("dve", lambda e, p=p, c0=c0, ntok=ntok: e.tensor_copy(out=lrT[:, c0:c0 + ntok], in_=p[0:32, 0:ntok]),
                                  r=[pk], w=["lrT"])
                        for ti in range(nt):
                            p = pj[pjc % 4]
                            pk = "pj%d" % (pjc % 4)
                            pjc += 1
                            for kc in range(8):
                                I("pe", lambda e, kc=kc, p=p, u=u, ti=ti: e.matmul(
                                    p[:, :], lhsT=u[:, kc, ti * 128:(ti + 1) * 128], rhs=winb[:, kc, 1536:2048],
                                    start=(kc == 0), stop=(kc == 7)), r=["winb", uk], w=[pk])
                            I("act", lambda e, p=p, t=t0 + ti: e.copy(out=vtok[:, t, :], in_=p[:, :]), r=[pk], w=["vtok"])
                    kb.barrier()

                if upto == "M1a":
                    sR.close()
                    return nc
                with contextlib.ExitStack() as s2:
                    xc = sb(s2, "xc", [128, S])
                    gr = sb(s2, "gr", [128, S])
                    gi_ = sb(s2, "gi", [128, S])
                    aa = sb(s2, "aa", [128, S])
                    bb = sb(s2, "bb", [128, S])
                    hf_ = sb(s2, "hf", [128, S])
                    hb_ = sb(s2, "hb", [128, S])
                    pg = [ps(s2, "pg%d" % i, [128, 512]) for i in range(4)]
                    pgc = 0
                    segs = ((0, CTX), (CTX, S))
                    for cc in range(4):
                        I("dve", lambda e, cc=cc: e.tensor_scalar(
                            out=xc[:], in0=rxT[:, cc, :], scalar1=cw[:, cc, 2:3], scalar2=cb[:, cc:cc + 1],
                            op0=ALU.mult, op1=ALU.add), r=["rxT", "cw", "cb"], w=["xc"])
                        for (a0, a1) in segs:
                            for j, sh in ((0, -2), (1, -1), (3, 1)):
                                lo = max(a0, a0 - sh)
                                hi = min(a1, a1 - sh)
                                I("dve", lambda e, cc=cc, j=j, sh=sh, lo=lo, hi=hi: e.scalar_tensor_tensor(
                                    out=xc[:, lo:hi], in0=rxT[:, cc, lo + sh:hi + sh], scalar=cw[:, cc, j:j + 1],
                                    in1=xc[:, lo:hi], op0=ALU.mult, op1=ALU.add), r=["rxT", "cw", "xc"], w=["xc"])
                        for d in range(2):
                            for gi, (gt, gk, bias_t) in enumerate(((gr, "gr", rba), (gi_, "gi", rbx))):
                                for g in range(5):
                                    c0 = g * 512
                                    n = min(512, S - c0)
                                    p = pg[pgc % 4]
                                    pk = "pg%d" % (pgc % 4)
                                    pgc += 1
                                    I("pe", lambda e, p=p, gi=gi, d=d, cc=cc, c0=c0, n=n: e.matmul(
                                        p[:, 0:n], lhsT=wbd[:, gi, d, cc, :], rhs=xc[:, c0:c0 + n], start=True, stop=True),
                                      r=["wbd", "xc"], w=[pk])
                                    I("act", lambda e, p=p, gt=gt, bias_t=bias_t, d=d, cc=cc, c0=c0, n=n: e.activation(
                                        out=gt[:, c0:c0 + n], in_=p[:, 0:n], func=AF.Sigmoid, bias=bias_t[:, d, cc:cc + 1]),
                                      r=[pk], w=[gk])
                            I("act", lambda e, d=d, cc=cc: e.activation(out=aa[:], in_=gr[:], func=AF.Exp, scale=coef[:, d, cc:cc + 1]),
                              r=["gr", "coef"], w=["aa"])
                            I("pool", lambda e: e.tensor_tensor(out=bb[:], in0=aa[:], in1=aa[:], op=ALU.mult), r=["aa"], w=["bb"])
                            I("act", lambda e: e.activation(out=bb[:], in_=bb[:], func=AF.Sqrt, scale=-1.0, bias=1.0), r=["bb"], w=["bb"])
                            I("pool", lambda e: e.tensor_tensor(out=bb[:], in0=bb[:], in1=gi_[:], op=ALU.mult), r=["bb", "gi"], w=["bb"])
                            I("pool", lambda e: e.tensor_tensor(out=bb[:], in0=bb[:], in1=xc[:], op=ALU.mult), r=["bb", "xc"], w=["bb"])
                            if d == 0:
                                I("dve", lambda e: e.tensor_tensor_scan(out=hf_[:], data0=aa[:], data1=bb[:], initial=0.0,
                                                                         op0=ALU.mult, op1=ALU.add), r=["aa", "bb"], w=["hf"])
                            else:
                                I("dve", lambda e: e.tensor_tensor_scan(out=hb_[:, 0:CTX][:, ::-1], data0=aa[:, 0:CTX][:, ::-1],
                                                                         data1=bb[:, 0:CTX][:, ::-1], initial=0.0,
                                                                         op0=ALU.mult, op1=ALU.add), r=["aa", "bb"], w=["hb"])
                                I("dve", lambda e: e.tensor_tensor_scan(out=hb_[:, CTX:S][:, ::-1], data0=aa[:, CTX:S][:, ::-1],
                                                                         data1=bb[:, CTX:S][:, ::-1], initial=hb_[:, 0:1],
                                                                         op0=ALU.mult, op1=ALU.add), r=["aa", "bb", "hb"], w=["hb"])
                        I("dve", lambda e: e.tensor_tensor(out=hf_[:, CTX:S], in0=hf_[:, CTX:S], in1=hb_[:, CTX:S], op=ALU.add),
                          r=["hf", "hb"], w=["hf"])
                        I("dve", lambda e, cc=cc: e.tensor_tensor(out=mgT[:, cc, :], in0=mgT[:, cc, :], in1=hf_[:, CTX:S], op=ALU.mult),
                          r=["hf", "mgT"], w=["mgT"])
                    kb.barrier()

                if upto == "M1b":
                    sR.close()
                    return nc
                sR.close()
                with contextlib.ExitStack() as s3:
                    otot = sb(s3, "otot", [128, 4, SEQ])
                    scr = sb(s3, "scr", [128, 2, S])
                    la = scr[:, 0, :]
                    bc = scr[:, 1, :]
                    dsg_all = scr[:, :, :].rearrange("p a n -> p (a n)").rearrange("p (c v) -> p c v", v=256)
                    eb = sb(s3, "eb", [128, S])
                    qd = sb(s3, "qd", [128, SEQ], BF16)
                    ki = sb(s3, "ki", [128, S], BF16)
                    kih = [sb(s3, "kih%d" % i, [128, S], BF16) for i in range(2)]
                    kit = [sb(s3, "kit%d" % i, [128, 128], BF16) for i in range(2)]
                    gam = sb(s3, "gam", [128, NT])
                    Sst = [sb(s3, "Sst%d" % i, [128, 256]) for i in range(2)]
                    Sbf_all = sb(s3, "Sbfa", [128, 16, 256], BF16)
                    scb_all = sb(s3, "scba", [128, 16, 256], BF16)
                    pz = [ps(s3, "pz%d" % i, [128, 512]) for i in range(1)]
                    ptr = [ps(s3, "ptr%d" % i, [128, 128], BF16) for i in range(2)]
                    pds = [ps(s3, "pds%d" % i, [128, 256]) for i in range(2)]
                    psc = [ps(s3, "psc%d" % i, [128, 256]) for i in range(2)]
                    po = [ps(s3, "po%d" % i, [128, 256]) for i in range(1)]
                    zc = 0
                    cn = 0
                    first_o = {0: True, 1: True}
                    for d in range(2):
                        order = list(range(NT)) if d == 0 else [1, 0] + list(range(NT - 1, 1, -1))
                        mk, mkk = (maskf, "maskf") if d == 0 else (maskb, "maskb")
                        for pr in range(2):
                            for g in range(5):
                                c0 = g * 512
                                n = min(512, S - c0)
                                p = pz[0]
                                pk = "pz0"
                                I("pe", lambda e, p=p, d=d, pr=pr, c0=c0, n=n: e.matmul(
                                    p[:, 0:n], lhsT=wg[:, d, pr * 128:(pr + 1) * 128], rhs=lrT[:, c0:c0 + n], start=True, stop=True),
                                  r=["wg", "lrT"], w=[pk])
                                I("act", lambda e, p=p, d=d, pr=pr, c0=c0, n=n: e.activation(
                                    out=la[:, c0:c0 + n], in_=p[:, 0:n], func=AF.Exp, scale=-1.0, bias=ngbg[:, d, pr:pr + 1]),
                                  r=[pk, "ngbg"], w=["la"])
                            I("act", lambda e: e.activation(out=la, in_=la, func=AF.Ln, bias=1.0), r=["la"], w=["la"])
                            for n_ in range(NT):
                                c0 = n_ * 128
                                if d == 0:
                                    I("dve", lambda e, c0=c0: e.tensor_tensor_scan(
                                        out=bc[:, c0:c0 + 128], data0=ones[:, :], data1=la[:, c0:c0 + 128], initial=0.0,
                                        op0=ALU.mult, op1=ALU.add), r=["la", "ones"], w=["bc"])
                                else:
                                    I("dve", lambda e, c0=c0: e.tensor_tensor_scan(
                                        out=bc[:, c0:c0 + 128][:, ::-1], data0=ones[:, :], data1=la[:, c0:c0 + 128][:, ::-1], initial=0.0,
                                        op0=ALU.mult, op1=ALU.add), r=["la", "ones"], w=["bc"])
                            I("act", lambda e: e.activation(out=eb[:], in_=bc, func=AF.Exp, scale=-1.0 / 16.0), r=["bc"], w=["eb"])
                            I("act", lambda e: e.activation(out=la, in_=bc, func=AF.Exp, scale=1.0 / 16.0), r=["bc"], w=["la"])
                            I("pool", lambda e, pr=pr: e.tensor_tensor(out=qd[:], in0=qT[:, pr, :], in1=eb[:, CTX:S], op=ALU.mult),
                              r=["qT", "eb"], w=["qd"])
                            I("dve", lambda e, pr=pr: e.tensor_tensor(out=ki[:], in0=kT[:, pr, :], in1=la, op=ALU.mult),
                              r=["kT", "la"], w=["ki"])
                            for h in range(2):
                                if h == 0:
                                    I("act", lambda e, h=h: e.mul(out=kih[h][:], in_=ki[:], mul=hm[:, h:h + 1]),
                                      r=["ki", "hm"], w=["kih%d" % h])
                                else:
                                    I("dve", lambda e, h=h: e.tensor_scalar_mul(out=kih[h][:], in0=ki[:], scalar1=hm[:, h:h + 1]),
                                      r=["ki", "hm"], w=["kih%d" % h])
                            gsrc = eb[:, 127::128] if d == 0 else eb[:, 0::128]
                            I("dve", lambda e, gsrc=gsrc: e.tensor_copy(out=gam[:], in_=gsrc), r=["eb"], w=["gam"])
                            I("dve", lambda e: e.memset(Sst[0][:], 0.0), w=["Sst0"])

                            def tr_(n_):
                                j = n_ % 2
                                I("pe", lambda e: e.transpose(ptr[j][:, :], ki[:, n_ * 128:(n_ + 1) * 128], identb[:]),
                                  r=["ki", "identb"], w=["ptr%d" % j])
                                I("act", lambda e: e.copy(out=kit[j][:], in_=ptr[j][:, :]), r=["ptr%d" % j], w=["kit%d" % j])

                            tr_(0)
                            for n_ in range(NT):
                                if n_ + 1 < NT:
                                    tr_(n_ + 1)
                                j = n_ % 2
                                dk = "la" if n_ < 9 else "bc"
                                I("pe", lambda e, j=j, n_=n_: e.matmul(
                                    pds[j][:, :], lhsT=kit[j][:, :], rhs=vtok[:, n_, pr * 256:(pr + 1) * 256], start=True, stop=True),
                                  r=["kit%d" % j, "vtok"], w=["pds%d" % j])
                                I("act", lambda e, j=j, n_=n_: e.mul(
                                    out=dsg_all[:, n_, :], in_=pds[j][:, :], mul=gam[:, n_:n_ + 1]),
                                  r=["pds%d" % j, "gam"], w=[dk])
                                if n_ >= 2:
                                    c0 = n_ * 128
                                    l0 = c0 - CTX
                                    for h in range(2):
                                        I("pe", lambda e, h=h, c0=c0, l0=l0, j=j: e.matmul(
                                            psc[j][:, h * 128:(h + 1) * 128], lhsT=kih[h][:, c0:c0 + 128],
                                            rhs=qd[:, l0:l0 + 128], start=True, stop=True),
                                          r=["kih%d" % h, "qd"], w=["psc%d" % j])
                                    I("dve", lambda e, j=j, n_=n_: e.tensor_tensor(out=scb_all[:, n_ - 2, :], in0=psc[j][:, :], in1=mk[:], op=ALU.mult),
                                      r=["psc%d" % j, mkk], w=["scba"])
                            for k_, n_ in enumerate(order):
                                a_, b_ = k_ % 2, (k_ + 1) % 2
                                dk = "la" if n_ < 9 else "bc"
                                if n_ >= 2:
                                    I("pool", lambda e, a_=a_, n_=n_: e.tensor_tensor(out=Sbf_all[:, n_ - 2, :], in0=Sst[a_][:], in1=bdm[:], op=ALU.mult),
                                      r=["Sst%d" % a_, "bdm"], w=["Sbfa"])
                                I("dve", lambda e, a_=a_, b_=b_, n_=n_: e.scalar_tensor_tensor(
                                    out=Sst[b_][:], in0=Sst[a_][:], scalar=gam[:, n_:n_ + 1], in1=dsg_all[:, n_, :],
                                    op0=ALU.mult, op1=ALU.add), r=["Sst%d" % a_, "gam", dk], w=["Sst%d" % b_])
                            pbanks = [(po[0], "po0"), (psc[0], "psc0"), (psc[1], "psc1")]
                            for n_ in range(2, NT):
                                l0 = n_ * 128 - CTX
                                pp, ppk = pbanks[n_ % len(pbanks)]
                                for h in range(2):
                                    hd = pr * 2 + h
                                    I("pe", lambda e, h=h, hd=hd, n_=n_, pp=pp: e.matmul(
                                        pp[:, h * 128:(h + 1) * 128], lhsT=vtok[:, n_, hd * 128:(hd + 1) * 128],
                                        rhs=scb_all[:, n_ - 2, h * 128:(h + 1) * 128], start=True, stop=False),
                                      r=["vtok", "scba"], w=[ppk])
                                    I("pe", lambda e, h=h, n_=n_, l0=l0, pp=pp: e.matmul(
                                        pp[:, h * 128:(h + 1) * 128], lhsT=Sbf_all[:, n_ - 2, h * 128:(h + 1) * 128],
                                        rhs=qd[:, l0:l0 + 128], start=False, stop=True),
                                      r=["Sbfa", "qd"], w=[ppk])
                                ov = otot[:, pr * 2:pr * 2 + 2, l0:l0 + 128]
                                pv_ = pp[:, :].rearrange("p (h t) -> p h t", h=2)
                                if d == 0:
                                    I("act", lambda e, ov=ov, pv_=pv_: e.copy(out=ov, in_=pv_), r=[ppk], w=["otot"])
                                else:
                                    I("dve", lambda e, ov=ov, pv_=pv_: e.tensor_tensor(out=ov, in0=ov, in1=pv_, op=ALU.add),
                                      r=[ppk, "otot"], w=["otot"])
                    sqo = sb(s3, "sqo", [128, 512])
                    rs = sb(s3, "rs", [128, 512])
                    tmpo = sb(s3, "tmpo", [128, 512])
                    for hd in range(4):
                        for g in range(4):
                            c0 = g * 512
                            p = pz[0]
                            pk = "pz0"
                            I("act", lambda e, hd=hd, c0=c0: e.activation(out=sqo[:], in_=otot[:, hd, c0:c0 + 512], func=AF.Square),
                              r=["otot"], w=["sqo"])
                            I("pe", lambda e, p=p: e.matmul(p[:, :], lhsT=ones[:, :], rhs=sqo[:], start=True, stop=True),
                              r=["ones", "sqo"], w=[pk])
                            I("act", lambda e, p=p: e.activation(out=rs[:], in_=p[:, :], func=AF.Sqrt, scale=1.0 / 128.0, bias=epsb[:]),
                              r=[pk, "epsb"], w=["rs"])
                            I("dve", lambda e: e.reciprocal(out=rs[:], in_=rs[:]), r=["rs"], w=["rs"])
                            I("dve", lambda e, hd=hd, c0=c0: e.scalar_tensor_tensor(
                                out=tmpo[:], in0=otot[:, hd, c0:c0 + 512], scalar=gng[:, 0:1], in1=rs[:], op0=ALU.mult, op1=ALU.mult),
                              r=["otot", "gng", "rs"], w=["tmpo"])
                            I("pool", lambda e, hd=hd, c0=c0: e.tensor_tensor(
                                out=mgT[:, 4 + hd, c0:c0 + 512], in0=mgT[:, 4 + hd, c0:c0 + 512], in1=tmpo[:], op=ALU.mult),
                              r=["tmpo", "mgT"], w=["mgT"])
                    kb.barrier()

                if upto == "M1c":
                    return nc
                with contextlib.ExitStack() as s4:
                    woutb = sb(s4, "woutb", [128, 8, D], BF16)
                    wst2 = [sb(s4, "wso%d" % i, [128, D]) for i in range(2)]
                    xt = [sb(s4, "xo%d" % i, [128, D]) for i in range(2)]
                    pt = [sb(s4, "po_%d" % i, [128, D]) for i in range(2)]
                    tm = [sb(s4, "tm%d" % i, [128, D]) for i in range(2)]
                    pw = [ps(s4, "pw%d" % i, [128, 2, 512]) for i in range(2)]
                    for kc in range(8):
                        w_ = wst2[kc % 2]
                        Dm(w_[:], wout_d[kc * 128:(kc + 1) * 128, :], w=["wso%d" % (kc % 2)])
                        I("dve", lambda e, w_=w_, kc=kc: e.tensor_copy(out=woutb[:, kc, :], in_=w_[:]), r=["wso%d" % (kc % 2)], w=["woutb"])
                    for t in range(16):
                        pb = t % 2
                        l0 = t * 128
                        Dm(xt[pb][:], x_d[b, l0:l0 + 128, :], w=["xo%d" % pb])
                        Dm(pt[pb][:], pos_d[l0:l0 + 128, :], w=["po_%d" % pb])
                        I("pool", lambda e, pb=pb: e.tensor_tensor(out=xt[pb][:], in0=xt[pb][:], in1=pt[pb][:], op=ALU.add),
                          r=["xo%d" % pb, "po_%d" % pb], w=["xo%d" % pb])
                        for hf in range(2):
                            for mc in range(8):
                                I("pe", lambda e, pb=pb, hf=hf, mc=mc, l0=l0: e.matmul(
                                    pw[pb][:, hf, :], lhsT=mgT[:, mc, l0:l0 + 128], rhs=woutb[:, mc, hf * 512:(hf + 1) * 512],
                                    start=(mc == 0), stop=(mc == 7)), r=["mgT", "woutb"], w=["pw%d" % pb])
                        I("dve", lambda e, pb=pb: e.tensor_tensor(
                            out=tm[pb][:], in0=pw[pb][:, :, :].rearrange("p a n -> p (a n)"), in1=g1gate[:, b, :], op=ALU.mult),
                          r=["pw%d" % pb, "g1gate"], w=["tm%d" % pb])
                        I("pool", lambda e, pb=pb: e.tensor_tensor(out=tm[pb][:], in0=tm[pb][:], in1=xt[pb][:], op=ALU.add),
                          r=["tm%d" % pb, "xo%d" % pb], w=["tm%d" % pb])
                        Dm(x1_d[b * SEQ + l0:b * SEQ + l0 + 128, :], tm[pb][:], r=["tm%d" % pb], w=["x1s"])
                    kb.barrier()
                if upto == "M1d":
                    return nc

    if upto == "M1":
        return nc
    with contextlib.ExitStack() as sc:
        uin = [sb(sc, "uin%d" % i, [128, 4, D]) for i in range(2)]
        ubf = [sb(sc, "ubf%d" % i, [128, 4, D], BF16) for i in range(2)]
        uts = [sb(sc, "uts%d" % i, [128, 8, 512], BF16) for i in range(2)]
        vin = [sb(sc, "vin%d" % i, [128, 4, D]) for i in range(2)]
        vbf = [sb(sc, "vbf%d" % i, [128, 4, D], BF16) for i in range(2)]
        ptc = [ps(sc, "ptc%d" % i, [128, 8, 128], BF16) for i in range(4)]
        tcn = 0
        for g in range(32):
            j = g % 2
            e0 = g * 512
            Dm(uin[j][:], pu_d[e0:e0 + 512, :].rearrange("(c p) n -> p c n", p=128), w=["uin%d" % j])
            Dm(vin[j][:], pv_d[e0:e0 + 512, :].rearrange("(c p) n -> p c n", p=128), w=["vin%d" % j])
            I("dve", lambda e, j=j: e.tensor_copy(out=ubf[j][:], in_=uin[j][:]), r=["uin%d" % j], w=["ubf%d" % j])
            if g % 2 == 0:
                I("dve", lambda e, j=j: e.tensor_copy(out=vbf[j][:], in_=vin[j][:]), r=["vin%d" % j], w=["vbf%d" % j])
            else:
                I("act", lambda e, j=j: e.copy(out=vbf[j][:], in_=vin[j][:]), r=["vin%d" % j], w=["vbf%d" % j])
            Dm(vs_d[e0:e0 + 512, :].rearrange("(c p) n -> p c n", p=128), vbf[j][:], r=["vbf%d" % j], w=["v_scr"])
            for c in range(4):
                p = ptc[tcn % 4]
                pk = "ptc%d" % (tcn % 4)
                tcn += 1
                for fc in range(8):
                    I("pe", lambda e, p=p, j=j, c=c, fc=fc: e.transpose(p[:, fc, :], ubf[j][:, c, fc * 128:(fc + 1) * 128], identb[:]),
                      r=["ubf%d" % j, "identb"], w=[pk])
                I("act", lambda e, p=p, j=j, c=c: e.copy(out=uts[j][:, :, c * 128:(c + 1) * 128], in_=p[:, :, :]),
                  r=[pk], w=["uts%d" % j])
            for hh in range(2):
                Dm(ut_d[2 * g + hh, :, :, :], uts[j][:, :, hh * 256:(hh + 1) * 256], r=["uts%d" % j], w=["ut_scr"])
        kb.barrier()

    if upto == "C":
        return nc
    with contextlib.ExitStack() as sp_:
        g2row = sb(sp_, "g2row", [128, D])
        gfrow = sb(sp_, "gfrow", [128, D])
        g2gate = sb(sp_, "g2gate", [128, D])
        wqb = sb(sp_, "wqb", [128, 8, 2048], BF16)
        keysb = sb(sp_, "keysb", [128, 16, 128], BF16)
        WT = sb(sp_, "WT", [128, TB, 128], BF16)
        NBUF = 4
        utb = [sb(sp_, "utb%d" % i, [128, 8, 256], BF16) for i in range(NBUF)]
        vtb = [sb(sp_, "vtb%d" % i, [128, 2, D], BF16) for i in range(NBUF)]
        x1s_ = sb(sp_, "x1s_", [128, D])
        u2T = [sb(sp_, "u2T%d" % i, [128, 8, TB], BF16) for i in range(2)]
        q2T = sb(sp_, "q2T", [128, 16, TB], BF16)
        sc_ = sb(sp_, "sc", [128, 16, 128])
        eq = sc_[:, :, :].rearrange("p (h a) (b c) -> p h (a b) c", a=2, c=16)
        cs = sb(sp_, "cs", [128, 8, 256])
        A16 = [sc_[:, 8 * i:8 * i + 4, :].rearrange("p a n -> p (a n)").bitcast(BF16).rearrange("p (t i) -> p t i", i=64) for i in range(2)]
        B16 = [cs[:, 4 * i:4 * i + 4, :].rearrange("p a n -> p (a n)").bitcast(BF16).rearrange("p (t i) -> p t i", i=128) for i in range(2)]
        tv = sb(sp_, "tv", [128, 16, 16])
        tix = sb(sp_, "tix", [128, 16, 16], U32)
        tif = sb(sp_, "tif", [128, 16, 16])
        sv = sb(sp_, "sv", [128, 8, 16])
        spx = sb(sp_, "spx", [128, 8, 16], U32)
        spi = sb(sp_, "spi", [128, 8, 16], U32)
        pf = sb(sp_, "pf", [128, 8, 16])
        qf = sb(sp_, "qf", [128, 8, 16])
        If_ = sb(sp_, "If", [128, 128])
        Jf_ = sb(sp_, "Jf", [128, 128])
        Wf_ = sb(sp_, "Wf", [128, 128])
        zs = sb(sp_, "zs", [128, 8])
        IT = sb(sp_, "IT", [128, 2, 128])
        JT = sb(sp_, "JT", [128, 2, 128])
        WTr = sb(sp_, "WTr", [128, 2, 128])
        NSL = 3
        gl = [sb(sp_, "gl%d" % i, [128, 2 * TB]) for i in range(NSL)]
        awt = [sb(sp_, "awt%d" % i, [128, 2 * TB], BF16) for i in range(NSL)]
        sq2 = sb(sp_, "sq2", [128, D])
        ss2 = sb(sp_, "ss2", [128, 1])
        ss3 = sb(sp_, "ss3", [128, 1])
        py = [ps(sp_, "py%d" % i, [128, 2, 512]) for i in range(2)]
        pg_ = [ps(sp_, "pg%d" % i, [128, 512]) for i in range(3)]
        pb_ = [ps(sp_, "pb%d" % i, [128, 512]) for i in range(1)]
        if dbg:
            print("phase P sbuf bytes remaining:", nc.sbuf_bytes_remaining)

        Dm(g2row[:], g2_d[0:1, :].partition_broadcast(128), w=["g2row"])
        Dm(gfrow[:], gf_d[0:1, :].partition_broadcast(128), w=["gfrow"])
        for kc in range(8):
            for hh in range(2):
                Dm(sq2[:], wq_d[kc * 128:(kc + 1) * 128, hh * 1024:(hh + 1) * 1024], w=["sq2"])
                I("dve", lambda e, kc=kc, hh=hh: e.tensor_copy(out=wqb[:, kc, hh * 1024:(hh + 1) * 1024], in_=sq2[:]), r=["sq2"], w=["wqb"])
        for hh in range(2):
            Dm(sq2[:], keysT_d[:, hh * 8:(hh + 1) * 8, :].rearrange("p a n -> p (a n)"), w=["sq2"])
            I("dve", lambda e, hh=hh: e.tensor_copy(out=keysb[:, hh * 8:(hh + 1) * 8, :].rearrange("p a n -> p (a n)"), in_=sq2[:]), r=["sq2"], w=["keysb"])

        NG = 64
        nblk = 2 * SEQ // TB
        if upto is not None and upto.startswith('P'):
            nblk = int(upto[1:])
        NBB = SEQ // TB

        def load_group(gi):
            j = gi % NBUF
            e0 = (gi % NG) * 256
            Dm(utb[j][:], ut_d[gi % NG, :, :, :], r=["ut_scr"], w=["utb%d" % j])
            Dm(vtb[j][:], vs_d[e0:e0 + 256, :].rearrange("(c p) n -> p c n", p=128), r=["v_scr"], w=["vtb%d" % j])

        total_groups = nblk * NG
        for g_ in range(min(NBUF, total_groups)):
            load_group(g_)
        pbc = [0]

        def nextpb():
            k = 0
            pbc[0] += 1
            return pb_[k], "pb%d" % k

        LOOK = 2
        PULL = 4
        SC = ["sc_%d" % i for i in range(16)]
        CS = ["cs_%d" % i for i in range(8)]
        TV = ["tv_%d" % i for i in range(16)]
        TIX = ["tix_%d" % i for i in range(16)]
        SV = ["sv_%d" % i for i in range(8)]
        SPX = ["spx_%d" % i for i in range(8)]
        SCH = [SC[0:4], SC[8:12]]
        CSH = [CS[0:4], CS[4:8]]

        def stageS(tb_):
            b = tb_ // NBB
            r0 = tb_ * TB
            u2 = u2T[tb_ % 2]
            uk = "u2T%d" % (tb_ % 2)
            for ti in range(2):
                Dm(x1s_[:], x1_d[r0 + ti * 128:r0 + (ti + 1) * 128, :], r=["x1s"], w=["x1s_"])
                I("act", lambda e: e.activation(out=sq2[:], in_=x1s_[:], func=AF.Square, accum_out=ss2[:]),
                  r=["x1s_"], w=["sq2", "ss2"])
                I("act", lambda e: e.activation(out=ss2[:], in_=ss2[:], func=AF.Sqrt, scale=1.0 / D, bias=epsb[:]),
                  r=["ss2", "epsb"], w=["ss2"])
                I("dve", lambda e: e.reciprocal(out=ss2[:], in_=ss2[:]), r=["ss2"], w=["ss2"])
                I("dve", lambda e: e.scalar_tensor_tensor(
                    out=sq2[:], in0=x1s_[:], scalar=ss2[:], in1=g2row[:], op0=ALU.mult, op1=ALU.mult),
                  r=["x1s_", "ss2", "g2row"], w=["sq2"])
                yield
                for hf in range(2):
                    p, pk = nextpb()
                    for f4 in range(4):
                        fc = hf * 4 + f4
                        I("pe", lambda e, p=p, fc=fc, f4=f4: e.transpose(p[:, f4 * 128:(f4 + 1) * 128], sq2[:, fc * 128:(fc + 1) * 128], ident[:]),
                          r=["sq2", "ident"], w=[pk])
                    yield
                    yield
                    for f4 in range(4):
                        fc = hf * 4 + f4
                        I("dve", lambda e, p=p, fc=fc, f4=f4, ti=ti, b=b: e.tensor_scalar(
                            out=u2[:, fc, ti * 128:(ti + 1) * 128], in0=p[:, f4 * 128:(f4 + 1) * 128],
                            scalar1=modp[:, 4, fc, b:b + 1], scalar2=modp[:, 3, fc, b:b + 1], op0=ALU.mult, op1=ALU.add),
                          r=[pk, "modp"], w=[uk])
                    yield
            for hp in range(16):
                p, pk = nextpb()
                for kc in range(8):
                    I("pe", lambda e, p=p, hp=hp, kc=kc: e.matmul(
                        p[:, 0:TB], lhsT=wqb[:, kc, hp * 128:(hp + 1) * 128], rhs=u2[:, kc, :], start=(kc == 0), stop=(kc == 7)),
                      r=["wqb", uk], w=[pk])
                yield
                yield
                I("act", lambda e, p=p, hp=hp: e.copy(out=q2T[:, hp, :], in_=p[:, 0:TB]), r=[pk], w=["q2T"])
                yield
            for ti in range(2):
                for qd_ in range(4):
                    p, pk = nextpb()
                    for hh in range(4):
                        hp = qd_ * 4 + hh
                        I("pe", lambda e, p=p, hp=hp, hh=hh, ti=ti: e.matmul(
                            p[:, hh * 128:(hh + 1) * 128], lhsT=q2T[:, hp, ti * 128:(ti + 1) * 128], rhs=keysb[:, hp, :],
                            start=True, stop=True), r=["q2T", "keysb"], w=[pk])
                    yield
                    yield
                    I("act", lambda e, p=p, qd_=qd_: e.copy(
                        out=sc_[:, qd_ * 4:(qd_ + 1) * 4, :], in_=p[:, :].rearrange("p (a n) -> p a n", n=128)),
                      r=[pk], w=SC[qd_ * 4:(qd_ + 1) * 4])
                    yield
                for hp in range(16):
                    I("dve", lambda e, hp=hp: e.max(out=tv[:, hp, 0:8], in_=sc_[:, hp, :]), r=["sc_%d" % hp], w=["tv_%d" % hp])
                    if hp % 4 == 3:
                        yield
                for hp in range(16):
                    I("dve", lambda e, hp=hp: e.max_index(out=tix[:, hp, 0:8], in_max=tv[:, hp, 0:8], in_values=sc_[:, hp, :]),
                      r=["sc_%d" % hp, "tv_%d" % hp], w=["tix_%d" % hp])
                    if hp % 4 == 3:
                        yield
                for hp in range(16):
                    I("dve", lambda e, hp=hp: e.match_replace(out=sc_[:, hp, :], in_to_replace=tv[:, hp, 0:8], in_values=sc_[:, hp, :], imm_value=NEG),
                      r=["tv_%d" % hp], w=["sc_%d" % hp])
                    if hp % 4 == 3:
                        yield
                for hp in range(16):
                    I("dve", lambda e, hp=hp: e.max(out=tv[:, hp, 8:16], in_=sc_[:, hp, :]), r=["sc_%d" % hp], w=["tv_%d" % hp])
                    if hp % 4 == 3:
                        yield
                for hp in range(16):
                    I("dve", lambda e, hp=hp: e.max_index(out=tix[:, hp, 8:16], in_max=tv[:, hp, 8:16], in_values=sc_[:, hp, :]),
                      r=["sc_%d" % hp, "tv_%d" % hp], w=["tix_%d" % hp])
                    if hp % 4 == 3:
                        yield
                I("dve", lambda e: e.tensor_copy(out=tif[:], in_=tix[:]), r=TIX, w=["tif"])
                tv4 = tv[:, :, :].rearrange("p (h s) k -> p h s k", s=2)
                tif4 = tif[:, :, :].rearrange("p (h s) k -> p h s k", s=2)
                I("dve", lambda e, tv4=tv4: e.tensor_tensor(
                    out=cs[:, :, :].rearrange("p h (a c) -> p h a c", c=16),
                    in0=tv4[:, :, 0, :].unsqueeze(3).to_broadcast([128, 8, 16, 16]),
                    in1=tv4[:, :, 1, :].unsqueeze(2).to_broadcast([128, 8, 16, 16]), op=ALU.add), r=TV, w=CS)
                yield
                for h in range(8):
                    I("dve", lambda e, h=h: e.max(out=sv[:, h, 0:8], in_=cs[:, h, :]), r=["cs_%d" % h], w=["sv_%d" % h])
                yield
                for h in range(8):
                    I("dve", lambda e, h=h: e.max_index(out=spx[:, h, 0:8], in_max=sv[:, h, 0:8], in_values=cs[:, h, :]),
                      r=["cs_%d" % h, "sv_%d" % h], w=["spx_%d" % h])
                yield
                for h in range(8):
                    I("dve", lambda e, h=h: e.match_replace(out=cs[:, h, :], in_to_replace=sv[:, h, 0:8], in_values=cs[:, h, :], imm_value=NEG),
                      r=["sv_%d" % h], w=["cs_%d" % h])
                yield
                for h in range(8):
                    I("dve", lambda e, h=h: e.max(out=sv[:, h, 8:16], in_=cs[:, h, :]), r=["cs_%d" % h], w=["sv_%d" % h])
                yield
                for h in range(8):
                    I("dve", lambda e, h=h: e.max_index(out=spx[:, h, 8:16], in_max=sv[:, h, 8:16], in_values=cs[:, h, :]),
                      r=["cs_%d" % h, "sv_%d" % h], w=["spx_%d" % h])
                yield
                I("dve", lambda e: e.tensor_single_scalar(out=spi[:], in_=spx[:], scalar=4, op=ALU.logical_shift_right), r=SPX, w=["spi"])
                I("dve", lambda e: e.tensor_copy(out=pf[:], in_=spi[:]), r=["spi"], w=["pf"])
                I("dve", lambda e: e.tensor_single_scalar(out=spi[:], in_=spx[:], scalar=15, op=ALU.bitwise_and), r=SPX + ["pf"], w=["spi"])
                I("dve", lambda e: e.tensor_copy(out=qf[:], in_=spi[:]), r=["spi"], w=["qf"])
                yield
                io16 = iota[:, 0:16].unsqueeze(1).unsqueeze(1).to_broadcast([128, 8, 16, 16])
                for (rf, side, dst, dk) in ((pf, 0, If_, "If"), (qf, 1, Jf_, "Jf")):
                    I("dve", lambda e, rf=rf: e.tensor_tensor(
                        out=eq, in0=rf[:, :, :].unsqueeze(3).to_broadcast([128, 8, 16, 16]), in1=io16, op=ALU.is_equal),
                      r=["pf", "qf", "iota"], w=SC)
                    yield
                    I("dve", lambda e, side=side, tif4=tif4: e.tensor_tensor(
                        out=eq, in0=eq, in1=tif4[:, :, side, :].unsqueeze(2).to_broadcast([128, 8, 16, 16]), op=ALU.mult),
                      r=SC + ["tif"], w=SC)
                    yield
                    I("dve", lambda e, dst=dst: e.tensor_reduce(
                        out=dst[:, :].rearrange("p (h k) -> p h k", k=16), in_=eq, axis=AX.X, op=ALU.add), r=SC, w=[dk])
                    yield
                I("dve", lambda e: e.tensor_tensor(out=sv[:], in0=sv[:], in1=sv[:, :, 0:1].to_broadcast([128, 8, 16]), op=ALU.subtract),
                  r=SV, w=SV)
                I("act", lambda e: e.activation(out=sv[:], in_=sv[:], func=AF.Exp), r=SV, w=SV)
                I("dve", lambda e: e.tensor_reduce(out=zs[:], in_=sv[:], axis=AX.X, op=ALU.add), r=SV, w=["zs"])
                I("dve", lambda e: e.reciprocal(out=zs[:], in_=zs[:]), r=["zs"], w=["zs"])
                I("dve", lambda e: e.tensor_tensor(
                    out=Wf_[:, :].rearrange("p (h k) -> p h k", k=16), in0=sv[:], in1=zs[:, :].unsqueeze(2).to_broadcast([128, 8, 16]),
                    op=ALU.mult), r=SV + ["zs"], w=["Wf"])
                yield
                for (src, sk, dst, dk) in ((If_, "If", IT, "IT"), (Jf_, "Jf", JT, "JT"), (Wf_, "Wf", WTr, "WTr")):
                    p, pk = nextpb()
                    yield
                    I("pe", lambda e, p=p, src=src: e.transpose(p[:, 0:128], src[:], ident[:]), r=[sk, "ident"], w=[pk])
                    yield
                    yield
                    I("act", lambda e, p=p, dst=dst, ti=ti: e.copy(out=dst[:, ti, :], in_=p[:, 0:128]), r=[pk], w=[dk])
                yield

        def stageW(tb_, half):
            iob = iota[:, :].unsqueeze(1).to_broadcast([128, 16, 128])
            ioh = iota[:, half * 64:(half + 1) * 64].unsqueeze(1).to_broadcast([128, 16, 64])
            wkey = "WT%d" % half

            def gen(bi):
                ti = bi // 8
                j2 = bi % 2
                t0 = (bi % 8) * 16
                I("dve", lambda e: e.tensor_tensor(
                    out=A16[j2], in0=ioh, in1=IT[:, ti, t0:t0 + 16].unsqueeze(2).to_broadcast([128, 16, 64]), op=ALU.is_equal),
                  r=["iota", "IT"], w=SCH[j2])
                I("dve", lambda e: e.tensor_tensor(
                    out=B16[j2], in0=iob, in1=JT[:, ti, t0:t0 + 16].unsqueeze(2).to_broadcast([128, 16, 128]), op=ALU.is_equal),
                  r=["iota", "JT"], w=CSH[j2])
                I("pool", lambda e: e.tensor_tensor(
                    out=A16[j2], in0=A16[j2], in1=WTr[:, ti, t0:t0 + 16].unsqueeze(2).to_broadcast([128, 16, 64]), op=ALU.mult),
                  r=SCH[j2] + ["WTr"], w=SCH[j2])

            def mm(bi, t8):
                ti = bi // 8
                j2 = bi % 2
                t0 = (bi % 8) * 16
                p, pk = nextpb()
                for tt in range(8):
                    tl = t8 * 8 + tt
                    I("pe", lambda e, tt=tt, tl=tl: e.matmul(
                        p[:, tt * 64:(tt + 1) * 64], lhsT=B16[j2][:, tl, :], rhs=A16[j2][:, tl, :], start=True, stop=True),
                      r=SCH[j2] + CSH[j2], w=[pk])
                return p, pk, ti * 128 + t0 + t8 * 8

            def cp(p, pk, tg0):
                I("act", lambda e: e.copy(
                    out=WT[:, tg0:tg0 + 8, half * 64:(half + 1) * 64], in_=p[:, :].rearrange("p (t i) -> p t i", i=64)),
                  r=[pk], w=[wkey])

            gen(0)
            yield
            for bi in range(16):
                if bi + 1 < 16:
                    gen(bi + 1)
                yield
                a = mm(bi, 0)
                yield
                yield
                cp(*a)
                yield
                a = mm(bi, 1)
                yield
                yield
                cp(*a)
                yield

        def gemm_and_epilogue(tb_, bg):
            b = tb_ // NBB
            u2 = u2T[tb_ % 2]
            uk = "u2T%d" % (tb_ % 2)
            slots = {}

            def pull(ic, n):
                for ent in bg:
                    if ent[0] is None:
                        continue
                    if ent[1] > ic:
                        return
                    while n > 0:
                        try:
                            next(ent[0])
                            n -= 1
                        except StopIteration:
                            ent[0] = None
                            break
                    if n == 0:
                        return

            def drain(pred):
                for ent in bg:
                    if ent[0] is not None and pred(ent):
                        for _ in ent[0]:
                            pass
                        ent[0] = None

            def emitU(pr):
                gi = tb_ * NG + pr
                j = gi % NBUF
                sl = pr % NSL
                p = pg_[sl]
                pk = "pgs%d" % sl
                slots[pr] = sl
                for c in range(2):
                    for kc in range(8):
                        I("pe", lambda e, p=p, j=j, c=c, kc=kc: e.matmul(
                            p[:, c * TB:(c + 1) * TB], lhsT=utb[j][:, kc, c * 128:(c + 1) * 128], rhs=u2[:, kc, :],
                            start=(kc == 0), stop=(kc == 7)), r=["utb%d" % j, uk], w=[pk])
                I("act", lambda e, p=p, sl=sl: e.activation(out=gl[sl][:], in_=p[:, :], func=AF.Gelu), r=[pk], w=["gl%d" % sl])
                I("dve", lambda e, sl=sl, pr=pr: e.tensor_tensor(
                    out=awt[sl][:, :].rearrange("p (c t) -> p c t", c=2), in0=gl[sl][:, :].rearrange("p (c t) -> p c t", c=2),
                    in1=WT[:, :, 2 * pr:2 * pr + 2].rearrange("p t i -> p i t"), op=ALU.mult),
                  r=["gl%d" % sl, "WT%d" % (pr // 32)], w=["awt%d" % sl])

            def emitV(pr):
                gi = tb_ * NG + pr
                j = gi % NBUF
                sl = slots.pop(pr)
                for c in range(2):
                    ic = 2 * pr + c
                    for ti in range(2):
                        for hf in range(2):
                            I("pe", lambda e, sl=sl, ti=ti, hf=hf, j=j, c=c, ic=ic: e.matmul(
                                py[ti][:, hf, :], lhsT=awt[sl][:, c * TB + ti * 128:c * TB + (ti + 1) * 128],
                                rhs=vtb[j][:, c, hf * 512:(hf + 1) * 512],
                                start=(ic == 0), stop=(ic == 127)), r=["awt%d" % sl, "vtb%d" % j], w=["py%d" % ti])
                if gi + NBUF < total_groups:
                    load_group(gi + NBUF)

            for pr in range(-LOOK, 64):
                if pr + LOOK < 64:
                    nu = pr + LOOK
                    if nu == 32:
                        drain(lambda ent: ent[2] <= 64)
                    emitU(nu)
                    if pr >= 0:
                        pull(2 * pr, PULL)
                if pr >= 0:
                    emitV(pr)
                    pull(2 * pr, PULL)
            drain(lambda ent: True)
            for ti in range(2):
                r0 = tb_ * TB + ti * 128
                Dm(x1s_[:], x1_d[r0:r0 + 128, :], r=["x1s"], w=["x1s_"])
                I("dve", lambda e, ti=ti: e.tensor_tensor(
                    out=sq2[:], in0=py[ti][:, :, :].rearrange("p a n -> p (a n)"), in1=g2gate[:], op=ALU.mult),
                  r=["py%d" % ti, "g2gate"], w=["sq2"])
                I("pool", lambda e: e.tensor_tensor(out=sq2[:], in0=sq2[:], in1=x1s_[:], op=ALU.add), r=["sq2", "x1s_"], w=["sq2"])
                I("act", lambda e: e.activation(out=x1s_[:], in_=sq2[:], func=AF.Square, accum_out=ss3[:]), r=["sq2"], w=["x1s_", "ss3"])
                I("act", lambda e: e.activation(out=ss3[:], in_=ss3[:], func=AF.Sqrt, scale=1.0 / D, bias=epsb[:]),
                  r=["ss3", "epsb"], w=["ss3"])
                I("dve", lambda e: e.reciprocal(out=ss3[:], in_=ss3[:]), r=["ss3"], w=["ss3"])
                I("dve", lambda e: e.scalar_tensor_tensor(
                    out=x1s_[:], in0=sq2[:], scalar=ss3[:], in1=gfrow[:], op0=ALU.mult, op1=ALU.mult),
                  r=["sq2", "ss3", "gfrow"], w=["x1s_"])
                l0 = (tb_ % NBB) * TB + ti * 128
                Dm(out_d[b, l0:l0 + 128, :], x1s_[:], r=["x1s_"], w=["out"])

        Dm(g2gate[:], grow_d[2:3, :].partition_broadcast(128), r=["grow_scr"], w=["g2gate"])
        for _ in stageS(0):
            pass
        if upto == "S0":
            kb.barrier()
            return nc
        for _ in stageW(0, 0):
            pass
        if upto == "S1":
            kb.barrier()
            return nc
        for tb_ in range(nblk):
            bg = [[stageW(tb_, 1), 0, 64]]
            if tb_ + 1 < nblk:
                bg.append([stageS(tb_ + 1), 0, 999])
                bg.append([stageW(tb_ + 1, 0), 64, 999])
            gemm_and_epilogue(tb_, bg)
            if tb_ + 1 < nblk and (tb_ + 1) % NBB == 0:
                Dm(g2gate[:], grow_d[2 + (tb_ + 1) // NBB:3 + (tb_ + 1) // NBB, :].partition_broadcast(128), r=["grow_scr"], w=["g2gate"])
        kb.barrier(engines=("sp",))
    return nc


def _pos_table():
    rows = SEQ // 64
    r, col = np.meshgrid(np.arange(rows, dtype=np.float32), np.arange(64, dtype=np.float32), indexing="ij")
    quarter = D // 4
    omega = (np.float32(10000.0) ** (-np.arange(quarter, dtype=np.float32) / np.float32(quarter))).astype(np.float32)

    def emb(p):
        ang = p.reshape(-1)[:, None].astype(np.float32) * omega[None, :]
        return np.concatenate([np.sin(ang), np.cos(ang)], axis=-1)

    return np.concatenate([emb(r), emb(col)], axis=-1).astype(np.float32)


_NC_CACHE = {}


def _fm(v, nchunk):
    return np.ascontiguousarray(np.asarray(v, np.float32).reshape(nchunk, 128).T)


def kernel(x, c, ctx, c_ctx, ada_w, ada_b, norm1_g, w_in, conv_w, conv_b, rg_w_a, rg_b_a, rg_w_x, rg_b_x, rg_lambda,
           gla_w_g, gla_b_g, gla_norm_g, w_out, norm2_g, peer_w_q, peer_keys, peer_u, peer_v, final_norm_g):
    if "nc" not in _NC_CACHE:
        _NC_CACHE["nc"] = build()
    nc = _NC_CACHE["nc"]
    in_maps = make_in_maps(x, c, ctx, c_ctx, ada_w, ada_b, norm1_g, w_in, conv_w, conv_b, rg_w_a, rg_b_a, rg_w_x, rg_b_x, rg_lambda,
                           gla_w_g, gla_b_g, gla_norm_g, w_out, norm2_g, peer_w_q, peer_keys, peer_u, peer_v, final_norm_g)
    res = run_bass_kernel_spmd(nc, in_maps, core_ids=list(range(NCORES)))
    out = np.concatenate([np.asarray(r["out"], dtype=np.float32) for r in res.results], axis=0)
    return out


def make_in_maps(x, c, ctx, c_ctx, ada_w, ada_b, norm1_g, w_in, conv_w, conv_b, rg_w_a, rg_b_a, rg_w_x, rg_b_x, rg_lambda,
                 gla_w_g, gla_b_g, gla_norm_g, w_out, norm2_g, peer_w_q, peer_keys, peer_u, peer_v, final_norm_g):
    f = lambda a: np.ascontiguousarray(np.asarray(a, dtype=np.float32))
    x, c, ctx, c_ctx = f(x), f(c), f(ctx), f(c_ctx)
    jj, ii = np.meshgrid(np.arange(128), np.arange(128), indexing="ij")
    mf = (jj <= ii).astype(np.float32)
    mb = (jj >= ii).astype(np.float32)
    shared = {
        "ada_w": f(ada_w[0]),
        "ada_b": f(ada_b[0]).reshape(1, -1),
        "ada_bT": _fm(ada_b[0], 48),
        "norm1_g": f(norm1_g[0]).reshape(1, -1),
        "norm2_g": f(norm2_g[0]).reshape(1, -1),
        "final_g": f(final_norm_g).reshape(1, -1),
        "w_in": f(w_in[0]),
        "convwT": np.ascontiguousarray(f(conv_w[0]).reshape(4, 4, 128).transpose(2, 1, 0)),
        "convbT": _fm(conv_b[0], 4),
        "rg_w_a": f(rg_w_a[0]),
        "rg_w_x": f(rg_w_x[0]),
        "rgbaT": np.ascontiguousarray(f(rg_b_a[0]).reshape(2, 4, 128).transpose(2, 0, 1)),
        "rgbxT": np.ascontiguousarray(f(rg_b_x[0]).reshape(2, 4, 128).transpose(2, 0, 1)),
        "rglamT": np.ascontiguousarray(f(rg_lambda[0]).reshape(2, 4, 128).transpose(2, 0, 1)),
        "gla_w_g": f(gla_w_g[0]),
        "glabgT": np.ascontiguousarray(f(gla_b_g[0]).reshape(2, 2, 128).transpose(2, 0, 1)),
        "glangT": f(gla_norm_g[0]).reshape(128, 1),
        "w_out": f(w_out[0]),
        "peer_w_q": f(peer_w_q[0]),
        "keysT": np.ascontiguousarray(f(peer_keys[0]).reshape(16, 128, 128).transpose(2, 0, 1)),
        "peer_u": f(peer_u[0]),
        "peer_v": f(peer_v[0]),
        "pos": _pos_table(),
        "ident": np.eye(128, dtype=np.float32),
        "iota": np.tile(np.arange(128, dtype=np.float32)[None, :], (128, 1)),
        "maskf": np.ascontiguousarray(np.concatenate([mf, mf], axis=1)),
        "maskb": np.ascontiguousarray(np.concatenate([mb, mb], axis=1)),
    }
    in_maps = []
    for i in range(NCORES):
        b0 = 2 * i
        cm = np.stack([c[b0], c[b0 + 1], c_ctx], axis=0)
        cT = np.ascontiguousarray(cm.reshape(3, 8, 128).transpose(2, 1, 0))
        m = dict(shared)
        m["x"] = np.ascontiguousarray(x[b0:b0 + 2])
        m["ctx"] = np.ascontiguousarray(ctx[b0:b0 + 2])
        m["cT"] = cT
        in_maps.append(m)
    return in_maps
```

```python
import contextlib
import numpy as np
import concourse.bass as bass
import concourse.mybir as mybir
from concourse.bass_utils import run_bass_kernel_spmd

F32 = mybir.dt.float32
BF16 = mybir.dt.bfloat16
U32 = mybir.dt.uint32
ALU = mybir.AluOpType
AF = mybir.ActivationFunctionType
AX = mybir.AxisListType

NCORES = 8
D = 1024
SEQ = 2048
CTX = 256
S = CTX + SEQ
NT = S // 128
INC = 2592
NEXP = 16384
TB = 256
EPS = 1e-6
NEG = -1.0e30


class KB:
    def __init__(self):
        self.nc = bass.Bass("TRN2", target_bir_lowering=False)
        nc = self.nc
        self.eng = {"pe": nc.tensor, "act": nc.scalar, "dve": nc.vector, "pool": nc.gpsimd, "sp": nc.sync}
        self.stack = contextlib.ExitStack()
        self.sem = {}
        self.cnt = {}
        for n in self.eng:
            self.sem[("e", n)] = self.stack.enter_context(nc.semaphore("s_" + n))
            self.cnt[n] = 0
        self.nd = 24
        self.dcnt = [0] * self.nd
        for i in range(self.nd):
            self.sem[("d", i)] = self.stack.enter_context(nc.semaphore("d%d" % i))
        self.dnext = 0
        self.waited = {}
        self.lastw = {}
        self.readers = {}

    def _wait(self, en, deps):
        for sk, v in deps.items():
            if en == "pe" and sk == ("e", "pe"):
                continue
            if self.waited.get((en, sk), 0) >= v:
                continue
            self.eng[en].wait_ge(self.sem[sk], v)
            self.waited[(en, sk)] = v

    def _deps(self, r, w):
        d = {}
        for k in r:
            t = self.lastw.get(k)
            if t:
                d[t[0]] = max(d.get(t[0], 0), t[1])
        for k in w:
            t = self.lastw.get(k)
            if t:
                d[t[0]] = max(d.get(t[0], 0), t[1])
            for sk, v in self.readers.get(k, {}).items():
                d[sk] = max(d.get(sk, 0), v)
        return d

    def _commit(self, tok, r, w):
        for k in w:
            self.lastw[k] = tok
            self.readers[k] = {}
        for k in r:
            rd = self.readers.setdefault(k, {})
            rd[tok[0]] = max(rd.get(tok[0], 0), tok[1])

    def I(self, en, fn, r=(), w=()):
        self._wait(en, self._deps(r, w))
        inst = fn(self.eng[en])
        self.cnt[en] += 1
        inst.then_inc(self.sem[("e", en)], 1)
        self._commit((("e", en), self.cnt[en]), r, w)

    def D(self, out, in_, r=(), w=(), q="sp"):
        i = self.dnext
        self.dnext = (i + 1) % self.nd
        deps = self._deps(r, w)
        if self.dcnt[i] > 0:
            deps[("d", i)] = max(deps.get(("d", i), 0), self.dcnt[i])
        self._wait(q, deps)
        inst = self.eng[q].dma_start(out=out, in_=in_)
        self.dcnt[i] += 16
        inst.then_inc(self.sem[("d", i)], 16)
        self._commit((("d", i), self.dcnt[i]), r, w)

    def barrier(self, engines=("pe", "act", "dve", "pool", "sp")):
        deps = {}
        for n in self.eng:
            if self.cnt[n] > 0:
                deps[("e", n)] = self.cnt[n]
        for i in range(self.nd):
            if self.dcnt[i] > 0:
                deps[("d", i)] = self.dcnt[i]
        for en in engines:
            d = dict(deps)
            d.pop(("e", en), None)
            self._wait(en, d)
        self.lastw = {}
        self.readers = {}


def build(upto=None, dbg=False):
    kb = KB()
    nc = kb.nc
    I, Dm = kb.I, kb.D

    def dram(name, shape, dt=F32, kind="ExternalInput"):
        return nc.dram_tensor(name, list(shape), dt, kind=kind).ap()

    x_d = dram("x", [2, SEQ, D])
    ctx_d = dram("ctx", [2, CTX, D])
    cT_d = dram("cT", [128, 8, 3])
    adaw_d = dram("ada_w", [D, 6 * D])
    adab_d = dram("ada_b", [1, 6 * D])
    adabT_d = dram("ada_bT", [128, 48])
    g1_d = dram("norm1_g", [1, D])
    g2_d = dram("norm2_g", [1, D])
    gf_d = dram("final_g", [1, D])
    win_d = dram("w_in", [D, INC])
    convw_d = dram("convwT", [128, 4, 4])
    convb_d = dram("convbT", [128, 4])
    rgwa_d = dram("rg_w_a", [2, 8, 64, 64])
    rgwx_d = dram("rg_w_x", [2, 8, 64, 64])
    rgba_d = dram("rgbaT", [128, 2, 4])
    rgbx_d = dram("rgbxT", [128, 2, 4])
    rglam_d = dram("rglamT", [128, 2, 4])
    glawg_d = dram("gla_w_g", [2, 16, 256])
    glabg_d = dram("glabgT", [128, 2, 2])
    glang_d = dram("glangT", [128, 1])
    wout_d = dram("w_out", [D, D])
    wq_d = dram("peer_w_q", [D, 2048])
    keysT_d = dram("keysT", [128, 16, 128])
    pu_d = dram("peer_u", [NEXP, D])
    pv_d = dram("peer_v", [NEXP, D])
    pos_d = dram("pos", [SEQ, D])
    ident_d = dram("ident", [128, 128])
    iota_d = dram("iota", [128, 128])
    maskf_d = dram("maskf", [128, 256])
    maskb_d = dram("maskb", [128, 256])
    out_d = dram("out", [2, SEQ, D], kind="ExternalOutput")
    x1_d = dram("x1s", [2 * SEQ, D], kind="ExternalOutput" if dbg else "Internal")
    grow_d = dram("grow_scr", [4, D], kind="Internal")
    ut_d = dram("ut_scr", [NEXP // 256, 128, 8, 256], BF16, kind="Internal")
    vs_d = dram("v_scr", [NEXP, D], BF16, kind="Internal")

    es = contextlib.ExitStack()

    uid = [0]

    def sb(stack, name, shape, dt=F32):
        uid[0] += 1
        return stack.enter_context(nc.sbuf_tensor("sb%d_%s" % (uid[0], name), list(shape), dt))

    def ps(stack, name, shape, dt=F32):
        uid[0] += 1
        return stack.enter_context(nc.psum_tensor("ps%d_%s" % (uid[0], name), list(shape), dt))

    ident = sb(es, "ident", [128, 128])
    identb = sb(es, "identb", [128, 128], BF16)
    iota = sb(es, "iota", [128, 128])
    ones = sb(es, "ones", [128, 128])
    modp = sb(es, "modp", [128, 6, 8, 3])
    epsb = sb(es, "epsb", [128, 1])

    Dm(ident[:], ident_d[:, :], w=["ident"])
    Dm(iota[:], iota_d[:, :], w=["iota"])
    I("dve", lambda e: e.tensor_copy(out=identb[:], in_=ident[:]), r=["ident"], w=["identb"])
    I("dve", lambda e: e.memset(ones[:], 1.0), w=["ones"])
    I("dve", lambda e: e.memset(epsb[:], EPS), w=["epsb"])

    with contextlib.ExitStack() as st:
        cT = sb(st, "cT", [128, 8, 3])
        scT = sb(st, "scT", [128, 8, 3])
        rep = sb(st, "rep", [128, 2, 8, 128])
        abT = sb(st, "abT", [128, 48])
        abrow = sb(st, "abrow", [128, D])
        growt = [sb(st, "growt%d" % i, [128, D]) for i in range(2)]
        aw = [sb(st, "aw%d" % i, [128, 8, D]) for i in range(2)]
        pm = [ps(st, "pm%d" % i, [128, 512]) for i in range(4)]
        Dm(cT[:], cT_d[:, :, :], w=["cT"])
        Dm(abT[:], adabT_d[:, :], w=["abT"])
        I("act", lambda e: e.activation(out=scT[:], in_=cT[:], func=AF.Silu), r=["cT"], w=["scT"])
        for b in range(2):
            I("dve", lambda e, b=b: e.tensor_copy(out=rep[:, b], in_=scT[:, :, b:b + 1].to_broadcast([128, 8, 128])),
              r=["scT"], w=["rep"])
        pi = 0
        for m in range(6):
            a = aw[m % 2]
            ak = "aw%d" % (m % 2)
            Dm(a[:], adaw_d[:, m * D:(m + 1) * D].rearrange("(kc p) n -> p kc n", p=128), w=[ak])
            if m in (0, 1, 3, 4):
                p = pm[pi % 4]
                pk = "pm%d" % (pi % 4)
                pi += 1
                for fc in range(8):
                    for kc in range(8):
                        I("pe", lambda e, fc=fc, kc=kc, p=p, a=a: e.matmul(
                            p[:, fc * 3:fc * 3 + 3], lhsT=a[:, kc, fc * 128:(fc + 1) * 128], rhs=scT[:, kc, :],
                            start=(kc == 0), stop=(kc == 7)), r=[ak, "scT"], w=[pk])
                I("dve", lambda e, m=m, p=p: e.tensor_tensor(
                    out=modp[:, m], in0=p[:, 0:24].rearrange("p (f c) -> p f c", c=3),
                    in1=abT[:, m * 8:(m + 1) * 8].unsqueeze(2).to_broadcast([128, 8, 3]), op=ALU.add),
                  r=[pk, "abT"], w=["modp"])
                if m in (1, 4):
                    I("dve", lambda e, m=m: e.tensor_scalar_add(out=modp[:, m], in0=modp[:, m], scalar1=1.0),
                      r=["modp"], w=["modp"])
            else:
                mi = 0 if m == 2 else 1
                Dm(abrow[:], adab_d[0:1, m * D:(m + 1) * D].partition_broadcast(128), w=["abrow"])
                for b in range(2):
                    for hf in range(2):
                        p = pm[pi % 4]
                        pk = "pm%d" % (pi % 4)
                        pi += 1
                        for kc in range(8):
                            I("pe", lambda e, kc=kc, p=p, a=a, b=b, hf=hf: e.matmul(
                                p[:, :], lhsT=rep[:, b, kc, :], rhs=a[:, kc, hf * 512:(hf + 1) * 512],
                                start=(kc == 0), stop=(kc == 7)), r=[ak, "rep"], w=[pk])
                        I("dve", lambda e, p=p, b=b, hf=hf, mi=mi: e.tensor_tensor(
                            out=growt[b][:, hf * 512:(hf + 1) * 512], in0=p[:, :],
                            in1=abrow[:, hf * 512:(hf + 1) * 512], op=ALU.add),
                          r=[pk, "abrow"], w=["growt%d" % b])
                    Dm(grow_d[mi * 2 + b:mi * 2 + b + 1, :], growt[b][0:1, :], r=["growt%d" % b], w=["grow_scr"])
        kb.barrier()
    if upto == "M0":
        return nc

    with contextlib.ExitStack() as sm:
        g1row = sb(sm, "g1row", [128, D])
        g1gate = sb(sm, "g1gate", [128, 2, D])
        for b in range(2):
            Dm(g1gate[:, b, :], grow_d[b:b + 1, :].partition_broadcast(128), r=["grow_scr"], w=["g1gate"])
        maskf = sb(sm, "maskf", [128, 256], BF16)
        maskb = sb(sm, "maskb", [128, 256], BF16)
        mstage = sb(sm, "mstage", [128, 256])
        cw = sb(sm, "cw", [128, 4, 4])
        cb = sb(sm, "cb", [128, 4])
        rba = sb(sm, "rba", [128, 2, 4])
        rbx = sb(sm, "rbx", [128, 2, 4])
        rlam = sb(sm, "rlam", [128, 2, 4])
        coef = sb(sm, "coef", [128, 2, 4])
        gbg = sb(sm, "gbg", [128, 2, 2])
        ngbg = sb(sm, "ngbg", [128, 2, 2])
        gng = sb(sm, "gng", [128, 1])
        wg = sb(sm, "wg", [32, 2, 256])
        wbd = sb(sm, "wbd", [128, 2, 2, 4, 128])
        hm = sb(sm, "hm", [128, 2])
        bdm = sb(sm, "bdm", [128, 256])
        I("dve", lambda e: e.memset(hm[:], 0.0), w=["hm"])
        I("dve", lambda e: e.memset(hm[0:64, 0:1], 1.0), w=["hm"])
        I("dve", lambda e: e.memset(hm[64:128, 1:2], 1.0), w=["hm"])
        I("dve", lambda e: e.memset(bdm[:], 0.0), w=["bdm"])
        I("dve", lambda e: e.memset(bdm[0:64, 0:128], 1.0), w=["bdm"])
        I("dve", lambda e: e.memset(bdm[64:128, 128:256], 1.0), w=["bdm"])
        Dm(g1row[:], g1_d[0:1, :].partition_broadcast(128), w=["g1row"])
        Dm(mstage[:], maskf_d[:, :], w=["mstage"])
        I("dve", lambda e: e.tensor_copy(out=maskf[:], in_=mstage[:]), r=["mstage"], w=["maskf"])
        Dm(mstage[:], maskb_d[:, :], w=["mstage"])
        I("dve", lambda e: e.tensor_copy(out=maskb[:], in_=mstage[:]), r=["mstage"], w=["maskb"])
        Dm(cw[:], convw_d[:, :, :], w=["cw"])
        Dm(cb[:], convb_d[:, :], w=["cb"])
        Dm(rba[:], rgba_d[:, :, :], w=["rba"])
        Dm(rbx[:], rgbx_d[:, :, :], w=["rbx"])
        Dm(rlam[:], rglam_d[:, :, :], w=["rlam"])
        Dm(gbg[:], glabg_d[:, :, :], w=["gbg"])
        Dm(gng[:], glang_d[:, :], w=["gng"])
        I("act", lambda e: e.activation(out=coef[:], in_=rlam[:], func=AF.Exp, scale=-1.0), r=["rlam"], w=["coef"])
        I("act", lambda e: e.activation(out=coef[:], in_=coef[:], func=AF.Ln, bias=1.0), r=["coef"], w=["coef"])
        I("dve", lambda e: e.tensor_scalar_mul(out=coef[:], in0=coef[:], scalar1=-8.0), r=["coef"], w=["coef"])
        I("dve", lambda e: e.tensor_scalar_mul(out=ngbg[:], in0=gbg[:], scalar1=-1.0), r=["gbg"], w=["ngbg"])
        I("dve", lambda e: e.memset(wg[:], 0.0), w=["wg"])
        for d in range(2):
            Dm(wg[16 * d:16 * d + 16, d, :], glawg_d[d, :, :], w=["wg"])
        I("dve", lambda e: e.memset(wbd[:], 0.0), w=["wbd"])
        for gi, src in enumerate((rgwa_d, rgwx_d)):
            for d in range(2):
                for cc in range(4):
                    for h in range(2):
                        Dm(wbd[64 * h:64 * h + 64, gi, d, cc, 64 * h:64 * h + 64], src[d, 2 * cc + h, :, :], w=["wbd"])

        for b in range(2):
            with contextlib.ExitStack() as sbt:
                mgT = sb(sbt, "mgT", [128, 8, SEQ], BF16)
                qT = sb(sbt, "qT", [128, 2, SEQ], BF16)
                kT = sb(sbt, "kT", [128, 2, S], BF16)
                vtok = sb(sbt, "vtok", [128, NT, 512], BF16)
                lrT = sb(sbt, "lrT", [32, S])
                sR = contextlib.ExitStack()
                rxT = sb(sR, "rxT", [128, 4, S], BF16)
                with contextlib.ExitStack() as s1:
                    winb = sb(s1, "winb", [128, 8, INC], BF16)
                    wst0 = sb(s1, "wst0", [128, INC // 2])
                    wst = [wst0, wst0]
                    uT = [sb(s1, "uT%d" % i, [128, 8, 512], BF16) for i in range(2)]
                    xt = [sb(s1, "xt%d" % i, [128, D]) for i in range(2)]
                    pt0 = sb(s1, "pt0", [128, D])
                    pt = [pt0, pt0]
                    xn = [sb(s1, "xn%d" % i, [128, D], BF16) for i in range(2)]
                    sq = sb(s1, "sqj", [128, D], BF16)
                    ss = [sb(s1, "ss%d" % i, [128, 1]) for i in range(2)]
                    tp = [ps(s1, "tp%d" % i, [128, 8, 128], BF16) for i in range(2)]
                    pj = [ps(s1, "pj%d" % i, [128, 512]) for i in range(4)]
                    hw = INC // 2
                    for kc in range(8):
                        for hh in range(2):
                            w_ = wst[hh]
                            Dm(w_[:], win_d[kc * 128:(kc + 1) * 128, hh * hw:(hh + 1) * hw], w=["wst0"])
                            I("pool" if hh else "dve", lambda e, w_=w_, kc=kc, hh=hh: e.tensor_copy(
                                out=winb[:, kc, hh * hw:(hh + 1) * hw], in_=w_[:]), r=["wst0"], w=["winb"])
                    ngroups = 5
                    pjc = 0
                    for g in range(ngroups):
                        t0 = g * 4
                        nt = min(4, NT - t0)
                        ntok = nt * 128
                        u = uT[g % 2]
                        uk = "uT%d" % (g % 2)
                        for ti in range(nt):
                            t = t0 + ti
                            pb = t % 2
                            xk, xnk, ssk, tpk, ptk = "xt%d" % pb, "xn%d" % pb, "ss%d" % pb, "tp%d" % pb, "pt0"
                            if t < 2:
                                Dm(xt[pb][:], ctx_d[b, t * 128:(t + 1) * 128, :], w=[xk])
                                col = 2
                            else:
                                l0 = (t - 2) * 128
                                Dm(xt[pb][:], x_d[b, l0:l0 + 128, :], w=[xk])
                                Dm(pt[pb][:], pos_d[l0:l0 + 128, :], w=[ptk])
                                I("pool", lambda e, pb=pb: e.tensor_tensor(out=xt[pb][:], in0=xt[pb][:], in1=pt[pb][:], op=ALU.add),
                                  r=[xk, ptk], w=[xk])
                                col = b
                            I("act", lambda e, pb=pb: e.activation(out=sq[:], in_=xt[pb][:], func=AF.Square, accum_out=ss[pb][:]),
                              r=[xk], w=["sqj", ssk])
                            I("act", lambda e, pb=pb: e.activation(out=ss[pb][:], in_=ss[pb][:], func=AF.Sqrt, scale=1.0 / D, bias=epsb[:]),
                              r=[ssk, "epsb"], w=[ssk])
                            I("dve", lambda e, pb=pb: e.reciprocal(out=ss[pb][:], in_=ss[pb][:]), r=[ssk], w=[ssk])
                            I("dve", lambda e, pb=pb: e.scalar_tensor_tensor(
                                out=xn[pb][:], in0=xt[pb][:], scalar=ss[pb][:], in1=g1row[:], op0=ALU.mult, op1=ALU.mult),
                              r=[xk, ssk, "g1row"], w=[xnk])
                            for fc in range(8):
                                I("pe", lambda e, pb=pb, fc=fc: e.transpose(tp[pb][:, fc, :], xn[pb][:, fc * 128:(fc + 1) * 128], identb[:]),
                                  r=[xnk, "identb"], w=[tpk])
                            for fc in range(8):
                                I("dve" if fc % 2 else "pool" if False else "dve", lambda e, pb=pb, fc=fc, ti=ti, u=u, col=col: e.tensor_scalar(
                                    out=u[:, fc, ti * 128:(ti + 1) * 128], in0=tp[pb][:, fc, :],
                                    scalar1=modp[:, 1, fc, col:col + 1], scalar2=modp[:, 0, fc, col:col + 1],
                                    op0=ALU.mult, op1=ALU.add), r=[tpk, "modp"], w=[uk])
                        c0 = t0 * 128
                        lat0 = max(c0, CTX)
                        for cch in range(21):
                            cs_ = cch * 128
                            ncol = 128 if cch < 20 else 32
                            p = pj[pjc % 4]
                            pk = "pj%d" % (pjc % 4)
                            pjc += 1
                            if 12 <= cch < 16:
                                continue
                            lat_only = (4 <= cch < 10) or (16 <= cch < 20)
                            if lat_only and lat0 >= c0 + ntok:
                                continue
                            for kc in range(8):
                                I("pe", lambda e, kc=kc, p=p, u=u, cs_=cs_, ncol=ncol, ntok=ntok: e.matmul(
                                    p[0:ncol, 0:ntok], lhsT=winb[:, kc, cs_:cs_ + ncol], rhs=u[:, kc, 0:ntok],
                                    start=(kc == 0), stop=(kc == 7)), r=["winb", uk], w=[pk])
                            o0 = lat0 - c0
                            if cch < 4:
                                I("act", lambda e, p=p, cch=cch, c0=c0, ntok=ntok: e.copy(out=rxT[:, cch, c0:c0 + ntok], in_=p[:, 0:ntok]),
                                  r=[pk], w=["rxT"])
                            elif cch < 8:
                                I("act", lambda e, p=p, cch=cch, o0=o0, lat0=lat0, ntok=ntok: e.activation(
                                    out=mgT[:, cch - 4, lat0 - CTX:lat0 - CTX + ntok - o0], in_=p[:, o0:ntok], func=AF.Gelu),
                                  r=[pk], w=["mgT"])
                            elif cch < 10:
                                I("dve", lambda e, p=p, cch=cch, o0=o0, lat0=lat0, ntok=ntok: e.tensor_scalar_mul(
                                    out=qT[:, cch - 8, lat0 - CTX:lat0 - CTX + ntok - o0], in0=p[:, o0:ntok], scalar1=0.125),
                                  r=[pk], w=["qT"])
                            elif cch < 12:
                                I("dve", lambda e, p=p, cch=cch, c0=c0, ntok=ntok: e.tensor_copy(out=kT[:, cch - 10, c0:c0 + ntok], in_=p[:, 0:ntok]),
                                  r=[pk], w=["kT"])
                            elif cch < 20:
                                I("act", lambda e, p=p, cch=cch, o0=o0, lat0=lat0, ntok=ntok: e.activation(
                                    out=mgT[:, 4 + cch - 16, lat0 - CTX:lat0 - CTX + ntok - o0], in_=p[:, o0:ntok], func=AF.Silu),
                                  r=[pk], w=["mgT"])
                            else:
                                I("dve", lambda e, p=p, c0=c0, ntok=ntok: e.tensor_copy(out=lrT[:, c0:c0 + ntok], in_=p[0:32, 0:ntok]),
                                  r=[pk], w=["lrT"])
                        for ti in range(nt):
                            p = pj[pjc % 4]
                            pk = "pj%d" % (pjc % 4)
                            pjc += 1
                            for kc in range(8):
                                I("pe", lambda e, kc=kc, p=p, u=u, ti=ti: e.matmul(
                                    p[:, :], lhsT=u[:, kc, ti * 128:(ti + 1) * 128], rhs=winb[:, kc, 1536:2048],
                                    start=(kc == 0), stop=(kc == 7)), r=["winb", uk], w=[pk])
                            I("act", lambda e, p=p, t=t0 + ti: e.copy(out=vtok[:, t, :], in_=p[:, :]), r=[pk], w=["vtok"])
                    kb.barrier()

                if upto == "M1a":
                    sR.close()
                    return nc
                with contextlib.ExitStack() as s2:
                    xc = sb(s2, "xc", [128, S])
                    gr = sb(s2, "gr", [128, S])
                    gi_ = sb(s2, "gi", [128, S])
                    aa = sb(s2, "aa", [128, S])
                    bb = sb(s2, "bb", [128, S])
                    hf_ = sb(s2, "hf", [128, S])
                    hb_ = sb(s2, "hb", [128, S])
                    pg = [ps(s2, "pg%d" % i, [128, 512]) for i in range(4)]
                    pgc = 0
                    segs = ((0, CTX), (CTX, S))
                    for cc in range(4):
                        I("dve", lambda e, cc=cc: e.tensor_scalar(
                            out=xc[:], in0=rxT[:, cc, :], scalar1=cw[:, cc, 2:3], scalar2=cb[:, cc:cc + 1],
                            op0=ALU.mult, op1=ALU.add), r=["rxT", "cw", "cb"], w=["xc"])
                        for (a0, a1) in segs:
                            for j, sh in ((0, -2), (1, -1), (3, 1)):
                                lo = max(a0, a0 - sh)
                                hi = min(a1, a1 - sh)
                                I("dve", lambda e, cc=cc, j=j, sh=sh, lo=lo, hi=hi: e.scalar_tensor_tensor(
                                    out=xc[:, lo:hi], in0=rxT[:, cc, lo + sh:hi + sh], scalar=cw[:, cc, j:j + 1],
                                    in1=xc[:, lo:hi], op0=ALU.mult, op1=ALU.add), r=["rxT", "cw", "xc"], w=["xc"])
                        for d in range(2):
                            for gi, (gt, gk, bias_t) in enumerate(((gr, "gr", rba), (gi_, "gi", rbx))):
                                for g in range(5):
                                    c0 = g * 512
                                    n = min(512, S - c0)
                                    p = pg[pgc % 4]
                                    pk = "pg%d" % (pgc % 4)
                                    pgc += 1
                                    I("pe", lambda e, p=p, gi=gi, d=d, cc=cc, c0=c0, n=n: e.matmul(
                                        p[:, 0:n], lhsT=wbd[:, gi, d, cc, :], rhs=xc[:, c0:c0 + n], start=True, stop=True),
                                      r=["wbd", "xc"], w=[pk])
                                    I("act", lambda e, p=p, gt=gt, bias_t=bias_t, d=d, cc=cc, c0=c0, n=n: e.activation(
                                        out=gt[:, c0:c0 + n], in_=p[:, 0:n], func=AF.Sigmoid, bias=bias_t[:, d, cc:cc + 1]),
                                      r=[pk], w=[gk])
                            I("act", lambda e, d=d, cc=cc: e.activation(out=aa[:], in_=gr[:], func=AF.Exp, scale=coef[:, d, cc:cc + 1]),
                              r=["gr", "coef"], w=["aa"])
                            I("pool", lambda e: e.tensor_tensor(out=bb[:], in0=aa[:], in1=aa[:], op=ALU.mult), r=["aa"], w=["bb"])
                            I("act", lambda e: e.activation(out=bb[:], in_=bb[:], func=AF.Sqrt, scale=-1.0, bias=1.0), r=["bb"], w=["bb"])
                            I("pool", lambda e: e.tensor_tensor(out=bb[:], in0=bb[:], in1=gi_[:], op=ALU.mult), r=["bb", "gi"], w=["bb"])
                            I("pool", lambda e: e.tensor_tensor(out=bb[:], in0=bb[:], in1=xc[:], op=ALU.mult), r=["bb", "xc"], w=["bb"])
                            if d == 0:
                                I("dve", lambda e: e.tensor_tensor_scan(out=hf_[:], data0=aa[:], data1=bb[:], initial=0.0,
                                                                         op0=ALU.mult, op1=ALU.add), r=["aa", "bb"], w=["hf"])
                            else:
                                I("dve", lambda e: e.tensor_tensor_scan(out=hb_[:, 0:CTX][:, ::-1], data0=aa[:, 0:CTX][:, ::-1],
                                                                         data1=bb[:, 0:CTX][:, ::-1], initial=0.0,
                                                                         op0=ALU.mult, op1=ALU.add), r=["aa", "bb"], w=["hb"])
                                I("dve", lambda e: e.tensor_tensor_scan(out=hb_[:, CTX:S][:, ::-1], data0=aa[:, CTX:S][:, ::-1],
                                                                         data1=bb[:, CTX:S][:, ::-1], initial=hb_[:, 0:1],
                                                                         op0=ALU.mult, op1=ALU.add), r=["aa", "bb", "hb"], w=["hb"])
                        I("dve", lambda e: e.tensor_tensor(out=hf_[:, CTX:S], in0=hf_[:, CTX:S], in1=hb_[:, CTX:S], op=ALU.add),
                          r=["hf", "hb"], w=["hf"])
                        I("dve", lambda e, cc=cc: e.tensor_tensor(out=mgT[:, cc, :], in0=mgT[:, cc, :], in1=hf_[:, CTX:S], op=ALU.mult),
                          r=["hf", "mgT"], w=["mgT"])
                    kb.barrier()

                if upto == "M1b":
                    sR.close()
                    return nc
                sR.close()
                with contextlib.ExitStack() as s3:
                    otot = sb(s3, "otot", [128, 4, SEQ])
                    scr = sb(s3, "scr", [128, 2, S])
                    la = scr[:, 0, :]
                    bc = scr[:, 1, :]
                    dsg_all = scr[:, :, :].rearrange("p a n -> p (a n)").rearrange("p (c v) -> p c v", v=256)
                    eb = sb(s3, "eb", [128, S])
                    qd = sb(s3, "qd", [128, SEQ], BF16)
                    ki = sb(s3, "ki", [128, S], BF16)
                    kih = [sb(s3, "kih%d" % i, [128, S], BF16) for i in range(2)]
                    kit = [sb(s3, "kit%d" % i, [128, 128], BF16) for i in range(2)]
                    gam = sb(s3, "gam", [128, NT])
                    Sst = [sb(s3, "Sst%d" % i, [128, 256]) for i in range(2)]
                    Sbf_all = sb(s3, "Sbfa", [128, 16, 256], BF16)
                    scb_all = sb(s3, "scba", [128, 16, 256], BF16)
                    pz = [ps(s3, "pz%d" % i, [128, 512]) for i in range(1)]
                    ptr = [ps(s3, "ptr%d" % i, [128, 128], BF16) for i in range(2)]
                    pds = [ps(s3, "pds%d" % i, [128, 256]) for i in range(2)]
                    psc = [ps(s3, "psc%d" % i, [128, 256]) for i in range(2)]
                    po = [ps(s3, "po%d" % i, [128, 256]) for i in range(1)]
                    zc = 0
                    cn = 0
                    first_o = {0: True, 1: True}
                    for d in range(2):
                        order = list(range(NT)) if d == 0 else [1, 0] + list(range(NT - 1, 1, -1))
                        mk, mkk = (maskf, "maskf") if d == 0 else (maskb, "maskb")
                        for pr in range(2):
                            for g in range(5):
                                c0 = g * 512
                                n = min(512, S - c0)
                                p = pz[0]
                                pk = "pz0"
                                I("pe", lambda e, p=p, d=d, pr=pr, c0=c0, n=n: e.matmul(
                                    p[:, 0:n], lhsT=wg[:, d, pr * 128:(pr + 1) * 128], rhs=lrT[:, c0:c0 + n], start=True, stop=True),
                                  r=["wg", "lrT"], w=[pk])
                                I("act", lambda e, p=p, d=d, pr=pr, c0=c0, n=n: e.activation(
                                    out=la[:, c0:c0 + n], in_=p[:, 0:n], func=AF.Exp, scale=-1.0, bias=ngbg[:, d, pr:pr + 1]),
                                  r=[pk, "ngbg"], w=["la"])
                            I("act", lambda e: e.activation(out=la, in_=la, func=AF.Ln, bias=1.0), r=["la"], w=["la"])
                            for n_ in range(NT):
                                c0 = n_ * 128
                                if d == 0:
                                    I("dve", lambda e, c0=c0: e.tensor_tensor_scan(
                                        out=bc[:, c0:c0 + 128], data0=ones[:, :], data1=la[:, c0:c0 + 128], initial=0.0,
                                        op0=ALU.mult, op1=ALU.add), r=["la", "ones"], w=["bc"])
                                else:
                                    I("dve", lambda e, c0=c0: e.tensor_tensor_scan(
                                        out=bc[:, c0:c0 + 128][:, ::-1], data0=ones[:, :], data1=la[:, c0:c0 + 128][:, ::-1], initial=0.0,
                                        op0=ALU.mult, op1=ALU.add), r=["la", "ones"], w=["bc"])
                            I("act", lambda e: e.activation(out=eb[:], in_=bc, func=AF.Exp, scale=-1.0 / 16.0), r=["bc"], w=["eb"])
                            I("act", lambda e: e.activation(out=la, in_=bc, func=AF.Exp, scale=1.0 / 16.0), r=["bc"], w=["la"])
                            I("pool", lambda e, pr=pr: e.tensor_tensor(out=qd[:], in0=qT[:, pr, :], in1=eb[:, CTX:S], op=ALU.mult),
                              r=["qT", "eb"], w=["qd"])
                            I("dve", lambda e, pr=pr: e.tensor_tensor(out=ki[:], in0=kT[:, pr, :], in1=la, op=ALU.mult),
                              r=["kT", "la"], w=["ki"])
                            for h in range(2):
                                if h == 0:
                                    I("act", lambda e, h=h: e.mul(out=kih[h][:], in_=ki[:], mul=hm[:, h:h + 1]),
                                      r=["ki", "hm"], w=["kih%d" % h])
                                else:
                                    I("dve", lambda e, h=h: e.tensor_scalar_mul(out=kih[h][:], in0=ki[:], scalar1=hm[:, h:h + 1]),
                                      r=["ki", "hm"], w=["kih%d" % h])
                            gsrc = eb[:, 127::128] if d == 0 else eb[:, 0::128]
                            I("dve", lambda e, gsrc=gsrc: e.tensor_copy(out=gam[:], in_=gsrc), r=["eb"], w=["gam"])
                            I("dve", lambda e: e.memset(Sst[0][:], 0.0), w=["Sst0"])

                            def tr_(n_):
                                j = n_ % 2
                                I("pe", lambda e: e.transpose(ptr[j][:, :], ki[:, n_ * 128:(n_ + 1) * 128], identb[:]),
                                  r=["ki", "identb"], w=["ptr%d" % j])
                                I("act", lambda e: e.copy(out=kit[j][:], in_=ptr[j][:, :]), r=["ptr%d" % j], w=["kit%d" % j])

                            tr_(0)
                            for n_ in range(NT):
                                if n_ + 1 < NT:
                                    tr_(n_ + 1)
                                j = n_ % 2
                                dk = "la" if n_ < 9 else "bc"
                                I("pe", lambda e, j=j, n_=n_: e.matmul(
                                    pds[j][:, :], lhsT=kit[j][:, :], rhs=vtok[:, n_, pr * 256:(pr + 1) * 256], start=True, stop=True),
                                  r=["kit%d" % j, "vtok"], w=["pds%d" % j])
                                I("act", lambda e, j=j, n_=n_: e.mul(
                                    out=dsg_all[:, n_, :], in_=pds[j][:, :], mul=gam[:, n_:n_ + 1]),
                                  r=["pds%d" % j, "gam"], w=[dk])
                                if n_ >= 2:
                                    c0 = n_ * 128
                                    l0 = c0 - CTX
                                    for h in range(2):
                                        I("pe", lambda e, h=h, c0=c0, l0=l0, j=j: e.matmul(
                                            psc[j][:, h * 128:(h + 1) * 128], lhsT=kih[h][:, c0:c0 + 128],
                                            rhs=qd[:, l0:l0 + 128], start=True, stop=True),
                                          r=["kih%d" % h, "qd"], w=["psc%d" % j])
                                    I("dve", lambda e, j=j, n_=n_: e.tensor_tensor(out=scb_all[:, n_ - 2, :], in0=psc[j][:, :], in1=mk[:], op=ALU.mult),
                                      r=["psc%d" % j, mkk], w=["scba"])
                            for k_, n_ in enumerate(order):
                                a_, b_ = k_ % 2, (k_ + 1) % 2
                                dk = "la" if n_ < 9 else "bc"
                                if n_ >= 2:
                                    I("pool", lambda e, a_=a_, n_=n_: e.tensor_tensor(out=Sbf_all[:, n_ - 2, :], in0=Sst[a_][:], in1=bdm[:], op=ALU.mult),
                                      r=["Sst%d" % a_, "bdm"], w=["Sbfa"])
                                I("dve", lambda e, a_=a_, b_=b_, n_=n_: e.scalar_tensor_tensor(
                                    out=Sst[b_][:], in0=Sst[a_][:], scalar=gam[:, n_:n_ + 1], in1=dsg_all[:, n_, :],
                                    op0=ALU.mult, op1=ALU.add), r=["Sst%d" % a_, "gam", dk], w=["Sst%d" % b_])
                            pbanks = [(po[0], "po0"), (psc[0], "psc0"), (psc[1], "psc1")]
                            for n_ in range(2, NT):
                                l0 = n_ * 128 - CTX
                                pp, ppk = pbanks[n_ % len(pbanks)]
                                for h in range(2):
                                    hd = pr * 2 + h
                                    I("pe", lambda e, h=h, hd=hd, n_=n_, pp=pp: e.matmul(
                                        pp[:, h * 128:(h + 1) * 128], lhsT=vtok[:, n_, hd * 128:(hd + 1) * 128],
                                        rhs=scb_all[:, n_ - 2, h * 128:(h + 1) * 128], start=True, stop=False),
                                      r=["vtok", "scba"], w=[ppk])
                                    I("pe", lambda e, h=h, n_=n_, l0=l0, pp=pp: e.matmul(
                                        pp[:, h * 128:(h + 1) * 128], lhsT=Sbf_all[:, n_ - 2, h * 128:(h + 1) * 128],
                                        rhs=qd[:, l0:l0 + 128], start=False, stop=True),
                                      r=["Sbfa", "qd"], w=[ppk])
                                ov = otot[:, pr * 2:pr * 2 + 2, l0:l0 + 128]
                                pv_ = pp[:, :].rearrange("p (h t) -> p h t", h=2)
                                if d == 0:
                                    I("act", lambda e, ov=ov, pv_=pv_: e.copy(out=ov, in_=pv_), r=[ppk], w=["otot"])
                                else:
                                    I("dve", lambda e, ov=ov, pv_=pv_: e.tensor_tensor(out=ov, in0=ov, in1=pv_, op=ALU.add),
                                      r=[ppk, "otot"], w=["otot"])
                    sqo = sb(s3, "sqo", [128, 512])
                    rs = sb(s3, "rs", [128, 512])
                    tmpo = sb(s3, "tmpo", [128, 512])
                    for hd in range(4):
                        for g in range(4):
                            c0 = g * 512
                            p = pz[0]
                            pk = "pz0"
                            I("act", lambda e, hd=hd, c0=c0: e.activation(out=sqo[:], in_=otot[:, hd, c0:c0 + 512], func=AF.Square),
                              r=["otot"], w=["sqo"])
                            I("pe", lambda e, p=p: e.matmul(p[:, :], lhsT=ones[:, :], rhs=sqo[:], start=True, stop=True),
                              r=["ones", "sqo"], w=[pk])
                            I("act", lambda e, p=p: e.activation(out=rs[:], in_=p[:, :], func=AF.Sqrt, scale=1.0 / 128.0, bias=epsb[:]),
                              r=[pk, "epsb"], w=["rs"])
                            I("dve", lambda e: e.reciprocal(out=rs[:], in_=rs[:]), r=["rs"], w=["rs"])
                            I("dve", lambda e, hd=hd, c0=c0: e.scalar_tensor_tensor(
                                out=tmpo[:], in0=otot[:, hd, c0:c0 + 512], scalar=gng[:, 0:1], in1=rs[:], op0=ALU.mult, op1=ALU.mult),
                              r=["otot", "gng", "rs"], w=["tmpo"])
                            I("pool", lambda e, hd=hd, c0=c0: e.tensor_tensor(
                                out=mgT[:, 4 + hd, c0:c0 + 512], in0=mgT[:, 4 + hd, c0:c0 + 512], in1=tmpo[:], op=ALU.mult),
                              r=["tmpo", "mgT"], w=["mgT"])
                    kb.barrier()

                if upto == "M1c":
                    return nc
                with contextlib.ExitStack() as s4:
                    woutb = sb(s4, "woutb", [128, 8, D], BF16)
                    wst2 = [sb(s4, "wso%d" % i, [128, D]) for i in range(2)]
                    xt = [sb(s4, "xo%d" % i, [128, D]) for i in range(2)]
                    pt = [sb(s4, "po_%d" % i, [128, D]) for i in range(2)]
                    tm = [sb(s4, "tm%d" % i, [128, D]) for i in range(2)]
                    pw = [ps(s4, "pw%d" % i, [128, 2, 512]) for i in range(2)]
                    for kc in range(8):
                        w_ = wst2[kc % 2]
                        Dm(w_[:], wout_d[kc * 128:(kc + 1) * 128, :], w=["wso%d" % (kc % 2)])
                        I("dve", lambda e, w_=w_, kc=kc: e.tensor_copy(out=woutb[:, kc, :], in_=w_[:]), r=["wso%d" % (kc % 2)], w=["woutb"])
                    for t in range(16):
                        pb = t % 2
                        l0 = t * 128
                        Dm(xt[pb][:], x_d[b, l0:l0 + 128, :], w=["xo%d" % pb])
                        Dm(pt[pb][:], pos_d[l0:l0 + 128, :], w=["po_%d" % pb])
                        I("pool", lambda e, pb=pb: e.tensor_tensor(out=xt[pb][:], in0=xt[pb][:], in1=pt[pb][:], op=ALU.add),
                          r=["xo%d" % pb, "po_%d" % pb], w=["xo%d" % pb])
                        for hf in range(2):
                            for mc in range(8):
                                I("pe", lambda e, pb=pb, hf=hf, mc=mc, l0=l0: e.matmul(
                                    pw[pb][:, hf, :], lhsT=mgT[:, mc, l0:l0 + 128], rhs=woutb[:, mc, hf * 512:(hf + 1) * 512],
                                    start=(mc == 0), stop=(mc == 7)), r=["mgT", "woutb"], w=["pw%d" % pb])
                        I("dve", lambda e, pb=pb: e.tensor_tensor(
                            out=tm[pb][:], in0=pw[pb][:, :, :].rearrange("p a n -> p (a n)"), in1=g1gate[:, b, :], op=ALU.mult),
                          r=["pw%d" % pb, "g1gate"], w=["tm%d" % pb])
                        I("pool", lambda e, pb=pb: e.tensor_tensor(out=tm[pb][:], in0=tm[pb][:], in1=xt[pb][:], op=ALU.add),
                          r=["tm%d" % pb, "xo%d" % pb], w=["tm%d" % pb])
                        Dm(x1_d[b * SEQ + l0:b * SEQ + l0 + 128, :], tm[pb][:], r=["tm%d" % pb], w=["x1s"])
                    kb.barrier()
                if upto == "M1d":
                    return nc

    if upto == "M1":
        return nc
    with contextlib.ExitStack() as sc:
        uin = [sb(sc, "uin%d" % i, [128, 4, D]) for i in range(2)]
        ubf = [sb(sc, "ubf%d" % i, [128, 4, D], BF16) for i in range(2)]
        uts = [sb(sc, "uts%d" % i, [128, 8, 512], BF16) for i in range(2)]
        vin = [sb(sc, "vin%d" % i, [128, 4, D]) for i in range(2)]
        vbf = [sb(sc, "vbf%d" % i, [128, 4, D], BF16) for i in range(2)]
        ptc = [ps(sc, "ptc%d" % i, [128, 8, 128], BF16) for i in range(4)]
        tcn = 0
        for g in range(32):
            j = g % 2
            e0 = g * 512
            Dm(uin[j][:], pu_d[e0:e0 + 512, :].rearrange("(c p) n -> p c n", p=128), w=["uin%d" % j])
            Dm(vin[j][:], pv_d[e0:e0 + 512, :].rearrange("(c p) n -> p c n", p=128), w=["vin%d" % j])
            I("dve", lambda e, j=j: e.tensor_copy(out=ubf[j][:], in_=uin[j][:]), r=["uin%d" % j], w=["ubf%d" % j])
            if g % 2 == 0:
                I("dve", lambda e, j=j: e.tensor_copy(out=vbf[j][:], in_=vin[j][:]), r=["vin%d" % j], w=["vbf%d" % j])
            else:
                I("act", lambda e, j=j: e.copy(out=vbf[j][:], in_=vin[j][:]), r=["vin%d" % j], w=["vbf%d" % j])
            Dm(vs_d[e0:e0 + 512, :].rearrange("(c p) n -> p c n", p=128), vbf[j][:], r=["vbf%d" % j], w=["v_scr"])
            for c in range(4):
                p = ptc[tcn % 4]
                pk = "ptc%d" % (tcn % 4)
                tcn += 1
                for fc in range(8):
                    I("pe", lambda e, p=p, j=j, c=c, fc=fc: e.transpose(p[:, fc, :], ubf[j][:, c, fc * 128:(fc + 1) * 128], identb[:]),
                      r=["ubf%d" % j, "identb"], w=[pk])
                I("act", lambda e, p=p, j=j, c=c: e.copy(out=uts[j][:, :, c * 128:(c + 1) * 128], in_=p[:, :, :]),
                  r=[pk], w=["uts%d" % j])
            for hh in range(2):
                Dm(ut_d[2 * g + hh, :, :, :], uts[j][:, :, hh * 256:(hh + 1) * 256], r=["uts%d" % j], w=["ut_scr"])
        kb.barrier()

    if upto == "C":
        return nc
    with contextlib.ExitStack() as sp_:
        g2row = sb(sp_, "g2row", [128, D])
        gfrow = sb(sp_, "gfrow", [128, D])
        g2gate = sb(sp_, "g2gate", [128, D])
        wqb = sb(sp_, "wqb", [128, 8, 2048], BF16)
        keysb = sb(sp_, "keysb", [128, 16, 128], BF16)
        WT = sb(sp_, "WT", [128, TB, 128], BF16)
        NBUF = 4
        utb = [sb(sp_, "utb%d" % i, [128, 8, 256], BF16) for i in range(NBUF)]
        vtb = [sb(sp_, "vtb%d" % i, [128, 2, D], BF16) for i in range(NBUF)]
        x1s_ = sb(sp_, "x1s_", [128, D])
        u2T = [sb(sp_, "u2T%d" % i, [128, 8, TB], BF16) for i in range(2)]
        q2T = sb(sp_, "q2T", [128, 16, TB], BF16)
        sc_ = sb(sp_, "sc", [128, 16, 128])
        eq = sc_[:, :, :].rearrange("p (h a) (b c) -> p h (a b) c", a=2, c=16)
        cs = sb(sp_, "cs", [128, 8, 256])
        A16 = [sc_[:, 8 * i:8 * i + 4, :].rearrange("p a n -> p (a n)").bitcast(BF16).rearrange("p (t i) -> p t i", i=64) for i in range(2)]
        B16 = [cs[:, 4 * i:4 * i + 4, :].rearrange("p a n -> p (a n)").bitcast(BF16).rearrange("p (t i) -> p t i", i=128) for i in range(2)]
        tv = sb(sp_, "tv", [128, 16, 16])
        tix = sb(sp_, "tix", [128, 16, 16], U32)
        tif = sb(sp_, "tif", [128, 16, 16])
        sv = sb(sp_, "sv", [128, 8, 16])
        spx = sb(sp_, "spx", [128, 8, 16], U32)
        spi = sb(sp_, "spi", [128, 8, 16], U32)
        pf = sb(sp_, "pf", [128, 8, 16])
        qf = sb(sp_, "qf", [128, 8, 16])
        If_ = sb(sp_, "If", [128, 128])
        Jf_ = sb(sp_, "Jf", [128, 128])
        Wf_ = sb(sp_, "Wf", [128, 128])
        zs = sb(sp_, "zs", [128, 8])
        IT = sb(sp_, "IT", [128, 2, 128])
        JT = sb(sp_, "JT", [128, 2, 128])
        WTr = sb(sp_, "WTr", [128, 2, 128])
        NSL = 3
        gl = [sb(sp_, "gl%d" % i, [128, 2 * TB]) for i in range(NSL)]
        awt = [sb(sp_, "awt%d" % i, [128, 2 * TB], BF16) for i in range(NSL)]
        sq2 = sb(sp_, "sq2", [128, D])
        ss2 = sb(sp_, "ss2", [128, 1])
        ss3 = sb(sp_, "ss3", [128, 1])
        py = [ps(sp_, "py%d" % i, [128, 2, 512]) for i in range(2)]
        pg_ = [ps(sp_, "pg%d" % i, [128, 512]) for i in range(3)]
        pb_ = [ps(sp_, "pb%d" % i, [128, 512]) for i in range(1)]
        if dbg:
            print("phase P sbuf bytes remaining:", nc.sbuf_bytes_remaining)

        Dm(g2row[:], g2_d[0:1, :].partition_broadcast(128), w=["g2row"])
        Dm(gfrow[:], gf_d[0:1, :].partition_broadcast(128), w=["gfrow"])
        for kc in range(8):
            for hh in range(2):
                Dm(sq2[:], wq_d[kc * 128:(kc + 1) * 128, hh * 1024:(hh + 1) * 1024], w=["sq2"])
                I("dve", lambda e, kc=kc, hh=hh: e.tensor_copy(out=wqb[:, kc, hh * 1024:(hh + 1) * 1024], in_=sq2[:]), r=["sq2"], w=["wqb"])
        for hh in range(2):
            Dm(sq2[:], keysT_d[:, hh * 8:(hh + 1) * 8, :].rearrange("p a n -> p (a n)"), w=["sq2"])
            I("dve", lambda e, hh=hh: e.tensor_copy(out=keysb[:, hh * 8:(hh + 1) * 8, :].rearrange("p a n -> p (a n)"), in_=sq2[:]), r=["sq2"], w=["keysb"])

        NG = 64
        nblk = 2 * SEQ // TB
        if upto is not None and upto.startswith('P'):
            nblk = int(upto[1:])
        NBB = SEQ // TB

        def load_group(gi):
            j = gi % NBUF
            e0 = (gi % NG) * 256
            Dm(utb[j][:], ut_d[gi % NG, :, :, :], r=["ut_scr"], w=["utb%d" % j])
            Dm(vtb[j][:], vs_d[e0:e0 + 256, :].rearrange("(c p) n -> p c n", p=128), r=["v_scr"], w=["vtb%d" % j])

        total_groups = nblk * NG
        for g_ in range(min(NBUF, total_groups)):
            load_group(g_)
        pbc = [0]

        def nextpb():
            k = 0
            pbc[0] += 1
            return pb_[k], "pb%d" % k

        LOOK = 2
        PULL = 3
        SC = ["sc_%d" % i for i in range(16)]
        CS = ["cs_%d" % i for i in range(8)]
        TV = ["tv_%d" % i for i in range(16)]
        TIX = ["tix_%d" % i for i in range(16)]
        SV = ["sv_%d" % i for i in range(8)]
        SPX = ["spx_%d" % i for i in range(8)]
        SCH = [SC[0:4], SC[8:12]]
        CSH = [CS[0:4], CS[4:8]]

        def stageS(tb_):
            b = tb_ // NBB
            r0 = tb_ * TB
            u2 = u2T[tb_ % 2]
            uk = "u2T%d" % (tb_ % 2)
            for ti in range(2):
                Dm(x1s_[:], x1_d[r0 + ti * 128:r0 + (ti + 1) * 128, :], r=["x1s"], w=["x1s_"])
                I("act", lambda e: e.activation(out=sq2[:], in_=x1s_[:], func=AF.Square, accum_out=ss2[:]),
                  r=["x1s_"], w=["sq2", "ss2"])
                I("act", lambda e: e.activation(out=ss2[:], in_=ss2[:], func=AF.Sqrt, scale=1.0 / D, bias=epsb[:]),
                  r=["ss2", "epsb"], w=["ss2"])
                I("dve", lambda e: e.reciprocal(out=ss2[:], in_=ss2[:]), r=["ss2"], w=["ss2"])
                I("dve", lambda e: e.scalar_tensor_tensor(
                    out=sq2[:], in0=x1s_[:], scalar=ss2[:], in1=g2row[:], op0=ALU.mult, op1=ALU.mult),
                  r=["x1s_", "ss2", "g2row"], w=["sq2"])
                yield
                for hf in range(2):
                    p, pk = nextpb()
                    for f4 in range(4):
                        fc = hf * 4 + f4
                        I("pe", lambda e, p=p, fc=fc, f4=f4: e.transpose(p[:, f4 * 128:(f4 + 1) * 128], sq2[:, fc * 128:(fc + 1) * 128], ident[:]),
                          r=["sq2", "ident"], w=[pk])
                    yield
                    yield
                    for f4 in range(4):
                        fc = hf * 4 + f4
                        I("dve", lambda e, p=p, fc=fc, f4=f4, ti=ti, b=b: e.tensor_scalar(
                            out=u2[:, fc, ti * 128:(ti + 1) * 128], in0=p[:, f4 * 128:(f4 + 1) * 128],
                            scalar1=modp[:, 4, fc, b:b + 1], scalar2=modp[:, 3, fc, b:b + 1], op0=ALU.mult, op1=ALU.add),
                          r=[pk, "modp"], w=[uk])
                    yield
            for hp in range(16):
                p, pk = nextpb()
                for kc in range(8):
                    I("pe", lambda e, p=p, hp=hp, kc=kc: e.matmul(
                        p[:, 0:TB], lhsT=wqb[:, kc, hp * 128:(hp + 1) * 128], rhs=u2[:, kc, :], start=(kc == 0), stop=(kc == 7)),
                      r=["wqb", uk], w=[pk])
                yield
                yield
                I("act", lambda e, p=p, hp=hp: e.copy(out=q2T[:, hp, :], in_=p[:, 0:TB]), r=[pk], w=["q2T"])
                yield
            for ti in range(2):
                for qd_ in range(4):
                    p, pk = nextpb()
                    for hh in range(4):
                        hp = qd_ * 4 + hh
                        I("pe", lambda e, p=p, hp=hp, hh=hh, ti=ti: e.matmul(
                            p[:, hh * 128:(hh + 1) * 128], lhsT=q2T[:, hp, ti * 128:(ti + 1) * 128], rhs=keysb[:, hp, :],
                            start=True, stop=True), r=["q2T", "keysb"], w=[pk])
                    yield
                    yield
                    I("act", lambda e, p=p, qd_=qd_: e.copy(
                        out=sc_[:, qd_ * 4:(qd_ + 1) * 4, :], in_=p[:, :].rearrange("p (a n) -> p a n", n=128)),
                      r=[pk], w=SC[qd_ * 4:(qd_ + 1) * 4])
                    yield
                for hp in range(16):
                    I("dve", lambda e, hp=hp: e.max(out=tv[:, hp, 0:8], in_=sc_[:, hp, :]), r=["sc_%d" % hp], w=["tv_%d" % hp])
                    if hp % 4 == 3:
                        yield
                for hp in range(16):
                    I("dve", lambda e, hp=hp: e.max_index(out=tix[:, hp, 0:8], in_max=tv[:, hp, 0:8], in_values=sc_[:, hp, :]),
                      r=["sc_%d" % hp, "tv_%d" % hp], w=["tix_%d" % hp])
                    if hp % 4 == 3:
                        yield
                for hp in range(16):
                    I("dve", lambda e, hp=hp: e.match_replace(out=sc_[:, hp, :], in_to_replace=tv[:, hp, 0:8], in_values=sc_[:, hp, :], imm_value=NEG),
                      r=["tv_%d" % hp], w=["sc_%d" % hp])
                    if hp % 4 == 3:
                        yield
                for hp in range(16):
                    I("dve", lambda e, hp=hp: e.max(out=tv[:, hp, 8:16], in_=sc_[:, hp, :]), r=["sc_%d" % hp], w=["tv_%d" % hp])
                    if hp % 4 == 3:
                        yield
                for hp in range(16):
                    I("dve", lambda e, hp=hp: e.max_index(out=tix[:, hp, 8:16], in_max=tv[:, hp, 8:16], in_values=sc_[:, hp, :]),
                      r=["sc_%d" % hp, "tv_%d" % hp], w=["tix_%d" % hp])
                    if hp % 4 == 3:
                        yield
                I("dve", lambda e: e.tensor_copy(out=tif[:], in_=tix[:]), r=TIX, w=["tif"])
                tv4 = tv[:, :, :].rearrange("p (h s) k -> p h s k", s=2)
                tif4 = tif[:, :, :].rearrange("p (h s) k -> p h s k", s=2)
                I("dve", lambda e, tv4=tv4: e.tensor_tensor(
                    out=cs[:, :, :].rearrange("p h (a c) -> p h a c", c=16),
                    in0=tv4[:, :, 0, :].unsqueeze(3).to_broadcast([128, 8, 16, 16]),
                    in1=tv4[:, :, 1, :].unsqueeze(2).to_broadcast([128, 8, 16, 16]), op=ALU.add), r=TV, w=CS)
                yield
                for h in range(8):
                    I("dve", lambda e, h=h: e.max(out=sv[:, h, 0:8], in_=cs[:, h, :]), r=["cs_%d" % h], w=["sv_%d" % h])
                yield
                for h in range(8):
                    I("dve", lambda e, h=h: e.max_index(out=spx[:, h, 0:8], in_max=sv[:, h, 0:8], in_values=cs[:, h, :]),
                      r=["cs_%d" % h, "sv_%d" % h], w=["spx_%d" % h])
                yield
                for h in range(8):
                    I("dve", lambda e, h=h: e.match_replace(out=cs[:, h, :], in_to_replace=sv[:, h, 0:8], in_values=cs[:, h, :], imm_value=NEG),
                      r=["sv_%d" % h], w=["cs_%d" % h])
                yield
                for h in range(8):
                    I("dve", lambda e, h=h: e.max(out=sv[:, h, 8:16], in_=cs[:, h, :]), r=["cs_%d" % h], w=["sv_%d" % h])
                yield
                for h in range(8):
                    I("dve", lambda e, h=h: e.max_index(out=spx[:, h, 8:16], in_max=sv[:, h, 8:16], in_values=cs[:, h, :]),
                      r=["cs_%d" % h, "sv_%d" % h], w=["spx_%d" % h])
                yield
                I("dve", lambda e: e.tensor_single_scalar(out=spi[:], in_=spx[:], scalar=4, op=ALU.logical_shift_right), r=SPX, w=["spi"])
                I("dve", lambda e: e.tensor_copy(out=pf[:], in_=spi[:]), r=["spi"], w=["pf"])
                I("dve", lambda e: e.tensor_single_scalar(out=spi[:], in_=spx[:], scalar=15, op=ALU.bitwise_and), r=SPX + ["pf"], w=["spi"])
                I("dve", lambda e: e.tensor_copy(out=qf[:], in_=spi[:]), r=["spi"], w=["qf"])
                yield
                io16 = iota[:, 0:16].unsqueeze(1).unsqueeze(1).to_broadcast([128, 8, 16, 16])
                for (rf, side, dst, dk) in ((pf, 0, If_, "If"), (qf, 1, Jf_, "Jf")):
                    I("dve", lambda e, rf=rf: e.tensor_tensor(
                        out=eq, in0=rf[:, :, :].unsqueeze(3).to_broadcast([128, 8, 16, 16]), in1=io16, op=ALU.is_equal),
                      r=["pf", "qf", "iota"], w=SC)
                    yield
                    I("dve", lambda e, side=side, tif4=tif4: e.tensor_tensor(
                        out=eq, in0=eq, in1=tif4[:, :, side, :].unsqueeze(2).to_broadcast([128, 8, 16, 16]), op=ALU.mult),
                      r=SC + ["tif"], w=SC)
                    yield
                    I("dve", lambda e, dst=dst: e.tensor_reduce(
                        out=dst[:, :].rearrange("p (h k) -> p h k", k=16), in_=eq, axis=AX.X, op=ALU.add), r=SC, w=[dk])
                    yield
                I("dve", lambda e: e.tensor_tensor(out=sv[:], in0=sv[:], in1=sv[:, :, 0:1].to_broadcast([128, 8, 16]), op=ALU.subtract),
                  r=SV, w=SV)
                I("act", lambda e: e.activation(out=sv[:], in_=sv[:], func=AF.Exp), r=SV, w=SV)
                I("dve", lambda e: e.tensor_reduce(out=zs[:], in_=sv[:], axis=AX.X, op=ALU.add), r=SV, w=["zs"])
                I("dve", lambda e: e.reciprocal(out=zs[:], in_=zs[:]), r=["zs"], w=["zs"])
                I("dve", lambda e: e.tensor_tensor(
                    out=Wf_[:, :].rearrange("p (h k) -> p h k", k=16), in0=sv[:], in1=zs[:, :].unsqueeze(2).to_broadcast([128, 8, 16]),
                    op=ALU.mult), r=SV + ["zs"], w=["Wf"])
                yield
                for (src, sk, dst, dk) in ((If_, "If", IT, "IT"), (Jf_, "Jf", JT, "JT"), (Wf_, "Wf", WTr, "WTr")):
                    p, pk = nextpb()
                    yield
                    I("pe", lambda e, p=p, src=src: e.transpose(p[:, 0:128], src[:], ident[:]), r=[sk, "ident"], w=[pk])
                    yield
                    yield
                    I("act", lambda e, p=p, dst=dst, ti=ti: e.copy(out=dst[:, ti, :], in_=p[:, 0:128]), r=[pk], w=[dk])
                yield

        def stageW(tb_, half):
            iob = iota[:, :].unsqueeze(1).to_broadcast([128, 16, 128])
            ioh = iota[:, half * 64:(half + 1) * 64].unsqueeze(1).to_broadcast([128, 16, 64])
            wkey = "WT%d" % half

            def gen(bi):
                ti = bi // 8
                j2 = bi % 2
                t0 = (bi % 8) * 16
                I("dve", lambda e: e.tensor_tensor(
                    out=A16[j2], in0=ioh, in1=IT[:, ti, t0:t0 + 16].unsqueeze(2).to_broadcast([128, 16, 64]), op=ALU.is_equal),
                  r=["iota", "IT"], w=SCH[j2])
                I("dve", lambda e: e.tensor_tensor(
                    out=B16[j2], in0=iob, in1=JT[:, ti, t0:t0 + 16].unsqueeze(2).to_broadcast([128, 16, 128]), op=ALU.is_equal),
                  r=["iota", "JT"], w=CSH[j2])
                I("pool", lambda e: e.tensor_tensor(
                    out=A16[j2], in0=A16[j2], in1=WTr[:, ti, t0:t0 + 16].unsqueeze(2).to_broadcast([128, 16, 64]), op=ALU.mult),
                  r=SCH[j2] + ["WTr"], w=SCH[j2])

            def mm(bi, t8):
                ti = bi // 8
                j2 = bi % 2
                t0 = (bi % 8) * 16
                p, pk = nextpb()
                for tt in range(8):
                    tl = t8 * 8 + tt
                    I("pe", lambda e, tt=tt, tl=tl: e.matmul(
                        p[:, tt * 64:(tt + 1) * 64], lhsT=B16[j2][:, tl, :], rhs=A16[j2][:, tl, :], start=True, stop=True),
                      r=SCH[j2] + CSH[j2], w=[pk])
                return p, pk, ti * 128 + t0 + t8 * 8

            def cp(p, pk, tg0):
                I("act", lambda e: e.copy(
                    out=WT[:, tg0:tg0 + 8, half * 64:(half + 1) * 64], in_=p[:, :].rearrange("p (t i) -> p t i", i=64)),
                  r=[pk], w=[wkey])

            gen(0)
            yield
            for bi in range(16):
                if bi + 1 < 16:
                    gen(bi + 1)
                yield
                a = mm(bi, 0)
                yield
                yield
                cp(*a)
                yield
                a = mm(bi, 1)
                yield
                yield
                cp(*a)
                yield

        def gemm_and_epilogue(tb_, bg):
            b = tb_ // NBB
            u2 = u2T[tb_ % 2]
            uk = "u2T%d" % (tb_ % 2)
            slots = {}

            def pull(ic, n):
                for ent in bg:
                    if ent[0] is None:
                        continue
                    if ent[1] > ic:
                        return
                    while n > 0:
                        try:
                            next(ent[0])
                            n -= 1
                        except StopIteration:
                            ent[0] = None
                            break
                    if n == 0:
                        return

            def drain(pred):
                for ent in bg:
                    if ent[0] is not None and pred(ent):
                        for _ in ent[0]:
                            pass
                        ent[0] = None

            def emitU(pr):
                gi = tb_ * NG + pr
                j = gi % NBUF
                sl = pr % NSL
                p = pg_[sl]
                pk = "pgs%d" % sl
                slots[pr] = sl
                for c in range(2):
                    for kc in range(8):
                        I("pe", lambda e, p=p, j=j, c=c, kc=kc: e.matmul(
                            p[:, c * TB:(c + 1) * TB], lhsT=utb[j][:, kc, c * 128:(c + 1) * 128], rhs=u2[:, kc, :],
                            start=(kc == 0), stop=(kc == 7)), r=["utb%d" % j, uk], w=[pk])
                I("act", lambda e, p=p, sl=sl: e.activation(out=gl[sl][:], in_=p[:, :], func=AF.Gelu), r=[pk], w=["gl%d" % sl])
                I("pool", lambda e, sl=sl, pr=pr: e.tensor_tensor(
                    out=awt[sl][:, :].rearrange("p (c t) -> p c t", c=2), in0=gl[sl][:, :].rearrange("p (c t) -> p c t", c=2),
                    in1=WT[:, :, 2 * pr:2 * pr + 2].rearrange("p t i -> p i t"), op=ALU.mult),
                  r=["gl%d" % sl, "WT%d" % (pr // 32)], w=["awt%d" % sl])

            def emitV(pr):
                gi = tb_ * NG + pr
                j = gi % NBUF
                sl = slots.pop(pr)
                for c in range(2):
                    ic = 2 * pr + c
                    for ti in range(2):
                        for hf in range(2):
                            I("pe", lambda e, sl=sl, ti=ti, hf=hf, j=j, c=c, ic=ic: e.matmul(
                                py[ti][:, hf, :], lhsT=awt[sl][:, c * TB + ti * 128:c * TB + (ti + 1) * 128],
                                rhs=vtb[j][:, c, hf * 512:(hf + 1) * 512],
                                start=(ic == 0), stop=(ic == 127)), r=["awt%d" % sl, "vtb%d" % j], w=["py%d" % ti])
                if gi + NBUF < total_groups:
                    load_group(gi + NBUF)

            for pr in range(-LOOK, 64):
                if pr + LOOK < 64:
                    nu = pr + LOOK
                    if nu == 32:
                        drain(lambda ent: ent[2] <= 64)
                    emitU(nu)
                    if pr >= 0:
                        pull(2 * pr, PULL)
                if pr >= 0:
                    emitV(pr)
                    pull(2 * pr, PULL)
            drain(lambda ent: True)
            for ti in range(2):
                r0 = tb_ * TB + ti * 128
                Dm(x1s_[:], x1_d[r0:r0 + 128, :], r=["x1s"], w=["x1s_"])
                I("dve", lambda e, ti=ti: e.tensor_tensor(
                    out=sq2[:], in0=py[ti][:, :, :].rearrange("p a n -> p (a n)"), in1=g2gate[:], op=ALU.mult),
                  r=["py%d" % ti, "g2gate"], w=["sq2"])
                I("pool", lambda e: e.tensor_tensor(out=sq2[:], in0=sq2[:], in1=x1s_[:], op=ALU.add), r=["sq2", "x1s_"], w=["sq2"])
                I("act", lambda e: e.activation(out=x1s_[:], in_=sq2[:], func=AF.Square, accum_out=ss3[:]), r=["sq2"], w=["x1s_", "ss3"])
                I("act", lambda e: e.activation(out=ss3[:], in_=ss3[:], func=AF.Sqrt, scale=1.0 / D, bias=epsb[:]),
                  r=["ss3", "epsb"], w=["ss3"])
                I("dve", lambda e: e.reciprocal(out=ss3[:], in_=ss3[:]), r=["ss3"], w=["ss3"])
                I("dve", lambda e: e.scalar_tensor_tensor(
                    out=x1s_[:], in0=sq2[:], scalar=ss3[:], in1=gfrow[:], op0=ALU.mult, op1=ALU.mult),
                  r=["sq2", "ss3", "gfrow"], w=["x1s_"])
                l0 = (tb_ % NBB) * TB + ti * 128
                Dm(out_d[b, l0:l0 + 128, :], x1s_[:], r=["x1s_"], w=["out"])

        Dm(g2gate[:], grow_d[2:3, :].partition_broadcast(128), r=["grow_scr"], w=["g2gate"])
        for _ in stageS(0):
            pass
        if upto == "S0":
            kb.barrier()
            return nc
        for _ in stageW(0, 0):
            pass
        if upto == "S1":
            kb.barrier()
            return nc
        for tb_ in range(nblk):
            bg = [[stageW(tb_, 1), 0, 64]]
            if tb_ + 1 < nblk:
                bg.append([stageS(tb_ + 1), 0, 999])
                bg.append([stageW(tb_ + 1, 0), 64, 999])
            gemm_and_epilogue(tb_, bg)
            if tb_ + 1 < nblk and (tb_ + 1) % NBB == 0:
                Dm(g2gate[:], grow_d[2 + (tb_ + 1) // NBB:3 + (tb_ + 1) // NBB, :].partition_broadcast(128), r=["grow_scr"], w=["g2gate"])
        kb.barrier(engines=("sp",))
    return nc


def _pos_table():
    rows = SEQ // 64
    r, col = np.meshgrid(np.arange(rows, dtype=np.float32), np.arange(64, dtype=np.float32), indexing="ij")
    quarter = D // 4
    omega = (np.float32(10000.0) ** (-np.arange(quarter, dtype=np.float32) / np.float32(quarter))).astype(np.float32)

    def emb(p):
        ang = p.reshape(-1)[:, None].astype(np.float32) * omega[None, :]
        return np.concatenate([np.sin(ang), np.cos(ang)], axis=-1)

    return np.concatenate([emb(r), emb(col)], axis=-1).astype(np.float32)


_NC_CACHE = {}


def _fm(v, nchunk):
    return np.ascontiguousarray(np.asarray(v, np.float32).reshape(nchunk, 128).T)


def kernel(x, c, ctx, c_ctx, ada_w, ada_b, norm1_g, w_in, conv_w, conv_b, rg_w_a, rg_b_a, rg_w_x, rg_b_x, rg_lambda,
           gla_w_g, gla_b_g, gla_norm_g, w_out, norm2_g, peer_w_q, peer_keys, peer_u, peer_v, final_norm_g):
    if "nc" not in _NC_CACHE:
        _NC_CACHE["nc"] = build()
    nc = _NC_CACHE["nc"]
    in_maps = make_in_maps(x, c, ctx, c_ctx, ada_w, ada_b, norm1_g, w_in, conv_w, conv_b, rg_w_a, rg_b_a, rg_w_x, rg_b_x, rg_lambda,
                           gla_w_g, gla_b_g, gla_norm_g, w_out, norm2_g, peer_w_q, peer_keys, peer_u, peer_v, final_norm_g)
    res = run_bass_kernel_spmd(nc, in_maps, core_ids=list(range(NCORES)))
    out = np.concatenate([np.asarray(r["out"], dtype=np.float32) for r in res.results], axis=0)
    return out


def make_in_maps(x, c, ctx, c_ctx, ada_w, ada_b, norm1_g, w_in, conv_w, conv_b, rg_w_a, rg_b_a, rg_w_x, rg_b_x, rg_lambda,
                 gla_w_g, gla_b_g, gla_norm_g, w_out, norm2_g, peer_w_q, peer_keys, peer_u, peer_v, final_norm_g):
    f = lambda a: np.ascontiguousarray(np.asarray(a, dtype=np.float32))
    x, c, ctx, c_ctx = f(x), f(c), f(ctx), f(c_ctx)
    jj, ii = np.meshgrid(np.arange(128), np.arange(128), indexing="ij")
    mf = (jj <= ii).astype(np.float32)
    mb = (jj >= ii).astype(np.float32)
    shared = {
        "ada_w": f(ada_w[0]),
        "ada_b": f(ada_b[0]).reshape(1, -1),
        "ada_bT": _fm(ada_b[0], 48),
        "norm1_g": f(norm1_g[0]).reshape(1, -1),
        "norm2_g": f(norm2_g[0]).reshape(1, -1),
        "final_g": f(final_norm_g).reshape(1, -1),
        "w_in": f(w_in[0]),
        "convwT": np.ascontiguousarray(f(conv_w[0]).reshape(4, 4, 128).transpose(2, 1, 0)),
        "convbT": _fm(conv_b[0], 4),
        "rg_w_a": f(rg_w_a[0]),
        "rg_w_x": f(rg_w_x[0]),
        "rgbaT": np.ascontiguousarray(f(rg_b_a[0]).reshape(2, 4, 128).transpose(2, 0, 1)),
        "rgbxT": np.ascontiguousarray(f(rg_b_x[0]).reshape(2, 4, 128).transpose(2, 0, 1)),
        "rglamT": np.ascontiguousarray(f(rg_lambda[0]).reshape(2, 4, 128).transpose(2, 0, 1)),
        "gla_w_g": f(gla_w_g[0]),
        "glabgT": np.ascontiguousarray(f(gla_b_g[0]).reshape(2, 2, 128).transpose(2, 0, 1)),
        "glangT": f(gla_norm_g[0]).reshape(128, 1),
        "w_out": f(w_out[0]),
        "peer_w_q": f(peer_w_q[0]),
        "keysT": np.ascontiguousarray(f(peer_keys[0]).reshape(16, 128, 128).transpose(2, 0, 1)),
        "peer_u": f(peer_u[0]),
        "peer_v": f(peer_v[0]),
        "pos": _pos_table(),
        "ident": np.eye(128, dtype=np.float32),
        "iota": np.tile(np.arange(128, dtype=np.float32)[None, :], (128, 1)),
        "maskf": np.ascontiguousarray(np.concatenate([mf, mf], axis=1)),
        "maskb": np.ascontiguousarray(np.concatenate([mb, mb], axis=1)),
    }
    in_maps = []
    for i in range(NCORES):
        b0 = 2 * i
        cm = np.stack([c[b0], c[b0 + 1], c_ctx], axis=0)
        cT = np.ascontiguousarray(cm.reshape(3, 8, 128).transpose(2, 1, 0))
        m = dict(shared)
        m["x"] = np.ascontiguousarray(x[b0:b0 + 2])
        m["ctx"] = np.ascontiguousarray(ctx[b0:b0 + 2])
        m["cT"] = cT
        in_maps.append(m)
    return in_maps
```

```python
import contextlib
import numpy as np
import concourse.bass as bass
import concourse.mybir as mybir
from concourse.bass_utils import run_bass_kernel_spmd

F32 = mybir.dt.float32
BF16 = mybir.dt.bfloat16
U32 = mybir.dt.uint32
ALU = mybir.AluOpType
AF = mybir.ActivationFunctionType
AX = mybir.AxisListType

NCORES = 8
D = 1024
SEQ = 2048
CTX = 256
S = CTX + SEQ
NT = S // 128
INC = 2592
NEXP = 16384
TB = 256
EPS = 1e-6
NEG = -1.0e30


class KB:
    def __init__(self):
        self.nc = bass.Bass("TRN2", target_bir_lowering=False)
        nc = self.nc
        self.eng = {"pe": nc.tensor, "act": nc.scalar, "dve": nc.vector, "pool": nc.gpsimd, "sp": nc.sync}
        self.stack = contextlib.ExitStack()
        self.sem = {}
        self.cnt = {}
        for n in self.eng:
            self.sem[("e", n)] = self.stack.enter_context(nc.semaphore("s_" + n))
            self.cnt[n] = 0
        self.nd = 24
        self.dcnt = [0] * self.nd
        for i in range(self.nd):
            self.sem[("d", i)] = self.stack.enter_context(nc.semaphore("d%d" % i))
        self.dnext = 0
        self.waited = {}
        self.lastw = {}
        self.readers = {}

    def _wait(self, en, deps):
        for sk, v in deps.items():
            if en == "pe" and sk == ("e", "pe"):
                continue
            if self.waited.get((en, sk), 0) >= v:
                continue
            self.eng[en].wait_ge(self.sem[sk], v)
            self.waited[(en, sk)] = v

    def _deps(self, r, w):
        d = {}
        for k in r:
            t = self.lastw.get(k)
            if t:
                d[t[0]] = max(d.get(t[0], 0), t[1])
        for k in w:
            t = self.lastw.get(k)
            if t:
                d[t[0]] = max(d.get(t[0], 0), t[1])
            for sk, v in self.readers.get(k, {}).items():
                d[sk] = max(d.get(sk, 0), v)
        return d

    def _commit(self, tok, r, w):
        for k in w:
            self.lastw[k] = tok
            self.readers[k] = {}
        for k in r:
            rd = self.readers.setdefault(k, {})
            rd[tok[0]] = max(rd.get(tok[0], 0), tok[1])

    def I(self, en, fn, r=(), w=()):
        self._wait(en, self._deps(r, w))
        inst = fn(self.eng[en])
        self.cnt[en] += 1
        inst.then_inc(self.sem[("e", en)], 1)
        self._commit((("e", en), self.cnt[en]), r, w)

    def D(self, out, in_, r=(), w=(), q="sp"):
        i = self.dnext
        self.dnext = (i + 1) % self.nd
        deps = self._deps(r, w)
        if self.dcnt[i] > 0:
            deps[("d", i)] = max(deps.get(("d", i), 0), self.dcnt[i])
        self._wait(q, deps)
        inst = self.eng[q].dma_start(out=out, in_=in_)
        self.dcnt[i] += 16
        inst.then_inc(self.sem[("d", i)], 16)
        self._commit((("d", i), self.dcnt[i]), r, w)

    def barrier(self, engines=("pe", "act", "dve", "pool", "sp")):
        deps = {}
        for n in self.eng:
            if self.cnt[n] > 0:
                deps[("e", n)] = self.cnt[n]
        for i in range(self.nd):
            if self.dcnt[i] > 0:
                deps[("d", i)] = self.dcnt[i]
        for en in engines:
            d = dict(deps)
            d.pop(("e", en), None)
            self._wait(en, d)
        self.lastw = {}
        self.readers = {}


def build(upto=None, dbg=False):
    kb = KB()
    nc = kb.nc
    I, Dm = kb.I, kb.D

    def dram(name, shape, dt=F32, kind="ExternalInput"):
        return nc.dram_tensor(name, list(shape), dt, kind=kind).ap()

    x_d = dram("x", [2, SEQ, D])
    ctx_d = dram("ctx", [2, CTX, D])
    cT_d = dram("cT", [128, 8, 3])
    adaw_d = dram("ada_w", [D, 6 * D])
    adab_d = dram("ada_b", [1, 6 * D])
    adabT_d = dram("ada_bT", [128, 48])
    g1_d = dram("norm1_g", [1, D])
    g2_d = dram("norm2_g", [1, D])
    gf_d = dram("final_g", [1, D])
    win_d = dram("w_in", [D, INC])
    convw_d = dram("convwT", [128, 4, 4])
    convb_d = dram("convbT", [128, 4])
    rgwa_d = dram("rg_w_a", [2, 8, 64, 64])
    rgwx_d = dram("rg_w_x", [2, 8, 64, 64])
    rgba_d = dram("rgbaT", [128, 2, 4])
    rgbx_d = dram("rgbxT", [128, 2, 4])
    rglam_d = dram("rglamT", [128, 2, 4])
    glawg_d = dram("gla_w_g", [2, 16, 256])
    glabg_d = dram("glabgT", [128, 2, 2])
    glang_d = dram("glangT", [128, 1])
    wout_d = dram("w_out", [D, D])
    wq_d = dram("peer_w_q", [D, 2048])
    keysT_d = dram("keysT", [128, 16, 128])
    pu_d = dram("peer_u", [NEXP, D])
    pv_d = dram("peer_v", [NEXP, D])
    pos_d = dram("pos", [SEQ, D])
    ident_d = dram("ident", [128, 128])
    iota_d = dram("iota", [128, 128])
    maskf_d = dram("maskf", [128, 256])
    maskb_d = dram("maskb", [128, 256])
    out_d = dram("out", [2, SEQ, D], kind="ExternalOutput")
    x1_d = dram("x1s", [2 * SEQ, D], kind="ExternalOutput" if dbg else "Internal")
    grow_d = dram("grow_scr", [4, D], kind="Internal")
    ut_d = dram("ut_scr", [NEXP // 256, 128, 8, 256], BF16, kind="Internal")
    vs_d = dram("v_scr", [NEXP, D], BF16, kind="Internal")

    es = contextlib.ExitStack()

    uid = [0]

    def sb(stack, name, shape, dt=F32):
        uid[0] += 1
        return stack.enter_context(nc.sbuf_tensor("sb%d_%s" % (uid[0], name), list(shape), dt))

    def ps(stack, name, shape, dt=F32):
        uid[0] += 1
        return stack.enter_context(nc.psum_tensor("ps%d_%s" % (uid[0], name), list(shape), dt))

    ident = sb(es, "ident", [128, 128])
    identb = sb(es, "identb", [128, 128], BF16)
    iota = sb(es, "iota", [128, 128])
    ones = sb(es, "ones", [128, 128])
    modp = sb(es, "modp", [128, 6, 8, 3])
    epsb = sb(es, "epsb", [128, 1])

    Dm(ident[:], ident_d[:, :], w=["ident"])
    Dm(iota[:], iota_d[:, :], w=["iota"])
    I("dve", lambda e: e.tensor_copy(out=identb[:], in_=ident[:]), r=["ident"], w=["identb"])
    I("dve", lambda e: e.memset(ones[:], 1.0), w=["ones"])
    I("dve", lambda e: e.memset(epsb[:], EPS), w=["epsb"])

    with contextlib.ExitStack() as st:
        cT = sb(st, "cT", [128, 8, 3])
        scT = sb(st, "scT", [128, 8, 3])
        rep = sb(st, "rep", [128, 2, 8, 128])
        abT = sb(st, "abT", [128, 48])
        abrow = sb(st, "abrow", [128, D])
        growt = [sb(st, "growt%d" % i, [128, D]) for i in range(2)]
        aw = [sb(st, "aw%d" % i, [128, 8, D]) for i in range(2)]
        pm = [ps(st, "pm%d" % i, [128, 512]) for i in range(4)]
        Dm(cT[:], cT_d[:, :, :], w=["cT"])
        Dm(abT[:], adabT_d[:, :], w=["abT"])
        I("act", lambda e: e.activation(out=scT[:], in_=cT[:], func=AF.Silu), r=["cT"], w=["scT"])
        for b in range(2):
            I("dve", lambda e, b=b: e.tensor_copy(out=rep[:, b], in_=scT[:, :, b:b + 1].to_broadcast([128, 8, 128])),
              r=["scT"], w=["rep"])
        pi = 0
        for m in range(6):
            a = aw[m % 2]
            ak = "aw%d" % (m % 2)
            Dm(a[:], adaw_d[:, m * D:(m + 1) * D].rearrange("(kc p) n -> p kc n", p=128), w=[ak])
            if m in (0, 1, 3, 4):
                p = pm[pi % 4]
                pk = "pm%d" % (pi % 4)
                pi += 1
                for fc in range(8):
                    for kc in range(8):
                        I("pe", lambda e, fc=fc, kc=kc, p=p, a=a: e.matmul(
                            p[:, fc * 3:fc * 3 + 3], lhsT=a[:, kc, fc * 128:(fc + 1) * 128], rhs=scT[:, kc, :],
                            start=(kc == 0), stop=(kc == 7)), r=[ak, "scT"], w=[pk])
                I("dve", lambda e, m=m, p=p: e.tensor_tensor(
                    out=modp[:, m], in0=p[:, 0:24].rearrange("p (f c) -> p f c", c=3),
                    in1=abT[:, m * 8:(m + 1) * 8].unsqueeze(2).to_broadcast([128, 8, 3]), op=ALU.add),
                  r=[pk, "abT"], w=["modp"])
                if m in (1, 4):
                    I("dve", lambda e, m=m: e.tensor_scalar_add(out=modp[:, m], in0=modp[:, m], scalar1=1.0),
                      r=["modp"], w=["modp"])
            else:
                mi = 0 if m == 2 else 1
                Dm(abrow[:], adab_d[0:1, m * D:(m + 1) * D].partition_broadcast(128), w=["abrow"])
                for b in range(2):
                    for hf in range(2):
                        p = pm[pi % 4]
                        pk = "pm%d" % (pi % 4)
                        pi += 1
                        for kc in range(8):
                            I("pe", lambda e, kc=kc, p=p, a=a, b=b, hf=hf: e.matmul(
                                p[:, :], lhsT=rep[:, b, kc, :], rhs=a[:, kc, hf * 512:(hf + 1) * 512],
                                start=(kc == 0), stop=(kc == 7)), r=[ak, "rep"], w=[pk])
                        I("dve", lambda e, p=p, b=b, hf=hf, mi=mi: e.tensor_tensor(
                            out=growt[b][:, hf * 512:(hf + 1) * 512], in0=p[:, :],
                            in1=abrow[:, hf * 512:(hf + 1) * 512], op=ALU.add),
                          r=[pk, "abrow"], w=["growt%d" % b])
                    Dm(grow_d[mi * 2 + b:mi * 2 + b + 1, :], growt[b][0:1, :], r=["growt%d" % b], w=["grow_scr"])
        kb.barrier()
    if upto == "M0":
        return nc

    with contextlib.ExitStack() as sm:
        g1row = sb(sm, "g1row", [128, D])
        g1gate = sb(sm, "g1gate", [128, 2, D])
        for b in range(2):
            Dm(g1gate[:, b, :], grow_d[b:b + 1, :].partition_broadcast(128), r=["grow_scr"], w=["g1gate"])
        maskf = sb(sm, "maskf", [128, 256], BF16)
        maskb = sb(sm, "maskb", [128, 256], BF16)
        mstage = sb(sm, "mstage", [128, 256])
        cw = sb(sm, "cw", [128, 4, 4])
        cb = sb(sm, "cb", [128, 4])
        rba = sb(sm, "rba", [128, 2, 4])
        rbx = sb(sm, "rbx", [128, 2, 4])
        rlam = sb(sm, "rlam", [128, 2, 4])
        coef = sb(sm, "coef", [128, 2, 4])
        gbg = sb(sm, "gbg", [128, 2, 2])
        ngbg = sb(sm, "ngbg", [128, 2, 2])
        gng = sb(sm, "gng", [128, 1])
        wg = sb(sm, "wg", [32, 2, 256])
        wbd = sb(sm, "wbd", [128, 2, 2, 4, 128])
        hm = sb(sm, "hm", [128, 2])
        bdm = sb(sm, "bdm", [128, 256])
        I("dve", lambda e: e.memset(hm[:], 0.0), w=["hm"])
        I("dve", lambda e: e.memset(hm[0:64, 0:1], 1.0), w=["hm"])
        I("dve", lambda e: e.memset(hm[64:128, 1:2], 1.0), w=["hm"])
        I("dve", lambda e: e.memset(bdm[:], 0.0), w=["bdm"])
        I("dve", lambda e: e.memset(bdm[0:64, 0:128], 1.0), w=["bdm"])
        I("dve", lambda e: e.memset(bdm[64:128, 128:256], 1.0), w=["bdm"])
        Dm(g1row[:], g1_d[0:1, :].partition_broadcast(128), w=["g1row"])
        Dm(mstage[:], maskf_d[:, :], w=["mstage"])
        I("dve", lambda e: e.tensor_copy(out=maskf[:], in_=mstage[:]), r=["mstage"], w=["maskf"])
        Dm(mstage[:], maskb_d[:, :], w=["mstage"])
        I("dve", lambda e: e.tensor_copy(out=maskb[:], in_=mstage[:]), r=["mstage"], w=["maskb"])
        Dm(cw[:], convw_d[:, :, :], w=["cw"])
        Dm(cb[:], convb_d[:, :], w=["cb"])
        Dm(rba[:], rgba_d[:, :, :], w=["rba"])
        Dm(rbx[:], rgbx_d[:, :, :], w=["rbx"])
        Dm(rlam[:], rglam_d[:, :, :], w=["rlam"])
        Dm(gbg[:], glabg_d[:, :, :], w=["gbg"])
        Dm(gng[:], glang_d[:, :], w=["gng"])
        I("act", lambda e: e.activation(out=coef[:], in_=rlam[:], func=AF.Exp, scale=-1.0), r=["rlam"], w=["coef"])
        I("act", lambda e: e.activation(out=coef[:], in_=coef[:], func=AF.Ln, bias=1.0), r=["coef"], w=["coef"])
        I("dve", lambda e: e.tensor_scalar_mul(out=coef[:], in0=coef[:], scalar1=-8.0), r=["coef"], w=["coef"])
        I("dve", lambda e: e.tensor_scalar_mul(out=ngbg[:], in0=gbg[:], scalar1=-1.0), r=["gbg"], w=["ngbg"])
        I("dve", lambda e: e.memset(wg[:], 0.0), w=["wg"])
        for d in range(2):
            Dm(wg[16 * d:16 * d + 16, d, :], glawg_d[d, :, :], w=["wg"])
        I("dve", lambda e: e.memset(wbd[:], 0.0), w=["wbd"])
        for gi, src in enumerate((rgwa_d, rgwx_d)):
            for d in range(2):
                for cc in range(4):
                    for h in range(2):
                        Dm(wbd[64 * h:64 * h + 64, gi, d, cc, 64 * h:64 * h + 64], src[d, 2 * cc + h, :, :], w=["wbd"])

        for b in range(2):
            with contextlib.ExitStack() as sbt:
                mgT = sb(sbt, "mgT", [128, 8, SEQ], BF16)
                qT = sb(sbt, "qT", [128, 2, SEQ], BF16)
                kT = sb(sbt, "kT", [128, 2, S], BF16)
                vtok = sb(sbt, "vtok", [128, NT, 512], BF16)
                lrT = sb(sbt, "lrT", [32, S])
                sR = contextlib.ExitStack()
                rxT = sb(sR, "rxT", [128, 4, S], BF16)
                with contextlib.ExitStack() as s1:
                    winb = sb(s1, "winb", [128, 8, INC], BF16)
                    wst0 = sb(s1, "wst0", [128, INC // 2])
                    wst = [wst0, wst0]
                    uT = [sb(s1, "uT%d" % i, [128, 8, 512], BF16) for i in range(2)]
                    xt = [sb(s1, "xt%d" % i, [128, D]) for i in range(2)]
                    pt0 = sb(s1, "pt0", [128, D])
                    pt = [pt0, pt0]
                    xn = [sb(s1, "xn%d" % i, [128, D], BF16) for i in range(2)]
                    sq = sb(s1, "sqj", [128, D], BF16)
                    ss = [sb(s1, "ss%d" % i, [128, 1]) for i in range(2)]
                    tp = [ps(s1, "tp%d" % i, [128, 8, 128], BF16) for i in range(2)]
                    pj = [ps(s1, "pj%d" % i, [128, 512]) for i in range(4)]
                    hw = INC // 2
                    for kc in range(8):
                        for hh in range(2):
                            w_ = wst[hh]
                            Dm(w_[:], win_d[kc * 128:(kc + 1) * 128, hh * hw:(hh + 1) * hw], w=["wst0"])
                            I("pool" if hh else "dve", lambda e, w_=w_, kc=kc, hh=hh: e.tensor_copy(
                                out=winb[:, kc, hh * hw:(hh + 1) * hw], in_=w_[:]), r=["wst0"], w=["winb"])
                    ngroups = 5
                    pjc = 0
                    for g in range(ngroups):
                        t0 = g * 4
                        nt = min(4, NT - t0)
                        ntok = nt * 128
                        u = uT[g % 2]
                        uk = "uT%d" % (g % 2)
                        for ti in range(nt):
                            t = t0 + ti
                            pb = t % 2
                            xk, xnk, ssk, tpk, ptk = "xt%d" % pb, "xn%d" % pb, "ss%d" % pb, "tp%d" % pb, "pt0"
                            if t < 2:
                                Dm(xt[pb][:], ctx_d[b, t * 128:(t + 1) * 128, :], w=[xk])
                                col = 2
                            else:
                                l0 = (t - 2) * 128
                                Dm(xt[pb][:], x_d[b, l0:l0 + 128, :], w=[xk])
                                Dm(pt[pb][:], pos_d[l0:l0 + 128, :], w=[ptk])
                                I("pool", lambda e, pb=pb: e.tensor_tensor(out=xt[pb][:], in0=xt[pb][:], in1=pt[pb][:], op=ALU.add),
                                  r=[xk, ptk], w=[xk])
                                col = b
                            I("act", lambda e, pb=pb: e.activation(out=sq[:], in_=xt[pb][:], func=AF.Square, accum_out=ss[pb][:]),
                              r=[xk], w=["sqj", ssk])
                            I("act", lambda e, pb=pb: e.activation(out=ss[pb][:], in_=ss[pb][:], func=AF.Sqrt, scale=1.0 / D, bias=epsb[:]),
                              r=[ssk, "epsb"], w=[ssk])
                            I("dve", lambda e, pb=pb: e.reciprocal(out=ss[pb][:], in_=ss[pb][:]), r=[ssk], w=[ssk])
                            I("dve", lambda e, pb=pb: e.scalar_tensor_tensor(
                                out=xn[pb][:], in0=xt[pb][:], scalar=ss[pb][:], in1=g1row[:], op0=ALU.mult, op1=ALU.mult),
                              r=[xk, ssk, "g1row"], w=[xnk])
                            for fc in range(8):
                                I("pe", lambda e, pb=pb, fc=fc: e.transpose(tp[pb][:, fc, :], xn[pb][:, fc * 128:(fc + 1) * 128], identb[:]),
                                  r=[xnk, "identb"], w=[tpk])
                            for fc in range(8):
                                I("dve" if fc % 2 else "pool" if False else "dve", lambda e, pb=pb, fc=fc, ti=ti, u=u, col=col: e.tensor_scalar(
                                    out=u[:, fc, ti * 128:(ti + 1) * 128], in0=tp[pb][:, fc, :],
                                    scalar1=modp[:, 1, fc, col:col + 1], scalar2=modp[:, 0, fc, col:col + 1],
                                    op0=ALU.mult, op1=ALU.add), r=[tpk, "modp"], w=[uk])
                        c0 = t0 * 128
                        lat0 = max(c0, CTX)
                        for cch in range(21):
                            cs_ = cch * 128
                            ncol = 128 if cch < 20 else 32
                            p = pj[pjc % 4]
                            pk = "pj%d" % (pjc % 4)
                            pjc += 1
                            if 12 <= cch < 16:
                                continue
                            lat_only = (4 <= cch < 10) or (16 <= cch < 20)
                            if lat_only and lat0 >= c0 + ntok:
                                continue
                            for kc in range(8):
                                I("pe", lambda e, kc=kc, p=p, u=u, cs_=cs_, ncol=ncol, ntok=ntok: e.matmul(
                                    p[0:ncol, 0:ntok], lhsT=winb[:, kc, cs_:cs_ + ncol], rhs=u[:, kc, 0:ntok],
                                    start=(kc == 0), stop=(kc == 7)), r=["winb", uk], w=[pk])
                            o0 = lat0 - c0
                            if cch < 4:
                                I("act", lambda e, p=p, cch=cch, c0=c0, ntok=ntok: e.copy(out=rxT[:, cch, c0:c0 + ntok], in_=p[:, 0:ntok]),
                                  r=[pk], w=["rxT"])
                            elif cch < 8:
                                I("act", lambda e, p=p, cch=cch, o0=o0, lat0=lat0, ntok=ntok: e.activation(
                                    out=mgT[:, cch - 4, lat0 - CTX:lat0 - CTX + ntok - o0], in_=p[:, o0:ntok], func=AF.Gelu),
                                  r=[pk], w=["mgT"])
                            elif cch < 10:
                                I("dve", lambda e, p=p, cch=cch, o0=o0, lat0=lat0, ntok=ntok: e.tensor_scalar_mul(
                                    out=qT[:, cch - 8, lat0 - CTX:lat0 - CTX + ntok - o0], in0=p[:, o0:ntok], scalar1=0.125),
                                  r=[pk], w=["qT"])
                            elif cch < 12:
                                I("dve", lambda e, p=p, cch=cch, c0=c0, ntok=ntok: e.tensor_copy(out=kT[:, cch - 10, c0:c0 + ntok], in_=p[:, 0:ntok]),
                                  r=[pk], w=["kT"])
                            elif cch < 20:
                                I("act", lambda e, p=p, cch=cch, o0=o0, lat0=lat0, ntok=ntok: e.activation(
                                    out=mgT[:, 4 + cch - 16, lat0 - CTX:lat0 - CTX + ntok - o0], in_=p[:, o0:ntok], func=AF.Silu),
                                  r=[pk], w=["mgT"])
                            else:
                                I("dve", lambda e, p=p, c0=c0, ntok=ntok: e.tensor_copy(out=lrT[:, c0:c0 + ntok], in_=p[0:32, 0:ntok]),
                                  r=[pk], w=["lrT"])
                        for ti in range(nt):
                            p = pj[pjc % 4]
                            pk = "pj%d" % (pjc % 4)
                            pjc += 1
                            for kc in range(8):
                                I("pe", lambda e, kc=kc, p=p, u=u, ti=ti: e.matmul(
                                    p[:, :], lhsT=u[:, kc, ti * 128:(ti + 1) * 128], rhs=winb[:, kc, 1536:2048],
                                    start=(kc == 0), stop=(kc == 7)), r=["winb", uk], w=[pk])
                            I("act", lambda e, p=p, t=t0 + ti: e.copy(out=vtok[:, t, :], in_=p[:, :]), r=[pk], w=["vtok"])
                    kb.barrier()

                if upto == "M1a":
                    sR.close()
                    return nc
                with contextlib.ExitStack() as s2:
                    xc = sb(s2, "xc", [128, S])
                    gr = sb(s2, "gr", [128, S])
                    gi_ = sb(s2, "gi", [128, S])
                    aa = sb(s2, "aa", [128, S])
                    bb = sb(s2, "bb", [128, S])
                    hf_ = sb(s2, "hf", [128, S])
                    hb_ = sb(s2, "hb", [128, S])
                    pg = [ps(s2, "pg%d" % i, [128, 512]) for i in range(4)]
                    pgc = 0
                    segs = ((0, CTX), (CTX, S))
                    for cc in range(4):
                        I("dve", lambda e, cc=cc: e.tensor_scalar(
                            out=xc[:], in0=rxT[:, cc, :], scalar1=cw[:, cc, 2:3], scalar2=cb[:, cc:cc + 1],
                            op0=ALU.mult, op1=ALU.add), r=["rxT", "cw", "cb"], w=["xc"])
                        for (a0, a1) in segs:
                            for j, sh in ((0, -2), (1, -1), (3, 1)):
                                lo = max(a0, a0 - sh)
                                hi = min(a1, a1 - sh)
                                I("dve", lambda e, cc=cc, j=j, sh=sh, lo=lo, hi=hi: e.scalar_tensor_tensor(
                                    out=xc[:, lo:hi], in0=rxT[:, cc, lo + sh:hi + sh], scalar=cw[:, cc, j:j + 1],
                                    in1=xc[:, lo:hi], op0=ALU.mult, op1=ALU.add), r=["rxT", "cw", "xc"], w=["xc"])
                        for d in range(2):
                            for gi, (gt, gk, bias_t) in enumerate(((gr, "gr", rba), (gi_, "gi", rbx))):
                                for g in range(5):
                                    c0 = g * 512
                                    n = min(512, S - c0)
                                    p = pg[pgc % 4]
                                    pk = "pg%d" % (pgc % 4)
                                    pgc += 1
                                    I("pe", lambda e, p=p, gi=gi, d=d, cc=cc, c0=c0, n=n: e.matmul(
                                        p[:, 0:n], lhsT=wbd[:, gi, d, cc, :], rhs=xc[:, c0:c0 + n], start=True, stop=True),
                                      r=["wbd", "xc"], w=[pk])
                                    I("act", lambda e, p=p, gt=gt, bias_t=bias_t, d=d, cc=cc, c0=c0, n=n: e.activation(
                                        out=gt[:, c0:c0 + n], in_=p[:, 0:n], func=AF.Sigmoid, bias=bias_t[:, d, cc:cc + 1]),
                                      r=[pk], w=[gk])
                            I("act", lambda e, d=d, cc=cc: e.activation(out=aa[:], in_=gr[:], func=AF.Exp, scale=coef[:, d, cc:cc + 1]),
                              r=["gr", "coef"], w=["aa"])
                            I("pool", lambda e: e.tensor_tensor(out=bb[:], in0=aa[:], in1=aa[:], op=ALU.mult), r=["aa"], w=["bb"])
                            I("act", lambda e: e.activation(out=bb[:], in_=bb[:], func=AF.Sqrt, scale=-1.0, bias=1.0), r=["bb"], w=["bb"])
                            I("pool", lambda e: e.tensor_tensor(out=bb[:], in0=bb[:], in1=gi_[:], op=ALU.mult), r=["bb", "gi"], w=["bb"])
                            I("pool", lambda e: e.tensor_tensor(out=bb[:], in0=bb[:], in1=xc[:], op=ALU.mult), r=["bb", "xc"], w=["bb"])
                            if d == 0:
                                I("dve", lambda e: e.tensor_tensor_scan(out=hf_[:], data0=aa[:], data1=bb[:], initial=0.0,
                                                                         op0=ALU.mult, op1=ALU.add), r=["aa", "bb"], w=["hf"])
                            else:
                                I("dve", lambda e: e.tensor_tensor_scan(out=hb_[:, 0:CTX][:, ::-1], data0=aa[:, 0:CTX][:, ::-1],
                                                                         data1=bb[:, 0:CTX][:, ::-1], initial=0.0,
                                                                         op0=ALU.mult, op1=ALU.add), r=["aa", "bb"], w=["hb"])
                                I("dve", lambda e: e.tensor_tensor_scan(out=hb_[:, CTX:S][:, ::-1], data0=aa[:, CTX:S][:, ::-1],
                                                                         data1=bb[:, CTX:S][:, ::-1], initial=hb_[:, 0:1],
                                                                         op0=ALU.mult, op1=ALU.add), r=["aa", "bb", "hb"], w=["hb"])
                        I("dve", lambda e: e.tensor_tensor(out=hf_[:, CTX:S], in0=hf_[:, CTX:S], in1=hb_[:, CTX:S], op=ALU.add),
                          r=["hf", "hb"], w=["hf"])
                        I("dve", lambda e, cc=cc: e.tensor_tensor(out=mgT[:, cc, :], in0=mgT[:, cc, :], in1=hf_[:, CTX:S], op=ALU.mult),
                          r=["hf", "mgT"], w=["mgT"])
                    kb.barrier()

                if upto == "M1b":
                    sR.close()
                    return nc
                sR.close()
                with contextlib.ExitStack() as s3:
                    otot = sb(s3, "otot", [128, 4, SEQ])
                    scr = sb(s3, "scr", [128, 2, S])
                    la = scr[:, 0, :]
                    bc = scr[:, 1, :]
                    dsg_all = scr[:, :, :].rearrange("p a n -> p (a n)").rearrange("p (c v) -> p c v", v=256)
                    eb = sb(s3, "eb", [128, S])
                    qd = sb(s3, "qd", [128, SEQ], BF16)
                    ki = sb(s3, "ki", [128, S], BF16)
                    kih = [sb(s3, "kih%d" % i, [128, S], BF16) for i in range(2)]
                    kit = [sb(s3, "kit%d" % i, [128, 128], BF16) for i in range(2)]
                    gam = sb(s3, "gam", [128, NT])
                    Sst = [sb(s3, "Sst%d" % i, [128, 256]) for i in range(2)]
                    Sbf_all = sb(s3, "Sbfa", [128, 16, 256], BF16)
                    scb_all = sb(s3, "scba", [128, 16, 256], BF16)
                    pz = [ps(s3, "pz%d" % i, [128, 512]) for i in range(1)]
                    ptr = [ps(s3, "ptr%d" % i, [128, 128], BF16) for i in range(2)]
                    pds = [ps(s3, "pds%d" % i, [128, 256]) for i in range(2)]
                    psc = [ps(s3, "psc%d" % i, [128, 256]) for i in range(2)]
                    po = [ps(s3, "po%d" % i, [128, 256]) for i in range(1)]
                    zc = 0
                    cn = 0
                    first_o = {0: True, 1: True}
                    for d in range(2):
                        order = list(range(NT)) if d == 0 else [1, 0] + list(range(NT - 1, 1, -1))
                        mk, mkk = (maskf, "maskf") if d == 0 else (maskb, "maskb")
                        for pr in range(2):
                            for g in range(5):
                                c0 = g * 512
                                n = min(512, S - c0)
                                p = pz[0]
                                pk = "pz0"
                                I("pe", lambda e, p=p, d=d, pr=pr, c0=c0, n=n: e.matmul(
                                    p[:, 0:n], lhsT=wg[:, d, pr * 128:(pr + 1) * 128], rhs=lrT[:, c0:c0 + n], start=True, stop=True),
                                  r=["wg", "lrT"], w=[pk])
                                I("act", lambda e, p=p, d=d, pr=pr, c0=c0, n=n: e.activation(
                                    out=la[:, c0:c0 + n], in_=p[:, 0:n], func=AF.Exp, scale=-1.0, bias=ngbg[:, d, pr:pr + 1]),
                                  r=[pk, "ngbg"], w=["la"])
                            I("act", lambda e: e.activation(out=la, in_=la, func=AF.Ln, bias=1.0), r=["la"], w=["la"])
                            for n_ in range(NT):
                                c0 = n_ * 128
                                if d == 0:
                                    I("dve", lambda e, c0=c0: e.tensor_tensor_scan(
                                        out=bc[:, c0:c0 + 128], data0=ones[:, :], data1=la[:, c0:c0 + 128], initial=0.0,
                                        op0=ALU.mult, op1=ALU.add), r=["la", "ones"], w=["bc"])
                                else:
                                    I("dve", lambda e, c0=c0: e.tensor_tensor_scan(
                                        out=bc[:, c0:c0 + 128][:, ::-1], data0=ones[:, :], data1=la[:, c0:c0 + 128][:, ::-1], initial=0.0,
                                        op0=ALU.mult, op1=ALU.add), r=["la", "ones"], w=["bc"])
                            I("act", lambda e: e.activation(out=eb[:], in_=bc, func=AF.Exp, scale=-1.0 / 16.0), r=["bc"], w=["eb"])
                            I("act", lambda e: e.activation(out=la, in_=bc, func=AF.Exp, scale=1.0 / 16.0), r=["bc"], w=["la"])
                            I("pool", lambda e, pr=pr: e.tensor_tensor(out=qd[:], in0=qT[:, pr, :], in1=eb[:, CTX:S], op=ALU.mult),
                              r=["qT", "eb"], w=["qd"])
                            I("dve", lambda e, pr=pr: e.tensor_tensor(out=ki[:], in0=kT[:, pr, :], in1=la, op=ALU.mult),
                              r=["kT", "la"], w=["ki"])
                            for h in range(2):
                                if h == 0:
                                    I("act", lambda e, h=h: e.mul(out=kih[h][:], in_=ki[:], mul=hm[:, h:h + 1]),
                                      r=["ki", "hm"], w=["kih%d" % h])
                                else:
                                    I("dve", lambda e, h=h: e.tensor_scalar_mul(out=kih[h][:], in0=ki[:], scalar1=hm[:, h:h + 1]),
                                      r=["ki", "hm"], w=["kih%d" % h])
                            gsrc = eb[:, 127::128] if d == 0 else eb[:, 0::128]
                            I("dve", lambda e, gsrc=gsrc: e.tensor_copy(out=gam[:], in_=gsrc), r=["eb"], w=["gam"])
                            I("dve", lambda e: e.memset(Sst[0][:], 0.0), w=["Sst0"])

                            def tr_(n_):
                                j = n_ % 2
                                I("pe", lambda e: e.transpose(ptr[j][:, :], ki[:, n_ * 128:(n_ + 1) * 128], identb[:]),
                                  r=["ki", "identb"], w=["ptr%d" % j])
                                I("act", lambda e: e.copy(out=kit[j][:], in_=ptr[j][:, :]), r=["ptr%d" % j], w=["kit%d" % j])

                            tr_(0)
                            for n_ in range(NT):
                                if n_ + 1 < NT:
                                    tr_(n_ + 1)
                                j = n_ % 2
                                dk = "la" if n_ < 9 else "bc"
                                I("pe", lambda e, j=j, n_=n_: e.matmul(
                                    pds[j][:, :], lhsT=kit[j][:, :], rhs=vtok[:, n_, pr * 256:(pr + 1) * 256], start=True, stop=True),
                                  r=["kit%d" % j, "vtok"], w=["pds%d" % j])
                                I("act", lambda e, j=j, n_=n_: e.mul(
                                    out=dsg_all[:, n_, :], in_=pds[j][:, :], mul=gam[:, n_:n_ + 1]),
                                  r=["pds%d" % j, "gam"], w=[dk])
                                if n_ >= 2:
                                    c0 = n_ * 128
                                    l0 = c0 - CTX
                                    for h in range(2):
                                        I("pe", lambda e, h=h, c0=c0, l0=l0, j=j: e.matmul(
                                            psc[j][:, h * 128:(h + 1) * 128], lhsT=kih[h][:, c0:c0 + 128],
                                            rhs=qd[:, l0:l0 + 128], start=True, stop=True),
                                          r=["kih%d" % h, "qd"], w=["psc%d" % j])
                                    I("dve", lambda e, j=j, n_=n_: e.tensor_tensor(out=scb_all[:, n_ - 2, :], in0=psc[j][:, :], in1=mk[:], op=ALU.mult),
                                      r=["psc%d" % j, mkk], w=["scba"])
                            for k_, n_ in enumerate(order):
                                a_, b_ = k_ % 2, (k_ + 1) % 2
                                dk = "la" if n_ < 9 else "bc"
                                if n_ >= 2:
                                    I("pool", lambda e, a_=a_, n_=n_: e.tensor_tensor(out=Sbf_all[:, n_ - 2, :], in0=Sst[a_][:], in1=bdm[:], op=ALU.mult),
                                      r=["Sst%d" % a_, "bdm"], w=["Sbfa"])
                                I("dve", lambda e, a_=a_, b_=b_, n_=n_: e.scalar_tensor_tensor(
                                    out=Sst[b_][:], in0=Sst[a_][:], scalar=gam[:, n_:n_ + 1], in1=dsg_all[:, n_, :],
                                    op0=ALU.mult, op1=ALU.add), r=["Sst%d" % a_, "gam", dk], w=["Sst%d" % b_])
                            pbanks = [(po[0], "po0"), (psc[0], "psc0"), (psc[1], "psc1")]
                            for n_ in range(2, NT):
                                l0 = n_ * 128 - CTX
                                pp, ppk = pbanks[n_ % len(pbanks)]
                                for h in range(2):
                                    hd = pr * 2 + h
                                    I("pe", lambda e, h=h, hd=hd, n_=n_, pp=pp: e.matmul(
                                        pp[:, h * 128:(h + 1) * 128], lhsT=vtok[:, n_, hd * 128:(hd + 1) * 128],
                                        rhs=scb_all[:, n_ - 2, h * 128:(h + 1) * 128], start=True, stop=False),
                                      r=["vtok", "scba"], w=[ppk])
                                    I("pe", lambda e, h=h, n_=n_, l0=l0, pp=pp: e.matmul(
                                        pp[:, h * 128:(h + 1) * 128], lhsT=Sbf_all[:, n_ - 2, h * 128:(h + 1) * 128],
                                        rhs=qd[:, l0:l0 + 128], start=False, stop=True),
                                      r=["Sbfa", "qd"], w=[ppk])
                                ov = otot[:, pr * 2:pr * 2 + 2, l0:l0 + 128]
                                pv_ = pp[:, :].rearrange("p (h t) -> p h t", h=2)
                                if d == 0:
                                    I("act", lambda e, ov=ov, pv_=pv_: e.copy(out=ov, in_=pv_), r=[ppk], w=["otot"])
                                else:
                                    I("dve", lambda e, ov=ov, pv_=pv_: e.tensor_tensor(out=ov, in0=ov, in1=pv_, op=ALU.add),
                                      r=[ppk, "otot"], w=["otot"])
                    sqo = sb(s3, "sqo", [128, 512])
                    rs = sb(s3, "rs", [128, 512])
                    tmpo = sb(s3, "tmpo", [128, 512])
                    for hd in range(4):
                        for g in range(4):
                            c0 = g * 512
                            p = pz[0]
                            pk = "pz0"
                            I("act", lambda e, hd=hd, c0=c0: e.activation(out=sqo[:], in_=otot[:, hd, c0:c0 + 512], func=AF.Square),
                              r=["otot"], w=["sqo"])
                            I("pe", lambda e, p=p: e.matmul(p[:, :], lhsT=ones[:, :], rhs=sqo[:], start=True, stop=True),
                              r=["ones", "sqo"], w=[pk])
                            I("act", lambda e, p=p: e.activation(out=rs[:], in_=p[:, :], func=AF.Sqrt, scale=1.0 / 128.0, bias=epsb[:]),
                              r=[pk, "epsb"], w=["rs"])
                            I("dve", lambda e: e.reciprocal(out=rs[:], in_=rs[:]), r=["rs"], w=["rs"])
                            I("dve", lambda e, hd=hd, c0=c0: e.scalar_tensor_tensor(
                                out=tmpo[:], in0=otot[:, hd, c0:c0 + 512], scalar=gng[:, 0:1], in1=rs[:], op0=ALU.mult, op1=ALU.mult),
                              r=["otot", "gng", "rs"], w=["tmpo"])
                            I("pool", lambda e, hd=hd, c0=c0: e.tensor_tensor(
                                out=mgT[:, 4 + hd, c0:c0 + 512], in0=mgT[:, 4 + hd, c0:c0 + 512], in1=tmpo[:], op=ALU.mult),
                              r=["tmpo", "mgT"], w=["mgT"])
                    kb.barrier()

                if upto == "M1c":
                    return nc
                with contextlib.ExitStack() as s4:
                    woutb = sb(s4, "woutb", [128, 8, D], BF16)
                    wst2 = [sb(s4, "wso%d" % i, [128, D]) for i in range(2)]
                    xt = [sb(s4, "xo%d" % i, [128, D]) for i in range(2)]
                    pt = [sb(s4, "po_%d" % i, [128, D]) for i in range(2)]
                    tm = [sb(s4, "tm%d" % i, [128, D]) for i in range(2)]
                    pw = [ps(s4, "pw%d" % i, [128, 2, 512]) for i in range(2)]
                    for kc in range(8):
                        w_ = wst2[kc % 2]
                        Dm(w_[:], wout_d[kc * 128:(kc + 1) * 128, :], w=["wso%d" % (kc % 2)])
                        I("dve", lambda e, w_=w_, kc=kc: e.tensor_copy(out=woutb[:, kc, :], in_=w_[:]), r=["wso%d" % (kc % 2)], w=["woutb"])
                    for t in range(16):
                        pb = t % 2
                        l0 = t * 128
                        Dm(xt[pb][:], x_d[b, l0:l0 + 128, :], w=["xo%d" % pb])
                        Dm(pt[pb][:], pos_d[l0:l0 + 128, :], w=["po_%d" % pb])
                        I("pool", lambda e, pb=pb: e.tensor_tensor(out=xt[pb][:], in0=xt[pb][:], in1=pt[pb][:], op=ALU.add),
                          r=["xo%d" % pb, "po_%d" % pb], w=["xo%d" % pb])
                        for hf in range(2):
                            for mc in range(8):
                                I("pe", lambda e, pb=pb, hf=hf, mc=mc, l0=l0: e.matmul(
                                    pw[pb][:, hf, :], lhsT=mgT[:, mc, l0:l0 + 128], rhs=woutb[:, mc, hf * 512:(hf + 1) * 512],
                                    start=(mc == 0), stop=(mc == 7)), r=["mgT", "woutb"], w=["pw%d" % pb])
                        I("dve", lambda e, pb=pb: e.tensor_tensor(
                            out=tm[pb][:], in0=pw[pb][:, :, :].rearrange("p a n -> p (a n)"), in1=g1gate[:, b, :], op=ALU.mult),
                          r=["pw%d" % pb, "g1gate"], w=["tm%d" % pb])
                        I("pool", lambda e, pb=pb: e.tensor_tensor(out=tm[pb][:], in0=tm[pb][:], in1=xt[pb][:], op=ALU.add),
                          r=["tm%d" % pb, "xo%d" % pb], w=["tm%d" % pb])
                        Dm(x1_d[b * SEQ + l0:b * SEQ + l0 + 128, :], tm[pb][:], r=["tm%d" % pb], w=["x1s"])
                    kb.barrier()
                if upto == "M1d":
                    return nc

    if upto == "M1":
        return nc
    with contextlib.ExitStack() as sc:
        uin = [sb(sc, "uin%d" % i, [128, 4, D]) for i in range(2)]
        ubf = [sb(sc, "ubf%d" % i, [128, 4, D], BF16) for i in range(2)]
        uts = [sb(sc, "uts%d" % i, [128, 8, 512], BF16) for i in range(2)]
        vin = [sb(sc, "vin%d" % i, [128, 4, D]) for i in range(2)]
        vbf = [sb(sc, "vbf%d" % i, [128, 4, D], BF16) for i in range(2)]
        ptc = [ps(sc, "ptc%d" % i, [128, 8, 128], BF16) for i in range(4)]
        tcn = 0
        for g in range(32):
            j = g % 2
            e0 = g * 512
            Dm(uin[j][:], pu_d[e0:e0 + 512, :].rearrange("(c p) n -> p c n", p=128), w=["uin%d" % j])
            Dm(vin[j][:], pv_d[e0:e0 + 512, :].rearrange("(c p) n -> p c n", p=128), w=["vin%d" % j])
            I("dve", lambda e, j=j: e.tensor_copy(out=ubf[j][:], in_=uin[j][:]), r=["uin%d" % j], w=["ubf%d" % j])
            if g % 2 == 0:
                I("dve", lambda e, j=j: e.tensor_copy(out=vbf[j][:], in_=vin[j][:]), r=["vin%d" % j], w=["vbf%d" % j])
            else:
                I("act", lambda e, j=j: e.copy(out=vbf[j][:], in_=vin[j][:]), r=["vin%d" % j], w=["vbf%d" % j])
            Dm(vs_d[e0:e0 + 512, :].rearrange("(c p) n -> p c n", p=128), vbf[j][:], r=["vbf%d" % j], w=["v_scr"], q="pool")
            for c in range(4):
                p = ptc[tcn % 4]
                pk = "ptc%d" % (tcn % 4)
                tcn += 1
                for fc in range(8):
                    I("pe", lambda e, p=p, j=j, c=c, fc=fc: e.transpose(p[:, fc, :], ubf[j][:, c, fc * 128:(fc + 1) * 128], identb[:]),
                      r=["ubf%d" % j, "identb"], w=[pk])
                I("act", lambda e, p=p, j=j, c=c: e.copy(out=uts[j][:, :, c * 128:(c + 1) * 128], in_=p[:, :, :]),
                  r=[pk], w=["uts%d" % j])
            for hh in range(2):
                Dm(ut_d[2 * g + hh, :, :, :], uts[j][:, :, hh * 256:(hh + 1) * 256], r=["uts%d" % j], w=["ut_scr"], q="pool")
        kb.barrier()

    if upto == "C":
        return nc
    with contextlib.ExitStack() as sp_:
        g2row = sb(sp_, "g2row", [128, D])
        gfrow = sb(sp_, "gfrow", [128, D])
        g2gate = sb(sp_, "g2gate", [128, D])
        wqb = sb(sp_, "wqb", [128, 8, 2048], BF16)
        keysb = sb(sp_, "keysb", [128, 16, 128], BF16)
        WT = sb(sp_, "WT", [128, TB, 128], BF16)
        NBUF = 4
        utb = [sb(sp_, "utb%d" % i, [128, 8, 256], BF16) for i in range(NBUF)]
        vtb = [sb(sp_, "vtb%d" % i, [128, 2, D], BF16) for i in range(NBUF)]
        x1s_ = sb(sp_, "x1s_", [128, D])
        u2T = [sb(sp_, "u2T%d" % i, [128, 8, TB], BF16) for i in range(2)]
        q2T = sb(sp_, "q2T", [128, 16, TB], BF16)
        sc_ = sb(sp_, "sc", [128, 16, 128])
        eq = sc_[:, :, :].rearrange("p (h a) (b c) -> p h (a b) c", a=2, c=16)
        cs = sb(sp_, "cs", [128, 8, 256])
        A16 = [sc_[:, 8 * i:8 * i + 4, :].rearrange("p a n -> p (a n)").bitcast(BF16).rearrange("p (t i) -> p t i", i=64) for i in range(2)]
        B16 = [cs[:, 4 * i:4 * i + 4, :].rearrange("p a n -> p (a n)").bitcast(BF16).rearrange("p (t i) -> p t i", i=128) for i in range(2)]
        tv = sb(sp_, "tv", [128, 16, 16])
        tix = sb(sp_, "tix", [128, 16, 16], U32)
        tif = sb(sp_, "tif", [128, 16, 16])
        sv = sb(sp_, "sv", [128, 8, 16])
        sve = sb(sp_, "sve", [128, 8, 16])
        spx = sb(sp_, "spx", [128, 8, 16], U32)
        spi = sb(sp_, "spi", [128, 8, 16], U32)
        pf = sb(sp_, "pf", [128, 8, 16])
        qf = sb(sp_, "qf", [128, 8, 16])
        If_ = sb(sp_, "If", [128, 128])
        Jf_ = sb(sp_, "Jf", [128, 128])
        Wf_ = sb(sp_, "Wf", [128, 128])
        zs = sb(sp_, "zs", [128, 8])
        IT = sb(sp_, "IT", [128, 2, 128])
        JT = sb(sp_, "JT", [128, 2, 128])
        WTr = sb(sp_, "WTr", [128, 2, 128])
        NSL = 3
        gl = [sb(sp_, "gl%d" % i, [128, 2 * TB]) for i in range(NSL)]
        awt = [sb(sp_, "awt%d" % i, [128, 2 * TB], BF16) for i in range(NSL)]
        sq2 = sb(sp_, "sq2", [128, D])
        ss2 = sb(sp_, "ss2", [128, 1])
        ss3 = sb(sp_, "ss3", [128, 1])
        py = [ps(sp_, "py%d" % i, [128, 2, 512]) for i in range(2)]
        pg_ = [ps(sp_, "pg%d" % i, [128, 512]) for i in range(3)]
        pb_ = [ps(sp_, "pb%d" % i, [128, 512]) for i in range(1)]
        if dbg:
            print("phase P sbuf bytes remaining:", nc.sbuf_bytes_remaining)

        Dm(g2row[:], g2_d[0:1, :].partition_broadcast(128), w=["g2row"])
        Dm(gfrow[:], gf_d[0:1, :].partition_broadcast(128), w=["gfrow"])
        for kc in range(8):
            for hh in range(2):
                Dm(sq2[:], wq_d[kc * 128:(kc + 1) * 128, hh * 1024:(hh + 1) * 1024], w=["sq2"])
                I("dve", lambda e, kc=kc, hh=hh: e.tensor_copy(out=wqb[:, kc, hh * 1024:(hh + 1) * 1024], in_=sq2[:]), r=["sq2"], w=["wqb"])
        for hh in range(2):
            Dm(sq2[:], keysT_d[:, hh * 8:(hh + 1) * 8, :].rearrange("p a n -> p (a n)"), w=["sq2"])
            I("dve", lambda e, hh=hh: e.tensor_copy(out=keysb[:, hh * 8:(hh + 1) * 8, :].rearrange("p a n -> p (a n)"), in_=sq2[:]), r=["sq2"], w=["keysb"])

        NG = 64
        nblk = 2 * SEQ // TB
        if upto is not None and upto.startswith('P'):
            nblk = int(upto[1:])
        NBB = SEQ // TB

        def load_group(gi):
            j = gi % NBUF
            e0 = (gi % NG) * 256
            Dm(utb[j][:], ut_d[gi % NG, :, :, :], r=["ut_scr"], w=["utb%d" % j])
            Dm(vtb[j][:], vs_d[e0:e0 + 256, :].rearrange("(c p) n -> p c n", p=128), r=["v_scr"], w=["vtb%d" % j])

        total_groups = nblk * NG
        for g_ in range(min(NBUF, total_groups)):
            load_group(g_)
        pbc = [0]

        def nextpb():
            k = 0
            pbc[0] += 1
            return pb_[k], "pb%d" % k

        LOOK = 2
        PULL = 3
        SC = ["sc_%d" % i for i in range(16)]
        CS = ["cs_%d" % i for i in range(8)]
        TV = ["tv_%d" % i for i in range(16)]
        TIX = ["tix_%d" % i for i in range(16)]
        SV = ["sv_%d" % i for i in range(8)]
        SPX = ["spx_%d" % i for i in range(8)]
        SCH = [SC[0:4], SC[8:12]]
        CSH = [CS[0:4], CS[4:8]]

        def stageS(tb_):
            b = tb_ // NBB
            r0 = tb_ * TB
            u2 = u2T[tb_ % 2]
            uk = "u2T%d" % (tb_ % 2)
            for ti in range(2):
                Dm(x1s_[:], x1_d[r0 + ti * 128:r0 + (ti + 1) * 128, :], r=["x1s"], w=["x1s_"])
                I("act", lambda e: e.activation(out=sq2[:], in_=x1s_[:], func=AF.Square, accum_out=ss2[:]),
                  r=["x1s_"], w=["sq2", "ss2"])
                I("act", lambda e: e.activation(out=ss2[:], in_=ss2[:], func=AF.Sqrt, scale=1.0 / D, bias=epsb[:]),
                  r=["ss2", "epsb"], w=["ss2"])
                I("dve", lambda e: e.reciprocal(out=ss2[:], in_=ss2[:]), r=["ss2"], w=["ss2"])
                I("dve", lambda e: e.scalar_tensor_tensor(
                    out=sq2[:], in0=x1s_[:], scalar=ss2[:], in1=g2row[:], op0=ALU.mult, op1=ALU.mult),
                  r=["x1s_", "ss2", "g2row"], w=["sq2"])
                yield
                for hf in range(2):
                    p, pk = nextpb()
                    for f4 in range(4):
                        fc = hf * 4 + f4
                        I("pe", lambda e, p=p, fc=fc, f4=f4: e.transpose(p[:, f4 * 128:(f4 + 1) * 128], sq2[:, fc * 128:(fc + 1) * 128], ident[:]),
                          r=["sq2", "ident"], w=[pk])
                    yield
                    yield
                    for f4 in range(4):
                        fc = hf * 4 + f4
                        I("dve", lambda e, p=p, fc=fc, f4=f4, ti=ti, b=b: e.tensor_scalar(
                            out=u2[:, fc, ti * 128:(ti + 1) * 128], in0=p[:, f4 * 128:(f4 + 1) * 128],
                            scalar1=modp[:, 4, fc, b:b + 1], scalar2=modp[:, 3, fc, b:b + 1], op0=ALU.mult, op1=ALU.add),
                          r=[pk, "modp"], w=[uk])
                    yield
            for hp in range(16):
                p, pk = nextpb()
                for kc in range(8):
                    I("pe", lambda e, p=p, hp=hp, kc=kc: e.matmul(
                        p[:, 0:TB], lhsT=wqb[:, kc, hp * 128:(hp + 1) * 128], rhs=u2[:, kc, :], start=(kc == 0), stop=(kc == 7)),
                      r=["wqb", uk], w=[pk])
                yield
                yield
                I("act", lambda e, p=p, hp=hp: e.copy(out=q2T[:, hp, :], in_=p[:, 0:TB]), r=[pk], w=["q2T"])
                yield
            for ti in range(2):
                for qd_ in range(4):
                    p, pk = nextpb()
                    for hh in range(4):
                        hp = qd_ * 4 + hh
                        I("pe", lambda e, p=p, hp=hp, hh=hh, ti=ti: e.matmul(
                            p[:, hh * 128:(hh + 1) * 128], lhsT=q2T[:, hp, ti * 128:(ti + 1) * 128], rhs=keysb[:, hp, :],
                            start=True, stop=True), r=["q2T", "keysb"], w=[pk])
                    yield
                    yield
                    I("act", lambda e, p=p, qd_=qd_: e.copy(
                        out=sc_[:, qd_ * 4:(qd_ + 1) * 4, :], in_=p[:, :].rearrange("p (a n) -> p a n", n=128)),
                      r=[pk], w=SC[qd_ * 4:(qd_ + 1) * 4])
                    yield
                for hp in range(16):
                    I("dve", lambda e, hp=hp: e.max(out=tv[:, hp, 0:8], in_=sc_[:, hp, :]), r=["sc_%d" % hp], w=["tv_%d" % hp])
                    if hp % 4 == 3:
                        yield
                for hp in range(16):
                    I("dve", lambda e, hp=hp: e.max_index(out=tix[:, hp, 0:8], in_max=tv[:, hp, 0:8], in_values=sc_[:, hp, :]),
                      r=["sc_%d" % hp, "tv_%d" % hp], w=["tix_%d" % hp])
                    if hp % 4 == 3:
                        yield
                for hp in range(16):
                    I("dve", lambda e, hp=hp: e.match_replace(out=sc_[:, hp, :], in_to_replace=tv[:, hp, 0:8], in_values=sc_[:, hp, :], imm_value=NEG),
                      r=["tv_%d" % hp], w=["sc_%d" % hp])
                    if hp % 4 == 3:
                        yield
                for hp in range(16):
                    I("dve", lambda e, hp=hp: e.max(out=tv[:, hp, 8:16], in_=sc_[:, hp, :]), r=["sc_%d" % hp], w=["tv_%d" % hp])
                    if hp % 4 == 3:
                        yield
                for hp in range(16):
                    I("dve", lambda e, hp=hp: e.max_index(out=tix[:, hp, 8:16], in_max=tv[:, hp, 8:16], in_values=sc_[:, hp, :]),
                      r=["sc_%d" % hp, "tv_%d" % hp], w=["tix_%d" % hp])
                    if hp % 4 == 3:
                        yield
                I("dve", lambda e: e.tensor_copy(out=tif[:], in_=tix[:]), r=TIX, w=["tif"])
                tv4 = tv[:, :, :].rearrange("p (h s) k -> p h s k", s=2)
                tif4 = tif[:, :, :].rearrange("p (h s) k -> p h s k", s=2)
                I("dve", lambda e, tv4=tv4: e.tensor_tensor(
                    out=cs[:, :, :].rearrange("p h (a c) -> p h a c", c=16),
                    in0=tv4[:, :, 0, :].unsqueeze(3).to_broadcast([128, 8, 16, 16]),
                    in1=tv4[:, :, 1, :].unsqueeze(2).to_broadcast([128, 8, 16, 16]), op=ALU.add), r=TV, w=CS)
                yield
                for h in range(8):
                    I("dve", lambda e, h=h: e.max(out=sv[:, h, 0:8], in_=cs[:, h, :]), r=["cs_%d" % h], w=["sv_%d" % h])
                yield
                for h in range(8):
                    I("dve", lambda e, h=h: e.max_index(out=spx[:, h, 0:8], in_max=sv[:, h, 0:8], in_values=cs[:, h, :]),
                      r=["cs_%d" % h, "sv_%d" % h], w=["spx_%d" % h])
                yield
                for h in range(8):
                    I("dve", lambda e, h=h: e.match_replace(out=cs[:, h, :], in_to_replace=sv[:, h, 0:8], in_values=cs[:, h, :], imm_value=NEG),
                      r=["sv_%d" % h], w=["cs_%d" % h])
                yield
                for h in range(8):
                    I("dve", lambda e, h=h: e.max(out=sv[:, h, 8:16], in_=cs[:, h, :]), r=["cs_%d" % h], w=["sv_%d" % h])
                yield
                for h in range(8):
                    I("dve", lambda e, h=h: e.max_index(out=spx[:, h, 8:16], in_max=sv[:, h, 8:16], in_values=cs[:, h, :]),
                      r=["cs_%d" % h, "sv_%d" % h], w=["spx_%d" % h])
                yield
                I("dve", lambda e: e.tensor_single_scalar(out=spi[:], in_=spx[:], scalar=4, op=ALU.logical_shift_right), r=SPX, w=["spi"])
                I("dve", lambda e: e.tensor_copy(out=pf[:], in_=spi[:]), r=["spi"], w=["pf"])
                I("dve", lambda e: e.tensor_single_scalar(out=spi[:], in_=spx[:], scalar=15, op=ALU.bitwise_and), r=SPX + ["pf"], w=["spi"])
                I("dve", lambda e: e.tensor_copy(out=qf[:], in_=spi[:]), r=["spi"], w=["qf"])
                yield
                io16 = iota[:, 0:16].unsqueeze(1).unsqueeze(1).to_broadcast([128, 8, 16, 16])
                for (rf, side, dst, dk) in ((pf, 0, If_, "If"), (qf, 1, Jf_, "Jf")):
                    I("dve", lambda e, rf=rf: e.tensor_tensor(
                        out=eq, in0=rf[:, :, :].unsqueeze(3).to_broadcast([128, 8, 16, 16]), in1=io16, op=ALU.is_equal),
                      r=["pf", "qf", "iota"], w=SC)
                    yield
                    I("dve", lambda e, side=side, tif4=tif4: e.tensor_tensor(
                        out=eq, in0=eq, in1=tif4[:, :, side, :].unsqueeze(2).to_broadcast([128, 8, 16, 16]), op=ALU.mult),
                      r=SC + ["tif"], w=SC)
                    yield
                    I("dve", lambda e, dst=dst: e.tensor_reduce(
                        out=dst[:, :].rearrange("p (h k) -> p h k", k=16), in_=eq, axis=AX.X, op=ALU.add), r=SC, w=[dk])
                    yield
                I("dve", lambda e: e.tensor_tensor(out=sve[:], in0=sv[:], in1=sv[:, :, 0:1].to_broadcast([128, 8, 16]), op=ALU.subtract),
                  r=SV, w=["sve"])
                I("act", lambda e: e.activation(out=sve[:], in_=sve[:], func=AF.Exp), r=["sve"], w=["sve"])
                I("dve", lambda e: e.tensor_reduce(out=zs[:], in_=sve[:], axis=AX.X, op=ALU.add), r=["sve"], w=["zs"])
                I("dve", lambda e: e.reciprocal(out=zs[:], in_=zs[:]), r=["zs"], w=["zs"])
                I("dve", lambda e: e.tensor_tensor(
                    out=Wf_[:, :].rearrange("p (h k) -> p h k", k=16), in0=sve[:], in1=zs[:, :].unsqueeze(2).to_broadcast([128, 8, 16]),
                    op=ALU.mult), r=["sve", "zs"], w=["Wf"])
                yield
                for (src, sk, dst, dk) in ((If_, "If", IT, "IT"), (Jf_, "Jf", JT, "JT"), (Wf_, "Wf", WTr, "WTr")):
                    p, pk = nextpb()
                    yield
                    I("pe", lambda e, p=p, src=src: e.transpose(p[:, 0:128], src[:], ident[:]), r=[sk, "ident"], w=[pk])
                    yield
                    yield
                    I("act", lambda e, p=p, dst=dst, ti=ti: e.copy(out=dst[:, ti, :], in_=p[:, 0:128]), r=[pk], w=[dk])
                yield

        def stageW(tb_, half):
            iob = iota[:, :].unsqueeze(1).to_broadcast([128, 16, 128])
            ioh = iota[:, half * 64:(half + 1) * 64].unsqueeze(1).to_broadcast([128, 16, 64])
            wkey = "WT%d" % half

            def gen(bi):
                ti = bi // 8
                j2 = bi % 2
                t0 = (bi % 8) * 16
                I("dve", lambda e: e.tensor_tensor(
                    out=A16[j2], in0=ioh, in1=IT[:, ti, t0:t0 + 16].unsqueeze(2).to_broadcast([128, 16, 64]), op=ALU.is_equal),
                  r=["iota", "IT"], w=SCH[j2])
                I("dve", lambda e: e.tensor_tensor(
                    out=B16[j2], in0=iob, in1=JT[:, ti, t0:t0 + 16].unsqueeze(2).to_broadcast([128, 16, 128]), op=ALU.is_equal),
                  r=["iota", "JT"], w=CSH[j2])
                I("pool", lambda e: e.tensor_tensor(
                    out=A16[j2], in0=A16[j2], in1=WTr[:, ti, t0:t0 + 16].unsqueeze(2).to_broadcast([128, 16, 64]), op=ALU.mult),
                  r=SCH[j2] + ["WTr"], w=SCH[j2])

            def mm(bi, t8):
                ti = bi // 8
                j2 = bi % 2
                t0 = (bi % 8) * 16
                p, pk = nextpb()
                for tt in range(8):
                    tl = t8 * 8 + tt
                    I("pe", lambda e, tt=tt, tl=tl: e.matmul(
                        p[:, tt * 64:(tt + 1) * 64], lhsT=B16[j2][:, tl, :], rhs=A16[j2][:, tl, :], start=True, stop=True),
                      r=SCH[j2] + CSH[j2], w=[pk])
                return p, pk, ti * 128 + t0 + t8 * 8

            def cp(p, pk, tg0):
                I("act", lambda e: e.copy(
                    out=WT[:, tg0:tg0 + 8, half * 64:(half + 1) * 64], in_=p[:, :].rearrange("p (t i) -> p t i", i=64)),
                  r=[pk], w=[wkey])

            gen(0)
            yield
            for bi in range(16):
                if bi + 1 < 16:
                    gen(bi + 1)
                yield
                a = mm(bi, 0)
                yield
                yield
                cp(*a)
                yield
                a = mm(bi, 1)
                yield
                yield
                cp(*a)
                yield

        def gemm_and_epilogue(tb_, bg):
            b = tb_ // NBB
            u2 = u2T[tb_ % 2]
            uk = "u2T%d" % (tb_ % 2)
            slots = {}

            def pull(ic, n):
                for ent in bg:
                    if ent[0] is None:
                        continue
                    if ent[1] > ic:
                        return
                    while n > 0:
                        try:
                            next(ent[0])
                            n -= 1
                        except StopIteration:
                            ent[0] = None
                            break
                    if n == 0:
                        return

            def drain(pred):
                for ent in bg:
                    if ent[0] is not None and pred(ent):
                        for _ in ent[0]:
                            pass
                        ent[0] = None

            def emitU(pr):
                gi = tb_ * NG + pr
                j = gi % NBUF
                sl = pr % NSL
                p = pg_[sl]
                pk = "pgs%d" % sl
                slots[pr] = sl
                for c in range(2):
                    for kc in range(8):
                        I("pe", lambda e, p=p, j=j, c=c, kc=kc: e.matmul(
                            p[:, c * TB:(c + 1) * TB], lhsT=utb[j][:, kc, c * 128:(c + 1) * 128], rhs=u2[:, kc, :],
                            start=(kc == 0), stop=(kc == 7)), r=["utb%d" % j, uk], w=[pk])
                I("act", lambda e, p=p, sl=sl: e.activation(out=gl[sl][:], in_=p[:, :], func=AF.Gelu), r=[pk], w=["gl%d" % sl])
                I("pool", lambda e, sl=sl, pr=pr: e.tensor_tensor(
                    out=awt[sl][:, :].rearrange("p (c t) -> p c t", c=2), in0=gl[sl][:, :].rearrange("p (c t) -> p c t", c=2),
                    in1=WT[:, :, 2 * pr:2 * pr + 2].rearrange("p t i -> p i t"), op=ALU.mult),
                  r=["gl%d" % sl, "WT%d" % (pr // 32)], w=["awt%d" % sl])

            def emitV(pr):
                gi = tb_ * NG + pr
                j = gi % NBUF
                sl = slots.pop(pr)
                for c in range(2):
                    ic = 2 * pr + c
                    for ti in range(2):
                        for hf in range(2):
                            I("pe", lambda e, sl=sl, ti=ti, hf=hf, j=j, c=c, ic=ic: e.matmul(
                                py[ti][:, hf, :], lhsT=awt[sl][:, c * TB + ti * 128:c * TB + (ti + 1) * 128],
                                rhs=vtb[j][:, c, hf * 512:(hf + 1) * 512],
                                start=(ic == 0), stop=(ic == 127)), r=["awt%d" % sl, "vtb%d" % j], w=["py%d" % ti])
                if gi + NBUF < total_groups:
                    load_group(gi + NBUF)

            for pr in range(-LOOK, 64):
                if pr + LOOK < 64:
                    nu = pr + LOOK
                    if nu == 32:
                        drain(lambda ent: ent[2] <= 64)
                    emitU(nu)
                    if pr >= 0:
                        pull(2 * pr, PULL)
                if pr >= 0:
                    emitV(pr)
                    pull(2 * pr, PULL)
            drain(lambda ent: True)
            for ti in range(2):
                r0 = tb_ * TB + ti * 128
                Dm(x1s_[:], x1_d[r0:r0 + 128, :], r=["x1s"], w=["x1s_"])
                I("dve", lambda e, ti=ti: e.tensor_tensor(
                    out=sq2[:], in0=py[ti][:, :, :].rearrange("p a n -> p (a n)"), in1=g2gate[:], op=ALU.mult),
                  r=["py%d" % ti, "g2gate"], w=["sq2"])
                I("pool", lambda e: e.tensor_tensor(out=sq2[:], in0=sq2[:], in1=x1s_[:], op=ALU.add), r=["sq2", "x1s_"], w=["sq2"])
                I("act", lambda e: e.activation(out=x1s_[:], in_=sq2[:], func=AF.Square, accum_out=ss3[:]), r=["sq2"], w=["x1s_", "ss3"])
                I("act", lambda e: e.activation(out=ss3[:], in_=ss3[:], func=AF.Sqrt, scale=1.0 / D, bias=epsb[:]),
                  r=["ss3", "epsb"], w=["ss3"])
                I("dve", lambda e: e.reciprocal(out=ss3[:], in_=ss3[:]), r=["ss3"], w=["ss3"])
                I("dve", lambda e: e.scalar_tensor_tensor(
                    out=x1s_[:], in0=sq2[:], scalar=ss3[:], in1=gfrow[:], op0=ALU.mult, op1=ALU.mult),
                  r=["sq2", "ss3", "gfrow"], w=["x1s_"])
                l0 = (tb_ % NBB) * TB + ti * 128
                Dm(out_d[b, l0:l0 + 128, :], x1s_[:], r=["x1s_"], w=["out"])

        Dm(g2gate[:], grow_d[2:3, :].partition_broadcast(128), r=["grow_scr"], w=["g2gate"])
        for _ in stageS(0):
            pass
        if upto == "S0":
            kb.barrier()
            return nc
        for _ in stageW(0, 0):
            pass
        if upto == "S1":
            kb.barrier()
            return nc
        for tb_ in range(nblk):
            bg = [[stageW(tb_, 1), 0, 64]]
            if tb_ + 1 < nblk:
                bg.append([stageS(tb_ + 1), 0, 999])
                bg.append([stageW(tb_ + 1, 0), 64, 999])
            gemm_and_epilogue(tb_, bg)
            if tb_ + 1 < nblk and (tb_ + 1) % NBB == 0:
                Dm(g2gate[:], grow_d[2 + (tb_ + 1) // NBB:3 + (tb_ + 1) // NBB, :].partition_broadcast(128), r=["grow_scr"], w=["g2gate"])
        kb.barrier(engines=("sp",))
    return nc


def _pos_table():
    rows = SEQ // 64
    r, col = np.meshgrid(np.arange(rows, dtype=np.float32), np.arange(64, dtype=np.float32), indexing="ij")
    quarter = D // 4
    omega = (np.float32(10000.0) ** (-np.arange(quarter, dtype=np.float32) / np.float32(quarter))).astype(np.float32)

    def emb(p):
        ang = p.reshape(-1)[:, None].astype(np.float32) * omega[None, :]
        return np.concatenate([np.sin(ang), np.cos(ang)], axis=-1)

    return np.concatenate([emb(r), emb(col)], axis=-1).astype(np.float32)


_NC_CACHE = {}


def _fm(v, nchunk):
    return np.ascontiguousarray(np.asarray(v, np.float32).reshape(nchunk, 128).T)


def kernel(x, c, ctx, c_ctx, ada_w, ada_b, norm1_g, w_in, conv_w, conv_b, rg_w_a, rg_b_a, rg_w_x, rg_b_x, rg_lambda,
           gla_w_g, gla_b_g, gla_norm_g, w_out, norm2_g, peer_w_q, peer_keys, peer_u, peer_v, final_norm_g):
    if "nc" not in _NC_CACHE:
        _NC_CACHE["nc"] = build()
    nc = _NC_CACHE["nc"]
    in_maps = make_in_maps(x, c, ctx, c_ctx, ada_w, ada_b, norm1_g, w_in, conv_w, conv_b, rg_w_a, rg_b_a, rg_w_x, rg_b_x, rg_lambda,
                           gla_w_g, gla_b_g, gla_norm_g, w_out, norm2_g, peer_w_q, peer_keys, peer_u, peer_v, final_norm_g)
    res = run_bass_kernel_spmd(nc, in_maps, core_ids=list(range(NCORES)))
    out = np.concatenate([np.asarray(r["out"], dtype=np.float32) for r in res.results], axis=0)
    return out


def make_in_maps(x, c, ctx, c_ctx, ada_w, ada_b, norm1_g, w_in, conv_w, conv_b, rg_w_a, rg_b_a, rg_w_x, rg_b_x, rg_lambda,
                 gla_w_g, gla_b_g, gla_norm_g, w_out, norm2_g, peer_w_q, peer_keys, peer_u, peer_v, final_norm_g):
    f = lambda a: np.ascontiguousarray(np.asarray(a, dtype=np.float32))
    x, c, ctx, c_ctx = f(x), f(c), f(ctx), f(c_ctx)
    jj, ii = np.meshgrid(np.arange(128), np.arange(128), indexing="ij")
    mf = (jj <= ii).astype(np.float32)
    mb = (jj >= ii).astype(np.float32)
    shared = {
        "ada_w": f(ada_w[0]),
        "ada_b": f(ada_b[0]).reshape(1, -1),
        "ada_bT": _fm(ada_b[0], 48),
        "norm1_g": f(norm1_g[0]).reshape(1, -1),
        "norm2_g": f(norm2_g[0]).reshape(1, -1),
        "final_g": f(final_norm_g).reshape(1, -1),
        "w_in": f(w_in[0]),
        "convwT": np.ascontiguousarray(f(conv_w[0]).reshape(4, 4, 128).transpose(2, 1, 0)),
        "convbT": _fm(conv_b[0], 4),
        "rg_w_a": f(rg_w_a[0]),
        "rg_w_x": f(rg_w_x[0]),
        "rgbaT": np.ascontiguousarray(f(rg_b_a[0]).reshape(2, 4, 128).transpose(2, 0, 1)),
        "rgbxT": np.ascontiguousarray(f(rg_b_x[0]).reshape(2, 4, 128).transpose(2, 0, 1)),
        "rglamT": np.ascontiguousarray(f(rg_lambda[0]).reshape(2, 4, 128).transpose(2, 0, 1)),
        "gla_w_g": f(gla_w_g[0]),
        "glabgT": np.ascontiguousarray(f(gla_b_g[0]).reshape(2, 2, 128).transpose(2, 0, 1)),
        "glangT": f(gla_norm_g[0]).reshape(128, 1),
        "w_out": f(w_out[0]),
        "peer_w_q": f(peer_w_q[0]),
        "keysT": np.ascontiguousarray(f(peer_keys[0]).reshape(16, 128, 128).transpose(2, 0, 1)),
        "peer_u": f(peer_u[0]),
        "peer_v": f(peer_v[0]),
        "pos": _pos_table(),
        "ident": np.eye(128, dtype=np.float32),
        "iota": np.tile(np.arange(128, dtype=np.float32)[None, :], (128, 1)),
        "maskf": np.ascontiguousarray(np.concatenate([mf, mf], axis=1)),
        "maskb": np.ascontiguousarray(np.concatenate([mb, mb], axis=1)),
    }
    in_maps = []
    for i in range(NCORES):
        b0 = 2 * i
        cm = np.stack([c[b0], c[b0 + 1], c_ctx], axis=0)
        cT = np.ascontiguousarray(cm.reshape(3, 8, 128).transpose(2, 1, 0))
        m = dict(shared)
        m["x"] = np.ascontiguousarray(x[b0:b0 + 2])
        m["ctx"] = np.ascontiguousarray(ctx[b0:b0 + 2])
        m["cT"] = cT
        in_maps.append(m)
    return in_maps
```

```python
import contextlib
import numpy as np
import concourse.bass as bass
import concourse.mybir as mybir
from concourse.bass_utils import run_bass_kernel_spmd

F32 = mybir.dt.float32
BF16 = mybir.dt.bfloat16
U32 = mybir.dt.uint32
ALU = mybir.AluOpType
AF = mybir.ActivationFunctionType
AX = mybir.AxisListType

NCORES = 8
D = 1024
SEQ = 2048
CTX = 256
S = CTX + SEQ
NT = S // 128
INC = 2592
NEXP = 16384
TB = 256
EPS = 1e-6
NEG = -1.0e30


class KB:
    def __init__(self):
        self.nc = bass.Bass("TRN2", target_bir_lowering=False)
        nc = self.nc
        self.eng = {"pe": nc.tensor, "act": nc.scalar, "dve": nc.vector, "pool": nc.gpsimd, "sp": nc.sync}
        self.stack = contextlib.ExitStack()
        self.sem = {}
        self.cnt = {}
        for n in self.eng:
            self.sem[("e", n)] = self.stack.enter_context(nc.semaphore("s_" + n))
            self.cnt[n] = 0
        self.nd = 24
        self.dcnt = [0] * self.nd
        for i in range(self.nd):
            self.sem[("d", i)] = self.stack.enter_context(nc.semaphore("d%d" % i))
        self.dnext = 0
        self.waited = {}
        self.lastw = {}
        self.readers = {}

    def _wait(self, en, deps):
        for sk, v in deps.items():
            if en == "pe" and sk == ("e", "pe"):
                continue
            if self.waited.get((en, sk), 0) >= v:
                continue
            self.eng[en].wait_ge(self.sem[sk], v)
            self.waited[(en, sk)] = v

    def _deps(self, r, w):
        d = {}
        for k in r:
            t = self.lastw.get(k)
            if t:
                d[t[0]] = max(d.get(t[0], 0), t[1])
        for k in w:
            t = self.lastw.get(k)
            if t:
                d[t[0]] = max(d.get(t[0], 0), t[1])
            for sk, v in self.readers.get(k, {}).items():
                d[sk] = max(d.get(sk, 0), v)
        return d

    def _commit(self, tok, r, w):
        for k in w:
            self.lastw[k] = tok
            self.readers[k] = {}
        for k in r:
            rd = self.readers.setdefault(k, {})
            rd[tok[0]] = max(rd.get(tok[0], 0), tok[1])

    def I(self, en, fn, r=(), w=()):
        self._wait(en, self._deps(r, w))
        inst = fn(self.eng[en])
        self.cnt[en] += 1
        inst.then_inc(self.sem[("e", en)], 1)
        self._commit((("e", en), self.cnt[en]), r, w)

    def D(self, out, in_, r=(), w=(), q="sp"):
        i = self.dnext
        self.dnext = (i + 1) % self.nd
        deps = self._deps(r, w)
        if self.dcnt[i] > 0:
            deps[("d", i)] = max(deps.get(("d", i), 0), self.dcnt[i])
        self._wait(q, deps)
        inst = self.eng[q].dma_start(out=out, in_=in_)
        self.dcnt[i] += 16
        inst.then_inc(self.sem[("d", i)], 16)
        self._commit((("d", i), self.dcnt[i]), r, w)

    def barrier(self, engines=("pe", "act", "dve", "pool", "sp")):
        deps = {}
        for n in self.eng:
            if self.cnt[n] > 0:
                deps[("e", n)] = self.cnt[n]
        for i in range(self.nd):
            if self.dcnt[i] > 0:
                deps[("d", i)] = self.dcnt[i]
        for en in engines:
            d = dict(deps)
            d.pop(("e", en), None)
            self._wait(en, d)
        self.lastw = {}
        self.readers = {}


def build(upto=None, dbg=False):
    kb = KB()
    nc = kb.nc
    I, Dm = kb.I, kb.D

    def dram(name, shape, dt=F32, kind="ExternalInput"):
        return nc.dram_tensor(name, list(shape), dt, kind=kind).ap()

    x_d = dram("x", [2, SEQ, D])
    ctx_d = dram("ctx", [2, CTX, D])
    cT_d = dram("cT", [128, 8, 3])
    adaw_d = dram("ada_w", [D, 6 * D])
    adab_d = dram("ada_b", [1, 6 * D])
    adabT_d = dram("ada_bT", [128, 48])
    g1_d = dram("norm1_g", [1, D])
    g2_d = dram("norm2_g", [1, D])
    gf_d = dram("final_g", [1, D])
    win_d = dram("w_in", [D, INC])
    convw_d = dram("convwT", [128, 4, 4])
    convb_d = dram("convbT", [128, 4])
    rgwa_d = dram("rg_w_a", [2, 8, 64, 64])
    rgwx_d = dram("rg_w_x", [2, 8, 64, 64])
    rgba_d = dram("rgbaT", [128, 2, 4])
    rgbx_d = dram("rgbxT", [128, 2, 4])
    rglam_d = dram("rglamT", [128, 2, 4])
    glawg_d = dram("gla_w_g", [2, 16, 256])
    glabg_d = dram("glabgT", [128, 2, 2])
    glang_d = dram("glangT", [128, 1])
    wout_d = dram("w_out", [D, D])
    wq_d = dram("peer_w_q", [D, 2048])
    keysT_d = dram("keysT", [128, 16, 128])
    pu_d = dram("peer_u", [NEXP, D])
    pv_d = dram("peer_v", [NEXP, D])
    pos_d = dram("pos", [SEQ, D])
    ident_d = dram("ident", [128, 128])
    iota_d = dram("iota", [128, 128])
    maskf_d = dram("maskf", [128, 256])
    maskb_d = dram("maskb", [128, 256])
    out_d = dram("out", [2, SEQ, D], kind="ExternalOutput")
    x1_d = dram("x1s", [2 * SEQ, D], kind="ExternalOutput" if dbg else "Internal")
    grow_d = dram("grow_scr", [4, D], kind="Internal")
    ut_d = dram("ut_scr", [NEXP // 256, 128, 8, 256], BF16, kind="Internal")
    vs_d = dram("v_scr", [NEXP, D], BF16, kind="Internal")

    es = contextlib.ExitStack()

    uid = [0]

    def sb(stack, name, shape, dt=F32):
        uid[0] += 1
        return stack.enter_context(nc.sbuf_tensor("sb%d_%s" % (uid[0], name), list(shape), dt))

    def ps(stack, name, shape, dt=F32):
        uid[0] += 1
        return stack.enter_context(nc.psum_tensor("ps%d_%s" % (uid[0], name), list(shape), dt))

    ident = sb(es, "ident", [128, 128])
    identb = sb(es, "identb", [128, 128], BF16)
    iota = sb(es, "iota", [128, 128])
    ones = sb(es, "ones", [128, 128])
    modp = sb(es, "modp", [128, 6, 8, 3])
    epsb = sb(es, "epsb", [128, 1])

    Dm(ident[:], ident_d[:, :], w=["ident"])
    Dm(iota[:], iota_d[:, :], w=["iota"])
    I("dve", lambda e: e.tensor_copy(out=identb[:], in_=ident[:]), r=["ident"], w=["identb"])
    I("dve", lambda e: e.memset(ones[:], 1.0), w=["ones"])
    I("dve", lambda e: e.memset(epsb[:], EPS), w=["epsb"])

    with contextlib.ExitStack() as st:
        cT = sb(st, "cT", [128, 8, 3])
        scT = sb(st, "scT", [128, 8, 3])
        rep = sb(st, "rep", [128, 2, 8, 128])
        abT = sb(st, "abT", [128, 48])
        abrow = sb(st, "abrow", [128, D])
        growt = [sb(st, "growt%d" % i, [128, D]) for i in range(2)]
        aw = [sb(st, "aw%d" % i, [128, 8, D]) for i in range(2)]
        pm = [ps(st, "pm%d" % i, [128, 512]) for i in range(4)]
        Dm(cT[:], cT_d[:, :, :], w=["cT"])
        Dm(abT[:], adabT_d[:, :], w=["abT"])
        I("act", lambda e: e.activation(out=scT[:], in_=cT[:], func=AF.Silu), r=["cT"], w=["scT"])
        for b in range(2):
            I("dve", lambda e, b=b: e.tensor_copy(out=rep[:, b], in_=scT[:, :, b:b + 1].to_broadcast([128, 8, 128])),
              r=["scT"], w=["rep"])
        pi = 0
        for m in range(6):
            a = aw[m % 2]
            ak = "aw%d" % (m % 2)
            Dm(a[:], adaw_d[:, m * D:(m + 1) * D].rearrange("(kc p) n -> p kc n", p=128), w=[ak])
            if m in (0, 1, 3, 4):
                p = pm[pi % 4]
                pk = "pm%d" % (pi % 4)
                pi += 1
                for fc in range(8):
                    for kc in range(8):
                        I("pe", lambda e, fc=fc, kc=kc, p=p, a=a: e.matmul(
                            p[:, fc * 3:fc * 3 + 3], lhsT=a[:, kc, fc * 128:(fc + 1) * 128], rhs=scT[:, kc, :],
                            start=(kc == 0), stop=(kc == 7)), r=[ak, "scT"], w=[pk])
                I("dve", lambda e, m=m, p=p: e.tensor_tensor(
                    out=modp[:, m], in0=p[:, 0:24].rearrange("p (f c) -> p f c", c=3),
                    in1=abT[:, m * 8:(m + 1) * 8].unsqueeze(2).to_broadcast([128, 8, 3]), op=ALU.add),
                  r=[pk, "abT"], w=["modp"])
                if m in (1, 4):
                    I("dve", lambda e, m=m: e.tensor_scalar_add(out=modp[:, m], in0=modp[:, m], scalar1=1.0),
                      r=["modp"], w=["modp"])
            else:
                mi = 0 if m == 2 else 1
                Dm(abrow[:], adab_d[0:1, m * D:(m + 1) * D].partition_broadcast(128), w=["abrow"])
                for b in range(2):
                    for hf in range(2):
                        p = pm[pi % 4]
                        pk = "pm%d" % (pi % 4)
                        pi += 1
                        for kc in range(8):
                            I("pe", lambda e, kc=kc, p=p, a=a, b=b, hf=hf: e.matmul(
                                p[:, :], lhsT=rep[:, b, kc, :], rhs=a[:, kc, hf * 512:(hf + 1) * 512],
                                start=(kc == 0), stop=(kc == 7)), r=[ak, "rep"], w=[pk])
                        I("dve", lambda e, p=p, b=b, hf=hf, mi=mi: e.tensor_tensor(
                            out=growt[b][:, hf * 512:(hf + 1) * 512], in0=p[:, :],
                            in1=abrow[:, hf * 512:(hf + 1) * 512], op=ALU.add),
                          r=[pk, "abrow"], w=["growt%d" % b])
                    Dm(grow_d[mi * 2 + b:mi * 2 + b + 1, :], growt[b][0:1, :], r=["growt%d" % b], w=["grow_scr"])
        kb.barrier()
    if upto == "M0":
        return nc

    with contextlib.ExitStack() as sm:
        g1row = sb(sm, "g1row", [128, D])
        g1gate = sb(sm, "g1gate", [128, 2, D])
        for b in range(2):
            Dm(g1gate[:, b, :], grow_d[b:b + 1, :].partition_broadcast(128), r=["grow_scr"], w=["g1gate"])
        maskf = sb(sm, "maskf", [128, 256], BF16)
        maskb = sb(sm, "maskb", [128, 256], BF16)
        mstage = sb(sm, "mstage", [128, 256])
        cw = sb(sm, "cw", [128, 4, 4])
        cb = sb(sm, "cb", [128, 4])
        rba = sb(sm, "rba", [128, 2, 4])
        rbx = sb(sm, "rbx", [128, 2, 4])
        rlam = sb(sm, "rlam", [128, 2, 4])
        coef = sb(sm, "coef", [128, 2, 4])
        gbg = sb(sm, "gbg", [128, 2, 2])
        ngbg = sb(sm, "ngbg", [128, 2, 2])
        gng = sb(sm, "gng", [128, 1])
        wg = sb(sm, "wg", [32, 2, 256])
        wbd = sb(sm, "wbd", [128, 2, 2, 4, 128])
        hm = sb(sm, "hm", [128, 2])
        bdm = sb(sm, "bdm", [128, 256])
        I("dve", lambda e: e.memset(hm[:], 0.0), w=["hm"])
        I("dve", lambda e: e.memset(hm[0:64, 0:1], 1.0), w=["hm"])
        I("dve", lambda e: e.memset(hm[64:128, 1:2], 1.0), w=["hm"])
        I("dve", lambda e: e.memset(bdm[:], 0.0), w=["bdm"])
        I("dve", lambda e: e.memset(bdm[0:64, 0:128], 1.0), w=["bdm"])
        I("dve", lambda e: e.memset(bdm[64:128, 128:256], 1.0), w=["bdm"])
        Dm(g1row[:], g1_d[0:1, :].partition_broadcast(128), w=["g1row"])
        Dm(mstage[:], maskf_d[:, :], w=["mstage"])
        I("dve", lambda e: e.tensor_copy(out=maskf[:], in_=mstage[:]), r=["mstage"], w=["maskf"])
        Dm(mstage[:], maskb_d[:, :], w=["mstage"])
        I("dve", lambda e: e.tensor_copy(out=maskb[:], in_=mstage[:]), r=["mstage"], w=["maskb"])
        Dm(cw[:], convw_d[:, :, :], w=["cw"])
        Dm(cb[:], convb_d[:, :], w=["cb"])
        Dm(rba[:], rgba_d[:, :, :], w=["rba"])
        Dm(rbx[:], rgbx_d[:, :, :], w=["rbx"])
        Dm(rlam[:], rglam_d[:, :, :], w=["rlam"])
        Dm(gbg[:], glabg_d[:, :, :], w=["gbg"])
        Dm(gng[:], glang_d[:, :], w=["gng"])
        I("act", lambda e: e.activation(out=coef[:], in_=rlam[:], func=AF.Exp, scale=-1.0), r=["rlam"], w=["coef"])
        I("act", lambda e: e.activation(out=coef[:], in_=coef[:], func=AF.Ln, bias=1.0), r=["coef"], w=["coef"])
        I("dve", lambda e: e.tensor_scalar_mul(out=coef[:], in0=coef[:], scalar1=-8.0), r=["coef"], w=["coef"])
        I("dve", lambda e: e.tensor_scalar_mul(out=ngbg[:], in0=gbg[:], scalar1=-1.0), r=["gbg"], w=["ngbg"])
        I("dve", lambda e: e.memset(wg[:], 0.0), w=["wg"])
        for d in range(2):
            Dm(wg[16 * d:16 * d + 16, d, :], glawg_d[d, :, :], w=["wg"])
        I("dve", lambda e: e.memset(wbd[:], 0.0), w=["wbd"])
        for gi, src in enumerate((rgwa_d, rgwx_d)):
            for d in range(2):
                for cc in range(4):
                    for h in range(2):
                        Dm(wbd[64 * h:64 * h + 64, gi, d, cc, 64 * h:64 * h + 64], src[d, 2 * cc + h, :, :], w=["wbd"])

        for b in range(2):
            with contextlib.ExitStack() as sbt:
                mgT = sb(sbt, "mgT", [128, 8, SEQ], BF16)
                qT = sb(sbt, "qT", [128, 2, SEQ], BF16)
                kT = sb(sbt, "kT", [128, 2, S], BF16)
                vtok = sb(sbt, "vtok", [128, NT, 512], BF16)
                lrT = sb(sbt, "lrT", [32, S])
                sR = contextlib.ExitStack()
                rxT = sb(sR, "rxT", [128, 4, S], BF16)
                with contextlib.ExitStack() as s1:
                    winb = sb(s1, "winb", [128, 8, INC], BF16)
                    wst0 = sb(s1, "wst0", [128, INC // 2])
                    wst = [wst0, wst0]
                    uT = [sb(s1, "uT%d" % i, [128, 8, 512], BF16) for i in range(2)]
                    xt = [sb(s1, "xt%d" % i, [128, D]) for i in range(2)]
                    pt0 = sb(s1, "pt0", [128, D])
                    pt = [pt0, pt0]
                    xn = [sb(s1, "xn%d" % i, [128, D], BF16) for i in range(2)]
                    sq = sb(s1, "sqj", [128, D], BF16)
                    ss = [sb(s1, "ss%d" % i, [128, 1]) for i in range(2)]
                    tp = [ps(s1, "tp%d" % i, [128, 8, 128], BF16) for i in range(2)]
                    pj = [ps(s1, "pj%d" % i, [128, 512]) for i in range(4)]
                    hw = INC // 2
                    for kc in range(8):
                        for hh in range(2):
                            w_ = wst[hh]
                            Dm(w_[:], win_d[kc * 128:(kc + 1) * 128, hh * hw:(hh + 1) * hw], w=["wst0"])
                            I("pool" if hh else "dve", lambda e, w_=w_, kc=kc, hh=hh: e.tensor_copy(
                                out=winb[:, kc, hh * hw:(hh + 1) * hw], in_=w_[:]), r=["wst0"], w=["winb"])
                    ngroups = 5
                    pjc = 0
                    for g in range(ngroups):
                        t0 = g * 4
                        nt = min(4, NT - t0)
                        ntok = nt * 128
                        u = uT[g % 2]
                        uk = "uT%d" % (g % 2)
                        for ti in range(nt):
                            t = t0 + ti
                            pb = t % 2
                            xk, xnk, ssk, tpk, ptk = "xt%d" % pb, "xn%d" % pb, "ss%d" % pb, "tp%d" % pb, "pt0"
                            if t < 2:
                                Dm(xt[pb][:], ctx_d[b, t * 128:(t + 1) * 128, :], w=[xk])
                                col = 2
                            else:
                                l0 = (t - 2) * 128
                                Dm(xt[pb][:], x_d[b, l0:l0 + 128, :], w=[xk])
                                Dm(pt[pb][:], pos_d[l0:l0 + 128, :], w=[ptk])
                                I("pool", lambda e, pb=pb: e.tensor_tensor(out=xt[pb][:], in0=xt[pb][:], in1=pt[pb][:], op=ALU.add),
                                  r=[xk, ptk], w=[xk])
                                col = b
                            I("act", lambda e, pb=pb: e.activation(out=sq[:], in_=xt[pb][:], func=AF.Square, accum_out=ss[pb][:]),
                              r=[xk], w=["sqj", ssk])
                            I("act", lambda e, pb=pb: e.activation(out=ss[pb][:], in_=ss[pb][:], func=AF.Sqrt, scale=1.0 / D, bias=epsb[:]),
                              r=[ssk, "epsb"], w=[ssk])
                            I("dve", lambda e, pb=pb: e.reciprocal(out=ss[pb][:], in_=ss[pb][:]), r=[ssk], w=[ssk])
                            I("dve", lambda e, pb=pb: e.scalar_tensor_tensor(
                                out=xn[pb][:], in0=xt[pb][:], scalar=ss[pb][:], in1=g1row[:], op0=ALU.mult, op1=ALU.mult),
                              r=[xk, ssk, "g1row"], w=[xnk])
                            for fc in range(8):
                                I("pe", lambda e, pb=pb, fc=fc: e.transpose(tp[pb][:, fc, :], xn[pb][:, fc * 128:(fc + 1) * 128], identb[:]),
                                  r=[xnk, "identb"], w=[tpk])
                            for fc in range(8):
                                I("dve" if fc % 2 else "pool" if False else "dve", lambda e, pb=pb, fc=fc, ti=ti, u=u, col=col: e.tensor_scalar(
                                    out=u[:, fc, ti * 128:(ti + 1) * 128], in0=tp[pb][:, fc, :],
                                    scalar1=modp[:, 1, fc, col:col + 1], scalar2=modp[:, 0, fc, col:col + 1],
                                    op0=ALU.mult, op1=ALU.add), r=[tpk, "modp"], w=[uk])
                        c0 = t0 * 128
                        lat0 = max(c0, CTX)
                        for cch in range(21):
                            cs_ = cch * 128
                            ncol = 128 if cch < 20 else 32
                            p = pj[pjc % 4]
                            pk = "pj%d" % (pjc % 4)
                            pjc += 1
                            if 12 <= cch < 16:
                                continue
                            lat_only = (4 <= cch < 10) or (16 <= cch < 20)
                            if lat_only and lat0 >= c0 + ntok:
                                continue
                            for kc in range(8):
                                I("pe", lambda e, kc=kc, p=p, u=u, cs_=cs_, ncol=ncol, ntok=ntok: e.matmul(
                                    p[0:ncol, 0:ntok], lhsT=winb[:, kc, cs_:cs_ + ncol], rhs=u[:, kc, 0:ntok],
                                    start=(kc == 0), stop=(kc == 7)), r=["winb", uk], w=[pk])
                            o0 = lat0 - c0
                            if cch < 4:
                                I("act", lambda e, p=p, cch=cch, c0=c0, ntok=ntok: e.copy(out=rxT[:, cch, c0:c0 + ntok], in_=p[:, 0:ntok]),
                                  r=[pk], w=["rxT"])
                            elif cch < 8:
                                I("act", lambda e, p=p, cch=cch, o0=o0, lat0=lat0, ntok=ntok: e.activation(
                                    out=mgT[:, cch - 4, lat0 - CTX:lat0 - CTX + ntok - o0], in_=p[:, o0:ntok], func=AF.Gelu),
                                  r=[pk], w=["mgT"])
                            elif cch < 10:
                                I("dve", lambda e, p=p, cch=cch, o0=o0, lat0=lat0, ntok=ntok: e.tensor_scalar_mul(
                                    out=qT[:, cch - 8, lat0 - CTX:lat0 - CTX + ntok - o0], in0=p[:, o0:ntok], scalar1=0.125),
                                  r=[pk], w=["qT"])
                            elif cch < 12:
                                I("dve", lambda e, p=p, cch=cch, c0=c0, ntok=ntok: e.tensor_copy(out=kT[:, cch - 10, c0:c0 + ntok], in_=p[:, 0:ntok]),
                                  r=[pk], w=["kT"])
                            elif cch < 20:
                                I("act", lambda e, p=p, cch=cch, o0=o0, lat0=lat0, ntok=ntok: e.activation(
                                    out=mgT[:, 4 + cch - 16, lat0 - CTX:lat0 - CTX + ntok - o0], in_=p[:, o0:ntok], func=AF.Silu),
                                  r=[pk], w=["mgT"])
                            else:
                                I("dve", lambda e, p=p, c0=c0, ntok=ntok: e.tensor_copy(out=lrT[:, c0:c0 + ntok], in_=p[0:32, 0:ntok]),
                                  r=[pk], w=["lrT"])
                        for ti in range(nt):
                            p = pj[pjc % 4]
                            pk = "pj%d" % (pjc % 4)
                            pjc += 1
                            for kc in range(8):
                                I("pe", lambda e, kc=kc, p=p, u=u, ti=ti: e.matmul(
                                    p[:, :], lhsT=u[:, kc, ti * 128:(ti + 1) * 128], rhs=winb[:, kc, 1536:2048],
                                    start=(kc == 0), stop=(kc == 7)), r=["winb", uk], w=[pk])
                            I("act", lambda e, p=p, t=t0 + ti: e.copy(out=vtok[:, t, :], in_=p[:, :]), r=[pk], w=["vtok"])
                    kb.barrier()

                if upto == "M1a":
                    sR.close()
                    return nc
                with contextlib.ExitStack() as s2:
                    xc = sb(s2, "xc", [128, S])
                    gr = sb(s2, "gr", [128, S])
                    gi_ = sb(s2, "gi", [128, S])
                    aa = sb(s2, "aa", [128, S])
                    bb = sb(s2, "bb", [128, S])
                    hf_ = sb(s2, "hf", [128, S])
                    hb_ = sb(s2, "hb", [128, S])
                    pg = [ps(s2, "pg%d" % i, [128, 512]) for i in range(4)]
                    pgc = 0
                    segs = ((0, CTX), (CTX, S))
                    for cc in range(4):
                        I("dve", lambda e, cc=cc: e.tensor_scalar(
                            out=xc[:], in0=rxT[:, cc, :], scalar1=cw[:, cc, 2:3], scalar2=cb[:, cc:cc + 1],
                            op0=ALU.mult, op1=ALU.add), r=["rxT", "cw", "cb"], w=["xc"])
                        for (a0, a1) in segs:
                            for j, sh in ((0, -2), (1, -1), (3, 1)):
                                lo = max(a0, a0 - sh)
                                hi = min(a1, a1 - sh)
                                I("dve", lambda e, cc=cc, j=j, sh=sh, lo=lo, hi=hi: e.scalar_tensor_tensor(
                                    out=xc[:, lo:hi], in0=rxT[:, cc, lo + sh:hi + sh], scalar=cw[:, cc, j:j + 1],
                                    in1=xc[:, lo:hi], op0=ALU.mult, op1=ALU.add), r=["rxT", "cw", "xc"], w=["xc"])
                        for d in range(2):
                            for gi, (gt, gk, bias_t) in enumerate(((gr, "gr", rba), (gi_, "gi", rbx))):
                                for g in range(5):
                                    c0 = g * 512
                                    n = min(512, S - c0)
                                    p = pg[pgc % 4]
                                    pk = "pg%d" % (pgc % 4)
                                    pgc += 1
                                    I("pe", lambda e, p=p, gi=gi, d=d, cc=cc, c0=c0, n=n: e.matmul(
                                        p[:, 0:n], lhsT=wbd[:, gi, d, cc, :], rhs=xc[:, c0:c0 + n], start=True, stop=True),
                                      r=["wbd", "xc"], w=[pk])
                                    I("act", lambda e, p=p, gt=gt, bias_t=bias_t, d=d, cc=cc, c0=c0, n=n: e.activation(
                                        out=gt[:, c0:c0 + n], in_=p[:, 0:n], func=AF.Sigmoid, bias=bias_t[:, d, cc:cc + 1]),
                                      r=[pk], w=[gk])
                            I("act", lambda e, d=d, cc=cc: e.activation(out=aa[:], in_=gr[:], func=AF.Exp, scale=coef[:, d, cc:cc + 1]),
                              r=["gr", "coef"], w=["aa"])
                            I("pool", lambda e: e.tensor_tensor(out=bb[:], in0=aa[:], in1=aa[:], op=ALU.mult), r=["aa"], w=["bb"])
                            I("act", lambda e: e.activation(out=bb[:], in_=bb[:], func=AF.Sqrt, scale=-1.0, bias=1.0), r=["bb"], w=["bb"])
                            I("pool", lambda e: e.tensor_tensor(out=bb[:], in0=bb[:], in1=gi_[:], op=ALU.mult), r=["bb", "gi"], w=["bb"])
                            I("pool", lambda e: e.tensor_tensor(out=bb[:], in0=bb[:], in1=xc[:], op=ALU.mult), r=["bb", "xc"], w=["bb"])
                            if d == 0:
                                I("dve", lambda e: e.tensor_tensor_scan(out=hf_[:], data0=aa[:], data1=bb[:], initial=0.0,
                                                                         op0=ALU.mult, op1=ALU.add), r=["aa", "bb"], w=["hf"])
                            else:
                                I("dve", lambda e: e.tensor_tensor_scan(out=hb_[:, 0:CTX][:, ::-1], data0=aa[:, 0:CTX][:, ::-1],
                                                                         data1=bb[:, 0:CTX][:, ::-1], initial=0.0,
                                                                         op0=ALU.mult, op1=ALU.add), r=["aa", "bb"], w=["hb"])
                                I("dve", lambda e: e.tensor_tensor_scan(out=hb_[:, CTX:S][:, ::-1], data0=aa[:, CTX:S][:, ::-1],
                                                                         data1=bb[:, CTX:S][:, ::-1], initial=hb_[:, 0:1],
                                                                         op0=ALU.mult, op1=ALU.add), r=["aa", "bb", "hb"], w=["hb"])
                        I("dve", lambda e: e.tensor_tensor(out=hf_[:, CTX:S], in0=hf_[:, CTX:S], in1=hb_[:, CTX:S], op=ALU.add),
                          r=["hf", "hb"], w=["hf"])
                        I("dve", lambda e, cc=cc: e.tensor_tensor(out=mgT[:, cc, :], in0=mgT[:, cc, :], in1=hf_[:, CTX:S], op=ALU.mult),
                          r=["hf", "mgT"], w=["mgT"])
                    kb.barrier()

                if upto == "M1b":
                    sR.close()
                    return nc
                sR.close()
                with contextlib.ExitStack() as s3:
                    otot = sb(s3, "otot", [128, 4, SEQ])
                    scr = sb(s3, "scr", [128, 2, S])
                    la = scr[:, 0, :]
                    bc = scr[:, 1, :]
                    dsg_all = scr[:, :, :].rearrange("p a n -> p (a n)").rearrange("p (c v) -> p c v", v=256)
                    eb = sb(s3, "eb", [128, S])
                    qd = sb(s3, "qd", [128, SEQ], BF16)
                    ki = sb(s3, "ki", [128, S], BF16)
                    kih = [sb(s3, "kih%d" % i, [128, S], BF16) for i in range(2)]
                    kit = [sb(s3, "kit%d" % i, [128, 128], BF16) for i in range(2)]
                    gam = sb(s3, "gam", [128, NT])
                    Sst = [sb(s3, "Sst%d" % i, [128, 256]) for i in range(2)]
                    Sbf_all = sb(s3, "Sbfa", [128, 16, 256], BF16)
                    scb_all = sb(s3, "scba", [128, 16, 256], BF16)
                    pz = [ps(s3, "pz%d" % i, [128, 512]) for i in range(1)]
                    ptr = [ps(s3, "ptr%d" % i, [128, 128], BF16) for i in range(2)]
                    pds = [ps(s3, "pds%d" % i, [128, 256]) for i in range(2)]
                    psc = [ps(s3, "psc%d" % i, [128, 256]) for i in range(2)]
                    po = [ps(s3, "po%d" % i, [128, 256]) for i in range(1)]
                    zc = 0
                    cn = 0
                    first_o = {0: True, 1: True}
                    for d in range(2):
                        order = list(range(NT)) if d == 0 else [1, 0] + list(range(NT - 1, 1, -1))
                        mk, mkk = (maskf, "maskf") if d == 0 else (maskb, "maskb")
                        for pr in range(2):
                            for g in range(5):
                                c0 = g * 512
                                n = min(512, S - c0)
                                p = pz[0]
                                pk = "pz0"
                                I("pe", lambda e, p=p, d=d, pr=pr, c0=c0, n=n: e.matmul(
                                    p[:, 0:n], lhsT=wg[:, d, pr * 128:(pr + 1) * 128], rhs=lrT[:, c0:c0 + n], start=True, stop=True),
                                  r=["wg", "lrT"], w=[pk])
                                I("act", lambda e, p=p, d=d, pr=pr, c0=c0, n=n: e.activation(
                                    out=la[:, c0:c0 + n], in_=p[:, 0:n], func=AF.Exp, scale=-1.0, bias=ngbg[:, d, pr:pr + 1]),
                                  r=[pk, "ngbg"], w=["la"])
                            I("act", lambda e: e.activation(out=la, in_=la, func=AF.Ln, bias=1.0), r=["la"], w=["la"])
                            for n_ in range(NT):
                                c0 = n_ * 128
                                if d == 0:
                                    I("dve", lambda e, c0=c0: e.tensor_tensor_scan(
                                        out=bc[:, c0:c0 + 128], data0=ones[:, :], data1=la[:, c0:c0 + 128], initial=0.0,
                                        op0=ALU.mult, op1=ALU.add), r=["la", "ones"], w=["bc"])
                                else:
                                    I("dve", lambda e, c0=c0: e.tensor_tensor_scan(
                                        out=bc[:, c0:c0 + 128][:, ::-1], data0=ones[:, :], data1=la[:, c0:c0 + 128][:, ::-1], initial=0.0,
                                        op0=ALU.mult, op1=ALU.add), r=["la", "ones"], w=["bc"])
                            I("act", lambda e: e.activation(out=eb[:], in_=bc, func=AF.Exp, scale=-1.0 / 16.0), r=["bc"], w=["eb"])
                            I("act", lambda e: e.activation(out=la, in_=bc, func=AF.Exp, scale=1.0 / 16.0), r=["bc"], w=["la"])
                            I("pool", lambda e, pr=pr: e.tensor_tensor(out=qd[:], in0=qT[:, pr, :], in1=eb[:, CTX:S], op=ALU.mult),
                              r=["qT", "eb"], w=["qd"])
                            I("dve", lambda e, pr=pr: e.tensor_tensor(out=ki[:], in0=kT[:, pr, :], in1=la, op=ALU.mult),
                              r=["kT", "la"], w=["ki"])
                            for h in range(2):
                                if h == 0:
                                    I("act", lambda e, h=h: e.mul(out=kih[h][:], in_=ki[:], mul=hm[:, h:h + 1]),
                                      r=["ki", "hm"], w=["kih%d" % h])
                                else:
                                    I("dve", lambda e, h=h: e.tensor_scalar_mul(out=kih[h][:], in0=ki[:], scalar1=hm[:, h:h + 1]),
                                      r=["ki", "hm"], w=["kih%d" % h])
                            gsrc = eb[:, 127::128] if d == 0 else eb[:, 0::128]
                            I("dve", lambda e, gsrc=gsrc: e.tensor_copy(out=gam[:], in_=gsrc), r=["eb"], w=["gam"])
                            I("dve", lambda e: e.memset(Sst[0][:], 0.0), w=["Sst0"])

                            def tr_(n_):
                                j = n_ % 2
                                I("pe", lambda e: e.transpose(ptr[j][:, :], ki[:, n_ * 128:(n_ + 1) * 128], identb[:]),
                                  r=["ki", "identb"], w=["ptr%d" % j])
                                I("act", lambda e: e.copy(out=kit[j][:], in_=ptr[j][:, :]), r=["ptr%d" % j], w=["kit%d" % j])

                            tr_(0)
                            for n_ in range(NT):
                                if n_ + 1 < NT:
                                    tr_(n_ + 1)
                                j = n_ % 2
                                dk = "la" if n_ < 9 else "bc"
                                I("pe", lambda e, j=j, n_=n_: e.matmul(
                                    pds[j][:, :], lhsT=kit[j][:, :], rhs=vtok[:, n_, pr * 256:(pr + 1) * 256], start=True, stop=True),
                                  r=["kit%d" % j, "vtok"], w=["pds%d" % j])
                                I("act", lambda e, j=j, n_=n_: e.mul(
                                    out=dsg_all[:, n_, :], in_=pds[j][:, :], mul=gam[:, n_:n_ + 1]),
                                  r=["pds%d" % j, "gam"], w=[dk])
                                if n_ >= 2:
                                    c0 = n_ * 128
                                    l0 = c0 - CTX
                                    for h in range(2):
                                        I("pe", lambda e, h=h, c0=c0, l0=l0, j=j: e.matmul(
                                            psc[j][:, h * 128:(h + 1) * 128], lhsT=kih[h][:, c0:c0 + 128],
                                            rhs=qd[:, l0:l0 + 128], start=True, stop=True),
                                          r=["kih%d" % h, "qd"], w=["psc%d" % j])
                                    I("dve", lambda e, j=j, n_=n_: e.tensor_tensor(out=scb_all[:, n_ - 2, :], in0=psc[j][:, :], in1=mk[:], op=ALU.mult),
                                      r=["psc%d" % j, mkk], w=["scba"])
                            for k_, n_ in enumerate(order):
                                a_, b_ = k_ % 2, (k_ + 1) % 2
                                dk = "la" if n_ < 9 else "bc"
                                if n_ >= 2:
                                    I("pool", lambda e, a_=a_, n_=n_: e.tensor_tensor(out=Sbf_all[:, n_ - 2, :], in0=Sst[a_][:], in1=bdm[:], op=ALU.mult),
                                      r=["Sst%d" % a_, "bdm"], w=["Sbfa"])
                                I("dve", lambda e, a_=a_, b_=b_, n_=n_: e.scalar_tensor_tensor(
                                    out=Sst[b_][:], in0=Sst[a_][:], scalar=gam[:, n_:n_ + 1], in1=dsg_all[:, n_, :],
                                    op0=ALU.mult, op1=ALU.add), r=["Sst%d" % a_, "gam", dk], w=["Sst%d" % b_])
                            pbanks = [(po[0], "po0"), (psc[0], "psc0"), (psc[1], "psc1")]
                            for n_ in range(2, NT):
                                l0 = n_ * 128 - CTX
                                pp, ppk = pbanks[n_ % len(pbanks)]
                                for h in range(2):
                                    hd = pr * 2 + h
                                    I("pe", lambda e, h=h, hd=hd, n_=n_, pp=pp: e.matmul(
                                        pp[:, h * 128:(h + 1) * 128], lhsT=vtok[:, n_, hd * 128:(hd + 1) * 128],
                                        rhs=scb_all[:, n_ - 2, h * 128:(h + 1) * 128], start=True, stop=False),
                                      r=["vtok", "scba"], w=[ppk])
                                    I("pe", lambda e, h=h, n_=n_, l0=l0, pp=pp: e.matmul(
                                        pp[:, h * 128:(h + 1) * 128], lhsT=Sbf_all[:, n_ - 2, h * 128:(h + 1) * 128],
                                        rhs=qd[:, l0:l0 + 128], start=False, stop=True),
                                      r=["Sbfa", "qd"], w=[ppk])
                                ov = otot[:, pr * 2:pr * 2 + 2, l0:l0 + 128]
                                pv_ = pp[:, :].rearrange("p (h t) -> p h t", h=2)
                                if d == 0:
                                    I("act", lambda e, ov=ov, pv_=pv_: e.copy(out=ov, in_=pv_), r=[ppk], w=["otot"])
                                else:
                                    I("dve", lambda e, ov=ov, pv_=pv_: e.tensor_tensor(out=ov, in0=ov, in1=pv_, op=ALU.add),
                                      r=[ppk, "otot"], w=["otot"])
                    sqo = sb(s3, "sqo", [128, 512])
                    rs = sb(s3, "rs", [128, 512])
                    tmpo = sb(s3, "tmpo", [128, 512])
                    for hd in range(4):
                        for g in range(4):
                            c0 = g * 512
                            p = pz[0]
                            pk = "pz0"
                            I("act", lambda e, hd=hd, c0=c0: e.activation(out=sqo[:], in_=otot[:, hd, c0:c0 + 512], func=AF.Square),
                              r=["otot"], w=["sqo"])
                            I("pe", lambda e, p=p: e.matmul(p[:, :], lhsT=ones[:, :], rhs=sqo[:], start=True, stop=True),
                              r=["ones", "sqo"], w=[pk])
                            I("act", lambda e, p=p: e.activation(out=rs[:], in_=p[:, :], func=AF.Sqrt, scale=1.0 / 128.0, bias=epsb[:]),
                              r=[pk, "epsb"], w=["rs"])
                            I("dve", lambda e: e.reciprocal(out=rs[:], in_=rs[:]), r=["rs"], w=["rs"])
                            I("dve", lambda e, hd=hd, c0=c0: e.scalar_tensor_tensor(
                                out=tmpo[:], in0=otot[:, hd, c0:c0 + 512], scalar=gng[:, 0:1], in1=rs[:], op0=ALU.mult, op1=ALU.mult),
                              r=["otot", "gng", "rs"], w=["tmpo"])
                            I("pool", lambda e, hd=hd, c0=c0: e.tensor_tensor(
                                out=mgT[:, 4 + hd, c0:c0 + 512], in0=mgT[:, 4 + hd, c0:c0 + 512], in1=tmpo[:], op=ALU.mult),
                              r=["tmpo", "mgT"], w=["mgT"])
                    kb.barrier()

                if upto == "M1c":
                    return nc
                with contextlib.ExitStack() as s4:
                    woutb = sb(s4, "woutb", [128, 8, D], BF16)
                    wst2 = [sb(s4, "wso%d" % i, [128, D]) for i in range(2)]
                    xt = [sb(s4, "xo%d" % i, [128, D]) for i in range(2)]
                    pt = [sb(s4, "po_%d" % i, [128, D]) for i in range(2)]
                    tm = [sb(s4, "tm%d" % i, [128, D]) for i in range(2)]
                    pw = [ps(s4, "pw%d" % i, [128, 2, 512]) for i in range(2)]
                    for kc in range(8):
                        w_ = wst2[kc % 2]
                        Dm(w_[:], wout_d[kc * 128:(kc + 1) * 128, :], w=["wso%d" % (kc % 2)])
                        I("dve", lambda e, w_=w_, kc=kc: e.tensor_copy(out=woutb[:, kc, :], in_=w_[:]), r=["wso%d" % (kc % 2)], w=["woutb"])
                    for t in range(16):
                        pb = t % 2
                        l0 = t * 128
                        Dm(xt[pb][:], x_d[b, l0:l0 + 128, :], w=["xo%d" % pb])
                        Dm(pt[pb][:], pos_d[l0:l0 + 128, :], w=["po_%d" % pb])
                        I("pool", lambda e, pb=pb: e.tensor_tensor(out=xt[pb][:], in0=xt[pb][:], in1=pt[pb][:], op=ALU.add),
                          r=["xo%d" % pb, "po_%d" % pb], w=["xo%d" % pb])
                        for hf in range(2):
                            for mc in range(8):
                                I("pe", lambda e, pb=pb, hf=hf, mc=mc, l0=l0: e.matmul(
                                    pw[pb][:, hf, :], lhsT=mgT[:, mc, l0:l0 + 128], rhs=woutb[:, mc, hf * 512:(hf + 1) * 512],
                                    start=(mc == 0), stop=(mc == 7)), r=["mgT", "woutb"], w=["pw%d" % pb])
                        I("dve", lambda e, pb=pb: e.tensor_tensor(
                            out=tm[pb][:], in0=pw[pb][:, :, :].rearrange("p a n -> p (a n)"), in1=g1gate[:, b, :], op=ALU.mult),
                          r=["pw%d" % pb, "g1gate"], w=["tm%d" % pb])
                        I("pool", lambda e, pb=pb: e.tensor_tensor(out=tm[pb][:], in0=tm[pb][:], in1=xt[pb][:], op=ALU.add),
                          r=["tm%d" % pb, "xo%d" % pb], w=["tm%d" % pb])
                        Dm(x1_d[b * SEQ + l0:b * SEQ + l0 + 128, :], tm[pb][:], r=["tm%d" % pb], w=["x1s"], q="pool")
                    kb.barrier()
                if upto == "M1d":
                    return nc

    if upto == "M1":
        return nc
    with contextlib.ExitStack() as sc:
        uin = [sb(sc, "uin%d" % i, [128, 4, D]) for i in range(2)]
        ubf = [sb(sc, "ubf%d" % i, [128, 4, D], BF16) for i in range(2)]
        uts = [sb(sc, "uts%d" % i, [128, 8, 512], BF16) for i in range(2)]
        vin = [sb(sc, "vin%d" % i, [128, 4, D]) for i in range(2)]
        vbf = [sb(sc, "vbf%d" % i, [128, 4, D], BF16) for i in range(2)]
        ptc = [ps(sc, "ptc%d" % i, [128, 8, 128], BF16) for i in range(4)]
        tcn = 0
        for g in range(32):
            j = g % 2
            e0 = g * 512
            Dm(uin[j][:], pu_d[e0:e0 + 512, :].rearrange("(c p) n -> p c n", p=128), w=["uin%d" % j])
            Dm(vin[j][:], pv_d[e0:e0 + 512, :].rearrange("(c p) n -> p c n", p=128), w=["vin%d" % j])
            I("dve", lambda e, j=j: e.tensor_copy(out=ubf[j][:], in_=uin[j][:]), r=["uin%d" % j], w=["ubf%d" % j])
            if g % 2 == 0:
                I("dve", lambda e, j=j: e.tensor_copy(out=vbf[j][:], in_=vin[j][:]), r=["vin%d" % j], w=["vbf%d" % j])
            else:
                I("act", lambda e, j=j: e.copy(out=vbf[j][:], in_=vin[j][:]), r=["vin%d" % j], w=["vbf%d" % j])
            Dm(vs_d[e0:e0 + 512, :].rearrange("(c p) n -> p c n", p=128), vbf[j][:], r=["vbf%d" % j], w=["v_scr"], q="pool")
            for c in range(4):
                p = ptc[tcn % 4]
                pk = "ptc%d" % (tcn % 4)
                tcn += 1
                for fc in range(8):
                    I("pe", lambda e, p=p, j=j, c=c, fc=fc: e.transpose(p[:, fc, :], ubf[j][:, c, fc * 128:(fc + 1) * 128], identb[:]),
                      r=["ubf%d" % j, "identb"], w=[pk])
                I("act", lambda e, p=p, j=j, c=c: e.copy(out=uts[j][:, :, c * 128:(c + 1) * 128], in_=p[:, :, :]),
                  r=[pk], w=["uts%d" % j])
            for hh in range(2):
                Dm(ut_d[2 * g + hh, :, :, :], uts[j][:, :, hh * 256:(hh + 1) * 256], r=["uts%d" % j], w=["ut_scr"], q="pool")
        kb.barrier()

    if upto == "C":
        return nc
    with contextlib.ExitStack() as sp_:
        g2row = sb(sp_, "g2row", [128, D])
        gfrow = sb(sp_, "gfrow", [128, D])
        g2gate = sb(sp_, "g2gate", [128, D])
        wqb = sb(sp_, "wqb", [128, 8, 2048], BF16)
        keysb = sb(sp_, "keysb", [128, 16, 128], BF16)
        WT = sb(sp_, "WT", [128, TB, 128], BF16)
        NBUF = 4
        utb = [sb(sp_, "utb%d" % i, [128, 8, 256], BF16) for i in range(NBUF)]
        vtb = [sb(sp_, "vtb%d" % i, [128, 2, D], BF16) for i in range(NBUF)]
        x1s_ = sb(sp_, "x1s_", [128, D])
        u2T = [sb(sp_, "u2T%d" % i, [128, 8, TB], BF16) for i in range(2)]
        q2T = sb(sp_, "q2T", [128, 16, TB], BF16)
        sc_ = sb(sp_, "sc", [128, 16, 128])
        eq = sc_[:, :, :].rearrange("p (h a) (b c) -> p h (a b) c", a=2, c=16)
        cs = sb(sp_, "cs", [128, 8, 256])
        A16 = [sc_[:, 8 * i:8 * i + 4, :].rearrange("p a n -> p (a n)").bitcast(BF16).rearrange("p (t i) -> p t i", i=64) for i in range(2)]
        B16 = [cs[:, 4 * i:4 * i + 4, :].rearrange("p a n -> p (a n)").bitcast(BF16).rearrange("p (t i) -> p t i", i=128) for i in range(2)]
        tv = sb(sp_, "tv", [128, 16, 16])
        tix = sb(sp_, "tix", [128, 16, 16], U32)
        tif = sb(sp_, "tif", [128, 16, 16])
        sv = sb(sp_, "sv", [128, 8, 16])
        sve = sb(sp_, "sve", [128, 8, 16])
        spx = sb(sp_, "spx", [128, 8, 16], U32)
        spi = sb(sp_, "spi", [128, 8, 16], U32)
        pf = sb(sp_, "pf", [128, 8, 16])
        qf = sb(sp_, "qf", [128, 8, 16])
        If_ = sb(sp_, "If", [128, 128])
        Jf_ = sb(sp_, "Jf", [128, 128])
        Wf_ = sb(sp_, "Wf", [128, 128])
        zs = sb(sp_, "zs", [128, 8])
        IT = sb(sp_, "IT", [128, 2, 128])
        JT = sb(sp_, "JT", [128, 2, 128])
        WTr = sb(sp_, "WTr", [128, 2, 128])
        NSL = 3
        gl = [sb(sp_, "gl%d" % i, [128, 2 * TB]) for i in range(NSL)]
        awt = [sb(sp_, "awt%d" % i, [128, 2 * TB], BF16) for i in range(NSL)]
        sq2 = sb(sp_, "sq2", [128, D])
        ss2 = sb(sp_, "ss2", [128, 1])
        ss3 = sb(sp_, "ss3", [128, 1])
        py = [ps(sp_, "py%d" % i, [128, 2, 512]) for i in range(2)]
        pg_ = [ps(sp_, "pg%d" % i, [128, 512]) for i in range(3)]
        pb_ = [ps(sp_, "pb%d" % i, [128, 512]) for i in range(1)]
        if dbg:
            print("phase P sbuf bytes remaining:", nc.sbuf_bytes_remaining)

        Dm(g2row[:], g2_d[0:1, :].partition_broadcast(128), w=["g2row"])
        Dm(gfrow[:], gf_d[0:1, :].partition_broadcast(128), w=["gfrow"])
        for kc in range(8):
            for hh in range(2):
                Dm(sq2[:], wq_d[kc * 128:(kc + 1) * 128, hh * 1024:(hh + 1) * 1024], w=["sq2"])
                I("dve", lambda e, kc=kc, hh=hh: e.tensor_copy(out=wqb[:, kc, hh * 1024:(hh + 1) * 1024], in_=sq2[:]), r=["sq2"], w=["wqb"])
        for hh in range(2):
            Dm(sq2[:], keysT_d[:, hh * 8:(hh + 1) * 8, :].rearrange("p a n -> p (a n)"), w=["sq2"])
            I("dve", lambda e, hh=hh: e.tensor_copy(out=keysb[:, hh * 8:(hh + 1) * 8, :].rearrange("p a n -> p (a n)"), in_=sq2[:]), r=["sq2"], w=["keysb"])

        NG = 64
        nblk = 2 * SEQ // TB
        if upto is not None and upto.startswith('P'):
            nblk = int(upto[1:])
        NBB = SEQ // TB

        def load_group(gi):
            j = gi % NBUF
            e0 = (gi % NG) * 256
            Dm(utb[j][:], ut_d[gi % NG, :, :, :], r=["ut_scr"], w=["utb%d" % j])
            Dm(vtb[j][:], vs_d[e0:e0 + 256, :].rearrange("(c p) n -> p c n", p=128), r=["v_scr"], w=["vtb%d" % j])

        total_groups = nblk * NG
        for g_ in range(min(NBUF, total_groups)):
            load_group(g_)
        pbc = [0]

        def nextpb():
            k = 0
            pbc[0] += 1
            return pb_[k], "pb%d" % k

        LOOK = 2
        PULL = 3
        SC = ["sc_%d" % i for i in range(16)]
        CS = ["cs_%d" % i for i in range(8)]
        TV = ["tv_%d" % i for i in range(16)]
        TIX = ["tix_%d" % i for i in range(16)]
        SV = ["sv_%d" % i for i in range(8)]
        SPX = ["spx_%d" % i for i in range(8)]
        SCH = [SC[0:4], SC[8:12]]
        CSH = [CS[0:4], CS[4:8]]

        def stageS(tb_):
            b = tb_ // NBB
            r0 = tb_ * TB
            u2 = u2T[tb_ % 2]
            uk = "u2T%d" % (tb_ % 2)
            for ti in range(2):
                Dm(x1s_[:], x1_d[r0 + ti * 128:r0 + (ti + 1) * 128, :], r=["x1s"], w=["x1s_"])
                I("act", lambda e: e.activation(out=sq2[:], in_=x1s_[:], func=AF.Square, accum_out=ss2[:]),
                  r=["x1s_"], w=["sq2", "ss2"])
                I("act", lambda e: e.activation(out=ss2[:], in_=ss2[:], func=AF.Sqrt, scale=1.0 / D, bias=epsb[:]),
                  r=["ss2", "epsb"], w=["ss2"])
                I("dve", lambda e: e.reciprocal(out=ss2[:], in_=ss2[:]), r=["ss2"], w=["ss2"])
                I("dve", lambda e: e.scalar_tensor_tensor(
                    out=sq2[:], in0=x1s_[:], scalar=ss2[:], in1=g2row[:], op0=ALU.mult, op1=ALU.mult),
                  r=["x1s_", "ss2", "g2row"], w=["sq2"])
                yield
                for hf in range(2):
                    p, pk = nextpb()
                    for f4 in range(4):
                        fc = hf * 4 + f4
                        I("pe", lambda e, p=p, fc=fc, f4=f4: e.transpose(p[:, f4 * 128:(f4 + 1) * 128], sq2[:, fc * 128:(fc + 1) * 128], ident[:]),
                          r=["sq2", "ident"], w=[pk])
                    yield
                    yield
                    for f4 in range(4):
                        fc = hf * 4 + f4
                        I("dve", lambda e, p=p, fc=fc, f4=f4, ti=ti, b=b: e.tensor_scalar(
                            out=u2[:, fc, ti * 128:(ti + 1) * 128], in0=p[:, f4 * 128:(f4 + 1) * 128],
                            scalar1=modp[:, 4, fc, b:b + 1], scalar2=modp[:, 3, fc, b:b + 1], op0=ALU.mult, op1=ALU.add),
                          r=[pk, "modp"], w=[uk])
                    yield
            for hp in range(16):
                p, pk = nextpb()
                for kc in range(8):
                    I("pe", lambda e, p=p, hp=hp, kc=kc: e.matmul(
                        p[:, 0:TB], lhsT=wqb[:, kc, hp * 128:(hp + 1) * 128], rhs=u2[:, kc, :], start=(kc == 0), stop=(kc == 7)),
                      r=["wqb", uk], w=[pk])
                yield
                yield
                I("act", lambda e, p=p, hp=hp: e.copy(out=q2T[:, hp, :], in_=p[:, 0:TB]), r=[pk], w=["q2T"])
                yield
            for ti in range(2):
                for qd_ in range(4):
                    p, pk = nextpb()
                    for hh in range(4):
                        hp = qd_ * 4 + hh
                        I("pe", lambda e, p=p, hp=hp, hh=hh, ti=ti: e.matmul(
                            p[:, hh * 128:(hh + 1) * 128], lhsT=q2T[:, hp, ti * 128:(ti + 1) * 128], rhs=keysb[:, hp, :],
                            start=True, stop=True), r=["q2T", "keysb"], w=[pk])
                    yield
                    yield
                    I("act", lambda e, p=p, qd_=qd_: e.copy(
                        out=sc_[:, qd_ * 4:(qd_ + 1) * 4, :], in_=p[:, :].rearrange("p (a n) -> p a n", n=128)),
                      r=[pk], w=SC[qd_ * 4:(qd_ + 1) * 4])
                    yield
                for hp in range(16):
                    I("dve", lambda e, hp=hp: e.max(out=tv[:, hp, 0:8], in_=sc_[:, hp, :]), r=["sc_%d" % hp], w=["tv_%d" % hp])
                    if hp % 4 == 3:
                        yield
                for hp in range(16):
                    I("dve", lambda e, hp=hp: e.max_index(out=tix[:, hp, 0:8], in_max=tv[:, hp, 0:8], in_values=sc_[:, hp, :]),
                      r=["sc_%d" % hp, "tv_%d" % hp], w=["tix_%d" % hp])
                    if hp % 4 == 3:
                        yield
                for hp in range(16):
                    I("dve", lambda e, hp=hp: e.match_replace(out=sc_[:, hp, :], in_to_replace=tv[:, hp, 0:8], in_values=sc_[:, hp, :], imm_value=NEG),
                      r=["tv_%d" % hp], w=["sc_%d" % hp])
                    if hp % 4 == 3:
                        yield
                for hp in range(16):
                    I("dve", lambda e, hp=hp: e.max(out=tv[:, hp, 8:16], in_=sc_[:, hp, :]), r=["sc_%d" % hp], w=["tv_%d" % hp])
                    if hp % 4 == 3:
                        yield
                for hp in range(16):
                    I("dve", lambda e, hp=hp: e.max_index(out=tix[:, hp, 8:16], in_max=tv[:, hp, 8:16], in_values=sc_[:, hp, :]),
                      r=["sc_%d" % hp, "tv_%d" % hp], w=["tix_%d" % hp])
                    if hp % 4 == 3:
                        yield
                I("dve", lambda e: e.tensor_copy(out=tif[:], in_=tix[:]), r=TIX, w=["tif"])
                tv4 = tv[:, :, :].rearrange("p (h s) k -> p h s k", s=2)
                tif4 = tif[:, :, :].rearrange("p (h s) k -> p h s k", s=2)
                I("dve", lambda e, tv4=tv4: e.tensor_tensor(
                    out=cs[:, :, :].rearrange("p h (a c) -> p h a c", c=16),
                    in0=tv4[:, :, 0, :].unsqueeze(3).to_broadcast([128, 8, 16, 16]),
                    in1=tv4[:, :, 1, :].unsqueeze(2).to_broadcast([128, 8, 16, 16]), op=ALU.add), r=TV, w=CS)
                yield
                for h in range(8):
                    I("dve", lambda e, h=h: e.max(out=sv[:, h, 0:8], in_=cs[:, h, :]), r=["cs_%d" % h], w=["sv_%d" % h])
                yield
                for h in range(8):
                    I("dve", lambda e, h=h: e.max_index(out=spx[:, h, 0:8], in_max=sv[:, h, 0:8], in_values=cs[:, h, :]),
                      r=["cs_%d" % h, "sv_%d" % h], w=["spx_%d" % h])
                yield
                for h in range(8):
                    I("dve", lambda e, h=h: e.match_replace(out=cs[:, h, :], in_to_replace=sv[:, h, 0:8], in_values=cs[:, h, :], imm_value=NEG),
                      r=["sv_%d" % h], w=["cs_%d" % h])
                yield
                for h in range(8):
                    I("dve", lambda e, h=h: e.max(out=sv[:, h, 8:16], in_=cs[:, h, :]), r=["cs_%d" % h], w=["sv_%d" % h])
                yield
                for h in range(8):
                    I("dve", lambda e, h=h: e.max_index(out=spx[:, h, 8:16], in_max=sv[:, h, 8:16], in_values=cs[:, h, :]),
                      r=["cs_%d" % h, "sv_%d" % h], w=["spx_%d" % h])
                yield
                I("dve", lambda e: e.tensor_single_scalar(out=spi[:], in_=spx[:], scalar=4, op=ALU.logical_shift_right), r=SPX, w=["spi"])
                I("dve", lambda e: e.tensor_copy(out=pf[:], in_=spi[:]), r=["spi"], w=["pf"])
                I("dve", lambda e: e.tensor_single_scalar(out=spi[:], in_=spx[:], scalar=15, op=ALU.bitwise_and), r=SPX + ["pf"], w=["spi"])
                I("dve", lambda e: e.tensor_copy(out=qf[:], in_=spi[:]), r=["spi"], w=["qf"])
                yield
                io16 = iota[:, 0:16].unsqueeze(1).unsqueeze(1).to_broadcast([128, 8, 16, 16])
                for (rf, side, dst, dk) in ((pf, 0, If_, "If"), (qf, 1, Jf_, "Jf")):
                    I("dve", lambda e, rf=rf: e.tensor_tensor(
                        out=eq, in0=rf[:, :, :].unsqueeze(3).to_broadcast([128, 8, 16, 16]), in1=io16, op=ALU.is_equal),
                      r=["pf", "qf", "iota"], w=SC)
                    yield
                    I("dve", lambda e, side=side, tif4=tif4: e.tensor_tensor(
                        out=eq, in0=eq, in1=tif4[:, :, side, :].unsqueeze(2).to_broadcast([128, 8, 16, 16]), op=ALU.mult),
                      r=SC + ["tif"], w=SC)
                    yield
                    I("dve", lambda e, dst=dst: e.tensor_reduce(
                        out=dst[:, :].rearrange("p (h k) -> p h k", k=16), in_=eq, axis=AX.X, op=ALU.add), r=SC, w=[dk])
                    yield
                I("dve", lambda e: e.tensor_tensor(out=sve[:], in0=sv[:], in1=sv[:, :, 0:1].to_broadcast([128, 8, 16]), op=ALU.subtract),
                  r=SV, w=["sve"])
                I("act", lambda e: e.activation(out=sve[:], in_=sve[:], func=AF.Exp), r=["sve"], w=["sve"])
                I("dve", lambda e: e.tensor_reduce(out=zs[:], in_=sve[:], axis=AX.X, op=ALU.add), r=["sve"], w=["zs"])
                I("dve", lambda e: e.reciprocal(out=zs[:], in_=zs[:]), r=["zs"], w=["zs"])
                I("dve", lambda e: e.tensor_tensor(
                    out=Wf_[:, :].rearrange("p (h k) -> p h k", k=16), in0=sve[:], in1=zs[:, :].unsqueeze(2).to_broadcast([128, 8, 16]),
                    op=ALU.mult), r=["sve", "zs"], w=["Wf"])
                yield
                for (src, sk, dst, dk) in ((If_, "If", IT, "IT"), (Jf_, "Jf", JT, "JT"), (Wf_, "Wf", WTr, "WTr")):
                    p, pk = nextpb()
                    yield
                    I("pe", lambda e, p=p, src=src: e.transpose(p[:, 0:128], src[:], ident[:]), r=[sk, "ident"], w=[pk])
                    yield
                    yield
                    I("act", lambda e, p=p, dst=dst, ti=ti: e.copy(out=dst[:, ti, :], in_=p[:, 0:128]), r=[pk], w=[dk])
                yield

        def stageW(tb_, half):
            iob = iota[:, :].unsqueeze(1).to_broadcast([128, 16, 128])
            ioh = iota[:, half * 64:(half + 1) * 64].unsqueeze(1).to_broadcast([128, 16, 64])
            wkey = "WT%d" % half

            def gen(bi):
                ti = bi // 8
                j2 = bi % 2
                t0 = (bi % 8) * 16
                I("dve", lambda e: e.tensor_tensor(
                    out=A16[j2], in0=ioh, in1=IT[:, ti, t0:t0 + 16].unsqueeze(2).to_broadcast([128, 16, 64]), op=ALU.is_equal),
                  r=["iota", "IT"], w=SCH[j2])
                I("dve", lambda e: e.tensor_tensor(
                    out=B16[j2], in0=iob, in1=JT[:, ti, t0:t0 + 16].unsqueeze(2).to_broadcast([128, 16, 128]), op=ALU.is_equal),
                  r=["iota", "JT"], w=CSH[j2])
                I("pool", lambda e: e.tensor_tensor(
                    out=A16[j2], in0=A16[j2], in1=WTr[:, ti, t0:t0 + 16].unsqueeze(2).to_broadcast([128, 16, 64]), op=ALU.mult),
                  r=SCH[j2] + ["WTr"], w=SCH[j2])

            def mm(bi, t8):
                ti = bi // 8
                j2 = bi % 2
                t0 = (bi % 8) * 16
                p, pk = nextpb()
                for tt in range(8):
                    tl = t8 * 8 + tt
                    I("pe", lambda e, tt=tt, tl=tl: e.matmul(
                        p[:, tt * 64:(tt + 1) * 64], lhsT=B16[j2][:, tl, :], rhs=A16[j2][:, tl, :], start=True, stop=True),
                      r=SCH[j2] + CSH[j2], w=[pk])
                return p, pk, ti * 128 + t0 + t8 * 8

            def cp(p, pk, tg0):
                I("act", lambda e: e.copy(
                    out=WT[:, tg0:tg0 + 8, half * 64:(half + 1) * 64], in_=p[:, :].rearrange("p (t i) -> p t i", i=64)),
                  r=[pk], w=[wkey])

            gen(0)
            yield
            for bi in range(16):
                if bi + 1 < 16:
                    gen(bi + 1)
                yield
                a = mm(bi, 0)
                yield
                yield
                cp(*a)
                yield
                a = mm(bi, 1)
                yield
                yield
                cp(*a)
                yield

        def gemm_and_epilogue(tb_, bg):
            b = tb_ // NBB
            u2 = u2T[tb_ % 2]
            uk = "u2T%d" % (tb_ % 2)
            slots = {}

            def pull(ic, n):
                for ent in bg:
                    if ent[0] is None:
                        continue
                    if ent[1] > ic:
                        return
                    while n > 0:
                        try:
                            next(ent[0])
                            n -= 1
                        except StopIteration:
                            ent[0] = None
                            break
                    if n == 0:
                        return

            def drain(pred):
                for ent in bg:
                    if ent[0] is not None and pred(ent):
                        for _ in ent[0]:
                            pass
                        ent[0] = None

            def emitU(pr):
                gi = tb_ * NG + pr
                j = gi % NBUF
                sl = pr % NSL
                p = pg_[sl]
                pk = "pgs%d" % sl
                slots[pr] = sl
                for c in range(2):
                    for kc in range(8):
                        I("pe", lambda e, p=p, j=j, c=c, kc=kc: e.matmul(
                            p[:, c * TB:(c + 1) * TB], lhsT=utb[j][:, kc, c * 128:(c + 1) * 128], rhs=u2[:, kc, :],
                            start=(kc == 0), stop=(kc == 7)), r=["utb%d" % j, uk], w=[pk])
                I("act", lambda e, p=p, sl=sl: e.activation(out=gl[sl][:], in_=p[:, :], func=AF.Gelu), r=[pk], w=["gl%d" % sl])
                I("pool", lambda e, sl=sl, pr=pr: e.tensor_tensor(
                    out=awt[sl][:, :].rearrange("p (c t) -> p c t", c=2), in0=gl[sl][:, :].rearrange("p (c t) -> p c t", c=2),
                    in1=WT[:, :, 2 * pr:2 * pr + 2].rearrange("p t i -> p i t"), op=ALU.mult),
                  r=["gl%d" % sl, "WT%d" % (pr // 32)], w=["awt%d" % sl])

            def emitV(pr):
                gi = tb_ * NG + pr
                j = gi % NBUF
                sl = slots.pop(pr)
                for c in range(2):
                    ic = 2 * pr + c
                    for ti in range(2):
                        for hf in range(2):
                            I("pe", lambda e, sl=sl, ti=ti, hf=hf, j=j, c=c, ic=ic: e.matmul(
                                py[ti][:, hf, :], lhsT=awt[sl][:, c * TB + ti * 128:c * TB + (ti + 1) * 128],
                                rhs=vtb[j][:, c, hf * 512:(hf + 1) * 512],
                                start=(ic == 0), stop=(ic == 127)), r=["awt%d" % sl, "vtb%d" % j], w=["py%d" % ti])
                if gi + NBUF < total_groups:
                    load_group(gi + NBUF)

            for pr in range(-LOOK, 64):
                if pr + LOOK < 64:
                    nu = pr + LOOK
                    if nu == 32:
                        drain(lambda ent: ent[2] <= 64)
                    emitU(nu)
                    if pr >= 0:
                        pull(2 * pr, PULL)
                if pr >= 0:
                    emitV(pr)
                    pull(2 * pr, PULL)
            drain(lambda ent: True)
            for ti in range(2):
                r0 = tb_ * TB + ti * 128
                Dm(x1s_[:], x1_d[r0:r0 + 128, :], r=["x1s"], w=["x1s_"])
                I("dve", lambda e, ti=ti: e.tensor_tensor(
                    out=sq2[:], in0=py[ti][:, :, :].rearrange("p a n -> p (a n)"), in1=g2gate[:], op=ALU.mult),
                  r=["py%d" % ti, "g2gate"], w=["sq2"])
                I("pool", lambda e: e.tensor_tensor(out=sq2[:], in0=sq2[:], in1=x1s_[:], op=ALU.add), r=["sq2", "x1s_"], w=["sq2"])
                I("act", lambda e: e.activation(out=x1s_[:], in_=sq2[:], func=AF.Square, accum_out=ss3[:]), r=["sq2"], w=["x1s_", "ss3"])
                I("act", lambda e: e.activation(out=ss3[:], in_=ss3[:], func=AF.Sqrt, scale=1.0 / D, bias=epsb[:]),
                  r=["ss3", "epsb"], w=["ss3"])
                I("dve", lambda e: e.reciprocal(out=ss3[:], in_=ss3[:]), r=["ss3"], w=["ss3"])
                I("dve", lambda e: e.scalar_tensor_tensor(
                    out=x1s_[:], in0=sq2[:], scalar=ss3[:], in1=gfrow[:], op0=ALU.mult, op1=ALU.mult),
                  r=["sq2", "ss3", "gfrow"], w=["x1s_"])
                l0 = (tb_ % NBB) * TB + ti * 128
                Dm(out_d[b, l0:l0 + 128, :], x1s_[:], r=["x1s_"], w=["out"])

        Dm(g2gate[:], grow_d[2:3, :].partition_broadcast(128), r=["grow_scr"], w=["g2gate"])
        for _ in stageS(0):
            pass
        if upto == "S0":
            kb.barrier()
            return nc
        for _ in stageW(0, 0):
            pass
        if upto == "S1":
            kb.barrier()
            return nc
        for tb_ in range(nblk):
            bg = [[stageW(tb_, 1), 0, 64]]
            if tb_ + 1 < nblk:
                bg.append([stageS(tb_ + 1), 0, 999])
                bg.append([stageW(tb_ + 1, 0), 64, 999])
            gemm_and_epilogue(tb_, bg)
            if tb_ + 1 < nblk and (tb_ + 1) % NBB == 0:
                Dm(g2gate[:], grow_d[2 + (tb_ + 1) // NBB:3 + (tb_ + 1) // NBB, :].partition_broadcast(128), r=["grow_scr"], w=["g2gate"])
        kb.barrier(engines=("sp",))
    return nc


def _pos_table():
    rows = SEQ // 64
    r, col = np.meshgrid(np.arange(rows, dtype=np.float32), np.arange(64, dtype=np.float32), indexing="ij")
    quarter = D // 4
    omega = (np.float32(10000.0) ** (-np.arange(quarter, dtype=np.float32) / np.float32(quarter))).astype(np.float32)

    def emb(p):
        ang = p.reshape(-1)[:, None].astype(np.float32) * omega[None, :]
        return np.concatenate([np.sin(ang), np.cos(ang)], axis=-1)

    return np.concatenate([emb(r), emb(col)], axis=-1).astype(np.float32)


_NC_CACHE = {}


def _fm(v, nchunk):
    return np.ascontiguousarray(np.asarray(v, np.float32).reshape(nchunk, 128).T)


def kernel(x, c, ctx, c_ctx, ada_w, ada_b, norm1_g, w_in, conv_w, conv_b, rg_w_a, rg_b_a, rg_w_x, rg_b_x, rg_lambda,
           gla_w_g, gla_b_g, gla_norm_g, w_out, norm2_g, peer_w_q, peer_keys, peer_u, peer_v, final_norm_g):
    if "nc" not in _NC_CACHE:
        _NC_CACHE["nc"] = build()
    nc = _NC_CACHE["nc"]
    in_maps = make_in_maps(x, c, ctx, c_ctx, ada_w, ada_b, norm1_g, w_in, conv_w, conv_b, rg_w_a, rg_b_a, rg_w_x, rg_b_x, rg_lambda,
                           gla_w_g, gla_b_g, gla_norm_g, w_out, norm2_g, peer_w_q, peer_keys, peer_u, peer_v, final_norm_g)
    res = run_bass_kernel_spmd(nc, in_maps, core_ids=list(range(NCORES)))
    out = np.concatenate([np.asarray(r["out"], dtype=np.float32) for r in res.results], axis=0)
    return out


def make_in_maps(x, c, ctx, c_ctx, ada_w, ada_b, norm1_g, w_in, conv_w, conv_b, rg_w_a, rg_b_a, rg_w_x, rg_b_x, rg_lambda,
                 gla_w_g, gla_b_g, gla_norm_g, w_out, norm2_g, peer_w_q, peer_keys, peer_u, peer_v, final_norm_g):
    f = lambda a: np.ascontiguousarray(np.asarray(a, dtype=np.float32))
    x, c, ctx, c_ctx = f(x), f(c), f(ctx), f(c_ctx)
    jj, ii = np.meshgrid(np.arange(128), np.arange(128), indexing="ij")
    mf = (jj <= ii).astype(np.float32)
    mb = (jj >= ii).astype(np.float32)
    shared = {
        "ada_w": f(ada_w[0]),
        "ada_b": f(ada_b[0]).reshape(1, -1),
        "ada_bT": _fm(ada_b[0], 48),
        "norm1_g": f(norm1_g[0]).reshape(1, -1),
        "norm2_g": f(norm2_g[0]).reshape(1, -1),
        "final_g": f(final_norm_g).reshape(1, -1),
        "w_in": f(w_in[0]),
        "convwT": np.ascontiguousarray(f(conv_w[0]).reshape(4, 4, 128).transpose(2, 1, 0)),
        "convbT": _fm(conv_b[0], 4),
        "rg_w_a": f(rg_w_a[0]),
        "rg_w_x": f(rg_w_x[0]),
        "rgbaT": np.ascontiguousarray(f(rg_b_a[0]).reshape(2, 4, 128).transpose(2, 0, 1)),
        "rgbxT": np.ascontiguousarray(f(rg_b_x[0]).reshape(2, 4, 128).transpose(2, 0, 1)),
        "rglamT": np.ascontiguousarray(f(rg_lambda[0]).reshape(2, 4, 128).transpose(2, 0, 1)),
        "gla_w_g": f(gla_w_g[0]),
        "glabgT": np.ascontiguousarray(f(gla_b_g[0]).reshape(2, 2, 128).transpose(2, 0, 1)),
        "glangT": f(gla_norm_g[0]).reshape(128, 1),
        "w_out": f(w_out[0]),
        "peer_w_q": f(peer_w_q[0]),
        "keysT": np.ascontiguousarray(f(peer_keys[0]).reshape(16, 128, 128).transpose(2, 0, 1)),
        "peer_u": f(peer_u[0]),
        "peer_v": f(peer_v[0]),
        "pos": _pos_table(),
        "ident": np.eye(128, dtype=np.float32),
        "iota": np.tile(np.arange(128, dtype=np.float32)[None, :], (128, 1)),
        "maskf": np.ascontiguousarray(np.concatenate([mf, mf], axis=1)),
        "maskb": np.ascontiguousarray(np.concatenate([mb, mb], axis=1)),
    }
    in_maps = []
    for i in range(NCORES):
        b0 = 2 * i
        cm = np.stack([c[b0], c[b0 + 1], c_ctx], axis=0)
        cT = np.ascontiguousarray(cm.reshape(3, 8, 128).transpose(2, 1, 0))
        m = dict(shared)
        m["x"] = np.ascontiguousarray(x[b0:b0 + 2])
        m["ctx"] = np.ascontiguousarray(ctx[b0:b0 + 2])
        m["cT"] = cT
        in_maps.append(m)
    return in_maps
```
